# Optimizing a Trainium2 kernel written in Bass

```python
import jax, jax.numpy as jnp
from jax import lax
import numpy as np

D_MODEL = 1024
BATCH = 2
SEQ = 16384
DEPTH = 2

GRID_W = 64
CTX_LEN = 256
CHUNK = 64
CONV_W = 5
POOL_WINDOWS = (2, 4, 8, 16)
POOL_DIM = D_MODEL // 4
POOL_GROUP = POOL_DIM // len(POOL_WINDOWS)
SSD_DIM = 3 * D_MODEL // 8
SSD_HEAD_DIM = 64
SSD_HEADS = SSD_DIM // SSD_HEAD_DIM
SSD_GROUPS = 2
SSD_STATE = 128
SSD_BC = SSD_GROUPS * SSD_STATE
GDN_DIM = 3 * D_MODEL // 8
GDN_HEAD_DIM = 64
GDN_HEADS = GDN_DIM // GDN_HEAD_DIM
D_MIX = POOL_DIM + SSD_DIM + GDN_DIM
IN_SPLITS = (POOL_DIM, SSD_DIM, SSD_DIM + 2 * SSD_BC, 2 * SSD_HEADS, 3 * GDN_DIM, GDN_DIM, 2 * GDN_HEADS, 2 * GDN_HEADS)
IN_DIM = sum(IN_SPLITS)
N_EXPERTS = 16
EC_CAPACITY = 2
EXPERT_FF = D_MODEL // 2
EPS = 1e-6
F32 = jnp.float32

kernel_name = "hybrid_pool_ssd_gdn_ec_moe_dit"


def rms_norm(x, g):
    x32 = x.astype(F32)
    y = x32 * lax.rsqrt(jnp.mean(x32 * x32, axis=-1, keepdims=True) + EPS) * g.astype(F32)
    return y.astype(x.dtype)


def l2_norm(a):
    return a * lax.rsqrt(jnp.sum(a * a, axis=-1, keepdims=True) + EPS)


def modulate(h, shift, scale):
    return h * (1 + scale[:, None]) + shift[:, None]


def flip_t(a):
    return jnp.flip(a, axis=1)


def to_column_major(u):
    b, T, C = u.shape
    rows = T // GRID_W
    return u.reshape(b, rows, GRID_W, C).transpose(0, 2, 1, 3).reshape(b, T, C)


def to_raster(u):
    b, T, C = u.shape
    rows = T // GRID_W
    return u.reshape(b, GRID_W, rows, C).transpose(0, 2, 1, 3).reshape(b, T, C)


def to_chunks(a):
    b, T = a.shape[:2]
    a = a.reshape((b, T // CHUNK, CHUNK) + a.shape[2:])
    return jnp.moveaxis(a, 2, 3)


def dwconv_centred(u, w):
    k, ch = w.shape
    return lax.conv_general_dilated(u, w[:, None, :], window_strides=(1,), padding=[(k // 2, k // 2)],
                                    dimension_numbers=('NWC', 'WIO', 'NWC'), feature_group_count=ch)


def pool_branch(u, pool_w, pool_scale):
    b, T, _ = u.shape
    u32 = u.astype(F32)
    csum = jnp.concatenate([jnp.zeros((b, 1, POOL_DIM), F32), jnp.cumsum(u32, axis=1)], axis=1)
    t = jnp.arange(T)
    groups = []
    for gi, win in enumerate(POOL_WINDOWS):
        lo = jnp.clip(t - win // 2, 0, T)
        hi = jnp.clip(t + win // 2, 0, T)
        sl = slice(gi * POOL_GROUP, (gi + 1) * POOL_GROUP)
        cg = csum[:, :, sl]
        mean = (cg[:, hi] - cg[:, lo]) / (hi - lo).astype(F32)[None, :, None]
        groups.append(mean - u32[:, :, sl])
    d = jnp.stack(groups, axis=2)
    y = jnp.einsum('btgc,gcd->btgd', d, pool_w.astype(F32)).reshape(b, T, POOL_DIM) * pool_scale.astype(F32)
    return y.astype(u.dtype)


def ssd_chunked(xdt, log_a, bm, cm, s0):
    b, T, H, P = xdt.shape
    xc, bc, cc = to_chunks(xdt), to_chunks(bm), to_chunks(cm)
    ac = jnp.cumsum(to_chunks(log_a), axis=-1)
    lower = jnp.tril(jnp.ones((CHUNK, CHUNK), bool))
    decay = jnp.exp(jnp.where(lower, ac[..., :, None] - ac[..., None, :], -jnp.inf))
    y_intra = jnp.einsum('bchij,bchjp->bchip', jnp.einsum('bchis,bchjs->bchij', cc, bc) * decay, xc)
    states = jnp.einsum('bchjs,bchjp->bchsp', bc * jnp.exp(ac[..., -1:] - ac)[..., None], xc)
    chunk_decay = jnp.exp(ac[..., -1])

    def step(s, inp):
        st, dc = inp
        return s * dc[..., None, None] + st, s

    s_fin, s_start = lax.scan(step, s0, (jnp.moveaxis(states, 1, 0), jnp.moveaxis(chunk_decay, 1, 0)))
    s_start = jnp.moveaxis(s_start, 0, 1)
    y_inter = jnp.einsum('bchis,bchsp->bchip', cc * jnp.exp(ac)[..., None], s_start)
    y = jnp.moveaxis(y_intra + y_inter, 2, 3).reshape(b, T, H, P)
    return y, s_fin


def gdn_chunked(q, k, v, g, beta, s0):
    b, T, H, _ = q.shape
    V = v.shape[-1]
    qc, kc, vc = to_chunks(q), to_chunks(k), to_chunks(v)
    gc = jnp.cumsum(to_chunks(g), axis=-1)
    bc = to_chunks(beta)
    lower = jnp.tril(jnp.ones((CHUNK, CHUNK), bool))
    strict = jnp.tril(jnp.ones((CHUNK, CHUNK), bool), -1)
    decay = jnp.exp(jnp.where(lower, gc[..., :, None] - gc[..., None, :], -jnp.inf))
    kb = kc * bc[..., None]
    xm = jnp.where(strict, jnp.einsum('bnhik,bnhjk->bnhij', kb, kc) * decay, 0.0)
    eye = jnp.eye(CHUNK, dtype=xm.dtype)
    tm = lax.linalg.triangular_solve(xm + eye, jnp.broadcast_to(eye, xm.shape), left_side=True, lower=True,
                                     unit_diagonal=True)
    u = jnp.einsum('bnhij,bnhjv->bnhiv', tm, vc * bc[..., None])
    w = jnp.einsum('bnhij,bnhjk->bnhik', tm, kb * jnp.exp(gc)[..., None])
    attn = jnp.einsum('bnhik,bnhjk->bnhij', qc, kc) * decay
    qg = qc * jnp.exp(gc)[..., None]
    kend = kc * jnp.exp(gc[..., -1:] - gc)[..., None]
    gend = jnp.exp(gc[..., -1])

    def step(s, inp):
        u_i, w_i, a_i, qg_i, ke_i, ge_i = inp
        v_new = u_i - jnp.einsum('bhlk,bhkv->bhlv', w_i, s)
        o_i = jnp.einsum('bhlk,bhkv->bhlv', qg_i, s) + jnp.einsum('bhij,bhjv->bhiv', a_i, v_new)
        s = s * ge_i[..., None, None] + jnp.einsum('bhlk,bhlv->bhkv', ke_i, v_new)
        return s, o_i

    xs = (jnp.moveaxis(u, 1, 0), jnp.moveaxis(w, 1, 0), jnp.moveaxis(attn, 1, 0),
          jnp.moveaxis(qg, 1, 0), jnp.moveaxis(kend, 1, 0), jnp.moveaxis(gend, 1, 0))
    s_fin, o = lax.scan(step, s0, xs)
    o = jnp.moveaxis(jnp.moveaxis(o, 0, 1), 2, 3).reshape(b, T, H, V)
    return o, s_fin


def ssd_stream(z, xbc, dt_raw, conv_w, conv_b, a_log, dt_bias, d_skip, norm_g, s0_f, s0_b):
    b, T, _ = z.shape
    xbc = jax.nn.silu(dwconv_centred(xbc, conv_w) + conv_b).astype(F32)
    xs, bm, cm = jnp.split(xbc, [SSD_DIM, SSD_DIM + SSD_BC], axis=-1)
    xs = xs.reshape(b, T, SSD_HEADS, SSD_HEAD_DIM)
    rep = SSD_HEADS // SSD_GROUPS
    bm = jnp.repeat(bm.reshape(b, T, SSD_GROUPS, SSD_STATE), rep, axis=2)
    cm = jnp.repeat(cm.reshape(b, T, SSD_GROUPS, SSD_STATE), rep, axis=2)
    dt = jax.nn.softplus(dt_raw.astype(F32).reshape(b, T, 2, SSD_HEADS) + dt_bias.astype(F32))
    a = -jnp.exp(a_log.astype(F32))
    y_f, s_f = ssd_chunked(xs * dt[:, :, 0, :, None], dt[:, :, 0] * a[0], bm, cm, s0_f)
    y_b, s_b = ssd_chunked(flip_t(xs * dt[:, :, 1, :, None]), flip_t(dt[:, :, 1] * a[1]), flip_t(bm), flip_t(cm), s0_b)
    y = y_f + flip_t(y_b) + d_skip.astype(F32)[:, None] * xs
    y = y.reshape(b, T, SSD_DIM) * jax.nn.silu(z.astype(F32))
    return rms_norm(y, norm_g).astype(z.dtype), s_f, s_b


def gdn_stream(qkv, gate, a_raw, b_raw, conv_w, a_log, dt_bias, norm_g, s0_f, s0_b):
    b, T, _ = qkv.shape
    qkv = jax.nn.silu(dwconv_centred(qkv, conv_w)).astype(F32)
    q, k, v = [t.reshape(b, T, GDN_HEADS, GDN_HEAD_DIM) for t in jnp.split(qkv, 3, axis=-1)]
    q = l2_norm(q) * (GDN_HEAD_DIM ** -0.5)
    k = l2_norm(k)
    g = -jnp.exp(a_log.astype(F32)) * jax.nn.softplus(a_raw.astype(F32).reshape(b, T, 2, GDN_HEADS) + dt_bias.astype(F32))
    beta = jax.nn.sigmoid(b_raw.astype(F32).reshape(b, T, 2, GDN_HEADS))
    o_f, s_f = gdn_chunked(q, k, v, g[:, :, 0], beta[:, :, 0], s0_f)
    o_b, s_b = gdn_chunked(flip_t(q), flip_t(k), flip_t(v), flip_t(g[:, :, 1]), flip_t(beta[:, :, 1]), s0_b)
    o = rms_norm(o_f + flip_t(o_b), norm_g).reshape(b, T, GDN_DIM) * jax.nn.silu(gate.astype(F32))
    return o.astype(gate.dtype), s_f, s_b


def hybrid_mixer(h_ctx, h_lat, w_in, w_out, pool_w, pool_scale, ssd_conv_w, ssd_conv_b, ssd_a_log, ssd_dt_bias,
                 ssd_d, ssd_norm_g, gdn_conv_w, gdn_a_log, gdn_dt_bias, gdn_norm_g, with_ctx_out):
    b = h_lat.shape[0]
    cut = np.cumsum(IN_SPLITS)[:-1].tolist()
    pc = jnp.split(h_ctx @ w_in, cut, axis=-1)
    pl = jnp.split(h_lat @ w_in, cut, axis=-1)
    ssd_p = (ssd_conv_w, ssd_conv_b, ssd_a_log, ssd_dt_bias, ssd_d, ssd_norm_g)
    gdn_p = (gdn_conv_w, gdn_a_log, gdn_dt_bias, gdn_norm_g)
    zs = jnp.zeros((b, SSD_HEADS, SSD_STATE, SSD_HEAD_DIM), F32)
    zg = jnp.zeros((b, GDN_HEADS, GDN_HEAD_DIM, GDN_HEAD_DIM), F32)
    s_ctx, ssd_sf, ssd_sb = ssd_stream(pc[1], pc[2], pc[3], *ssd_p, zs, zs)
    g_ctx, gdn_sf, gdn_sb = gdn_stream(pc[4], pc[5], pc[6], pc[7], *gdn_p, zg, zg)
    s_lat, _, _ = ssd_stream(pl[1], pl[2], pl[3], *ssd_p, ssd_sf, ssd_sb)
    g_lat, _, _ = gdn_stream(to_column_major(pl[4]), to_column_major(pl[5]), to_column_major(pl[6]),
                             to_column_major(pl[7]), *gdn_p, gdn_sf, gdn_sb)
    g_lat = to_raster(g_lat)
    lat = jnp.concatenate([pool_branch(pl[0], pool_w, pool_scale), s_lat, g_lat], axis=-1) @ w_out
    if not with_ctx_out:
        return None, lat
    ctx = jnp.concatenate([pool_branch(pc[0], pool_w, pool_scale), s_ctx, g_ctx], axis=-1) @ w_out
    return ctx, lat


def expert_choice_ffn(h, router_w, w_gate, w_up, w_down):
    b, T, D = h.shape
    cap = EC_CAPACITY * T // N_EXPERTS
    aff = jax.nn.softmax(jnp.einsum('btd,de->bte', h.astype(F32), router_w.astype(F32)), axis=-1)
    gate, idx = lax.top_k(jnp.swapaxes(aff, 1, 2), cap)
    xe = jax.vmap(lambda hb, ib: hb[ib])(h, idx)
    hid = jax.nn.silu(jnp.einsum('becd,edf->becf', xe, w_gate)) * jnp.einsum('becd,edf->becf', xe, w_up)
    ye = jnp.einsum('becf,efd->becd', hid, w_down) * gate[..., None].astype(h.dtype)
    return jax.vmap(lambda ib, yb: jnp.zeros((T, D), yb.dtype).at[ib.reshape(-1)].add(yb.reshape(-1, D)))(idx, ye)


def setup_inputs(seed: int = 0) -> dict:
    key = jax.random.key(seed)
    ks = iter(jax.random.split(key, 40))

    def nrm(shape, s):
        return jax.random.normal(next(ks), shape, F32) * s

    def gain(shape):
        return 1.0 + nrm(shape, 0.05)

    def dt_bias(shape):
        dt = jnp.exp(jax.random.uniform(next(ks), shape, F32, np.log(1e-3), np.log(1e-1)))
        return dt + jnp.log(-jnp.expm1(-dt))

    def a_log(shape):
        return jnp.log(jax.random.uniform(next(ks), shape, F32, 1.0, 16.0))

    L = DEPTH
    return {
        "x": nrm((BATCH, SEQ, D_MODEL), 1.0),
        "c": nrm((BATCH, D_MODEL), 1.0),
        "ctx": nrm((BATCH, CTX_LEN, D_MODEL), 1.0),
        "c_ctx": nrm((D_MODEL,), 1.0),
        "norm1_g": gain((L, D_MODEL)),
        "norm2_g": gain((L, D_MODEL)),
        "ada_w": nrm((L, D_MODEL, 6 * D_MODEL), 0.5 * D_MODEL ** -0.5),
        "ada_b": nrm((L, 6 * D_MODEL), 0.02),
        "w_in": nrm((L, D_MODEL, IN_DIM), D_MODEL ** -0.5),
        "w_out": nrm((L, D_MIX, D_MODEL), D_MIX ** -0.5),
        "pool_w": nrm((L, len(POOL_WINDOWS), POOL_GROUP, POOL_GROUP), POOL_GROUP ** -0.5),
        "pool_scale": gain((L, POOL_DIM)),
        "ssd_conv_w": nrm((L, CONV_W, SSD_DIM + 2 * SSD_BC), CONV_W ** -0.5),
        "ssd_conv_b": nrm((L, SSD_DIM + 2 * SSD_BC), 0.02),
        "ssd_a_log": a_log((L, 2, SSD_HEADS)),
        "ssd_dt_bias": dt_bias((L, 2, SSD_HEADS)),
        "ssd_d": gain((L, SSD_HEADS)),
        "ssd_norm_g": gain((L, SSD_DIM)),
        "gdn_conv_w": nrm((L, CONV_W, 3 * GDN_DIM), CONV_W ** -0.5),
        "gdn_a_log": a_log((L, 2, GDN_HEADS)),
        "gdn_dt_bias": dt_bias((L, 2, GDN_HEADS)),
        "gdn_norm_g": gain((L, GDN_HEAD_DIM)),
        "router_w": nrm((L, D_MODEL, N_EXPERTS), D_MODEL ** -0.5),
        "exp_w_gate": nrm((L, N_EXPERTS, D_MODEL, EXPERT_FF), D_MODEL ** -0.5),
        "exp_w_up": nrm((L, N_EXPERTS, D_MODEL, EXPERT_FF), D_MODEL ** -0.5),
        "exp_w_down": nrm((L, N_EXPERTS, EXPERT_FF, D_MODEL), EXPERT_FF ** -0.5),
        "final_norm_g": gain((D_MODEL,)),
    }


def reference(x, c, ctx, c_ctx, norm1_g, norm2_g, ada_w, ada_b, w_in, w_out, pool_w, pool_scale, ssd_conv_w,
              ssd_conv_b, ssd_a_log, ssd_dt_bias, ssd_d, ssd_norm_g, gdn_conv_w, gdn_a_log, gdn_dt_bias, gdn_norm_g,
              router_w, exp_w_gate, exp_w_up, exp_w_down, final_norm_g):
    sc = jax.nn.silu(c)
    scc = jax.nn.silu(c_ctx)[None]
    for l in range(DEPTH):
        last = l == DEPTH - 1
        m_lat = jnp.split(sc @ ada_w[l] + ada_b[l], 6, axis=-1)
        m_ctx = jnp.split(scc @ ada_w[l] + ada_b[l], 6, axis=-1)
        h_lat = modulate(rms_norm(x, norm1_g[l]), m_lat[0], m_lat[1])
        h_ctx = modulate(rms_norm(ctx, norm1_g[l]), m_ctx[0], m_ctx[1])
        mix_ctx, mix_lat = hybrid_mixer(h_ctx, h_lat, w_in[l], w_out[l], pool_w[l], pool_scale[l], ssd_conv_w[l],
                                        ssd_conv_b[l], ssd_a_log[l], ssd_dt_bias[l], ssd_d[l], ssd_norm_g[l],
                                        gdn_conv_w[l], gdn_a_log[l], gdn_dt_bias[l], gdn_norm_g[l], not last)
        x = x + m_lat[2][:, None] * mix_lat
        h_lat = modulate(rms_norm(x, norm2_g[l]), m_lat[3], m_lat[4])
        x = x + m_lat[5][:, None] * expert_choice_ffn(h_lat, router_w[l], exp_w_gate[l], exp_w_up[l], exp_w_down[l])
        if not last:
            ctx = ctx + m_ctx[2][:, None] * mix_ctx
            h_ctx = modulate(rms_norm(ctx, norm2_g[l]), m_ctx[3], m_ctx[4])
            ctx = ctx + m_ctx[5][:, None] * expert_choice_ffn(h_ctx, router_w[l], exp_w_gate[l], exp_w_up[l],
                                                               exp_w_down[l])
    return rms_norm(x, final_norm_g)
```

```python
import contextlib
import numpy as np
import concourse.bass as bass
import concourse.mybir as mybir
from concourse.bass_utils import run_bass_kernel_spmd

F32 = mybir.dt.float32
BF16 = mybir.dt.bfloat16
ALU = mybir.AluOpType
AF = mybir.ActivationFunctionType
AX = mybir.AxisListType


class Buf:
    def __init__(self, t=None, name=""):
        self.t = t
        self.name = name
        self.w = None
        self.r = []
        self.excl = False

    def __getitem__(self, k):
        return self.t[k]


class Prog:
    ENGS = ("sync", "act", "dve", "pool", "pe")

    def __init__(self, nc, n_dma_sems=40):
        self.nc = nc
        self.es = contextlib.ExitStack()
        self.q = {e: [] for e in self.ENGS}
        self.EPOCH = 4000
        self.esem = {}
        self.seq = {e: 0 for e in ("act", "dve", "pool", "pe")}
        self.dsem = [nc.alloc_semaphore("ds_%d" % i) for i in range(n_dma_sems)]
        self.dcnt = [0] * n_dma_sems
        self.dlast = [None] * n_dma_sems
        self.dnext = 0
        self.waited = {e: {} for e in self.ENGS}
        self.nbuf = 0

    def sb(self, shape, dtype=F32, name=None):
        self.nbuf += 1
        name = "s_" + (name or "sb%d" % self.nbuf)
        t = self.es.enter_context(self.nc.sbuf_tensor(name, list(shape), dtype))
        return Buf(t, name)

    def ps(self, shape, dtype=F32, name=None):
        self.nbuf += 1
        name = "p_" + (name or "ps%d" % self.nbuf)
        t = self.es.enter_context(self.nc.psum_tensor(name, [128, 512], F32))
        b = Buf(t, name)
        b.excl = True
        return b

    def dram(self, name, shape, dtype=F32, kind="Internal"):
        t = self.nc.dram_tensor(name, list(shape), dtype, kind=kind)
        return Buf(t, name)

    def _need(self, eng, reads, writes):
        evs = []
        for b in reads:
            if b.w is not None:
                evs.append(b.w)
            if b.excl:
                evs.extend(b.r)
        for b in writes:
            if b.w is not None:
                evs.append(b.w)
            evs.extend(b.r)
        best = {}
        for (k, v) in evs:
            if eng == "pe" and k[0] == "pe":
                continue
            if best.get(k, 0) < v:
                best[k] = v
        out = []
        wd = self.waited[eng]
        for k, v in best.items():
            if wd.get(k, 0) >= v:
                continue
            wd[k] = v
            out.append((k, v))
        return out

    def _mark(self, ev, reads, writes):
        for b in reads:
            b.r.append(ev)
        for b in writes:
            b.w = ev
            b.r = []

    def op(self, eng, fn, reads=(), writes=()):
        waits = self._need(eng, reads, writes)
        ep, v = divmod(self.seq[eng], self.EPOCH)
        self.seq[eng] += 1
        key = (eng, ep)
        if key not in self.esem:
            self.esem[key] = self.nc.alloc_semaphore("es_%s_%d" % key)
        ev = (key, v + 1)
        self._mark(ev, reads, writes)
        self.q[eng].append((waits, fn, (key, 1)))

    def dma(self, queue, out, in_, reads=(), writes=(), **kw):
        i = self.dnext
        self.dnext = (self.dnext + 1) % len(self.dsem)
        waits = self._need(queue, reads, writes)
        key = ("d", i)
        if self.dcnt[i] > 0 and self.waited[queue].get(key, 0) < self.dcnt[i]:
            self.waited[queue][key] = self.dcnt[i]
            waits.append((key, self.dcnt[i]))
        self.dcnt[i] += 16
        ev = (key, self.dcnt[i])
        self._mark(ev, reads, writes)
        self.q[queue].append((waits, lambda e: e.dma_start(out=out, in_=in_, **kw), (key, 16)))
        return ev

    def mm(self, O, o, A, a, B, b, start=True, stop=True):
        self.op("pe", lambda e: e.matmul(o, lhsT=a, rhs=b, start=start, stop=stop), reads=[A, B], writes=[O])

    def tr(self, O, o, A, a, I, i):
        self.op("pe", lambda e: e.transpose(out=o, in_=a, identity=i), reads=[A, I], writes=[O])

    def act(self, O, o, A, a, func, bias=None, scale=1.0, accum=None, rd=(), wr=()):
        kw = {}
        if bias is not None:
            kw["bias"] = bias
        if accum is not None:
            kw["accum_out"] = accum
        self.op("act", lambda e: e.activation(out=o, in_=a, func=func, scale=scale, **kw),
                reads=[A] + list(rd), writes=[O] + list(wr))

    def tt(self, eng, O, o, A, a, B, b, op):
        self.op(eng, lambda e: e.tensor_tensor(out=o, in0=a, in1=b, op=op), reads=[A, B], writes=[O])

    def ts(self, eng, O, o, A, a, s1, s2, op0, op1=None, rd=()):
        if op1 is None:
            self.op(eng, lambda e: e.tensor_scalar(out=o, in0=a, scalar1=s1, scalar2=None, op0=op0),
                    reads=[A] + list(rd), writes=[O])
        else:
            self.op(eng, lambda e: e.tensor_scalar(out=o, in0=a, scalar1=s1, scalar2=s2, op0=op0, op1=op1),
                    reads=[A] + list(rd), writes=[O])

    def stt(self, eng, O, o, A, a, sc, B, b, op0, op1, rd=()):
        self.op(eng, lambda e: e.scalar_tensor_tensor(out=o, in0=a, scalar=sc, in1=b, op0=op0, op1=op1),
                reads=[A, B] + list(rd), writes=[O])

    def cp(self, eng, O, o, A, a):
        if eng == "act":
            self.op("act", lambda e: e.copy(out=o, in_=a), reads=[A], writes=[O])
        else:
            self.op(eng, lambda e: e.tensor_copy(out=o, in_=a), reads=[A], writes=[O])

    def _sem(self, k):
        if k[0] == "d":
            return self.dsem[k[1]]
        return self.esem[k]

    def finish(self, final_bufs):
        evs = []
        for b in final_bufs:
            if b.w is not None:
                evs.append(b.w)
        fw = []
        for (k, v) in evs:
            fw.append((k, v))
        nc = self.nc
        q = self.q
        semf = self._sem

        def replay(e, lst, tail=()):
            for waits, fn, inc in lst:
                for (k, v) in waits:
                    e.wait_ge(semf(k), v)
                ins = fn(e)
                ins.then_inc(semf(inc[0]), inc[1])
            for (k, v) in tail:
                e.wait_ge(semf(k), v)

        with nc.Block() as block:
            @block.sync
            def _(e):
                replay(e, q["sync"], fw)

            @block.scalar
            def _(e):
                replay(e, q["act"])

            @block.vector
            def _(e):
                replay(e, q["dve"])

            @block.gpsimd
            def _(e):
                replay(e, q["pool"])

            @block.tensor
            def _(e):
                replay(e, q["pe"])
        self.es.close()


D = 1024
IN_DIM = 3108
NCORE = 8


def run(nc, in_maps):
    res = run_bass_kernel_spmd(nc, in_maps, core_ids=list(range(NCORE)))
    return res.results


def build_k0():
    nc = bass.Bass("TRN2", target_bir_lowering=False)
    P = Prog(nc)
    cT = P.dram("cT", [128, 8, 3], F32, kind="ExternalInput")
    aw = P.dram("aw", [3, 8, 128, 512], F32, kind="ExternalInput")
    ab = P.dram("ab", [3, 512], F32, kind="ExternalInput")
    out = P.dram("out", [3, 3, 512], F32, kind="ExternalOutput")
    sc = P.sb([128, 8, 3])
    P.dma("sync", sc[:], cT.t.ap(), reads=[cT], writes=[sc])
    P.act(sc, sc[:], sc, sc[:], AF.Silu)
    for j in range(3):
        w = P.sb([128, 8, 512], name="w%d" % j)
        bt = P.sb([3, 512], name="b%d" % j)
        ot = P.sb([3, 512], name="o%d" % j)
        pm = P.ps([3, 512], name="pm%d" % j)
        P.dma(("sync", "act", "pool")[j], w[:], aw.t.ap()[j].rearrange("k p n -> p k n"), reads=[aw], writes=[w])
        P.dma("sync", bt[:], ab.t.ap()[j].partition_broadcast(3), reads=[ab], writes=[bt])
        for k in range(8):
            P.mm(pm, pm[0:3, :], sc, sc[:, k, :], w, w[:, k, :], start=(k == 0), stop=(k == 7))
        P.tt("dve", ot, ot[:], pm, pm[0:3, :], bt, bt[:], ALU.add)
        P.dma("sync", out.t.ap()[j], ot[:], reads=[ot], writes=[out])
    P.finish([out])
    return nc


def run_k0(c, c_ctx, ada_w, ada_b):
    L = ada_w.shape[0]
    cv = np.stack([c[0], c[1], c_ctx], axis=0)
    cT = np.ascontiguousarray(cv.reshape(3, 8, 128).transpose(2, 1, 0))
    nblk = L * 12
    assert nblk == 24
    in_maps = []
    for core in range(NCORE):
        aws, abs_ = [], []
        for j in range(3):
            b = core * 3 + j
            l, cb = divmod(b, 12)
            aws.append(ada_w[l][:, cb * 512:(cb + 1) * 512].reshape(8, 128, 512))
            abs_.append(ada_b[l][cb * 512:(cb + 1) * 512])
        in_maps.append({"cT": cT, "aw": np.ascontiguousarray(np.stack(aws)), "ab": np.ascontiguousarray(np.stack(abs_))})
    res = run(build_k0(), in_maps)
    mods = np.zeros((L, 3, 6144), np.float32)
    for core in range(NCORE):
        for j in range(3):
            b = core * 3 + j
            l, cb = divmod(b, 12)
            mods[l, :, cb * 512:(cb + 1) * 512] = res[core]["out"][j]
    return mods


def load_bcast(P, queue, dram_buf, n, name):
    t = P.sb([128, n], name=name)
    P.dma(queue, t[:], dram_buf.t.ap().partition_broadcast(128), reads=[dram_buf], writes=[t])
    return t


def norm_mod_tile(P, xt, h, A, B, scr, ss, eng2="pool"):
    P.act(scr, scr[:], xt, xt[:], AF.Square, scale=1.0 / 32.0, accum=ss[:], wr=[ss])
    P.ts("dve", ss, ss[:], ss, ss[:], 1e-6, None, ALU.add)
    P.op("act", lambda e: e.sqrt(out=ss[:], in_=ss[:]), reads=[ss], writes=[ss])
    P.op("dve", lambda e: e.reciprocal(out=ss[:], in_=ss[:]), reads=[ss], writes=[ss])
    P.stt("dve", h, h[:], xt, xt[:], ss[:, 0:1], A, A[:], ALU.mult, ALU.mult, rd=[ss])
    P.tt(eng2, h, h[:], h, h[:], B, B[:], ALU.add)


def transpose_tile(P, h, hT, ident, pts, nk=8, evac=("act", "dve")):
    for half in range((nk + 3) // 4):
        pt = pts[half % len(pts)]
        n4 = min(4, nk - half * 4)
        for q in range(n4):
            k = half * 4 + q
            P.tr(pt, pt[:, q * 128:(q + 1) * 128], h, h[:, k * 128:(k + 1) * 128], ident, ident[:])
        P.cp(evac[half % len(evac)], hT, hT[:, half * 4:half * 4 + n4, :],
             pt, pt[:, 0:n4 * 128].rearrange("p (k t) -> p k t", k=n4))


def load_weight_bf16(P, wdram_ap_fn, W, nk, ncols, stage, wdram, colchunk=None):
    qs = ("sync", "act", "pool")
    cs = ("pool", "dve", "act")
    for k in range(nk):
        st = stage[k % len(stage)]
        P.dma(qs[k % 3], st[:, 0:ncols], wdram_ap_fn(k), reads=[wdram], writes=[st])
        P.cp(cs[k % 3], W, W[:, k, :], st, st[:, 0:ncols])


def build_k1(NT, ncols=IN_DIM, n_lat=None):
    if n_lat is None:
        n_lat = NT - 1
    nc = bass.Bass("TRN2", target_bir_lowering=False)
    P = Prog(nc)
    xt = P.dram("xt", [NT, 128, D], F32, kind="ExternalInput")
    w = P.dram("w", [D, ncols], F32, kind="ExternalInput")
    g = P.dram("g", [D], F32, kind="ExternalInput")
    vecs = {n: P.dram(n, [D], F32, kind="ExternalInput") for n in ("sh_l", "sc_l", "sh_c", "sc_c")}
    idn = P.dram("idn", [128, 128], F32, kind="ExternalInput")
    out = P.dram("out", [NT, 128, ncols], F32, kind="ExternalOutput")

    ident = P.sb([128, 128], name="ident")
    P.dma("sync", ident[:], idn.t.ap(), reads=[idn], writes=[ident])
    gb = load_bcast(P, "act", g, D, "gb")
    A_l = load_bcast(P, "pool", vecs["sc_l"], D, "A_l")
    B_l = load_bcast(P, "sync", vecs["sh_l"], D, "B_l")
    A_c = load_bcast(P, "act", vecs["sc_c"], D, "A_c")
    B_c = load_bcast(P, "pool", vecs["sh_c"], D, "B_c")
    for A in (A_l, A_c):
        P.stt("dve", A, A[:], A, A[:], 1.0, gb, gb[:], ALU.add, ALU.mult)

    W = P.sb([128, 8, ncols], BF16, name="W")
    stage = [P.sb([128, ncols], name="stg%d" % i) for i in range(2)]
    load_weight_bf16(P, lambda k: w.t.ap()[k * 128:(k + 1) * 128, :], W, 8, ncols, stage, w)

    xs = [P.sb([128, D], name="x%d" % i) for i in range(2)]
    hs = [P.sb([128, D], name="h%d" % i) for i in range(2)]
    scr = P.sb([128, D], name="scr")
    sss = [P.sb([128, 1], name="ss%d" % i) for i in range(2)]
    hTs = [P.sb([128, 8, 128], BF16, name="hT%d" % i) for i in range(2)]
    outs = [P.sb([128, ncols], name="ot%d" % i) for i in range(2)]
    pts = [P.ps([128, 512], name="pt%d" % i) for i in range(2)]
    pms = [P.ps([128, 512], name="pm%d" % i) for i in range(4)]
    ncb = (ncols + 511) // 512
    pmi = 0
    for t in range(NT):
        x_, h_, ss_, hT_, o_ = xs[t % 2], hs[t % 2], sss[t % 2], hTs[t % 2], outs[t % 2]
        P.dma(("sync", "pool")[t % 2], x_[:], xt.t.ap()[t], reads=[xt], writes=[x_])
        A, B = (A_l, B_l) if t < n_lat else (A_c, B_c)
        norm_mod_tile(P, x_, h_, A, B, scr, ss_)
        transpose_tile(P, h_, hT_, ident, pts)
        for cb in range(ncb):
            c0 = cb * 512
            cw = min(512, ncols - c0)
            pm = pms[pmi % 4]
            pmi += 1
            for k in range(8):
                P.mm(pm, pm[:, 0:cw], hT_, hT_[:, k, :], W, W[:, k, c0:c0 + cw], start=(k == 0), stop=(k == 7))
            P.cp(("act", "dve")[cb % 2], o_, o_[:, c0:c0 + cw], pm, pm[:, 0:cw])
        P.dma("sync", out.t.ap()[t], o_[:], reads=[o_], writes=[out])
    P.finish([out])
    return nc


NCH = 130
NBLK = 65


def consts():
    t = np.arange(128)
    U = (t[:, None] <= t[None, :]).astype(np.float32)
    NEG = np.where(t[None, :] < t[:, None], -30000.0, 0.0).astype(np.float32)
    return {"idn": np.eye(128, dtype=np.float32), "U": U, "NEG": NEG, "ones": np.ones((128, 128), np.float32)}


def load_consts(P, names=("idn", "U", "NEG", "ones")):
    out = {}
    for i, n in enumerate(names):
        d = P.dram(n, [128, 128], F32, kind="ExternalInput")
        s = P.sb([128, 128], name="c_" + n)
        P.dma(("sync", "act", "pool")[i % 3], s[:], d.t.ap(), reads=[d], writes=[s])
        out[n] = s
    return out


def conv_block(P, eng, ut, acc, cv, cw, cb, npart, n=256, bias=True):
    sl = slice(0, npart)
    P.ts(eng, acc, acc[sl, 0:n], ut, ut[sl, 0:n], cw[sl, 0:1], None, ALU.mult, rd=[cw])
    for k in range(1, 5):
        P.stt(eng, acc, acc[sl, 0:n], ut, ut[sl, k:k + n], cw[sl, k:k + 1], acc, acc[sl, 0:n], ALU.mult, ALU.add, rd=[cw])
    if bias:
        P.act(cv, cv[sl, 0:n], acc, acc[sl, 0:n], AF.Silu, bias=cb[sl, 0:1], rd=[cb])
    else:
        P.act(cv, cv[sl, 0:n], acc, acc[sl, 0:n], AF.Silu)


def softplus_inplace(P, t, ap):
    P.act(t, ap, t, ap, AF.Exp)
    P.ts("dve", t, ap, t, ap, 1.0, None, ALU.add)
    P.act(t, ap, t, ap, AF.Ln)


def cum_tables(P, C, la, pC1, pC2, nh=3):
    N = NCH * nh
    flat = lambda b: b[:].rearrange("p c h -> p (c h)")
    T = {}
    for n in ("negac", "eac", "wj", "eL"):
        T[n] = P.sb([128, NCH, nh], name="tb_" + n)
    P.mm(pC1, pC1[:, 0:N], C["U"], C["U"][:], la, flat(la))
    P.mm(pC2, pC2[:, 0:N], C["ones"], C["ones"][:], la, flat(la))
    P.ts("dve", T["negac"], flat(T["negac"]), pC1, pC1[:, 0:N], -1.0, None, ALU.mult)
    P.act(T["eac"], flat(T["eac"]), pC1, pC1[:, 0:N], AF.Exp)
    P.act(T["eL"], flat(T["eL"]), pC2, pC2[:, 0:N], AF.Exp)
    P.tt("dve", T["wj"], flat(T["wj"]), pC2, pC2[:, 0:N], T["negac"], flat(T["negac"]), ALU.add)
    P.act(T["wj"], flat(T["wj"]), T["wj"], flat(T["wj"]), AF.Exp)
    return T


def decay_mats(P, C, la, T, c, pA, rhs_t, decT, nh=3):
    for h in range(nh):
        r = rhs_t[h % len(rhs_t)]
        P.ts(("dve", "pool")[h % 2], r, r[:], C["U"], C["U"][:], la[:, c, h:h + 1], None, ALU.mult, rd=[la])
        P.mm(pA, pA[:, h * 128:(h + 1) * 128], C["ones"], C["ones"][:], r, r[:], start=True, stop=False)
        P.mm(pA, pA[:, h * 128:(h + 1) * 128], C["idn"], C["idn"][:], C["NEG"], C["NEG"][:], start=False, stop=True)
    for h in range(nh):
        P.act(decT, decT[:, h, :], pA, pA[:, h * 128:(h + 1) * 128], AF.Exp, bias=T["negac"][:, c, h:h + 1], rd=[T["negac"]])


def build_k2s():
    nc = bass.Bass("TRN2", target_bir_lowering=False)
    P = Prog(nc)
    u = P.dram("u", [448, NBLK, 260], F32, kind="ExternalInput")
    cwd = P.dram("cw", [448, 5], F32, kind="ExternalInput")
    cbd = P.dram("cb", [448, 1], F32, kind="ExternalInput")
    dtr = P.dram("dtr", [128, NCH, 3], F32, kind="ExternalInput")
    dtb = P.dram("dtb", [3], F32, kind="ExternalInput")
    alog = P.dram("alog", [3], F32, kind="ExternalInput")
    yout = P.dram("y", [NCH, 128, 192], F32, kind="ExternalOutput")
    xout = P.dram("xs", [NCH, 128, 192], F32, kind="ExternalOutput")
    C = load_consts(P)
    offs = (0, 128, 192, 320)
    nps = (128, 64, 128, 128)
    cw, cb = [], []
    for i in range(4):
        a = P.sb([128, 5], name="cw%d" % i)
        b = P.sb([128, 1], name="cb%d" % i)
        P.dma("sync", a[0:nps[i], :], cwd.t.ap()[offs[i]:offs[i] + nps[i], :], reads=[cwd], writes=[a])
        P.dma("act", b[0:nps[i], :], cbd.t.ap()[offs[i]:offs[i] + nps[i], :], reads=[cbd], writes=[b])
        cw.append(a)
        cb.append(b)
    dt = P.sb([128, NCH, 3], name="dt")
    la = P.sb([128, NCH, 3], name="la")
    dtw = P.sb([128, NCH, 3], name="dtw")
    dtbb = P.sb([128, 3], name="dtbb")
    Ab = P.sb([128, 3], name="Ab")
    P.dma("sync", dt[:], dtr.t.ap(), reads=[dtr], writes=[dt])
    P.dma("act", dtbb[:], dtb.t.ap().partition_broadcast(128), reads=[dtb], writes=[dtbb])
    P.dma("pool", Ab[:], alog.t.ap().partition_broadcast(128), reads=[alog], writes=[Ab])
    P.act(Ab, Ab[:], Ab, Ab[:], AF.Exp)
    P.ts("dve", Ab, Ab[:], Ab, Ab[:], -1.0, None, ALU.mult)
    for h in range(3):
        P.ts("dve", dt, dt[:, :, h], dt, dt[:, :, h], dtbb[:, h:h + 1], None, ALU.add, rd=[dtbb])
    fl = lambda b: b[:].rearrange("p c h -> p (c h)")
    softplus_inplace(P, dt, fl(dt))
    for h in range(3):
        P.ts("dve", la, la[:, :, h], dt, dt[:, :, h], Ab[:, h:h + 1], None, ALU.mult, rd=[Ab])
    pC1 = P.ps([128, 512], name="pC1")
    pC2 = P.ps([128, 512], name="pC2")
    T = cum_tables(P, C, la, pC1, pC2)
    P.tt("dve", dtw, fl(dtw), dt, fl(dt), T["wj"], fl(T["wj"]), ALU.mult)

    pA = P.ps([128, 384], name="pA")
    pG = P.ps([128, 128], name="pG")
    pY1 = P.ps([128, 192], name="pY1")
    pY2 = P.ps([128, 192], name="pY2")
    pS = P.ps([128, 192], name="pS")
    pT = P.ps([128, 320], name="pT")
    S = P.sb([128, 192], name="S")
    P.op("dve", lambda e: e.memset(S[:], 0.0), writes=[S])
    ut = [[P.sb([128, 260], name="ut%d_%d" % (i, j)) for j in range(2)] for i in range(4)]
    acc = [P.sb([128, 256], name="acc%d" % i) for i in range(4)]
    cv = [[P.sb([128, 256], name="cv%d_%d" % (i, j)) for j in range(2)] for i in range(4)]
    rhs_t = [P.sb([128, 128], name="rhs%d" % i) for i in range(2)]
    decT = P.sb([128, 3, 128], name="decT")
    Wt = P.sb([128, 3, 128], name="Wt")
    tok = [P.sb([128, 320], name="tok%d" % i) for i in range(2)]
    xdt = P.sb([128, 192], name="xdt")
    xw = P.sb([128, 192], name="xw")
    ysb = P.sb([128, 192], name="ysb")
    yo = [P.sb([128, 192], name="yo%d" % i) for i in range(2)]
    for b in range(NBLK):
        j = b % 2
        for i in range(4):
            P.dma(("sync", "act", "pool", "sync")[i], ut[i][j][0:nps[i], :], u.t.ap()[offs[i]:offs[i] + nps[i], b, :],
                  reads=[u], writes=[ut[i][j]])
            conv_block(P, "dve", ut[i][j], acc[i], cv[i][j], cw[i], cb[i], nps[i])
        xA, xB, BT, CT = cv[0][j], cv[1][j], cv[2][j], cv[3][j]
        for cc in range(2):
            c = b * 2 + cc
            ck = slice(cc * 128, (cc + 1) * 128)
            tk = tok[c % 2]
            P.tr(pT, pT[:, 0:128], xA, xA[:, ck], C["idn"], C["idn"][:])
            P.tr(pT, pT[:, 128:192], xB, xB[0:64, ck], C["idn"], C["idn"][0:64, 0:64])
            P.tr(pT, pT[:, 192:320], BT, BT[:, ck], C["idn"], C["idn"][:])
            P.cp("act", tk, tk[:], pT, pT[:, 0:320])
            P.dma("sync", xout.t.ap()[c], tk[:, 0:192], reads=[tk], writes=[xout])
            decay_mats(P, C, la, T, c, pA, rhs_t, decT)
            P.mm(pG, pG[:, 0:128], BT, BT[:, ck], CT, CT[:, ck])
            for h in range(3):
                hs = slice(h * 64, (h + 1) * 64)
                P.tt("dve", Wt, Wt[:, h, :], pG, pG[:, 0:128], decT, decT[:, h, :], ALU.mult)
                P.ts("pool", xdt, xdt[:, hs], tk, tk[:, hs], dt[:, c, h:h + 1], None, ALU.mult, rd=[dt])
                P.ts("pool", xw, xw[:, hs], tk, tk[:, hs], dtw[:, c, h:h + 1], None, ALU.mult, rd=[dtw])
            for h in range(3):
                hs = slice(h * 64, (h + 1) * 64)
                P.mm(pY1, pY1[:, hs], Wt, Wt[:, h, :], xdt, xdt[:, hs])
                P.mm(pY2, pY2[:, hs], CT, CT[:, ck], S, S[:, hs])
                P.mm(pS, pS[:, hs], tk, tk[:, 192:320], xw, xw[:, hs])
            P.cp("act", ysb, ysb[:], pY1, pY1[:, 0:192])
            y_ = yo[c % 2]
            for h in range(3):
                hs = slice(h * 64, (h + 1) * 64)
                P.stt("dve", y_, y_[:, hs], pY2, pY2[:, hs], T["eac"][:, c, h:h + 1], ysb, ysb[:, hs], ALU.mult, ALU.add, rd=[T["eac"]])
            for h in range(3):
                hs = slice(h * 64, (h + 1) * 64)
                P.stt("dve", S, S[:, hs], S, S[:, hs], T["eL"][:, c, h:h + 1], pS, pS[:, hs], ALU.mult, ALU.add, rd=[T["eL"]])
            P.dma("sync", yout.t.ap()[c], y_[:], reads=[y_], writes=[yout])
    P.finish([yout, xout])
    return nc


def windows(seg, n=256):
    ch, T = seg.shape
    p = np.zeros((ch, T + 4), np.float32)
    p[:, 2:T + 2] = seg
    idx = (np.arange(T // n)[:, None] * n + np.arange(n + 4)[None, :])
    return p[:, idx]


def prep_k2s(pl, pc, conv_w, conv_b, a_log, dt_bias, core):
    s, d, g = core // 4, (core // 2) % 2, core % 2
    c0 = 256 + 384
    chans = np.concatenate([np.arange(g * 192, g * 192 + 192), 384 + g * 128 + np.arange(128), 384 + 256 + g * 128 + np.arange(128)])
    segs = []
    for arr in (pc[s], pl[s]):
        a = arr[:, c0 + chans]
        if d == 1:
            a = a[::-1]
        segs.append(windows(np.ascontiguousarray(a.T)))
    u = np.ascontiguousarray(np.concatenate(segs, axis=1))
    cw = conv_w[:, chans].T
    if d == 1:
        cw = cw[:, ::-1]
    dcol = 256 + 384 + 896 + d * 6 + g * 3
    dts = []
    for arr in (pc[s], pl[s]):
        a = arr[:, dcol:dcol + 3]
        if d == 1:
            a = a[::-1]
        dts.append(a)
    dtr = np.concatenate(dts, 0).reshape(NCH, 128, 3).transpose(1, 0, 2)
    m = {"u": u, "cw": np.ascontiguousarray(cw), "cb": np.ascontiguousarray(conv_b[chans][:, None]),
         "dtr": np.ascontiguousarray(dtr), "dtb": np.ascontiguousarray(dt_bias[d, g * 3:g * 3 + 3]),
         "alog": np.ascontiguousarray(a_log[d, g * 3:g * 3 + 3])}
    m.update(consts())
    return m


GRID_W = 64


def consts_g():
    c = consts()
    t = np.arange(128)
    c["POS"] = np.where(t[None, :] >= t[:, None], 30000.0, 0.0).astype(np.float32)
    return c


def build_k2g(nblk=NBLK):
    nc = bass.Bass("TRN2", target_bir_lowering=False)
    P = Prog(nc)
    u = P.dram("u", [576, NBLK, 260], F32, kind="ExternalInput")
    cwd = P.dram("cw", [576, 5], F32, kind="ExternalInput")
    ard = P.dram("araw", [128, NCH, 3], F32, kind="ExternalInput")
    brd = P.dram("braw", [128, NCH, 3], F32, kind="ExternalInput")
    dtb = P.dram("dtb", [3], F32, kind="ExternalInput")
    alog = P.dram("alog", [3], F32, kind="ExternalInput")
    oout = P.dram("o", [NCH, 128, 192], F32, kind="ExternalOutput")
    C = load_consts(P, ("idn", "U", "NEG", "ones", "POS"))
    idn = C["idn"]
    offs = (0, 128, 192, 320, 384, 512)
    nps = (128, 64, 128, 64, 128, 64)
    cw = []
    for i in range(6):
        a = P.sb([128, 5], name="cw%d" % i)
        P.dma(("sync", "act")[i % 2], a[0:nps[i], :], cwd.t.ap()[offs[i]:offs[i] + nps[i], :], reads=[cwd], writes=[a])
        cw.append(a)
    fl = lambda b: b[:].rearrange("p c h -> p (c h)")
    N3 = NCH * 3
    la = P.sb([128, NCH, 3], name="la")
    beta = P.sb([128, NCH, 3], name="beta")
    dtbb = P.sb([128, 3], name="dtbb")
    Ab = P.sb([128, 3], name="Ab")
    P.dma("sync", la[:], ard.t.ap(), reads=[ard], writes=[la])
    P.dma("pool", beta[:], brd.t.ap(), reads=[brd], writes=[beta])
    P.dma("act", dtbb[:], dtb.t.ap().partition_broadcast(128), reads=[dtb], writes=[dtbb])
    P.dma("pool", Ab[:], alog.t.ap().partition_broadcast(128), reads=[alog], writes=[Ab])
    P.act(Ab, Ab[:], Ab, Ab[:], AF.Exp)
    P.ts("dve", Ab, Ab[:], Ab, Ab[:], -1.0, None, ALU.mult)
    for h in range(3):
        P.ts("dve", la, la[:, :, h], la, la[:, :, h], dtbb[:, h:h + 1], None, ALU.add, rd=[dtbb])
    softplus_inplace(P, la, fl(la))
    for h in range(3):
        P.ts("dve", la, la[:, :, h], la, la[:, :, h], Ab[:, h:h + 1], None, ALU.mult, rd=[Ab])
    P.act(beta, fl(beta), beta, fl(beta), AF.Sigmoid)
    banks = [P.ps([128, 512], name="bank%d" % i) for i in range(8)]
    ac = P.sb([128, NCH, 3], name="ac")
    negac = P.sb([128, NCH, 3], name="negac")
    eac = P.sb([128, NCH, 3], name="eac")
    wj = P.sb([128, NCH, 3], name="wj")
    eL = P.sb([128, NCH, 3], name="eL")
    be = P.sb([128, NCH, 3], name="be")
    nbeta = P.sb([128, NCH, 3], name="nbeta")
    pC1, pC2 = banks[3], banks[4]
    P.mm(pC1, pC1[:, 0:N3], C["U"], C["U"][:], la, fl(la))
    P.mm(pC2, pC2[:, 0:N3], C["ones"], C["ones"][:], la, fl(la))
    P.cp("dve", ac, fl(ac), pC1, pC1[:, 0:N3])
    P.ts("dve", negac, fl(negac), ac, fl(ac), -1.0, None, ALU.mult)
    P.act(eac, fl(eac), ac, fl(ac), AF.Exp)
    P.act(eL, fl(eL), pC2, pC2[:, 0:N3], AF.Exp)
    P.tt("dve", wj, fl(wj), pC2, pC2[:, 0:N3], negac, fl(negac), ALU.add)
    P.act(wj, fl(wj), wj, fl(wj), AF.Exp)
    P.tt("dve", be, fl(be), beta, fl(beta), eac, fl(eac), ALU.mult)
    P.ts("dve", nbeta, fl(nbeta), beta, fl(beta), -1.0, None, ALU.mult)

    I3 = P.sb([128, 3, 128], name="I3")
    for h in range(3):
        P.cp("pool", I3, I3[:, h, :], idn, idn[:])
    S = P.sb([64, 192], name="S")
    P.op("dve", lambda e: e.memset(S[:], 0.0), writes=[S])

    ut = [[P.sb([128, 260], name="ut%d_%d" % (i, j)) for j in range(2)] for i in range(6)]
    acc = [P.sb([128, 256], name="acc%d" % i) for i in range(6)]
    cv = [[P.sb([128, 256], name="cv%d_%d" % (i, j)) for j in range(2)] for i in range(6)]
    qk = P.sb([128, 384], name="qk")
    vt = P.sb([128, 192], name="vt")
    sq = P.sb([128, 384], name="sq")
    rs = P.sb([128, 6], name="rs")
    qkn = P.sb([128, 384], name="qkn")
    kT = P.sb([64, 384], name="kT")
    qT = P.sb([64, 384], name="qT")
    kbe = P.sb([128, 192], name="kbe")
    vb = P.sb([128, 192], name="vb")
    kend = P.sb([128, 192], name="kend")
    rhs_t = [P.sb([128, 128], name="rhs%d" % i) for i in range(3)]
    decT = P.sb([128, 3, 128], name="decT")
    decS = P.sb([128, 3, 128], name="decS")
    Np = P.sb([128, 3, 128], name="Np")
    Mp = P.sb([128, 3, 128], name="Mp")
    Tt = P.sb([128, 3, 128], name="Tt")
    attnT = P.sb([128, 3, 128], name="attnT")
    usb = P.sb([128, 192], name="usb")
    wT = P.sb([64, 384], name="wT")
    vnew = P.sb([128, 192], name="vnew")
    o2 = P.sb([128, 192], name="o2")
    oo = [P.sb([128, 192], name="oo%d" % i) for i in range(2)]
    f3 = lambda b: b[:].rearrange("p h n -> p (h n)")
    H = lambda h: slice(h * 64, (h + 1) * 64)
    H2 = lambda h: slice(h * 128, (h + 1) * 128)

    for b in range(nblk):
        j = b % 2
        for i in range(6):
            P.dma(("sync", "act")[i % 2], ut[i][j][0:nps[i], :], u.t.ap()[offs[i]:offs[i] + nps[i], b, :],
                  reads=[u], writes=[ut[i][j]])
            conv_block(P, "dve", ut[i][j], acc[i], cv[i][j], cw[i], None, nps[i], bias=False)
        for cc in range(2):
            c = b * 2 + cc
            ck = slice(cc * 128, (cc + 1) * 128)
            pT1, pT2 = banks[0], banks[1]
            for a in range(3):
                pt = pT1 if a < 2 else pT2
                base = (a % 2) * 192
                t01, t2 = cv[2 * a][j], cv[2 * a + 1][j]
                P.tr(pt, pt[:, base:base + 128], t01, t01[:, ck], idn, idn[:])
                P.tr(pt, pt[:, base + 128:base + 192], t2, t2[0:64, ck], idn, idn[0:64, 0:64])
            P.cp("act", qk, qk[:], pT1, pT1[:, 0:384])
            P.cp("dve", vt, vt[:], pT2, pT2[:, 0:192])
            P.tt("pool", sq, sq[:], qk, qk[:], qk, qk[:], ALU.mult)
            P.op("dve", lambda e: e.reduce_sum(out=rs[:], in_=sq[:].rearrange("p (a d) -> p a d", d=64), axis=AX.X),
                 reads=[sq], writes=[rs])
            P.ts("dve", rs, rs[:], rs, rs[:], 1e-6, None, ALU.add)
            P.op("act", lambda e: e.sqrt(out=rs[:], in_=rs[:]), reads=[rs], writes=[rs])
            P.op("dve", lambda e: e.reciprocal(out=rs[:], in_=rs[:]), reads=[rs], writes=[rs])
            P.ts("dve", rs, rs[:, 0:3], rs, rs[:, 0:3], 0.125, None, ALU.mult)
            for a in range(6):
                P.ts(("dve", "pool")[a % 2], qkn, qkn[:, H(a)], qk, qk[:, H(a)], rs[:, a:a + 1], None, ALU.mult, rd=[rs])
            pKT, pQT = banks[2], banks[0]
            for h in range(3):
                P.tr(pKT, pKT[0:64, H2(h)], qkn, qkn[:, 192 + h * 64:192 + (h + 1) * 64], idn, idn[:])
                P.tr(pQT, pQT[0:64, H2(h)], qkn, qkn[:, h * 64:(h + 1) * 64], idn, idn[:])
            P.cp("act", kT, kT[:], pKT, pKT[0:64, 0:384])
            P.cp("dve", qT, qT[:], pQT, pQT[0:64, 0:384])
            for h in range(3):
                kn_h = qkn[:, 192 + h * 64:192 + (h + 1) * 64]
                P.ts("pool", kbe, kbe[:, H(h)], qkn, kn_h, be[:, c, h:h + 1], None, ALU.mult, rd=[be])
                P.ts("pool", vb, vb[:, H(h)], vt, vt[:, H(h)], beta[:, c, h:h + 1], None, ALU.mult, rd=[beta])
                P.ts("pool", kend, kend[:, H(h)], qkn, kn_h, wj[:, c, h:h + 1], None, ALU.mult, rd=[wj])
            pA1, pA2 = banks[0], banks[1]
            for h in range(3):
                r = rhs_t[h]
                P.ts(("dve", "pool")[h % 2], r, r[:], C["U"], C["U"][:], la[:, c, h:h + 1], None, ALU.mult, rd=[la])
                P.mm(pA1, pA1[:, H2(h)], C["ones"], C["ones"][:], r, r[:], start=True, stop=False)
                P.mm(pA1, pA1[:, H2(h)], idn, idn[:], C["NEG"], C["NEG"][:], start=False, stop=True)
                P.mm(pA2, pA2[:, H2(h)], C["ones"], C["ones"][:], r, r[:], start=True, stop=False)
                P.mm(pA2, pA2[:, H2(h)], idn, idn[:], C["POS"], C["POS"][:], start=False, stop=True)
            for h in range(3):
                P.act(decT, decT[:, h, :], pA1, pA1[:, H2(h)], AF.Exp, bias=negac[:, c, h:h + 1], rd=[negac])
                P.act(decS, decS[:, h, :], pA2, pA2[:, H2(h)], AF.Exp, bias=ac[:, c, h:h + 1], scale=-1.0, rd=[ac])
            pKK, pQK = banks[3], banks[4]
            for h in range(3):
                P.mm(pKK, pKK[:, H2(h)], kT, kT[:, H2(h)], kT, kT[:, H2(h)])
                P.mm(pQK, pQK[:, H2(h)], kT, kT[:, H2(h)], qT, qT[:, H2(h)])
            for h in range(3):
                P.stt("dve", Np, Np[:, h, :], pKK, pKK[:, H2(h)], nbeta[:, c, h:h + 1], decS, decS[:, h, :], ALU.mult, ALU.mult, rd=[nbeta])
                P.tt("dve", attnT, attnT[:, h, :], pQK, pQK[:, H2(h)], decT, decT[:, h, :], ALU.mult)
            pN, pM, pTt = banks[0], banks[1], banks[2]
            for h in range(3):
                P.tr(pM, pM[:, H2(h)], Np, Np[:, h, :], idn, idn[:])
            P.cp("act", Mp, f3(Mp), pM, pM[:, 0:384])
            P.tt("pool", Tt, f3(Tt), Mp, f3(Mp), I3, f3(I3), ALU.add)
            for step in range(6):
                last = step == 5
                for h in range(3):
                    P.mm(pN, pN[:, H2(h)], Mp, Mp[:, h, :], Np, Np[:, h, :])
                    if not last:
                        P.mm(pM, pM[:, H2(h)], Np, Np[:, h, :], Mp, Mp[:, h, :])
                P.cp("act", Np, f3(Np), pN, pN[:, 0:384])
                if not last:
                    P.cp("dve", Mp, f3(Mp), pM, pM[:, 0:384])
                for h in range(3):
                    P.mm(pTt, pTt[:, H2(h)], Np, Np[:, h, :], Tt, Tt[:, h, :])
                P.tt("dve", Tt, f3(Tt), Tt, f3(Tt), pTt, pTt[:, 0:384], ALU.add)
            pU, pWT = banks[0], banks[1]
            for h in range(3):
                P.mm(pU, pU[:, H(h)], Tt, Tt[:, h, :], vb, vb[:, H(h)])
                P.mm(pWT, pWT[0:64, H2(h)], kbe, kbe[:, H(h)], Tt, Tt[:, h, :])
            P.cp("act", usb, usb[:], pU, pU[:, 0:192])
            P.cp("dve", wT, wT[:], pWT, pWT[0:64, 0:384])
            pWS, pO1, pO2, pSn = banks[5], banks[6], banks[7], banks[5]
            for h in range(3):
                P.mm(pWS, pWS[:, H(h)], wT, wT[:, H2(h)], S, S[:, H(h)])
                P.mm(pO1, pO1[:, H(h)], qT, qT[:, H2(h)], S, S[:, H(h)])
            P.tt("dve", vnew, vnew[:], usb, usb[:], pWS, pWS[:, 0:192], ALU.subtract)
            for h in range(3):
                P.mm(pO2, pO2[:, H(h)], attnT, attnT[:, h, :], vnew, vnew[:, H(h)])
            for h in range(3):
                P.mm(pSn, pSn[0:64, H(h)], kend, kend[:, H(h)], vnew, vnew[:, H(h)])
            P.cp("act", o2, o2[:], pO2, pO2[:, 0:192])
            o_ = oo[c % 2]
            for h in range(3):
                P.stt("dve", o_, o_[:, H(h)], pO1, pO1[:, H(h)], eac[:, c, h:h + 1], o2, o2[:, H(h)], ALU.mult, ALU.add, rd=[eac])
            for h in range(3):
                P.stt("dve", S, S[:, H(h)], S, S[:, H(h)], eL[0:64, c, h:h + 1], pSn, pSn[0:64, H(h)], ALU.mult, ALU.add, rd=[eL])
            P.dma("sync", oout.t.ap()[c], o_[:], reads=[o_], writes=[oout])
    P.finish([oout])
    return nc


def to_cm(a):
    T, Cc = a.shape
    return a.reshape(T // GRID_W, GRID_W, Cc).transpose(1, 0, 2).reshape(T, Cc)


def from_cm(a):
    T, Cc = a.shape
    return a.reshape(GRID_W, T // GRID_W, Cc).transpose(1, 0, 2).reshape(T, Cc)


def prep_k2g(pl, pc, conv_w, a_log, dt_bias, core):
    s, d, g = core // 4, (core // 2) % 2, core % 2
    q0 = 1548
    chans = np.concatenate([a * 384 + g * 192 + np.arange(192) for a in range(3)])
    acol = 3084 + d * 6 + g * 3
    bcol = 3096 + d * 6 + g * 3
    segs, ars, brs = [], [], []
    for arr, cm in ((pc[s], False), (pl[s], True)):
        a = arr[:, q0 + chans]
        ar = arr[:, acol:acol + 3]
        br = arr[:, bcol:bcol + 3]
        if cm:
            a, ar, br = to_cm(a), to_cm(ar), to_cm(br)
        if d == 1:
            a, ar, br = a[::-1], ar[::-1], br[::-1]
        segs.append(windows(np.ascontiguousarray(a.T)))
        ars.append(ar)
        brs.append(br)
    cw = conv_w[:, chans].T
    if d == 1:
        cw = cw[:, ::-1]
    tm = lambda lst: np.ascontiguousarray(np.concatenate(lst, 0).reshape(NCH, 128, 3).transpose(1, 0, 2))
    m = {"u": np.ascontiguousarray(np.concatenate(segs, axis=1)), "cw": np.ascontiguousarray(cw),
         "araw": tm(ars), "braw": tm(brs), "dtb": np.ascontiguousarray(dt_bias[d, g * 3:g * 3 + 3]),
         "alog": np.ascontiguousarray(a_log[d, g * 3:g * 3 + 3])}
    m.update(consts_g())
    return m


PKW = 1024 + 256 + 7 * 384
POOL_WINDOWS = (2, 4, 8, 16)


def build_k3(NT, n_lat=None):
    if n_lat is None:
        n_lat = NT - 1
    nc = bass.Bass("TRN2", target_bir_lowering=False)
    P = Prog(nc)
    pk = P.dram("pk", [NT, 128, PKW], F32, kind="ExternalInput")
    halo = P.dram("halo", [NT, 16, 256], F32, kind="ExternalInput")
    band = P.dram("band", [NT, 144, 512], F32, kind="ExternalInput")
    wout = P.dram("wout", [D, D], F32, kind="ExternalInput")
    pwd = P.dram("pw", [64, 4, 128], F32, kind="ExternalInput")
    psd = P.dram("pscale", [128, 2], F32, kind="ExternalInput")
    rwd = P.dram("rw", [128, 8, 16], F32, kind="ExternalInput")
    idn = P.dram("idn", [128, 128], F32, kind="ExternalInput")
    v384 = {n: P.dram(n, [384], F32, kind="ExternalInput") for n in ("dvec", "sng", "gng")}
    v1k = {n: P.dram(n, [D], F32, kind="ExternalInput") for n in ("g1_l", "g1_c", "n2g", "sh_l", "sc_l", "sh_c", "sc_c")}
    x2o = P.dram("x2", [NT, 128, D], F32, kind="ExternalOutput")
    hTo = P.dram("h2T", [NT, 128, 8, 128], F32, kind="ExternalOutput")
    affo = P.dram("aff", [NT, 128, 16], F32, kind="ExternalOutput")

    ident = P.sb([128, 128], name="ident")
    P.dma("sync", ident[:], idn.t.ap(), reads=[idn], writes=[ident])
    pw = P.sb([64, 4, 128], name="pw")
    P.dma("act", pw[:], pwd.t.ap(), reads=[pwd], writes=[pw])
    psc = P.sb([128, 2], name="psc")
    P.dma("sync", psc[:], psd.t.ap(), reads=[psd], writes=[psc])
    rw = P.sb([128, 8, 16], name="rw")
    P.dma("act", rw[:], rwd.t.ap(), reads=[rwd], writes=[rw])
    b384 = {n: load_bcast(P, ("sync", "act")[i % 2], v384[n], 384, "b_" + n) for i, n in enumerate(v384)}
    b1k = {n: load_bcast(P, ("sync", "act")[i % 2], v1k[n], D, "b_" + n) for i, n in enumerate(v1k)}
    for n in ("sc_l", "sc_c"):
        A = b1k[n]
        P.stt("dve", A, A[:], A, A[:], 1.0, b1k["n2g"], b1k["n2g"][:], ALU.add, ALU.mult)
    W = P.sb([128, 8, D], BF16, name="W")
    stage = [P.sb([128, D], name="stg%d" % i) for i in range(2)]
    load_weight_bf16(P, lambda k: wout.t.ap()[k * 128:(k + 1) * 128, :], W, 8, D, stage, wout)

    pks = [P.sb([128, PKW], name="pk%d" % i) for i in range(2)]
    hls = [P.sb([16, 256], name="hl%d" % i) for i in range(2)]
    bds = [P.sb([128, 512], name="bd%d" % i) for i in range(2)]
    bd2s = [P.sb([16, 512], name="bdh%d" % i) for i in range(2)]
    dT = P.sb([64, 4, 128], name="dT")
    mixT = [P.sb([128, 8, 128], BF16, name="mixT%d" % i) for i in range(2)]
    t1 = P.sb([128, 384], name="t1")
    ys = P.sb([128, 384], name="ys")
    sz = P.sb([128, 384], name="sz")
    scr3 = P.sb([128, 384], name="scr3")
    ss1 = P.sb([128, 1], name="ss1")
    yo = P.sb([128, 768], name="yo")
    og = P.sb([128, 384], name="og")
    sq = P.sb([128, 384], name="sq")
    rs6 = P.sb([128, 6], name="rs6")
    sg = P.sb([128, 384], name="sg")
    mx = P.sb([128, 512], name="mx")
    x2s = [P.sb([128, D], name="x2_%d" % i) for i in range(2)]
    h2s = [P.sb([128, D], name="h2_%d" % i) for i in range(2)]
    scr = P.sb([128, D], name="scr")
    ss2 = P.sb([128, 1], name="ss2")
    h2Ts = [P.sb([128, 8, 128], name="h2T%d" % i) for i in range(2)]
    lg = P.sb([128, 16], name="lg")
    ex = P.sb([128, 16], name="ex")
    m1 = P.sb([128, 1], name="m1")
    s1 = P.sb([128, 1], name="s1")
    afs = [P.sb([128, 16], name="af%d" % i) for i in range(2)]
    pD, pP, pr = P.ps([1], name="pD"), P.ps([1], name="pP"), P.ps([1], name="pr")
    pts = [P.ps([1], name="pt%d" % i) for i in range(2)]
    pms = [P.ps([1], name="pm%d" % i) for i in range(2)]

    for t in range(NT):
        lat = t < n_lat
        pk_ = pks[t % 2]
        hl, bd, bd2, mT = hls[t % 2], bds[t % 2], bd2s[t % 2], mixT[t % 2]
        P.dma("sync", pk_[:], pk.t.ap()[t], reads=[pk], writes=[pk_])
        P.dma("act", hl[:], halo.t.ap()[t], reads=[halo], writes=[hl])
        P.dma("act", bd[:], band.t.ap()[t, 0:128, :], reads=[band], writes=[bd])
        P.dma("act", bd2[:], band.t.ap()[t, 128:144, :], reads=[band], writes=[bd2])
        xo, uo = 0, 1024
        yf, yb, xs, z, of, ob, gt = [slice(1280 + i * 384, 1280 + (i + 1) * 384) for i in range(7)]
        for g in range(4):
            P.mm(pD, pD[0:64, g * 128:(g + 1) * 128], pk_, pk_[:, uo + g * 64:uo + (g + 1) * 64], bd, bd[:, g * 128:(g + 1) * 128],
                 start=True, stop=False)
            P.mm(pD, pD[0:64, g * 128:(g + 1) * 128], hl, hl[:, g * 64:(g + 1) * 64], bd2, bd2[:, g * 128:(g + 1) * 128],
                 start=False, stop=True)
        P.cp("act", dT, dT[:].rearrange("p g t -> p (g t)"), pD, pD[0:64, :])
        for cch in range(2):
            for gg in range(2):
                g = cch * 2 + gg
                P.mm(pP, pP[:, cch * 128:(cch + 1) * 128], pw, pw[:, g, :], dT, dT[:, g, :], start=(gg == 0), stop=(gg == 1))
        for cch in range(2):
            P.ts("dve", mT, mT[:, cch, :], pP, pP[:, cch * 128:(cch + 1) * 128], psc[:, cch:cch + 1], None, ALU.mult, rd=[psc])
        P.tt("pool", ys, ys[:], pk_, pk_[:, yf], pk_, pk_[:, yb], ALU.add)
        P.tt("pool", t1, t1[:], pk_, pk_[:, xs], b384["dvec"], b384["dvec"][:], ALU.mult)
        P.tt("pool", ys, ys[:], ys, ys[:], t1, t1[:], ALU.add)
        P.act(sz, sz[:], pk_, pk_[:, z], AF.Silu)
        P.tt("dve", ys, ys[:], ys, ys[:], sz, sz[:], ALU.mult)
        P.act(scr3, scr3[:], ys, ys[:], AF.Square, scale=float(384 ** -0.5), accum=ss1[:], wr=[ss1])
        P.ts("dve", ss1, ss1[:], ss1, ss1[:], 1e-6, None, ALU.add)
        P.op("act", lambda e: e.sqrt(out=ss1[:], in_=ss1[:]), reads=[ss1], writes=[ss1])
        P.op("dve", lambda e: e.reciprocal(out=ss1[:], in_=ss1[:]), reads=[ss1], writes=[ss1])
        P.stt("dve", yo, yo[:, 0:384], ys, ys[:], ss1[:, 0:1], b384["sng"], b384["sng"][:], ALU.mult, ALU.mult, rd=[ss1])
        P.tt("pool", og, og[:], pk_, pk_[:, of], pk_, pk_[:, ob], ALU.add)
        P.tt("pool", sq, sq[:], og, og[:], og, og[:], ALU.mult)
        P.op("dve", lambda e: e.reduce_sum(out=rs6[:], in_=sq[:].rearrange("p (a d) -> p a d", d=64), axis=AX.X),
             reads=[sq], writes=[rs6])
        P.ts("dve", rs6, rs6[:], rs6, rs6[:], 1.0 / 64.0, 1e-6, ALU.mult, ALU.add)
        P.op("act", lambda e: e.sqrt(out=rs6[:], in_=rs6[:]), reads=[rs6], writes=[rs6])
        P.op("dve", lambda e: e.reciprocal(out=rs6[:], in_=rs6[:]), reads=[rs6], writes=[rs6])
        for a in range(6):
            P.ts(("dve", "pool")[a % 2], og, og[:, a * 64:(a + 1) * 64], og, og[:, a * 64:(a + 1) * 64], rs6[:, a:a + 1], None, ALU.mult, rd=[rs6])
        P.act(sg, sg[:], pk_, pk_[:, gt], AF.Silu)
        P.tt("pool", og, og[:], og, og[:], b384["gng"], b384["gng"][:], ALU.mult)
        P.tt("dve", yo, yo[:, 384:768], og, og[:], sg, sg[:], ALU.mult)
        for half in range(2):
            pt = pts[half]
            for q in range(3):
                k = half * 3 + q
                P.tr(pt, pt[:, q * 128:(q + 1) * 128], yo, yo[:, k * 128:(k + 1) * 128], ident, ident[:])
            P.cp(("act", "dve")[half], mT, mT[:, 2 + half * 3:5 + half * 3, :], pt, pt[:, 0:384].rearrange("p (k t) -> p k t", k=3))
        x2 = x2s[t % 2]
        g1 = b1k["g1_l"] if lat else b1k["g1_c"]
        for cb in range(2):
            pm = pms[cb]
            cs = slice(cb * 512, (cb + 1) * 512)
            for k in range(8):
                P.mm(pm, pm[:, :], mT, mT[:, k, :], W, W[:, k, cs], start=(k == 0), stop=(k == 7))
            P.tt("dve", mx, mx[:], pm, pm[:, :], g1, g1[:, cs], ALU.mult)
            P.tt("pool", x2, x2[:, cs], mx, mx[:], pk_, pk_[:, cb * 512:(cb + 1) * 512], ALU.add)
        P.dma("sync", x2o.t.ap()[t], x2[:], reads=[x2], writes=[x2o])
        h2, h2T = h2s[t % 2], h2Ts[t % 2]
        A2, B2 = (b1k["sc_l"], b1k["sh_l"]) if lat else (b1k["sc_c"], b1k["sh_c"])
        norm_mod_tile(P, x2, h2, A2, B2, scr, ss2)
        transpose_tile(P, h2, h2T, ident, pts)
        P.dma("sync", hTo.t.ap()[t], h2T[:], reads=[h2T], writes=[hTo])
        for k in range(8):
            P.mm(pr, pr[:, 0:16], h2T, h2T[:, k, :], rw, rw[:, k, :], start=(k == 0), stop=(k == 7))
        P.cp("act", lg, lg[:], pr, pr[:, 0:16])
        P.op("dve", lambda e: e.reduce_max(out=m1[:], in_=lg[:], axis=AX.X), reads=[lg], writes=[m1])
        P.ts("dve", m1, m1[:], m1, m1[:], -1.0, None, ALU.mult)
        P.act(ex, ex[:], lg, lg[:], AF.Exp, bias=m1[:, 0:1], accum=s1[:], rd=[m1], wr=[s1])
        P.op("dve", lambda e: e.reciprocal(out=s1[:], in_=s1[:]), reads=[s1], writes=[s1])
        af = afs[t % 2]
        P.ts("dve", af, af[:], ex, ex[:], s1[:, 0:1], None, ALU.mult, rd=[s1])
        P.dma("sync", affo.t.ap()[t], af[:], reads=[af], writes=[affo])
    P.finish([x2o, hTo, affo])
    return nc


def pool_band(pos, T):
    src = np.concatenate([pos, pos[0] - 8 + np.arange(8), pos[-1] + 1 + np.arange(8)])
    out = np.zeros((144, 4, 128), np.float32)
    for g, w in enumerate(POOL_WINDOWS):
        lo = np.clip(pos - w // 2, 0, T)
        hi = np.clip(pos + w // 2, 0, T)
        cnt = np.maximum(hi - lo, 1).astype(np.float32)
        m = (src[:, None] >= lo[None, :]) & (src[:, None] < hi[None, :])
        out[:, g, :] = m / cnt[None, :]
        out[np.arange(128), g, np.arange(128)] -= 1.0
    return out.reshape(144, 512)


NE = 16
FF = 512


def bisect_threshold(P, affs, J, kcap, ones, pcnt, name):
    lo = P.sb([128, NE], name=name + "_lo")
    hi = P.sb([128, NE], name=name + "_hi")
    mid = P.sb([128, NE], name=name + "_mid")
    cnt = P.sb([128, NE], name=name + "_cnt")
    pred = P.sb([128, NE], name=name + "_pred")
    tmp = P.sb([128, NE], name=name + "_tmp")
    cmp_ = P.sb([128, NE, J], name=name + "_cmp")
    P.op("dve", lambda e: e.memset(lo[:], 0.0), writes=[lo])
    P.op("dve", lambda e: e.memset(hi[:], 1.0), writes=[hi])
    for it in range(34):
        P.tt("dve", mid, mid[:], lo, lo[:], hi, hi[:], ALU.add)
        P.ts("dve", mid, mid[:], mid, mid[:], 0.5, None, ALU.mult)
        for e_ in range(NE):
            P.ts(("dve", "pool")[e_ % 2], cmp_, cmp_[:, e_, :], affs, affs[:, e_, :], mid[:, e_:e_ + 1], None, ALU.is_ge, rd=[mid])
        P.op("dve", lambda e: e.reduce_sum(out=cnt[:], in_=cmp_[:], axis=AX.X), reads=[cmp_], writes=[cnt])
        P.mm(pcnt, pcnt[:, 0:NE], ones, ones[:], cnt, cnt[:])
        P.ts("dve", pred, pred[:], pcnt, pcnt[:, 0:NE], float(kcap) - 0.5, None, ALU.is_ge)
        P.tt("dve", tmp, tmp[:], mid, mid[:], lo, lo[:], ALU.subtract)
        P.tt("dve", tmp, tmp[:], tmp, tmp[:], pred, pred[:], ALU.mult)
        P.tt("dve", lo, lo[:], lo, lo[:], tmp, tmp[:], ALU.add)
        P.tt("dve", tmp, tmp[:], hi, hi[:], mid, mid[:], ALU.subtract)
        P.tt("dve", tmp, tmp[:], tmp, tmp[:], pred, pred[:], ALU.mult)
        P.tt("dve", hi, hi[:], mid, mid[:], tmp, tmp[:], ALU.add)
    return lo


def build_k4(NT, n_lat, kcap_lat, kcap_ctx, J_lat, J_ctx, final_norm, passes):
    nc = bass.Bass("TRN2", target_bir_lowering=False)
    P = Prog(nc)
    x2d = P.dram("x2", [NT, 128, D], F32, kind="ExternalInput")
    hTd = P.dram("h2T", [NT, 128, 8, 128], F32, kind="ExternalInput")
    afd = P.dram("aff", [NT, 128, NE], F32, kind="ExternalInput")
    asl = P.dram("affs_l", [128, NE, J_lat], F32, kind="ExternalInput")
    asc = P.dram("affs_c", [128, NE, J_ctx], F32, kind="ExternalInput")
    wgd = P.dram("wg", [NE, D, FF], F32, kind="ExternalInput")
    wud = P.dram("wu", [NE, D, FF], F32, kind="ExternalInput")
    wdd = P.dram("wd", [NE, FF, D], F32, kind="ExternalInput")
    onesd = P.dram("ones", [128, 128], F32, kind="ExternalInput")
    v1k = {n: P.dram(n, [D], F32, kind="ExternalInput") for n in ("g2_l", "g2_c", "fng")}
    outd = P.dram("out", [NT, 128, D], F32, kind="ExternalOutput")

    ones = P.sb([128, 128], name="ones")
    P.dma("sync", ones[:], onesd.t.ap(), reads=[onesd], writes=[ones])
    b1k = {n: load_bcast(P, ("sync", "act")[i % 2], v1k[n], D, "b_" + n) for i, n in enumerate(v1k)}
    pcnt = P.ps([1], name="pcnt")
    affs_l = P.sb([128, NE, J_lat], name="affs_l")
    P.dma("sync", affs_l[:], asl.t.ap(), reads=[asl], writes=[affs_l])
    thr_l = bisect_threshold(P, affs_l, J_lat, kcap_lat, ones, pcnt, "bl")
    thr_c = None
    if n_lat < NT:
        affs_c = P.sb([128, NE, J_ctx], name="affs_c")
        P.dma("act", affs_c[:], asc.t.ap(), reads=[asc], writes=[affs_c])
        thr_c = bisect_threshold(P, affs_c, J_ctx, kcap_ctx, ones, pcnt, "bc")
    afo = P.sb([128, NT, NE], name="afo")
    gw = P.sb([128, NT, NE], name="gw")
    P.dma("sync", afo[:], afd.t.ap().rearrange("t p e -> p t e"), reads=[afd], writes=[afo])
    for t in range(NT):
        thr = thr_l if t < n_lat else thr_c
        P.tt("dve", gw, gw[:, t, :], afo, afo[:, t, :], thr, thr[:], ALU.is_ge)
        P.tt("dve", gw, gw[:, t, :], gw, gw[:, t, :], afo, afo[:, t, :], ALU.mult)

    maxt = max(sum(t1 - t0 for (t0, t1) in ps_) for ps_ in passes)
    hT = P.sb([128, 8, maxt * 128], BF16, name="hT")
    hst = [P.sb([128, 8, 128], name="hst%d" % i) for i in range(2)]
    acc = P.sb([128, maxt, D], name="acc")
    Wg = [P.sb([128, 8, FF], BF16, name="Wg%d" % i) for i in range(2)]
    Wu = [P.sb([128, 8, FF], BF16, name="Wu%d" % i) for i in range(2)]
    Wd = [P.sb([128, 4, D], BF16, name="Wd%d" % i) for i in range(2)]
    stg = [P.sb([128, 2048], name="stg%d" % i) for i in range(2)]
    sil = [P.sb([128, 512], name="sil%d" % i) for i in range(2)]
    hidT = [P.sb([128, 4, 512], BF16, name="hidT%d" % i) for i in range(2)]
    xt = [P.sb([128, D], name="xt%d" % i) for i in range(2)]
    scr = P.sb([128, D], name="scr")
    ssf = P.sb([128, 1], name="ssf")
    pg = [P.ps([1], name="pg%d" % i) for i in range(2)]
    pu = [P.ps([1], name="pu%d" % i) for i in range(2)]
    pd = [P.ps([1], name="pd%d" % i) for i in range(2)]
    sti = 0
    wi = 0
    gi = 0
    for ps_ in passes:
        tiles = [t for (t0, t1) in ps_ for t in range(t0, t1)]
        loc = {t: i for i, t in enumerate(tiles)}
        for i, t in enumerate(tiles):
            h_ = hst[i % 2]
            P.dma(("sync", "act")[i % 2], h_[:], hTd.t.ap()[t], reads=[hTd], writes=[h_])
            P.cp(("pool", "act")[i % 2], hT, hT[:, :, i * 128:(i + 1) * 128], h_, h_[:])
        P.op("pool", lambda e: e.memset(acc[:], 0.0), writes=[acc])
        for ex in range(NE):
            wg_, wu_, wd_ = Wg[wi % 2], Wu[wi % 2], Wd[wi % 2]
            wi += 1
            for (wt, src, nk) in ((wg_, wgd, 8), (wu_, wud, 8), (wd_, wdd, 4)):
                for hf in range(2):
                    s_ = stg[sti % 2]
                    sti += 1
                    k0, k1 = hf * nk // 2, (hf + 1) * nk // 2
                    sv = s_[:].rearrange("p (k f) -> p k f", k=nk // 2)
                    P.dma(("sync", "act")[sti % 2], sv, src.t.ap()[ex].rearrange("(k p) f -> p k f", p=128)[:, k0:k1, :],
                          reads=[src], writes=[s_])
                    P.cp(("pool", "act", "dve")[sti % 3], wt, wt[:, k0:k1, :], s_, sv)
            for (t0, t1) in ps_:
                n = (t1 - t0) * 128
                c0 = loc[t0] * 128
                hd = hidT[gi % 2]
                gi += 1
                for fc in range(4):
                    pg_, pu_, sl_ = pg[fc % 2], pu[fc % 2], sil[fc % 2]
                    for k in range(8):
                        P.mm(pg_, pg_[:, 0:n], wg_, wg_[:, k, fc * 128:(fc + 1) * 128], hT, hT[:, k, c0:c0 + n], start=(k == 0), stop=(k == 7))
                    for k in range(8):
                        P.mm(pu_, pu_[:, 0:n], wu_, wu_[:, k, fc * 128:(fc + 1) * 128], hT, hT[:, k, c0:c0 + n], start=(k == 0), stop=(k == 7))
                    P.act(sl_, sl_[:, 0:n], pg_, pg_[:, 0:n], AF.Silu)
                    P.tt("dve", hd, hd[:, fc, 0:n], pu_, pu_[:, 0:n], sl_, sl_[:, 0:n], ALU.mult)
                for t in range(t0, t1):
                    i = loc[t]
                    tl = slice((t - t0) * 128, (t - t0 + 1) * 128)
                    for half in range(2):
                        pd_ = pd[half]
                        cs = slice(half * 512, (half + 1) * 512)
                        for fc in range(4):
                            P.mm(pd_, pd_[:, :], hd, hd[:, fc, tl], wd_, wd_[:, fc, cs], start=(fc == 0), stop=(fc == 3))
                        P.stt("dve", acc, acc[:, i, cs], pd_, pd_[:, :], gw[:, t, ex:ex + 1], acc, acc[:, i, cs], ALU.mult, ALU.add, rd=[gw])
        for t in tiles:
            i = loc[t]
            x_ = xt[t % 2]
            g2 = b1k["g2_l"] if t < n_lat else b1k["g2_c"]
            P.dma("act", x_[:], x2d.t.ap()[t], reads=[x2d], writes=[x_])
            P.tt("pool", acc, acc[:, i, :], acc, acc[:, i, :], g2, g2[:], ALU.mult)
            P.tt("pool", x_, x_[:], x_, x_[:], acc, acc[:, i, :], ALU.add)
            if final_norm:
                P.act(scr, scr[:], x_, x_[:], AF.Square, scale=1.0 / 32.0, accum=ssf[:], wr=[ssf])
                P.ts("dve", ssf, ssf[:], ssf, ssf[:], 1e-6, None, ALU.add)
                P.op("act", lambda e: e.sqrt(out=ssf[:], in_=ssf[:]), reads=[ssf], writes=[ssf])
                P.op("dve", lambda e: e.reciprocal(out=ssf[:], in_=ssf[:]), reads=[ssf], writes=[ssf])
                P.stt("dve", x_, x_[:], x_, x_[:], ssf[:, 0:1], b1k["fng"], b1k["fng"][:], ALU.mult, ALU.mult, rd=[ssf])
            P.dma("sync", outd.t.ap()[t], x_[:], reads=[x_], writes=[outd])
    P.finish([outd])
    return nc


B, T, CTX = 2, 16384, 256
NLT = 32


def tiles_of(lat, ctx, core, with_ctx=True):
    s, q = core // 4, core % 4
    Cc = lat.shape[-1]
    lt = lat[s, q * 4096:(q + 1) * 4096].reshape(NLT, 128, Cc)
    if not with_ctx:
        return np.ascontiguousarray(lt)
    ct = np.zeros((1, 128, Cc), np.float32)
    n = min(128, CTX - q * 64)
    ct[0, :n] = ctx[s, q * 64:q * 64 + n]
    return np.concatenate([lt, ct], 0)


def untile(res, key, Cc, with_ctx=True):
    lat = np.zeros((B, T, Cc), np.float32)
    ctx = np.zeros((B, CTX, Cc), np.float32)
    for core in range(NCORE):
        s, q = core // 4, core % 4
        r = res[core][key]
        lat[s, q * 4096:(q + 1) * 4096] = r[:NLT].reshape(4096, Cc)
        if with_ctx:
            ctx[s, q * 64:(q + 1) * 64] = r[NLT, :64]
    return lat, ctx


def unseq(y, d, cm):
    y = y.reshape(NCH * 128, -1)
    c_, l_ = y[:CTX], y[CTX:]
    if d == 1:
        c_, l_ = c_[::-1], l_[::-1]
    if cm:
        l_ = from_cm(l_)
    return c_, l_


_band_cache = {}


def band_for(pos0, Tseq):
    key = (pos0 if (pos0 == 0 or pos0 + 128 + 8 > Tseq) else -1, Tseq)
    if key not in _band_cache:
        _band_cache[key] = pool_band(pos0 + np.arange(128), Tseq)
    return _band_cache[key]


def halo_for(u, pos0):
    Tseq = u.shape[0]
    h = np.zeros((16, 256), np.float32)
    for i in range(8):
        a = pos0 - 8 + i
        if 0 <= a < Tseq:
            h[i] = u[a]
        b_ = pos0 + 128 + i
        if 0 <= b_ < Tseq:
            h[8 + i] = u[b_]
    return h


def stage_proj(l, x, ctx, mods, p):
    f32 = _f32
    m = mods[l]
    seg = lambda r, i: f32(m[r, i * 1024:(i + 1) * 1024])
    idn = np.eye(128, dtype=np.float32)
    in_maps = []
    for core in range(NCORE):
        s = core // 4
        in_maps.append({"xt": tiles_of(x, ctx, core), "w": f32(p["w_in"][l]), "g": f32(p["norm1_g"][l]),
                        "sh_l": seg(s, 0), "sc_l": seg(s, 1), "sh_c": seg(2, 0), "sc_c": seg(2, 1), "idn": idn})
    res = run(build_k1(NLT + 1), in_maps)
    return untile(res, "out", 3108)


def stage_scans(l, pl, pc, p):
    f32 = _f32
    res = run(build_k2s(), [prep_k2s(pl, pc, f32(p["ssd_conv_w"][l]), f32(p["ssd_conv_b"][l]), f32(p["ssd_a_log"][l]),
                                     f32(p["ssd_dt_bias"][l]), core) for core in range(NCORE)])
    ys_l = np.zeros((B, 2, T, 384), np.float32)
    ys_c = np.zeros((B, 2, CTX, 384), np.float32)
    xs_l = np.zeros((B, T, 384), np.float32)
    xs_c = np.zeros((B, CTX, 384), np.float32)
    for core in range(NCORE):
        s, d, g = core // 4, (core // 2) % 2, core % 2
        c_, l_ = unseq(res[core]["y"], d, False)
        ys_l[s, d, :, g * 192:(g + 1) * 192] = l_
        ys_c[s, d, :, g * 192:(g + 1) * 192] = c_
        if d == 0:
            c_, l_ = unseq(res[core]["xs"], 0, False)
            xs_l[s, :, g * 192:(g + 1) * 192] = l_
            xs_c[s, :, g * 192:(g + 1) * 192] = c_
    del res
    res = run(build_k2g(), [prep_k2g(pl, pc, f32(p["gdn_conv_w"][l]), f32(p["gdn_a_log"][l]), f32(p["gdn_dt_bias"][l]), core)
                            for core in range(NCORE)])
    os_l = np.zeros((B, 2, T, 384), np.float32)
    os_c = np.zeros((B, 2, CTX, 384), np.float32)
    for core in range(NCORE):
        s, d, g = core // 4, (core // 2) % 2, core % 2
        c_, l_ = unseq(res[core]["o"], d, True)
        os_l[s, d, :, g * 192:(g + 1) * 192] = l_
        os_c[s, d, :, g * 192:(g + 1) * 192] = c_
    return ys_l, ys_c, xs_l, xs_c, os_l, os_c


def stage_post(l, x, ctx, pl, pc, scans, mods, p):
    f32 = _f32
    ys_l, ys_c, xs_l, xs_c, os_l, os_c = scans
    m = mods[l]
    seg = lambda r, i: f32(m[r, i * 1024:(i + 1) * 1024])
    idn = np.eye(128, dtype=np.float32)
    pwp = np.zeros((64, 4, 128), np.float32)
    for g in range(4):
        pwp[:, g, (g % 2) * 64:(g % 2) * 64 + 64] = p["pool_w"][l][g]
    common = {"wout": f32(p["w_out"][l]), "pw": pwp, "pscale": f32(np.asarray(p["pool_scale"][l]).reshape(2, 128).T),
              "rw": f32(np.asarray(p["router_w"][l]).reshape(8, 128, 16).transpose(1, 0, 2)), "idn": idn,
              "dvec": f32(np.repeat(np.asarray(p["ssd_d"][l]), 64)), "sng": f32(p["ssd_norm_g"][l]),
              "gng": f32(np.tile(np.asarray(p["gdn_norm_g"][l]), 6)), "n2g": f32(p["norm2_g"][l])}
    in_maps = []
    for core in range(NCORE):
        s, q = core // 4, core % 4
        ls = slice(q * 4096, (q + 1) * 4096)
        lat_p = np.concatenate([x[s, ls], pl[s, ls, 0:256], ys_l[s, 0, ls], ys_l[s, 1, ls], xs_l[s, ls], pl[s, ls, 256:640],
                                os_l[s, 0, ls], os_l[s, 1, ls], pl[s, ls, 2700:3084]], axis=-1).reshape(NLT, 128, PKW)
        n = min(128, CTX - q * 64)
        cs_ = slice(q * 64, q * 64 + n)
        ctx_p = np.zeros((1, 128, PKW), np.float32)
        ctx_p[0, :n] = np.concatenate([ctx[s, cs_], pc[s, cs_, 0:256], ys_c[s, 0, cs_], ys_c[s, 1, cs_], xs_c[s, cs_],
                                       pc[s, cs_, 256:640], os_c[s, 0, cs_], os_c[s, 1, cs_], pc[s, cs_, 2700:3084]], axis=-1)
        halo = np.stack([halo_for(pl[s, :, 0:256], q * 4096 + i * 128) for i in range(NLT)] + [halo_for(pc[s, :, 0:256], q * 64)])
        band = np.stack([band_for(q * 4096 + i * 128, T) for i in range(NLT)] + [band_for(q * 64, CTX)])
        d_ = {"pk": np.ascontiguousarray(np.concatenate([lat_p, ctx_p], 0)), "halo": f32(halo), "band": f32(band),
              "g1_l": seg(s, 2), "g1_c": seg(2, 2), "sh_l": seg(s, 3), "sc_l": seg(s, 4), "sh_c": seg(2, 3), "sc_c": seg(2, 4)}
        d_.update(common)
        in_maps.append(d_)
    return run(build_k3(NLT + 1), in_maps)


def stage_moe(l, res3, mods, p, last):
    f32 = _f32
    m = mods[l]
    seg = lambda r, i: f32(m[r, i * 1024:(i + 1) * 1024])
    ones = np.ones((128, 128), np.float32)
    aff_l, aff_c = untile(res3, "aff", 16)
    with_ctx = not last
    NT = NLT + 1 if with_ctx else NLT
    passes = [[(q * 8, q * 8 + 4), (q * 8 + 4, q * 8 + 8)] for q in range(4)]
    if with_ctx:
        passes[3].append((NLT, NLT + 1))
    in_maps = []
    for core in range(NCORE):
        s = core // 4
        in_maps.append({"x2": f32(res3[core]["x2"][:NT]), "h2T": f32(res3[core]["h2T"][:NT]), "aff": f32(res3[core]["aff"][:NT]),
                        "affs_l": f32(aff_l[s].reshape(128, 128, 16).transpose(0, 2, 1)),
                        "affs_c": f32(aff_c[s].reshape(128, 2, 16).transpose(0, 2, 1)),
                        "wg": f32(p["exp_w_gate"][l]), "wu": f32(p["exp_w_up"][l]), "wd": f32(p["exp_w_down"][l]), "ones": ones,
                        "g2_l": seg(s, 5), "g2_c": seg(2, 5), "fng": f32(p["final_norm_g"])})
    res = run(build_k4(NT, NLT, 2 * T // 16, 2 * CTX // 16, 128, 2, last, passes), in_maps)
    return untile(res, "out", 1024, with_ctx=with_ctx)


def _f32(a):
    return np.ascontiguousarray(np.asarray(a, dtype=np.float32))


def kernel(**p):
    x, ctx = _f32(p["x"]), _f32(p["ctx"])
    L = p["ada_w"].shape[0]
    mods = run_k0(_f32(p["c"]), _f32(p["c_ctx"]), _f32(p["ada_w"]), _f32(p["ada_b"]))
    for l in range(L):
        last = l == L - 1
        pl, pc = stage_proj(l, x, ctx, mods, p)
        scans = stage_scans(l, pl, pc, p)
        res3 = stage_post(l, x, ctx, pl, pc, scans, mods, p)
        del scans, pl, pc
        x, ctx_new = stage_moe(l, res3, mods, p, last)
        del res3
        if not last:
            ctx = ctx_new
    return x
```

```python
import contextlib
import numpy as np
import concourse.bass as bass
import concourse.mybir as mybir
from concourse.bass_utils import run_bass_kernel_spmd

F32 = mybir.dt.float32
BF16 = mybir.dt.bfloat16
ALU = mybir.AluOpType
AF = mybir.ActivationFunctionType
AX = mybir.AxisListType


class Buf:
    def __init__(self, t=None, name=""):
        self.t = t
        self.name = name
        self.w = None
        self.r = []
        self.excl = False

    def __getitem__(self, k):
        return self.t[k]


class Prog:
    ENGS = ("sync", "act", "dve", "pool", "pe")

    def __init__(self, nc, n_dma_sems=40):
        self.nc = nc
        self.es = contextlib.ExitStack()
        self.q = {e: [] for e in self.ENGS}
        self.EPOCH = 4000
        self.esem = {}
        self.seq = {e: 0 for e in ("act", "dve", "pool", "pe")}
        self.dsem = [nc.alloc_semaphore("ds_%d" % i) for i in range(n_dma_sems)]
        self.dcnt = [0] * n_dma_sems
        self.dlast = [None] * n_dma_sems
        self.dnext = 0
        self.waited = {e: {} for e in self.ENGS}
        self.nbuf = 0

    def sb(self, shape, dtype=F32, name=None):
        self.nbuf += 1
        name = "s_" + (name or "sb%d" % self.nbuf)
        t = self.es.enter_context(self.nc.sbuf_tensor(name, list(shape), dtype))
        return Buf(t, name)

    def ps(self, shape, dtype=F32, name=None):
        self.nbuf += 1
        name = "p_" + (name or "ps%d" % self.nbuf)
        t = self.es.enter_context(self.nc.psum_tensor(name, [128, 512], F32))
        b = Buf(t, name)
        b.excl = True
        return b

    def dram(self, name, shape, dtype=F32, kind="Internal"):
        t = self.nc.dram_tensor(name, list(shape), dtype, kind=kind)
        return Buf(t, name)

    def _need(self, eng, reads, writes):
        evs = []
        for b in reads:
            if b.w is not None:
                evs.append(b.w)
            if b.excl:
                evs.extend(b.r)
        for b in writes:
            if b.w is not None:
                evs.append(b.w)
            evs.extend(b.r)
        best = {}
        for (k, v) in evs:
            if eng == "pe" and k[0] == "pe":
                continue
            if best.get(k, 0) < v:
                best[k] = v
        out = []
        wd = self.waited[eng]
        for k, v in best.items():
            if wd.get(k, 0) >= v:
                continue
            wd[k] = v
            out.append((k, v))
        return out

    def _mark(self, ev, reads, writes):
        for b in reads:
            b.r.append(ev)
        for b in writes:
            b.w = ev
            b.r = []

    def op(self, eng, fn, reads=(), writes=()):
        waits = self._need(eng, reads, writes)
        ep, v = divmod(self.seq[eng], self.EPOCH)
        self.seq[eng] += 1
        key = (eng, ep)
        if key not in self.esem:
            self.esem[key] = self.nc.alloc_semaphore("es_%s_%d" % key)
        ev = (key, v + 1)
        self._mark(ev, reads, writes)
        self.q[eng].append((waits, fn, (key, 1)))

    def dma(self, queue, out, in_, reads=(), writes=(), **kw):
        i = self.dnext
        self.dnext = (self.dnext + 1) % len(self.dsem)
        waits = self._need(queue, reads, writes)
        key = ("d", i)
        if self.dcnt[i] > 0 and self.waited[queue].get(key, 0) < self.dcnt[i]:
            self.waited[queue][key] = self.dcnt[i]
            waits.append((key, self.dcnt[i]))
        self.dcnt[i] += 16
        ev = (key, self.dcnt[i])
        self._mark(ev, reads, writes)
        self.q[queue].append((waits, lambda e: e.dma_start(out=out, in_=in_, **kw), (key, 16)))
        return ev

    def mm(self, O, o, A, a, B, b, start=True, stop=True):
        self.op("pe", lambda e: e.matmul(o, lhsT=a, rhs=b, start=start, stop=stop), reads=[A, B], writes=[O])

    def tr(self, O, o, A, a, I, i):
        self.op("pe", lambda e: e.transpose(out=o, in_=a, identity=i), reads=[A, I], writes=[O])

    def act(self, O, o, A, a, func, bias=None, scale=1.0, accum=None, rd=(), wr=()):
        kw = {}
        if bias is not None:
            kw["bias"] = bias
        if accum is not None:
            kw["accum_out"] = accum
        self.op("act", lambda e: e.activation(out=o, in_=a, func=func, scale=scale, **kw),
                reads=[A] + list(rd), writes=[O] + list(wr))

    def tt(self, eng, O, o, A, a, B, b, op):
        self.op(eng, lambda e: e.tensor_tensor(out=o, in0=a, in1=b, op=op), reads=[A, B], writes=[O])

    def ts(self, eng, O, o, A, a, s1, s2, op0, op1=None, rd=()):
        if op1 is None:
            self.op(eng, lambda e: e.tensor_scalar(out=o, in0=a, scalar1=s1, scalar2=None, op0=op0),
                    reads=[A] + list(rd), writes=[O])
        else:
            self.op(eng, lambda e: e.tensor_scalar(out=o, in0=a, scalar1=s1, scalar2=s2, op0=op0, op1=op1),
                    reads=[A] + list(rd), writes=[O])

    def stt(self, eng, O, o, A, a, sc, B, b, op0, op1, rd=()):
        self.op(eng, lambda e: e.scalar_tensor_tensor(out=o, in0=a, scalar=sc, in1=b, op0=op0, op1=op1),
                reads=[A, B] + list(rd), writes=[O])

    def cp(self, eng, O, o, A, a):
        if eng == "act":
            self.op("act", lambda e: e.copy(out=o, in_=a), reads=[A], writes=[O])
        else:
            self.op(eng, lambda e: e.tensor_copy(out=o, in_=a), reads=[A], writes=[O])

    def _sem(self, k):
        if k[0] == "d":
            return self.dsem[k[1]]
        return self.esem[k]

    def finish(self, final_bufs):
        evs = []
        for b in final_bufs:
            if b.w is not None:
                evs.append(b.w)
        fw = []
        for (k, v) in evs:
            fw.append((k, v))
        nc = self.nc
        q = self.q
        semf = self._sem

        def replay(e, lst, tail=()):
            for waits, fn, inc in lst:
                for (k, v) in waits:
                    e.wait_ge(semf(k), v)
                ins = fn(e)
                ins.then_inc(semf(inc[0]), inc[1])
            for (k, v) in tail:
                e.wait_ge(semf(k), v)

        with nc.Block() as block:
            @block.sync
            def _(e):
                replay(e, q["sync"], fw)

            @block.scalar
            def _(e):
                replay(e, q["act"])

            @block.vector
            def _(e):
                replay(e, q["dve"])

            @block.gpsimd
            def _(e):
                replay(e, q["pool"])

            @block.tensor
            def _(e):
                replay(e, q["pe"])
        self.es.close()


D = 1024
IN_DIM = 3108
NCORE = 8


def run(nc, in_maps):
    res = run_bass_kernel_spmd(nc, in_maps, core_ids=list(range(NCORE)))
    return res.results


def build_k0():
    nc = bass.Bass("TRN2", target_bir_lowering=False)
    P = Prog(nc)
    cT = P.dram("cT", [128, 8, 3], F32, kind="ExternalInput")
    aw = P.dram("aw", [3, 8, 128, 512], F32, kind="ExternalInput")
    ab = P.dram("ab", [3, 512], F32, kind="ExternalInput")
    out = P.dram("out", [3, 3, 512], F32, kind="ExternalOutput")
    sc = P.sb([128, 8, 3])
    P.dma("sync", sc[:], cT.t.ap(), reads=[cT], writes=[sc])
    P.act(sc, sc[:], sc, sc[:], AF.Silu)
    for j in range(3):
        w = P.sb([128, 8, 512], name="w%d" % j)
        bt = P.sb([3, 512], name="b%d" % j)
        ot = P.sb([3, 512], name="o%d" % j)
        pm = P.ps([3, 512], name="pm%d" % j)
        P.dma(("sync", "act", "pool")[j], w[:], aw.t.ap()[j].rearrange("k p n -> p k n"), reads=[aw], writes=[w])
        P.dma("sync", bt[:], ab.t.ap()[j].partition_broadcast(3), reads=[ab], writes=[bt])
        for k in range(8):
            P.mm(pm, pm[0:3, :], sc, sc[:, k, :], w, w[:, k, :], start=(k == 0), stop=(k == 7))
        P.tt("dve", ot, ot[:], pm, pm[0:3, :], bt, bt[:], ALU.add)
        P.dma("sync", out.t.ap()[j], ot[:], reads=[ot], writes=[out])
    P.finish([out])
    return nc


def run_k0(c, c_ctx, ada_w, ada_b):
    L = ada_w.shape[0]
    cv = np.stack([c[0], c[1], c_ctx], axis=0)
    cT = np.ascontiguousarray(cv.reshape(3, 8, 128).transpose(2, 1, 0))
    nblk = L * 12
    assert nblk == 24
    in_maps = []
    for core in range(NCORE):
        aws, abs_ = [], []
        for j in range(3):
            b = core * 3 + j
            l, cb = divmod(b, 12)
            aws.append(ada_w[l][:, cb * 512:(cb + 1) * 512].reshape(8, 128, 512))
            abs_.append(ada_b[l][cb * 512:(cb + 1) * 512])
        in_maps.append({"cT": cT, "aw": np.ascontiguousarray(np.stack(aws)), "ab": np.ascontiguousarray(np.stack(abs_))})
    res = run(build_k0(), in_maps)
    mods = np.zeros((L, 3, 6144), np.float32)
    for core in range(NCORE):
        for j in range(3):
            b = core * 3 + j
            l, cb = divmod(b, 12)
            mods[l, :, cb * 512:(cb + 1) * 512] = res[core]["out"][j]
    return mods


def load_bcast(P, queue, dram_buf, n, name):
    t = P.sb([128, n], name=name)
    P.dma(queue, t[:], dram_buf.t.ap().partition_broadcast(128), reads=[dram_buf], writes=[t])
    return t


def norm_mod_tile(P, xt, h, A, B, scr, ss, eng2="pool"):
    P.act(scr, scr[:], xt, xt[:], AF.Square, scale=1.0 / 32.0, accum=ss[:], wr=[ss])
    P.ts("dve", ss, ss[:], ss, ss[:], 1e-6, None, ALU.add)
    P.op("act", lambda e: e.sqrt(out=ss[:], in_=ss[:]), reads=[ss], writes=[ss])
    P.op("dve", lambda e: e.reciprocal(out=ss[:], in_=ss[:]), reads=[ss], writes=[ss])
    P.stt("dve", h, h[:], xt, xt[:], ss[:, 0:1], A, A[:], ALU.mult, ALU.mult, rd=[ss])
    P.tt(eng2, h, h[:], h, h[:], B, B[:], ALU.add)


def transpose_tile(P, h, hT, ident, pts, nk=8, evac=("act", "dve")):
    for half in range((nk + 3) // 4):
        pt = pts[half % len(pts)]
        n4 = min(4, nk - half * 4)
        for q in range(n4):
            k = half * 4 + q
            P.tr(pt, pt[:, q * 128:(q + 1) * 128], h, h[:, k * 128:(k + 1) * 128], ident, ident[:])
        P.cp(evac[half % len(evac)], hT, hT[:, half * 4:half * 4 + n4, :],
             pt, pt[:, 0:n4 * 128].rearrange("p (k t) -> p k t", k=n4))


def load_weight_bf16(P, wdram_ap_fn, W, nk, ncols, stage, wdram, colchunk=None):
    qs = ("sync", "act", "pool")
    cs = ("pool", "dve", "act")
    for k in range(nk):
        st = stage[k % len(stage)]
        P.dma(qs[k % 3], st[:, 0:ncols], wdram_ap_fn(k), reads=[wdram], writes=[st])
        P.cp(cs[k % 3], W, W[:, k, :], st, st[:, 0:ncols])


def build_k1(NT, ncols=IN_DIM, n_lat=None):
    if n_lat is None:
        n_lat = NT - 1
    nc = bass.Bass("TRN2", target_bir_lowering=False)
    P = Prog(nc)
    xt = P.dram("xt", [NT, 128, D], F32, kind="ExternalInput")
    w = P.dram("w", [D, ncols], F32, kind="ExternalInput")
    g = P.dram("g", [D], F32, kind="ExternalInput")
    vecs = {n: P.dram(n, [D], F32, kind="ExternalInput") for n in ("sh_l", "sc_l", "sh_c", "sc_c")}
    idn = P.dram("idn", [128, 128], F32, kind="ExternalInput")
    out = P.dram("out", [NT, 128, ncols], F32, kind="ExternalOutput")

    ident = P.sb([128, 128], name="ident")
    P.dma("sync", ident[:], idn.t.ap(), reads=[idn], writes=[ident])
    gb = load_bcast(P, "act", g, D, "gb")
    A_l = load_bcast(P, "pool", vecs["sc_l"], D, "A_l")
    B_l = load_bcast(P, "sync", vecs["sh_l"], D, "B_l")
    A_c = load_bcast(P, "act", vecs["sc_c"], D, "A_c")
    B_c = load_bcast(P, "pool", vecs["sh_c"], D, "B_c")
    for A in (A_l, A_c):
        P.stt("dve", A, A[:], A, A[:], 1.0, gb, gb[:], ALU.add, ALU.mult)

    W = P.sb([128, 8, ncols], BF16, name="W")
    stage = [P.sb([128, ncols], name="stg%d" % i) for i in range(2)]
    load_weight_bf16(P, lambda k: w.t.ap()[k * 128:(k + 1) * 128, :], W, 8, ncols, stage, w)

    xs = [P.sb([128, D], name="x%d" % i) for i in range(2)]
    hs = [P.sb([128, D], name="h%d" % i) for i in range(2)]
    scr = P.sb([128, D], name="scr")
    sss = [P.sb([128, 1], name="ss%d" % i) for i in range(2)]
    hTs = [P.sb([128, 8, 128], BF16, name="hT%d" % i) for i in range(2)]
    outs = [P.sb([128, ncols], name="ot%d" % i) for i in range(2)]
    pts = [P.ps([128, 512], name="pt%d" % i) for i in range(2)]
    pms = [P.ps([128, 512], name="pm%d" % i) for i in range(4)]
    ncb = (ncols + 511) // 512
    pmi = 0
    for t in range(NT):
        x_, h_, ss_, hT_, o_ = xs[t % 2], hs[t % 2], sss[t % 2], hTs[t % 2], outs[t % 2]
        P.dma(("sync", "pool")[t % 2], x_[:], xt.t.ap()[t], reads=[xt], writes=[x_])
        A, B = (A_l, B_l) if t < n_lat else (A_c, B_c)
        norm_mod_tile(P, x_, h_, A, B, scr, ss_)
        transpose_tile(P, h_, hT_, ident, pts)
        for cb in range(ncb):
            c0 = cb * 512
            cw = min(512, ncols - c0)
            pm = pms[pmi % 4]
            pmi += 1
            for k in range(8):
                P.mm(pm, pm[:, 0:cw], hT_, hT_[:, k, :], W, W[:, k, c0:c0 + cw], start=(k == 0), stop=(k == 7))
            P.cp(("act", "dve")[cb % 2], o_, o_[:, c0:c0 + cw], pm, pm[:, 0:cw])
        P.dma("sync", out.t.ap()[t], o_[:], reads=[o_], writes=[out])
    P.finish([out])
    return nc


NCH = 130
NBLK = 65


def consts():
    t = np.arange(128)
    U = (t[:, None] <= t[None, :]).astype(np.float32)
    NEG = np.where(t[None, :] < t[:, None], -30000.0, 0.0).astype(np.float32)
    return {"idn": np.eye(128, dtype=np.float32), "U": U, "NEG": NEG, "ones": np.ones((128, 128), np.float32)}


def load_consts(P, names=("idn", "U", "NEG", "ones")):
    out = {}
    for i, n in enumerate(names):
        d = P.dram(n, [128, 128], F32, kind="ExternalInput")
        s = P.sb([128, 128], name="c_" + n)
        P.dma(("sync", "act", "pool")[i % 3], s[:], d.t.ap(), reads=[d], writes=[s])
        out[n] = s
    return out


def conv_block(P, eng, ut, acc, cv, cw, cb, npart, n=256, bias=True):
    sl = slice(0, npart)
    P.ts(eng, acc, acc[sl, 0:n], ut, ut[sl, 0:n], cw[sl, 0:1], None, ALU.mult, rd=[cw])
    for k in range(1, 5):
        P.stt(eng, acc, acc[sl, 0:n], ut, ut[sl, k:k + n], cw[sl, k:k + 1], acc, acc[sl, 0:n], ALU.mult, ALU.add, rd=[cw])
    if bias:
        P.act(cv, cv[sl, 0:n], acc, acc[sl, 0:n], AF.Silu, bias=cb[sl, 0:1], rd=[cb])
    else:
        P.act(cv, cv[sl, 0:n], acc, acc[sl, 0:n], AF.Silu)


def softplus_inplace(P, t, ap):
    P.act(t, ap, t, ap, AF.Exp)
    P.ts("dve", t, ap, t, ap, 1.0, None, ALU.add)
    P.act(t, ap, t, ap, AF.Ln)


def cum_tables(P, C, la, pC1, pC2, nh=3):
    N = NCH * nh
    flat = lambda b: b[:].rearrange("p c h -> p (c h)")
    T = {}
    for n in ("negac", "eac", "wj", "eL"):
        T[n] = P.sb([128, NCH, nh], name="tb_" + n)
    P.mm(pC1, pC1[:, 0:N], C["U"], C["U"][:], la, flat(la))
    P.mm(pC2, pC2[:, 0:N], C["ones"], C["ones"][:], la, flat(la))
    P.ts("dve", T["negac"], flat(T["negac"]), pC1, pC1[:, 0:N], -1.0, None, ALU.mult)
    P.act(T["eac"], flat(T["eac"]), pC1, pC1[:, 0:N], AF.Exp)
    P.act(T["eL"], flat(T["eL"]), pC2, pC2[:, 0:N], AF.Exp)
    P.tt("dve", T["wj"], flat(T["wj"]), pC2, pC2[:, 0:N], T["negac"], flat(T["negac"]), ALU.add)
    P.act(T["wj"], flat(T["wj"]), T["wj"], flat(T["wj"]), AF.Exp)
    return T


def decay_mats(P, C, la, T, c, pA, rhs_t, decT, nh=3):
    for h in range(nh):
        r = rhs_t[h % len(rhs_t)]
        P.ts(("dve", "pool")[h % 2], r, r[:], C["U"], C["U"][:], la[:, c, h:h + 1], None, ALU.mult, rd=[la])
        P.mm(pA, pA[:, h * 128:(h + 1) * 128], C["ones"], C["ones"][:], r, r[:], start=True, stop=False)
        P.mm(pA, pA[:, h * 128:(h + 1) * 128], C["idn"], C["idn"][:], C["NEG"], C["NEG"][:], start=False, stop=True)
    for h in range(nh):
        P.act(decT, decT[:, h, :], pA, pA[:, h * 128:(h + 1) * 128], AF.Exp, bias=T["negac"][:, c, h:h + 1], rd=[T["negac"]])


def build_k2s():
    nc = bass.Bass("TRN2", target_bir_lowering=False)
    P = Prog(nc)
    u = P.dram("u", [448, NBLK, 260], F32, kind="ExternalInput")
    cwd = P.dram("cw", [448, 5], F32, kind="ExternalInput")
    cbd = P.dram("cb", [448, 1], F32, kind="ExternalInput")
    dtr = P.dram("dtr", [128, NCH, 3], F32, kind="ExternalInput")
    dtb = P.dram("dtb", [3], F32, kind="ExternalInput")
    alog = P.dram("alog", [3], F32, kind="ExternalInput")
    yout = P.dram("y", [NCH, 128, 192], F32, kind="ExternalOutput")
    xout = P.dram("xs", [NCH, 128, 192], F32, kind="ExternalOutput")
    C = load_consts(P)
    offs = (0, 128, 192, 320)
    nps = (128, 64, 128, 128)
    cw, cb = [], []
    for i in range(4):
        a = P.sb([128, 5], name="cw%d" % i)
        b = P.sb([128, 1], name="cb%d" % i)
        P.dma("sync", a[0:nps[i], :], cwd.t.ap()[offs[i]:offs[i] + nps[i], :], reads=[cwd], writes=[a])
        P.dma("act", b[0:nps[i], :], cbd.t.ap()[offs[i]:offs[i] + nps[i], :], reads=[cbd], writes=[b])
        cw.append(a)
        cb.append(b)
    dt = P.sb([128, NCH, 3], name="dt")
    la = P.sb([128, NCH, 3], name="la")
    dtw = P.sb([128, NCH, 3], name="dtw")
    dtbb = P.sb([128, 3], name="dtbb")
    Ab = P.sb([128, 3], name="Ab")
    P.dma("sync", dt[:], dtr.t.ap(), reads=[dtr], writes=[dt])
    P.dma("act", dtbb[:], dtb.t.ap().partition_broadcast(128), reads=[dtb], writes=[dtbb])
    P.dma("pool", Ab[:], alog.t.ap().partition_broadcast(128), reads=[alog], writes=[Ab])
    P.act(Ab, Ab[:], Ab, Ab[:], AF.Exp)
    P.ts("dve", Ab, Ab[:], Ab, Ab[:], -1.0, None, ALU.mult)
    for h in range(3):
        P.ts("dve", dt, dt[:, :, h], dt, dt[:, :, h], dtbb[:, h:h + 1], None, ALU.add, rd=[dtbb])
    fl = lambda b: b[:].rearrange("p c h -> p (c h)")
    softplus_inplace(P, dt, fl(dt))
    for h in range(3):
        P.ts("dve", la, la[:, :, h], dt, dt[:, :, h], Ab[:, h:h + 1], None, ALU.mult, rd=[Ab])
    pC1 = P.ps([128, 512], name="pC1")
    pC2 = P.ps([128, 512], name="pC2")
    T = cum_tables(P, C, la, pC1, pC2)
    P.tt("dve", dtw, fl(dtw), dt, fl(dt), T["wj"], fl(T["wj"]), ALU.mult)

    pA = P.ps([128, 384], name="pA")
    pG = P.ps([128, 128], name="pG")
    pY1 = P.ps([128, 192], name="pY1")
    pY2 = P.ps([128, 192], name="pY2")
    pS = P.ps([128, 192], name="pS")
    pT = P.ps([128, 320], name="pT")
    S = P.sb([128, 192], name="S")
    P.op("dve", lambda e: e.memset(S[:], 0.0), writes=[S])
    ut = [[P.sb([128, 260], name="ut%d_%d" % (i, j)) for j in range(2)] for i in range(4)]
    acc = [P.sb([128, 256], name="acc%d" % i) for i in range(4)]
    cv = [[P.sb([128, 256], name="cv%d_%d" % (i, j)) for j in range(2)] for i in range(4)]
    rhs_t = [P.sb([128, 128], name="rhs%d" % i) for i in range(2)]
    decT = P.sb([128, 3, 128], name="decT")
    Wt = P.sb([128, 3, 128], name="Wt")
    tok = [P.sb([128, 320], name="tok%d" % i) for i in range(2)]
    xdt = P.sb([128, 192], name="xdt")
    xw = P.sb([128, 192], name="xw")
    ysb = P.sb([128, 192], name="ysb")
    yo = [P.sb([128, 192], name="yo%d" % i) for i in range(2)]
    for b in range(NBLK):
        j = b % 2
        for i in range(4):
            P.dma(("sync", "act", "pool", "sync")[i], ut[i][j][0:nps[i], :], u.t.ap()[offs[i]:offs[i] + nps[i], b, :],
                  reads=[u], writes=[ut[i][j]])
            conv_block(P, "dve", ut[i][j], acc[i], cv[i][j], cw[i], cb[i], nps[i])
        xA, xB, BT, CT = cv[0][j], cv[1][j], cv[2][j], cv[3][j]
        for cc in range(2):
            c = b * 2 + cc
            ck = slice(cc * 128, (cc + 1) * 128)
            tk = tok[c % 2]
            P.tr(pT, pT[:, 0:128], xA, xA[:, ck], C["idn"], C["idn"][:])
            P.tr(pT, pT[:, 128:192], xB, xB[0:64, ck], C["idn"], C["idn"][0:64, 0:64])
            P.tr(pT, pT[:, 192:320], BT, BT[:, ck], C["idn"], C["idn"][:])
            P.cp("act", tk, tk[:], pT, pT[:, 0:320])
            P.dma("sync", xout.t.ap()[c], tk[:, 0:192], reads=[tk], writes=[xout])
            decay_mats(P, C, la, T, c, pA, rhs_t, decT)
            P.mm(pG, pG[:, 0:128], BT, BT[:, ck], CT, CT[:, ck])
            for h in range(3):
                hs = slice(h * 64, (h + 1) * 64)
                P.tt("dve", Wt, Wt[:, h, :], pG, pG[:, 0:128], decT, decT[:, h, :], ALU.mult)
                P.ts("pool", xdt, xdt[:, hs], tk, tk[:, hs], dt[:, c, h:h + 1], None, ALU.mult, rd=[dt])
                P.ts("pool", xw, xw[:, hs], tk, tk[:, hs], dtw[:, c, h:h + 1], None, ALU.mult, rd=[dtw])
            for h in range(3):
                hs = slice(h * 64, (h + 1) * 64)
                P.mm(pY1, pY1[:, hs], Wt, Wt[:, h, :], xdt, xdt[:, hs])
                P.mm(pY2, pY2[:, hs], CT, CT[:, ck], S, S[:, hs])
                P.mm(pS, pS[:, hs], tk, tk[:, 192:320], xw, xw[:, hs])
            P.cp("act", ysb, ysb[:], pY1, pY1[:, 0:192])
            y_ = yo[c % 2]
            for h in range(3):
                hs = slice(h * 64, (h + 1) * 64)
                P.stt("dve", y_, y_[:, hs], pY2, pY2[:, hs], T["eac"][:, c, h:h + 1], ysb, ysb[:, hs], ALU.mult, ALU.add, rd=[T["eac"]])
            for h in range(3):
                hs = slice(h * 64, (h + 1) * 64)
                P.stt("dve", S, S[:, hs], S, S[:, hs], T["eL"][:, c, h:h + 1], pS, pS[:, hs], ALU.mult, ALU.add, rd=[T["eL"]])
            P.dma("sync", yout.t.ap()[c], y_[:], reads=[y_], writes=[yout])
    P.finish([yout, xout])
    return nc


def windows(seg, n=256):
    ch, T = seg.shape
    p = np.zeros((ch, T + 4), np.float32)
    p[:, 2:T + 2] = seg
    idx = (np.arange(T // n)[:, None] * n + np.arange(n + 4)[None, :])
    return p[:, idx]


def prep_k2s(pl, pc, conv_w, conv_b, a_log, dt_bias, core):
    s, d, g = core // 4, (core // 2) % 2, core % 2
    c0 = 256 + 384
    chans = np.concatenate([np.arange(g * 192, g * 192 + 192), 384 + g * 128 + np.arange(128), 384 + 256 + g * 128 + np.arange(128)])
    segs = []
    for arr in (pc[s], pl[s]):
        a = arr[:, c0 + chans]
        if d == 1:
            a = a[::-1]
        segs.append(windows(np.ascontiguousarray(a.T)))
    u = np.ascontiguousarray(np.concatenate(segs, axis=1))
    cw = conv_w[:, chans].T
    if d == 1:
        cw = cw[:, ::-1]
    dcol = 256 + 384 + 896 + d * 6 + g * 3
    dts = []
    for arr in (pc[s], pl[s]):
        a = arr[:, dcol:dcol + 3]
        if d == 1:
            a = a[::-1]
        dts.append(a)
    dtr = np.concatenate(dts, 0).reshape(NCH, 128, 3).transpose(1, 0, 2)
    m = {"u": u, "cw": np.ascontiguousarray(cw), "cb": np.ascontiguousarray(conv_b[chans][:, None]),
         "dtr": np.ascontiguousarray(dtr), "dtb": np.ascontiguousarray(dt_bias[d, g * 3:g * 3 + 3]),
         "alog": np.ascontiguousarray(a_log[d, g * 3:g * 3 + 3])}
    m.update(consts())
    return m


GRID_W = 64


def consts_g():
    c = consts()
    t = np.arange(128)
    c["POS"] = np.where(t[None, :] >= t[:, None], 30000.0, 0.0).astype(np.float32)
    return c


def build_k2g(nblk=NBLK):
    nc = bass.Bass("TRN2", target_bir_lowering=False)
    P = Prog(nc)
    u = P.dram("u", [576, NBLK, 260], F32, kind="ExternalInput")
    cwd = P.dram("cw", [576, 5], F32, kind="ExternalInput")
    ard = P.dram("araw", [128, NCH, 3], F32, kind="ExternalInput")
    brd = P.dram("braw", [128, NCH, 3], F32, kind="ExternalInput")
    dtb = P.dram("dtb", [3], F32, kind="ExternalInput")
    alog = P.dram("alog", [3], F32, kind="ExternalInput")
    oout = P.dram("o", [NCH, 128, 192], F32, kind="ExternalOutput")
    C = load_consts(P, ("idn", "U", "NEG", "ones", "POS"))
    idn = C["idn"]
    offs = (0, 128, 192, 320, 384, 512)
    nps = (128, 64, 128, 64, 128, 64)
    cw = []
    for i in range(6):
        a = P.sb([128, 5], name="cw%d" % i)
        P.dma(("sync", "act")[i % 2], a[0:nps[i], :], cwd.t.ap()[offs[i]:offs[i] + nps[i], :], reads=[cwd], writes=[a])
        cw.append(a)
    fl = lambda b: b[:].rearrange("p c h -> p (c h)")
    N3 = NCH * 3
    la = P.sb([128, NCH, 3], name="la")
    beta = P.sb([128, NCH, 3], name="beta")
    dtbb = P.sb([128, 3], name="dtbb")
    Ab = P.sb([128, 3], name="Ab")
    P.dma("sync", la[:], ard.t.ap(), reads=[ard], writes=[la])
    P.dma("pool", beta[:], brd.t.ap(), reads=[brd], writes=[beta])
    P.dma("act", dtbb[:], dtb.t.ap().partition_broadcast(128), reads=[dtb], writes=[dtbb])
    P.dma("pool", Ab[:], alog.t.ap().partition_broadcast(128), reads=[alog], writes=[Ab])
    P.act(Ab, Ab[:], Ab, Ab[:], AF.Exp)
    P.ts("dve", Ab, Ab[:], Ab, Ab[:], -1.0, None, ALU.mult)
    for h in range(3):
        P.ts("dve", la, la[:, :, h], la, la[:, :, h], dtbb[:, h:h + 1], None, ALU.add, rd=[dtbb])
    softplus_inplace(P, la, fl(la))
    for h in range(3):
        P.ts("dve", la, la[:, :, h], la, la[:, :, h], Ab[:, h:h + 1], None, ALU.mult, rd=[Ab])
    P.act(beta, fl(beta), beta, fl(beta), AF.Sigmoid)
    banks = [P.ps([128, 512], name="bank%d" % i) for i in range(8)]
    ac = P.sb([128, NCH, 3], name="ac")
    negac = P.sb([128, NCH, 3], name="negac")
    eac = P.sb([128, NCH, 3], name="eac")
    wj = P.sb([128, NCH, 3], name="wj")
    eL = P.sb([128, NCH, 3], name="eL")
    be = P.sb([128, NCH, 3], name="be")
    nbeta = P.sb([128, NCH, 3], name="nbeta")
    pC1, pC2 = banks[3], banks[4]
    P.mm(pC1, pC1[:, 0:N3], C["U"], C["U"][:], la, fl(la))
    P.mm(pC2, pC2[:, 0:N3], C["ones"], C["ones"][:], la, fl(la))
    P.cp("dve", ac, fl(ac), pC1, pC1[:, 0:N3])
    P.ts("dve", negac, fl(negac), ac, fl(ac), -1.0, None, ALU.mult)
    P.act(eac, fl(eac), ac, fl(ac), AF.Exp)
    P.act(eL, fl(eL), pC2, pC2[:, 0:N3], AF.Exp)
    P.tt("dve", wj, fl(wj), pC2, pC2[:, 0:N3], negac, fl(negac), ALU.add)
    P.act(wj, fl(wj), wj, fl(wj), AF.Exp)
    P.tt("dve", be, fl(be), beta, fl(beta), eac, fl(eac), ALU.mult)
    P.ts("dve", nbeta, fl(nbeta), beta, fl(beta), -1.0, None, ALU.mult)

    I3 = P.sb([128, 3, 128], name="I3")
    for h in range(3):
        P.cp("pool", I3, I3[:, h, :], idn, idn[:])
    S = P.sb([64, 192], name="S")
    P.op("dve", lambda e: e.memset(S[:], 0.0), writes=[S])

    ut = [[P.sb([128, 260], name="ut%d_%d" % (i, j)) for j in range(2)] for i in range(6)]
    acc = [P.sb([128, 256], name="acc%d" % i) for i in range(6)]
    cv = [[P.sb([128, 256], name="cv%d_%d" % (i, j)) for j in range(2)] for i in range(6)]

    def mkset(n):
        d = {}
        for nm, shp in (("qk", [128, 384]), ("vt", [128, 192]), ("sq", [128, 384]), ("rs", [128, 6]), ("qkn", [128, 384]),
                        ("kT", [64, 384]), ("kbe", [128, 192]), ("vb", [128, 192]), ("decT", [128, 3, 128]),
                        ("decS", [128, 3, 128]), ("Np", [128, 3, 128]), ("Mp", [128, 3, 128]), ("Tt", [128, 3, 128])):
            d[nm] = P.sb(shp, name="%s_%d" % (nm, n))
        d["rhs"] = [P.sb([128, 128], name="rhs%d_%d" % (i, n)) for i in range(3)]
        return d

    def mkhand(n, par):
        d = {}
        for nm, shp in (("usb", [128, 192]), ("wT", [64, 384]), ("qT", [64, 384]), ("attnT", [128, 3, 128]), ("kend", [128, 192])):
            d[nm] = P.sb(shp, name="%s_%d_%d" % (nm, n, par))
        return d

    sets = [mkset(0), mkset(1)]
    hands = [[mkhand(n, par) for par in range(2)] for n in range(2)]
    bankset = [banks[0:3], banks[3:6]]
    bR6, bR7 = banks[6], banks[7]
    vnew = P.sb([128, 192], name="vnew")
    o2 = P.sb([128, 192], name="o2")
    oo = [P.sb([128, 192], name="oo%d" % i) for i in range(2)]
    f3 = lambda b: b[:].rearrange("p h n -> p (h n)")
    H = lambda h: slice(h * 64, (h + 1) * 64)
    H2 = lambda h: slice(h * 128, (h + 1) * 128)

    def pre(c, cc, j, st, bk, hd):
        b0, b1, b2 = bk
        qk, vt, sq, rs, qkn, kT = st["qk"], st["vt"], st["sq"], st["rs"], st["qkn"], st["kT"]
        kbe, vb, decT, decS, Np, Mp, Tt, rhs_t = st["kbe"], st["vb"], st["decT"], st["decS"], st["Np"], st["Mp"], st["Tt"], st["rhs"]
        usb, wT, qT, attnT, kend = hd["usb"], hd["wT"], hd["qT"], hd["attnT"], hd["kend"]
        ck = slice(cc * 128, (cc + 1) * 128)
        pT1, pT2 = b0, b1
        for a in range(3):
            pt = pT1 if a < 2 else pT2
            base = (a % 2) * 192
            t01, t2 = cv[2 * a][j], cv[2 * a + 1][j]
            P.tr(pt, pt[:, base:base + 128], t01, t01[:, ck], idn, idn[:])
            P.tr(pt, pt[:, base + 128:base + 192], t2, t2[0:64, ck], idn, idn[0:64, 0:64])
        yield
        P.cp("act", qk, qk[:], pT1, pT1[:, 0:384])
        P.cp("dve", vt, vt[:], pT2, pT2[:, 0:192])
        yield
        P.tt("pool", sq, sq[:], qk, qk[:], qk, qk[:], ALU.mult)
        yield
        P.op("dve", lambda e: e.reduce_sum(out=rs[:], in_=sq[:].rearrange("p (a d) -> p a d", d=64), axis=AX.X),
             reads=[sq], writes=[rs])
        P.ts("dve", rs, rs[:], rs, rs[:], 1e-6, None, ALU.add)
        yield
        P.op("act", lambda e: e.sqrt(out=rs[:], in_=rs[:]), reads=[rs], writes=[rs])
        yield
        P.op("dve", lambda e: e.reciprocal(out=rs[:], in_=rs[:]), reads=[rs], writes=[rs])
        P.ts("dve", rs, rs[:, 0:3], rs, rs[:, 0:3], 0.125, None, ALU.mult)
        yield
        for a in range(6):
            P.ts(("dve", "pool")[a % 2], qkn, qkn[:, H(a)], qk, qk[:, H(a)], rs[:, a:a + 1], None, ALU.mult, rd=[rs])
        yield
        pKT, pQT = b2, b0
        for h in range(3):
            P.tr(pKT, pKT[0:64, H2(h)], qkn, qkn[:, 192 + h * 64:192 + (h + 1) * 64], idn, idn[:])
            P.tr(pQT, pQT[0:64, H2(h)], qkn, qkn[:, h * 64:(h + 1) * 64], idn, idn[:])
        yield
        P.cp("act", kT, kT[:], pKT, pKT[0:64, 0:384])
        P.cp("dve", qT, qT[:], pQT, pQT[0:64, 0:384])
        for h in range(3):
            kn_h = qkn[:, 192 + h * 64:192 + (h + 1) * 64]
            P.ts("pool", kbe, kbe[:, H(h)], qkn, kn_h, be[:, c, h:h + 1], None, ALU.mult, rd=[be])
            P.ts("pool", vb, vb[:, H(h)], vt, vt[:, H(h)], beta[:, c, h:h + 1], None, ALU.mult, rd=[beta])
            P.ts("pool", kend, kend[:, H(h)], qkn, kn_h, wj[:, c, h:h + 1], None, ALU.mult, rd=[wj])
        yield
        pA1, pA2 = b0, b1
        for h in range(3):
            r = rhs_t[h]
            P.ts(("dve", "pool")[h % 2], r, r[:], C["U"], C["U"][:], la[:, c, h:h + 1], None, ALU.mult, rd=[la])
        yield
        for h in range(3):
            r = rhs_t[h]
            P.mm(pA1, pA1[:, H2(h)], C["ones"], C["ones"][:], r, r[:], start=True, stop=False)
            P.mm(pA1, pA1[:, H2(h)], idn, idn[:], C["NEG"], C["NEG"][:], start=False, stop=True)
            P.mm(pA2, pA2[:, H2(h)], C["ones"], C["ones"][:], r, r[:], start=True, stop=False)
            P.mm(pA2, pA2[:, H2(h)], idn, idn[:], C["POS"], C["POS"][:], start=False, stop=True)
        yield
        for h in range(3):
            P.act(decT, decT[:, h, :], pA1, pA1[:, H2(h)], AF.Exp, bias=negac[:, c, h:h + 1], rd=[negac])
            P.act(decS, decS[:, h, :], pA2, pA2[:, H2(h)], AF.Exp, bias=ac[:, c, h:h + 1], scale=-1.0, rd=[ac])
        yield
        pKK, pQK = b2, b0
        for h in range(3):
            P.mm(pKK, pKK[:, H2(h)], kT, kT[:, H2(h)], kT, kT[:, H2(h)])
            P.mm(pQK, pQK[:, H2(h)], kT, kT[:, H2(h)], qT, qT[:, H2(h)])
        yield
        for h in range(3):
            P.stt("dve", Np, Np[:, h, :], pKK, pKK[:, H2(h)], nbeta[:, c, h:h + 1], decS, decS[:, h, :], ALU.mult, ALU.mult, rd=[nbeta])
            P.tt("dve", attnT, attnT[:, h, :], pQK, pQK[:, H2(h)], decT, decT[:, h, :], ALU.mult)
        yield
        pN, pM, pTt = b0, b1, b2
        for h in range(3):
            P.tr(pM, pM[:, H2(h)], Np, Np[:, h, :], idn, idn[:])
        yield
        P.cp("act", Mp, f3(Mp), pM, pM[:, 0:384])
        yield
        P.tt("pool", Tt, f3(Tt), Mp, f3(Mp), I3, f3(I3), ALU.add)
        for step in range(6):
            last = step == 5
            for h in range(3):
                P.mm(pN, pN[:, H2(h)], Mp, Mp[:, h, :], Np, Np[:, h, :])
                if not last:
                    P.mm(pM, pM[:, H2(h)], Np, Np[:, h, :], Mp, Mp[:, h, :])
            yield
            P.cp("act", Np, f3(Np), pN, pN[:, 0:384])
            if not last:
                P.cp("dve", Mp, f3(Mp), pM, pM[:, 0:384])
            yield
            for h in range(3):
                P.mm(pTt, pTt[:, H2(h)], Np, Np[:, h, :], Tt, Tt[:, h, :])
            yield
            P.tt("dve", Tt, f3(Tt), Tt, f3(Tt), pTt, pTt[:, 0:384], ALU.add)
        yield
        pU, pWT = b0, b1
        for h in range(3):
            P.mm(pU, pU[:, H(h)], Tt, Tt[:, h, :], vb, vb[:, H(h)])
            P.mm(pWT, pWT[0:64, H2(h)], kbe, kbe[:, H(h)], Tt, Tt[:, h, :])
        yield
        P.cp("act", usb, usb[:], pU, pU[:, 0:192])
        P.cp("dve", wT, wT[:], pWT, pWT[0:64, 0:384])
        yield

    def rec(c, hd):
        usb, wT, qT, attnT, kend = hd["usb"], hd["wT"], hd["qT"], hd["attnT"], hd["kend"]
        pWS, pO1, pO2, pSn = bR6, bR7, bR6, bR6
        for h in range(3):
            P.mm(pWS, pWS[:, H(h)], wT, wT[:, H2(h)], S, S[:, H(h)])
            P.mm(pO1, pO1[:, H(h)], qT, qT[:, H2(h)], S, S[:, H(h)])
        yield
        P.tt("dve", vnew, vnew[:], usb, usb[:], pWS, pWS[:, 0:192], ALU.subtract)
        yield
        for h in range(3):
            P.mm(pO2, pO2[:, H(h)], attnT, attnT[:, h, :], vnew, vnew[:, H(h)])
        yield
        P.cp("act", o2, o2[:], pO2, pO2[:, 0:192])
        yield
        for h in range(3):
            P.mm(pSn, pSn[0:64, H(h)], kend, kend[:, H(h)], vnew, vnew[:, H(h)])
        o_ = oo[c % 2]
        for h in range(3):
            P.stt("dve", o_, o_[:, H(h)], pO1, pO1[:, H(h)], eac[:, c, h:h + 1], o2, o2[:, H(h)], ALU.mult, ALU.add, rd=[eac])
        yield
        for h in range(3):
            P.stt("dve", S, S[:, H(h)], S, S[:, H(h)], eL[0:64, c, h:h + 1], pSn, pSn[0:64, H(h)], ALU.mult, ALU.add, rd=[eL])
        P.dma("sync", oout.t.ap()[c], o_[:], reads=[o_], writes=[oout])
        yield

    def chain(gs):
        for g in gs:
            for _ in g:
                yield

    def roundrobin(gens):
        gens = list(gens)
        while gens:
            for g in list(gens):
                try:
                    next(g)
                except StopIteration:
                    gens.remove(g)

    pending = []
    for b in range(nblk):
        j = b % 2
        for i in range(6):
            P.dma(("sync", "act")[i % 2], ut[i][j][0:nps[i], :], u.t.ap()[offs[i]:offs[i] + nps[i], b, :],
                  reads=[u], writes=[ut[i][j]])
            conv_block(P, "dve", ut[i][j], acc[i], cv[i][j], cw[i], None, nps[i], bias=False)
        gens = [pre(2 * b, 0, j, sets[0], bankset[0], hands[0][j]), pre(2 * b + 1, 1, j, sets[1], bankset[1], hands[1][j])]
        if pending:
            gens.append(chain(pending))
        roundrobin(gens)
        pending = [rec(2 * b, hands[0][j]), rec(2 * b + 1, hands[1][j])]
    roundrobin([chain(pending)])
    P.finish([oout])
    return nc


def to_cm(a):
    T, Cc = a.shape
    return a.reshape(T // GRID_W, GRID_W, Cc).transpose(1, 0, 2).reshape(T, Cc)


def from_cm(a):
    T, Cc = a.shape
    return a.reshape(GRID_W, T // GRID_W, Cc).transpose(1, 0, 2).reshape(T, Cc)


def prep_k2g(pl, pc, conv_w, a_log, dt_bias, core):
    s, d, g = core // 4, (core // 2) % 2, core % 2
    q0 = 1548
    chans = np.concatenate([a * 384 + g * 192 + np.arange(192) for a in range(3)])
    acol = 3084 + d * 6 + g * 3
    bcol = 3096 + d * 6 + g * 3
    segs, ars, brs = [], [], []
    for arr, cm in ((pc[s], False), (pl[s], True)):
        a = arr[:, q0 + chans]
        ar = arr[:, acol:acol + 3]
        br = arr[:, bcol:bcol + 3]
        if cm:
            a, ar, br = to_cm(a), to_cm(ar), to_cm(br)
        if d == 1:
            a, ar, br = a[::-1], ar[::-1], br[::-1]
        segs.append(windows(np.ascontiguousarray(a.T)))
        ars.append(ar)
        brs.append(br)
    cw = conv_w[:, chans].T
    if d == 1:
        cw = cw[:, ::-1]
    tm = lambda lst: np.ascontiguousarray(np.concatenate(lst, 0).reshape(NCH, 128, 3).transpose(1, 0, 2))
    m = {"u": np.ascontiguousarray(np.concatenate(segs, axis=1)), "cw": np.ascontiguousarray(cw),
         "araw": tm(ars), "braw": tm(brs), "dtb": np.ascontiguousarray(dt_bias[d, g * 3:g * 3 + 3]),
         "alog": np.ascontiguousarray(a_log[d, g * 3:g * 3 + 3])}
    m.update(consts_g())
    return m


PKW = 1024 + 256 + 7 * 384
POOL_WINDOWS = (2, 4, 8, 16)


def build_k3(NT, n_lat=None):
    if n_lat is None:
        n_lat = NT - 1
    nc = bass.Bass("TRN2", target_bir_lowering=False)
    P = Prog(nc)
    pk = P.dram("pk", [NT, 128, PKW], F32, kind="ExternalInput")
    halo = P.dram("halo", [NT, 16, 256], F32, kind="ExternalInput")
    band = P.dram("band", [NT, 144, 512], F32, kind="ExternalInput")
    wout = P.dram("wout", [D, D], F32, kind="ExternalInput")
    pwd = P.dram("pw", [64, 4, 128], F32, kind="ExternalInput")
    psd = P.dram("pscale", [128, 2], F32, kind="ExternalInput")
    rwd = P.dram("rw", [128, 8, 16], F32, kind="ExternalInput")
    idn = P.dram("idn", [128, 128], F32, kind="ExternalInput")
    v384 = {n: P.dram(n, [384], F32, kind="ExternalInput") for n in ("dvec", "sng", "gng")}
    v1k = {n: P.dram(n, [D], F32, kind="ExternalInput") for n in ("g1_l", "g1_c", "n2g", "sh_l", "sc_l", "sh_c", "sc_c")}
    x2o = P.dram("x2", [NT, 128, D], F32, kind="ExternalOutput")
    hTo = P.dram("h2T", [NT, 128, 8, 128], F32, kind="ExternalOutput")
    affo = P.dram("aff", [NT, 128, 16], F32, kind="ExternalOutput")

    ident = P.sb([128, 128], name="ident")
    P.dma("sync", ident[:], idn.t.ap(), reads=[idn], writes=[ident])
    pw = P.sb([64, 4, 128], name="pw")
    P.dma("act", pw[:], pwd.t.ap(), reads=[pwd], writes=[pw])
    psc = P.sb([128, 2], name="psc")
    P.dma("sync", psc[:], psd.t.ap(), reads=[psd], writes=[psc])
    rw = P.sb([128, 8, 16], name="rw")
    P.dma("act", rw[:], rwd.t.ap(), reads=[rwd], writes=[rw])
    b384 = {n: load_bcast(P, ("sync", "act")[i % 2], v384[n], 384, "b_" + n) for i, n in enumerate(v384)}
    b1k = {n: load_bcast(P, ("sync", "act")[i % 2], v1k[n], D, "b_" + n) for i, n in enumerate(v1k)}
    for n in ("sc_l", "sc_c"):
        A = b1k[n]
        P.stt("dve", A, A[:], A, A[:], 1.0, b1k["n2g"], b1k["n2g"][:], ALU.add, ALU.mult)
    W = P.sb([128, 8, D], BF16, name="W")
    stage = [P.sb([128, D], name="stg%d" % i) for i in range(2)]
    load_weight_bf16(P, lambda k: wout.t.ap()[k * 128:(k + 1) * 128, :], W, 8, D, stage, wout)

    pks = [P.sb([128, PKW], name="pk%d" % i) for i in range(2)]
    hls = [P.sb([16, 256], name="hl%d" % i) for i in range(2)]
    bds = [P.sb([128, 512], name="bd%d" % i) for i in range(2)]
    bd2s = [P.sb([16, 512], name="bdh%d" % i) for i in range(2)]
    dT = P.sb([64, 4, 128], name="dT")
    mixT = [P.sb([128, 8, 128], BF16, name="mixT%d" % i) for i in range(2)]
    t1 = P.sb([128, 384], name="t1")
    ys = P.sb([128, 384], name="ys")
    sz = P.sb([128, 384], name="sz")
    scr3 = P.sb([128, 384], name="scr3")
    ss1 = P.sb([128, 1], name="ss1")
    yo = P.sb([128, 768], name="yo")
    og = P.sb([128, 384], name="og")
    sq = P.sb([128, 384], name="sq")
    rs6 = P.sb([128, 6], name="rs6")
    sg = P.sb([128, 384], name="sg")
    mx = P.sb([128, 512], name="mx")
    x2s = [P.sb([128, D], name="x2_%d" % i) for i in range(2)]
    h2s = [P.sb([128, D], name="h2_%d" % i) for i in range(2)]
    scr = P.sb([128, D], name="scr")
    ss2 = P.sb([128, 1], name="ss2")
    h2Ts = [P.sb([128, 8, 128], name="h2T%d" % i) for i in range(2)]
    lg = P.sb([128, 16], name="lg")
    ex = P.sb([128, 16], name="ex")
    m1 = P.sb([128, 1], name="m1")
    s1 = P.sb([128, 1], name="s1")
    afs = [P.sb([128, 16], name="af%d" % i) for i in range(2)]
    pD, pP, pr = P.ps([1], name="pD"), P.ps([1], name="pP"), P.ps([1], name="pr")
    pts = [P.ps([1], name="pt%d" % i) for i in range(2)]
    pms = [P.ps([1], name="pm%d" % i) for i in range(2)]

    for t in range(NT):
        lat = t < n_lat
        pk_ = pks[t % 2]
        hl, bd, bd2, mT = hls[t % 2], bds[t % 2], bd2s[t % 2], mixT[t % 2]
        P.dma("sync", pk_[:], pk.t.ap()[t], reads=[pk], writes=[pk_])
        P.dma("act", hl[:], halo.t.ap()[t], reads=[halo], writes=[hl])
        P.dma("act", bd[:], band.t.ap()[t, 0:128, :], reads=[band], writes=[bd])
        P.dma("act", bd2[:], band.t.ap()[t, 128:144, :], reads=[band], writes=[bd2])
        xo, uo = 0, 1024
        yf, yb, xs, z, of, ob, gt = [slice(1280 + i * 384, 1280 + (i + 1) * 384) for i in range(7)]
        for g in range(4):
            P.mm(pD, pD[0:64, g * 128:(g + 1) * 128], pk_, pk_[:, uo + g * 64:uo + (g + 1) * 64], bd, bd[:, g * 128:(g + 1) * 128],
                 start=True, stop=False)
            P.mm(pD, pD[0:64, g * 128:(g + 1) * 128], hl, hl[:, g * 64:(g + 1) * 64], bd2, bd2[:, g * 128:(g + 1) * 128],
                 start=False, stop=True)
        P.cp("act", dT, dT[:].rearrange("p g t -> p (g t)"), pD, pD[0:64, :])
        for cch in range(2):
            for gg in range(2):
                g = cch * 2 + gg
                P.mm(pP, pP[:, cch * 128:(cch + 1) * 128], pw, pw[:, g, :], dT, dT[:, g, :], start=(gg == 0), stop=(gg == 1))
        for cch in range(2):
            P.ts("dve", mT, mT[:, cch, :], pP, pP[:, cch * 128:(cch + 1) * 128], psc[:, cch:cch + 1], None, ALU.mult, rd=[psc])
        P.tt("pool", ys, ys[:], pk_, pk_[:, yf], pk_, pk_[:, yb], ALU.add)
        P.tt("pool", t1, t1[:], pk_, pk_[:, xs], b384["dvec"], b384["dvec"][:], ALU.mult)
        P.tt("pool", ys, ys[:], ys, ys[:], t1, t1[:], ALU.add)
        P.act(sz, sz[:], pk_, pk_[:, z], AF.Silu)
        P.tt("dve", ys, ys[:], ys, ys[:], sz, sz[:], ALU.mult)
        P.act(scr3, scr3[:], ys, ys[:], AF.Square, scale=float(384 ** -0.5), accum=ss1[:], wr=[ss1])
        P.ts("dve", ss1, ss1[:], ss1, ss1[:], 1e-6, None, ALU.add)
        P.op("act", lambda e: e.sqrt(out=ss1[:], in_=ss1[:]), reads=[ss1], writes=[ss1])
        P.op("dve", lambda e: e.reciprocal(out=ss1[:], in_=ss1[:]), reads=[ss1], writes=[ss1])
        P.stt("dve", yo, yo[:, 0:384], ys, ys[:], ss1[:, 0:1], b384["sng"], b384["sng"][:], ALU.mult, ALU.mult, rd=[ss1])
        P.tt("pool", og, og[:], pk_, pk_[:, of], pk_, pk_[:, ob], ALU.add)
        P.tt("pool", sq, sq[:], og, og[:], og, og[:], ALU.mult)
        P.op("dve", lambda e: e.reduce_sum(out=rs6[:], in_=sq[:].rearrange("p (a d) -> p a d", d=64), axis=AX.X),
             reads=[sq], writes=[rs6])
        P.ts("dve", rs6, rs6[:], rs6, rs6[:], 1.0 / 64.0, 1e-6, ALU.mult, ALU.add)
        P.op("act", lambda e: e.sqrt(out=rs6[:], in_=rs6[:]), reads=[rs6], writes=[rs6])
        P.op("dve", lambda e: e.reciprocal(out=rs6[:], in_=rs6[:]), reads=[rs6], writes=[rs6])
        for a in range(6):
            P.ts(("dve", "pool")[a % 2], og, og[:, a * 64:(a + 1) * 64], og, og[:, a * 64:(a + 1) * 64], rs6[:, a:a + 1], None, ALU.mult, rd=[rs6])
        P.act(sg, sg[:], pk_, pk_[:, gt], AF.Silu)
        P.tt("pool", og, og[:], og, og[:], b384["gng"], b384["gng"][:], ALU.mult)
        P.tt("dve", yo, yo[:, 384:768], og, og[:], sg, sg[:], ALU.mult)
        for half in range(2):
            pt = pts[half]
            for q in range(3):
                k = half * 3 + q
                P.tr(pt, pt[:, q * 128:(q + 1) * 128], yo, yo[:, k * 128:(k + 1) * 128], ident, ident[:])
            P.cp(("act", "dve")[half], mT, mT[:, 2 + half * 3:5 + half * 3, :], pt, pt[:, 0:384].rearrange("p (k t) -> p k t", k=3))
        x2 = x2s[t % 2]
        g1 = b1k["g1_l"] if lat else b1k["g1_c"]
        for cb in range(2):
            pm = pms[cb]
            cs = slice(cb * 512, (cb + 1) * 512)
            for k in range(8):
                P.mm(pm, pm[:, :], mT, mT[:, k, :], W, W[:, k, cs], start=(k == 0), stop=(k == 7))
            P.tt("dve", mx, mx[:], pm, pm[:, :], g1, g1[:, cs], ALU.mult)
            P.tt("pool", x2, x2[:, cs], mx, mx[:], pk_, pk_[:, cb * 512:(cb + 1) * 512], ALU.add)
        P.dma("sync", x2o.t.ap()[t], x2[:], reads=[x2], writes=[x2o])
        h2, h2T = h2s[t % 2], h2Ts[t % 2]
        A2, B2 = (b1k["sc_l"], b1k["sh_l"]) if lat else (b1k["sc_c"], b1k["sh_c"])
        norm_mod_tile(P, x2, h2, A2, B2, scr, ss2)
        transpose_tile(P, h2, h2T, ident, pts)
        P.dma("sync", hTo.t.ap()[t], h2T[:], reads=[h2T], writes=[hTo])
        for k in range(8):
            P.mm(pr, pr[:, 0:16], h2T, h2T[:, k, :], rw, rw[:, k, :], start=(k == 0), stop=(k == 7))
        P.cp("act", lg, lg[:], pr, pr[:, 0:16])
        P.op("dve", lambda e: e.reduce_max(out=m1[:], in_=lg[:], axis=AX.X), reads=[lg], writes=[m1])
        P.ts("dve", m1, m1[:], m1, m1[:], -1.0, None, ALU.mult)
        P.act(ex, ex[:], lg, lg[:], AF.Exp, bias=m1[:, 0:1], accum=s1[:], rd=[m1], wr=[s1])
        P.op("dve", lambda e: e.reciprocal(out=s1[:], in_=s1[:]), reads=[s1], writes=[s1])
        af = afs[t % 2]
        P.ts("dve", af, af[:], ex, ex[:], s1[:, 0:1], None, ALU.mult, rd=[s1])
        P.dma("sync", affo.t.ap()[t], af[:], reads=[af], writes=[affo])
    P.finish([x2o, hTo, affo])
    return nc


def pool_band(pos, T):
    src = np.concatenate([pos, pos[0] - 8 + np.arange(8), pos[-1] + 1 + np.arange(8)])
    out = np.zeros((144, 4, 128), np.float32)
    for g, w in enumerate(POOL_WINDOWS):
        lo = np.clip(pos - w // 2, 0, T)
        hi = np.clip(pos + w // 2, 0, T)
        cnt = np.maximum(hi - lo, 1).astype(np.float32)
        m = (src[:, None] >= lo[None, :]) & (src[:, None] < hi[None, :])
        out[:, g, :] = m / cnt[None, :]
        out[np.arange(128), g, np.arange(128)] -= 1.0
    return out.reshape(144, 512)


NE = 16
FF = 512


def bisect_threshold(P, affs, J, kcap, ones, pcnt, name):
    lo = P.sb([128, NE], name=name + "_lo")
    hi = P.sb([128, NE], name=name + "_hi")
    mid = P.sb([128, NE], name=name + "_mid")
    cnt = P.sb([128, NE], name=name + "_cnt")
    pred = P.sb([128, NE], name=name + "_pred")
    tmp = P.sb([128, NE], name=name + "_tmp")
    cmp_ = P.sb([128, NE, J], name=name + "_cmp")
    P.op("dve", lambda e: e.memset(lo[:], 0.0), writes=[lo])
    P.op("dve", lambda e: e.memset(hi[:], 1.0), writes=[hi])
    for it in range(34):
        P.tt("dve", mid, mid[:], lo, lo[:], hi, hi[:], ALU.add)
        P.ts("dve", mid, mid[:], mid, mid[:], 0.5, None, ALU.mult)
        for e_ in range(NE):
            P.ts(("dve", "pool")[e_ % 2], cmp_, cmp_[:, e_, :], affs, affs[:, e_, :], mid[:, e_:e_ + 1], None, ALU.is_ge, rd=[mid])
        P.op("dve", lambda e: e.reduce_sum(out=cnt[:], in_=cmp_[:], axis=AX.X), reads=[cmp_], writes=[cnt])
        P.mm(pcnt, pcnt[:, 0:NE], ones, ones[:], cnt, cnt[:])
        P.ts("dve", pred, pred[:], pcnt, pcnt[:, 0:NE], float(kcap) - 0.5, None, ALU.is_ge)
        P.tt("dve", tmp, tmp[:], mid, mid[:], lo, lo[:], ALU.subtract)
        P.tt("dve", tmp, tmp[:], tmp, tmp[:], pred, pred[:], ALU.mult)
        P.tt("dve", lo, lo[:], lo, lo[:], tmp, tmp[:], ALU.add)
        P.tt("dve", tmp, tmp[:], hi, hi[:], mid, mid[:], ALU.subtract)
        P.tt("dve", tmp, tmp[:], tmp, tmp[:], pred, pred[:], ALU.mult)
        P.tt("dve", hi, hi[:], mid, mid[:], tmp, tmp[:], ALU.add)
    return lo


def build_k4(NT, n_lat, kcap_lat, kcap_ctx, J_lat, J_ctx, final_norm, passes):
    nc = bass.Bass("TRN2", target_bir_lowering=False)
    P = Prog(nc)
    x2d = P.dram("x2", [NT, 128, D], F32, kind="ExternalInput")
    hTd = P.dram("h2T", [NT, 128, 8, 128], F32, kind="ExternalInput")
    afd = P.dram("aff", [NT, 128, NE], F32, kind="ExternalInput")
    asl = P.dram("affs_l", [128, NE, J_lat], F32, kind="ExternalInput")
    asc = P.dram("affs_c", [128, NE, J_ctx], F32, kind="ExternalInput")
    wgd = P.dram("wg", [NE, D, FF], F32, kind="ExternalInput")
    wud = P.dram("wu", [NE, D, FF], F32, kind="ExternalInput")
    wdd = P.dram("wd", [NE, FF, D], F32, kind="ExternalInput")
    onesd = P.dram("ones", [128, 128], F32, kind="ExternalInput")
    v1k = {n: P.dram(n, [D], F32, kind="ExternalInput") for n in ("g2_l", "g2_c", "fng")}
    outd = P.dram("out", [NT, 128, D], F32, kind="ExternalOutput")

    ones = P.sb([128, 128], name="ones")
    P.dma("sync", ones[:], onesd.t.ap(), reads=[onesd], writes=[ones])
    b1k = {n: load_bcast(P, ("sync", "act")[i % 2], v1k[n], D, "b_" + n) for i, n in enumerate(v1k)}
    pcnt = P.ps([1], name="pcnt")
    affs_l = P.sb([128, NE, J_lat], name="affs_l")
    P.dma("sync", affs_l[:], asl.t.ap(), reads=[asl], writes=[affs_l])
    thr_l = bisect_threshold(P, affs_l, J_lat, kcap_lat, ones, pcnt, "bl")
    thr_c = None
    if n_lat < NT:
        affs_c = P.sb([128, NE, J_ctx], name="affs_c")
        P.dma("act", affs_c[:], asc.t.ap(), reads=[asc], writes=[affs_c])
        thr_c = bisect_threshold(P, affs_c, J_ctx, kcap_ctx, ones, pcnt, "bc")
    afo = P.sb([128, NT, NE], name="afo")
    gw = P.sb([128, NT, NE], name="gw")
    P.dma("sync", afo[:], afd.t.ap().rearrange("t p e -> p t e"), reads=[afd], writes=[afo])
    for t in range(NT):
        thr = thr_l if t < n_lat else thr_c
        P.tt("dve", gw, gw[:, t, :], afo, afo[:, t, :], thr, thr[:], ALU.is_ge)
        P.tt("dve", gw, gw[:, t, :], gw, gw[:, t, :], afo, afo[:, t, :], ALU.mult)

    maxt = max(sum(t1 - t0 for (t0, t1) in ps_) for ps_ in passes)
    hT = P.sb([128, 8, maxt * 128], BF16, name="hT")
    hst = [P.sb([128, 8, 128], name="hst%d" % i) for i in range(2)]
    acc = P.sb([128, maxt, D], name="acc")
    Wg = [P.sb([128, 8, FF], BF16, name="Wg%d" % i) for i in range(2)]
    Wu = [P.sb([128, 8, FF], BF16, name="Wu%d" % i) for i in range(2)]
    Wd = [P.sb([128, 4, D], BF16, name="Wd%d" % i) for i in range(2)]
    stg = [P.sb([128, 2048], name="stg%d" % i) for i in range(2)]
    sil = [P.sb([128, 512], name="sil%d" % i) for i in range(2)]
    hidT = [P.sb([128, 4, 512], BF16, name="hidT%d" % i) for i in range(2)]
    xt = [P.sb([128, D], name="xt%d" % i) for i in range(2)]
    scr = P.sb([128, D], name="scr")
    ssf = P.sb([128, 1], name="ssf")
    pg = [P.ps([1], name="pg%d" % i) for i in range(2)]
    pu = [P.ps([1], name="pu%d" % i) for i in range(2)]
    pd = [P.ps([1], name="pd%d" % i) for i in range(2)]
    sti = 0
    wi = 0
    gi = 0
    for ps_ in passes:
        tiles = [t for (t0, t1) in ps_ for t in range(t0, t1)]
        loc = {t: i for i, t in enumerate(tiles)}
        for i, t in enumerate(tiles):
            h_ = hst[i % 2]
            P.dma(("sync", "act")[i % 2], h_[:], hTd.t.ap()[t], reads=[hTd], writes=[h_])
            P.cp(("pool", "act")[i % 2], hT, hT[:, :, i * 128:(i + 1) * 128], h_, h_[:])
        P.op("pool", lambda e: e.memset(acc[:], 0.0), writes=[acc])
        for ex in range(NE):
            wg_, wu_, wd_ = Wg[wi % 2], Wu[wi % 2], Wd[wi % 2]
            wi += 1
            for (wt, src, nk) in ((wg_, wgd, 8), (wu_, wud, 8), (wd_, wdd, 4)):
                for hf in range(2):
                    s_ = stg[sti % 2]
                    sti += 1
                    k0, k1 = hf * nk // 2, (hf + 1) * nk // 2
                    sv = s_[:].rearrange("p (k f) -> p k f", k=nk // 2)
                    P.dma(("sync", "act")[sti % 2], sv, src.t.ap()[ex].rearrange("(k p) f -> p k f", p=128)[:, k0:k1, :],
                          reads=[src], writes=[s_])
                    P.cp(("pool", "act", "dve")[sti % 3], wt, wt[:, k0:k1, :], s_, sv)
            for (t0, t1) in ps_:
                n = (t1 - t0) * 128
                c0 = loc[t0] * 128
                hd = hidT[gi % 2]
                gi += 1
                for fc in range(4):
                    pg_, pu_, sl_ = pg[fc % 2], pu[fc % 2], sil[fc % 2]
                    for k in range(8):
                        P.mm(pg_, pg_[:, 0:n], wg_, wg_[:, k, fc * 128:(fc + 1) * 128], hT, hT[:, k, c0:c0 + n], start=(k == 0), stop=(k == 7))
                    for k in range(8):
                        P.mm(pu_, pu_[:, 0:n], wu_, wu_[:, k, fc * 128:(fc + 1) * 128], hT, hT[:, k, c0:c0 + n], start=(k == 0), stop=(k == 7))
                    P.act(sl_, sl_[:, 0:n], pg_, pg_[:, 0:n], AF.Silu)
                    P.tt("dve", hd, hd[:, fc, 0:n], pu_, pu_[:, 0:n], sl_, sl_[:, 0:n], ALU.mult)
                for t in range(t0, t1):
                    i = loc[t]
                    tl = slice((t - t0) * 128, (t - t0 + 1) * 128)
                    for half in range(2):
                        pd_ = pd[half]
                        cs = slice(half * 512, (half + 1) * 512)
                        for fc in range(4):
                            P.mm(pd_, pd_[:, :], hd, hd[:, fc, tl], wd_, wd_[:, fc, cs], start=(fc == 0), stop=(fc == 3))
                        P.stt("dve", acc, acc[:, i, cs], pd_, pd_[:, :], gw[:, t, ex:ex + 1], acc, acc[:, i, cs], ALU.mult, ALU.add, rd=[gw])
        for t in tiles:
            i = loc[t]
            x_ = xt[t % 2]
            g2 = b1k["g2_l"] if t < n_lat else b1k["g2_c"]
            P.dma("act", x_[:], x2d.t.ap()[t], reads=[x2d], writes=[x_])
            P.tt("pool", acc, acc[:, i, :], acc, acc[:, i, :], g2, g2[:], ALU.mult)
            P.tt("pool", x_, x_[:], x_, x_[:], acc, acc[:, i, :], ALU.add)
            if final_norm:
                P.act(scr, scr[:], x_, x_[:], AF.Square, scale=1.0 / 32.0, accum=ssf[:], wr=[ssf])
                P.ts("dve", ssf, ssf[:], ssf, ssf[:], 1e-6, None, ALU.add)
                P.op("act", lambda e: e.sqrt(out=ssf[:], in_=ssf[:]), reads=[ssf], writes=[ssf])
                P.op("dve", lambda e: e.reciprocal(out=ssf[:], in_=ssf[:]), reads=[ssf], writes=[ssf])
                P.stt("dve", x_, x_[:], x_, x_[:], ssf[:, 0:1], b1k["fng"], b1k["fng"][:], ALU.mult, ALU.mult, rd=[ssf])
            P.dma("sync", outd.t.ap()[t], x_[:], reads=[x_], writes=[outd])
    P.finish([outd])
    return nc


B, T, CTX = 2, 16384, 256
NLT = 32


def tiles_of(lat, ctx, core, with_ctx=True):
    s, q = core // 4, core % 4
    Cc = lat.shape[-1]
    lt = lat[s, q * 4096:(q + 1) * 4096].reshape(NLT, 128, Cc)
    if not with_ctx:
        return np.ascontiguousarray(lt)
    ct = np.zeros((1, 128, Cc), np.float32)
    n = min(128, CTX - q * 64)
    ct[0, :n] = ctx[s, q * 64:q * 64 + n]
    return np.concatenate([lt, ct], 0)


def untile(res, key, Cc, with_ctx=True):
    lat = np.zeros((B, T, Cc), np.float32)
    ctx = np.zeros((B, CTX, Cc), np.float32)
    for core in range(NCORE):
        s, q = core // 4, core % 4
        r = res[core][key]
        lat[s, q * 4096:(q + 1) * 4096] = r[:NLT].reshape(4096, Cc)
        if with_ctx:
            ctx[s, q * 64:(q + 1) * 64] = r[NLT, :64]
    return lat, ctx


def unseq(y, d, cm):
    y = y.reshape(NCH * 128, -1)
    c_, l_ = y[:CTX], y[CTX:]
    if d == 1:
        c_, l_ = c_[::-1], l_[::-1]
    if cm:
        l_ = from_cm(l_)
    return c_, l_


_band_cache = {}


def band_for(pos0, Tseq):
    key = (pos0 if (pos0 == 0 or pos0 + 128 + 8 > Tseq) else -1, Tseq)
    if key not in _band_cache:
        _band_cache[key] = pool_band(pos0 + np.arange(128), Tseq)
    return _band_cache[key]


def halo_for(u, pos0):
    Tseq = u.shape[0]
    h = np.zeros((16, 256), np.float32)
    for i in range(8):
        a = pos0 - 8 + i
        if 0 <= a < Tseq:
            h[i] = u[a]
        b_ = pos0 + 128 + i
        if 0 <= b_ < Tseq:
            h[8 + i] = u[b_]
    return h


def stage_proj(l, x, ctx, mods, p):
    f32 = _f32
    m = mods[l]
    seg = lambda r, i: f32(m[r, i * 1024:(i + 1) * 1024])
    idn = np.eye(128, dtype=np.float32)
    in_maps = []
    for core in range(NCORE):
        s = core // 4
        in_maps.append({"xt": tiles_of(x, ctx, core), "w": f32(p["w_in"][l]), "g": f32(p["norm1_g"][l]),
                        "sh_l": seg(s, 0), "sc_l": seg(s, 1), "sh_c": seg(2, 0), "sc_c": seg(2, 1), "idn": idn})
    res = run(build_k1(NLT + 1), in_maps)
    return untile(res, "out", 3108)


def stage_scans(l, pl, pc, p):
    f32 = _f32
    res = run(build_k2s(), [prep_k2s(pl, pc, f32(p["ssd_conv_w"][l]), f32(p["ssd_conv_b"][l]), f32(p["ssd_a_log"][l]),
                                     f32(p["ssd_dt_bias"][l]), core) for core in range(NCORE)])
    ys_l = np.zeros((B, 2, T, 384), np.float32)
    ys_c = np.zeros((B, 2, CTX, 384), np.float32)
    xs_l = np.zeros((B, T, 384), np.float32)
    xs_c = np.zeros((B, CTX, 384), np.float32)
    for core in range(NCORE):
        s, d, g = core // 4, (core // 2) % 2, core % 2
        c_, l_ = unseq(res[core]["y"], d, False)
        ys_l[s, d, :, g * 192:(g + 1) * 192] = l_
        ys_c[s, d, :, g * 192:(g + 1) * 192] = c_
        if d == 0:
            c_, l_ = unseq(res[core]["xs"], 0, False)
            xs_l[s, :, g * 192:(g + 1) * 192] = l_
            xs_c[s, :, g * 192:(g + 1) * 192] = c_
    del res
    res = run(build_k2g(), [prep_k2g(pl, pc, f32(p["gdn_conv_w"][l]), f32(p["gdn_a_log"][l]), f32(p["gdn_dt_bias"][l]), core)
                            for core in range(NCORE)])
    os_l = np.zeros((B, 2, T, 384), np.float32)
    os_c = np.zeros((B, 2, CTX, 384), np.float32)
    for core in range(NCORE):
        s, d, g = core // 4, (core // 2) % 2, core % 2
        c_, l_ = unseq(res[core]["o"], d, True)
        os_l[s, d, :, g * 192:(g + 1) * 192] = l_
        os_c[s, d, :, g * 192:(g + 1) * 192] = c_
    return ys_l, ys_c, xs_l, xs_c, os_l, os_c


def stage_post(l, x, ctx, pl, pc, scans, mods, p):
    f32 = _f32
    ys_l, ys_c, xs_l, xs_c, os_l, os_c = scans
    m = mods[l]
    seg = lambda r, i: f32(m[r, i * 1024:(i + 1) * 1024])
    idn = np.eye(128, dtype=np.float32)
    pwp = np.zeros((64, 4, 128), np.float32)
    for g in range(4):
        pwp[:, g, (g % 2) * 64:(g % 2) * 64 + 64] = p["pool_w"][l][g]
    common = {"wout": f32(p["w_out"][l]), "pw": pwp, "pscale": f32(np.asarray(p["pool_scale"][l]).reshape(2, 128).T),
              "rw": f32(np.asarray(p["router_w"][l]).reshape(8, 128, 16).transpose(1, 0, 2)), "idn": idn,
              "dvec": f32(np.repeat(np.asarray(p["ssd_d"][l]), 64)), "sng": f32(p["ssd_norm_g"][l]),
              "gng": f32(np.tile(np.asarray(p["gdn_norm_g"][l]), 6)), "n2g": f32(p["norm2_g"][l])}
    in_maps = []
    for core in range(NCORE):
        s, q = core // 4, core % 4
        ls = slice(q * 4096, (q + 1) * 4096)
        lat_p = np.concatenate([x[s, ls], pl[s, ls, 0:256], ys_l[s, 0, ls], ys_l[s, 1, ls], xs_l[s, ls], pl[s, ls, 256:640],
                                os_l[s, 0, ls], os_l[s, 1, ls], pl[s, ls, 2700:3084]], axis=-1).reshape(NLT, 128, PKW)
        n = min(128, CTX - q * 64)
        cs_ = slice(q * 64, q * 64 + n)
        ctx_p = np.zeros((1, 128, PKW), np.float32)
        ctx_p[0, :n] = np.concatenate([ctx[s, cs_], pc[s, cs_, 0:256], ys_c[s, 0, cs_], ys_c[s, 1, cs_], xs_c[s, cs_],
                                       pc[s, cs_, 256:640], os_c[s, 0, cs_], os_c[s, 1, cs_], pc[s, cs_, 2700:3084]], axis=-1)
        halo = np.stack([halo_for(pl[s, :, 0:256], q * 4096 + i * 128) for i in range(NLT)] + [halo_for(pc[s, :, 0:256], q * 64)])
        band = np.stack([band_for(q * 4096 + i * 128, T) for i in range(NLT)] + [band_for(q * 64, CTX)])
        d_ = {"pk": np.ascontiguousarray(np.concatenate([lat_p, ctx_p], 0)), "halo": f32(halo), "band": f32(band),
              "g1_l": seg(s, 2), "g1_c": seg(2, 2), "sh_l": seg(s, 3), "sc_l": seg(s, 4), "sh_c": seg(2, 3), "sc_c": seg(2, 4)}
        d_.update(common)
        in_maps.append(d_)
    return run(build_k3(NLT + 1), in_maps)


def stage_moe(l, res3, mods, p, last):
    f32 = _f32
    m = mods[l]
    seg = lambda r, i: f32(m[r, i * 1024:(i + 1) * 1024])
    ones = np.ones((128, 128), np.float32)
    aff_l, aff_c = untile(res3, "aff", 16)
    with_ctx = not last
    NT = NLT + 1 if with_ctx else NLT
    passes = [[(q * 8, q * 8 + 4), (q * 8 + 4, q * 8 + 8)] for q in range(4)]
    if with_ctx:
        passes[3].append((NLT, NLT + 1))
    in_maps = []
    for core in range(NCORE):
        s = core // 4
        in_maps.append({"x2": f32(res3[core]["x2"][:NT]), "h2T": f32(res3[core]["h2T"][:NT]), "aff": f32(res3[core]["aff"][:NT]),
                        "affs_l": f32(aff_l[s].reshape(128, 128, 16).transpose(0, 2, 1)),
                        "affs_c": f32(aff_c[s].reshape(128, 2, 16).transpose(0, 2, 1)),
                        "wg": f32(p["exp_w_gate"][l]), "wu": f32(p["exp_w_up"][l]), "wd": f32(p["exp_w_down"][l]), "ones": ones,
                        "g2_l": seg(s, 5), "g2_c": seg(2, 5), "fng": f32(p["final_norm_g"])})
    res = run(build_k4(NT, NLT, 2 * T // 16, 2 * CTX // 16, 128, 2, last, passes), in_maps)
    return untile(res, "out", 1024, with_ctx=with_ctx)


def _f32(a):
    return np.ascontiguousarray(np.asarray(a, dtype=np.float32))


def kernel(**p):
    x, ctx = _f32(p["x"]), _f32(p["ctx"])
    L = p["ada_w"].shape[0]
    mods = run_k0(_f32(p["c"]), _f32(p["c_ctx"]), _f32(p["ada_w"]), _f32(p["ada_b"]))
    for l in range(L):
        last = l == L - 1
        pl, pc = stage_proj(l, x, ctx, mods, p)
        scans = stage_scans(l, pl, pc, p)
        res3 = stage_post(l, x, ctx, pl, pc, scans, mods, p)
        del scans, pl, pc
        x, ctx_new = stage_moe(l, res3, mods, p, last)
        del res3
        if not last:
            ctx = ctx_new
    return x
```

```python
import contextlib
import numpy as np
import concourse.bass as bass
import concourse.mybir as mybir
from concourse.bass_utils import run_bass_kernel_spmd

F32 = mybir.dt.float32
BF16 = mybir.dt.bfloat16
ALU = mybir.AluOpType
AF = mybir.ActivationFunctionType
AX = mybir.AxisListType


class Buf:
    def __init__(self, t=None, name=""):
        self.t = t
        self.name = name
        self.w = None
        self.r = []
        self.excl = False

    def __getitem__(self, k):
        return self.t[k]


class Prog:
    ENGS = ("sync", "act", "dve", "pool", "pe")

    def __init__(self, nc, n_dma_sems=40):
        self.nc = nc
        self.es = contextlib.ExitStack()
        self.q = {e: [] for e in self.ENGS}
        self.EPOCH = 4000
        self.esem = {}
        self.seq = {e: 0 for e in ("act", "dve", "pool", "pe")}
        self.dsem = [nc.alloc_semaphore("ds_%d" % i) for i in range(n_dma_sems)]
        self.dcnt = [0] * n_dma_sems
        self.dlast = [None] * n_dma_sems
        self.dnext = 0
        self.waited = {e: {} for e in self.ENGS}
        self.nbuf = 0

    def sb(self, shape, dtype=F32, name=None):
        self.nbuf += 1
        name = "s_" + (name or "sb%d" % self.nbuf)
        t = self.es.enter_context(self.nc.sbuf_tensor(name, list(shape), dtype))
        return Buf(t, name)

    def ps(self, shape, dtype=F32, name=None):
        self.nbuf += 1
        name = "p_" + (name or "ps%d" % self.nbuf)
        t = self.es.enter_context(self.nc.psum_tensor(name, [128, 512], F32))
        b = Buf(t, name)
        b.excl = True
        return b

    def dram(self, name, shape, dtype=F32, kind="Internal"):
        t = self.nc.dram_tensor(name, list(shape), dtype, kind=kind)
        return Buf(t, name)

    def _need(self, eng, reads, writes):
        evs = []
        for b in reads:
            if b.w is not None:
                evs.append(b.w)
            if b.excl:
                evs.extend(b.r)
        for b in writes:
            if b.w is not None:
                evs.append(b.w)
            evs.extend(b.r)
        best = {}
        for (k, v) in evs:
            if eng == "pe" and k[0] == "pe":
                continue
            if best.get(k, 0) < v:
                best[k] = v
        out = []
        wd = self.waited[eng]
        for k, v in best.items():
            if wd.get(k, 0) >= v:
                continue
            wd[k] = v
            out.append((k, v))
        return out

    def _mark(self, ev, reads, writes):
        for b in reads:
            b.r.append(ev)
        for b in writes:
            b.w = ev
            b.r = []

    def op(self, eng, fn, reads=(), writes=()):
        waits = self._need(eng, reads, writes)
        ep, v = divmod(self.seq[eng], self.EPOCH)
        self.seq[eng] += 1
        key = (eng, ep)
        if key not in self.esem:
            self.esem[key] = self.nc.alloc_semaphore("es_%s_%d" % key)
        ev = (key, v + 1)
        self._mark(ev, reads, writes)
        self.q[eng].append((waits, fn, (key, 1)))

    def dma(self, queue, out, in_, reads=(), writes=(), **kw):
        i = self.dnext
        self.dnext = (self.dnext + 1) % len(self.dsem)
        waits = self._need(queue, reads, writes)
        key = ("d", i)
        if self.dcnt[i] > 0 and self.waited[queue].get(key, 0) < self.dcnt[i]:
            self.waited[queue][key] = self.dcnt[i]
            waits.append((key, self.dcnt[i]))
        self.dcnt[i] += 16
        ev = (key, self.dcnt[i])
        self._mark(ev, reads, writes)
        self.q[queue].append((waits, lambda e: e.dma_start(out=out, in_=in_, **kw), (key, 16)))
        return ev

    def mm(self, O, o, A, a, B, b, start=True, stop=True):
        self.op("pe", lambda e: e.matmul(o, lhsT=a, rhs=b, start=start, stop=stop), reads=[A, B], writes=[O])

    def tr(self, O, o, A, a, I, i):
        self.op("pe", lambda e: e.transpose(out=o, in_=a, identity=i), reads=[A, I], writes=[O])

    def act(self, O, o, A, a, func, bias=None, scale=1.0, accum=None, rd=(), wr=()):
        kw = {}
        if bias is not None:
            kw["bias"] = bias
        if accum is not None:
            kw["accum_out"] = accum
        self.op("act", lambda e: e.activation(out=o, in_=a, func=func, scale=scale, **kw),
                reads=[A] + list(rd), writes=[O] + list(wr))

    def tt(self, eng, O, o, A, a, B, b, op):
        self.op(eng, lambda e: e.tensor_tensor(out=o, in0=a, in1=b, op=op), reads=[A, B], writes=[O])

    def ts(self, eng, O, o, A, a, s1, s2, op0, op1=None, rd=()):
        if op1 is None:
            self.op(eng, lambda e: e.tensor_scalar(out=o, in0=a, scalar1=s1, scalar2=None, op0=op0),
                    reads=[A] + list(rd), writes=[O])
        else:
            self.op(eng, lambda e: e.tensor_scalar(out=o, in0=a, scalar1=s1, scalar2=s2, op0=op0, op1=op1),
                    reads=[A] + list(rd), writes=[O])

    def stt(self, eng, O, o, A, a, sc, B, b, op0, op1, rd=()):
        self.op(eng, lambda e: e.scalar_tensor_tensor(out=o, in0=a, scalar=sc, in1=b, op0=op0, op1=op1),
                reads=[A, B] + list(rd), writes=[O])

    def cp(self, eng, O, o, A, a):
        if eng == "act":
            self.op("act", lambda e: e.copy(out=o, in_=a), reads=[A], writes=[O])
        else:
            self.op(eng, lambda e: e.tensor_copy(out=o, in_=a), reads=[A], writes=[O])

    def _sem(self, k):
        if k[0] == "d":
            return self.dsem[k[1]]
        return self.esem[k]

    def finish(self, final_bufs):
        evs = []
        for b in final_bufs:
            if b.w is not None:
                evs.append(b.w)
        fw = []
        for (k, v) in evs:
            fw.append((k, v))
        nc = self.nc
        q = self.q
        semf = self._sem

        def replay(e, lst, tail=()):
            for waits, fn, inc in lst:
                for (k, v) in waits:
                    e.wait_ge(semf(k), v)
                ins = fn(e)
                ins.then_inc(semf(inc[0]), inc[1])
            for (k, v) in tail:
                e.wait_ge(semf(k), v)

        with nc.Block() as block:
            @block.sync
            def _(e):
                replay(e, q["sync"], fw)

            @block.scalar
            def _(e):
                replay(e, q["act"])

            @block.vector
            def _(e):
                replay(e, q["dve"])

            @block.gpsimd
            def _(e):
                replay(e, q["pool"])

            @block.tensor
            def _(e):
                replay(e, q["pe"])
        self.es.close()


D = 1024
IN_DIM = 3108
NCORE = 8


def run(nc, in_maps):
    res = run_bass_kernel_spmd(nc, in_maps, core_ids=list(range(NCORE)))
    return res.results


def build_k0():
    nc = bass.Bass("TRN2", target_bir_lowering=False)
    P = Prog(nc)
    cT = P.dram("cT", [128, 8, 3], F32, kind="ExternalInput")
    aw = P.dram("aw", [3, 8, 128, 512], F32, kind="ExternalInput")
    ab = P.dram("ab", [3, 512], F32, kind="ExternalInput")
    out = P.dram("out", [3, 3, 512], F32, kind="ExternalOutput")
    sc = P.sb([128, 8, 3])
    P.dma("sync", sc[:], cT.t.ap(), reads=[cT], writes=[sc])
    P.act(sc, sc[:], sc, sc[:], AF.Silu)
    for j in range(3):
        w = P.sb([128, 8, 512], name="w%d" % j)
        bt = P.sb([3, 512], name="b%d" % j)
        ot = P.sb([3, 512], name="o%d" % j)
        pm = P.ps([3, 512], name="pm%d" % j)
        P.dma(("sync", "act", "pool")[j], w[:], aw.t.ap()[j].rearrange("k p n -> p k n"), reads=[aw], writes=[w])
        P.dma("sync", bt[:], ab.t.ap()[j].partition_broadcast(3), reads=[ab], writes=[bt])
        for k in range(8):
            P.mm(pm, pm[0:3, :], sc, sc[:, k, :], w, w[:, k, :], start=(k == 0), stop=(k == 7))
        P.tt("dve", ot, ot[:], pm, pm[0:3, :], bt, bt[:], ALU.add)
        P.dma("sync", out.t.ap()[j], ot[:], reads=[ot], writes=[out])
    P.finish([out])
    return nc


def run_k0(c, c_ctx, ada_w, ada_b):
    L = ada_w.shape[0]
    cv = np.stack([c[0], c[1], c_ctx], axis=0)
    cT = np.ascontiguousarray(cv.reshape(3, 8, 128).transpose(2, 1, 0))
    nblk = L * 12
    assert nblk == 24
    in_maps = []
    for core in range(NCORE):
        aws, abs_ = [], []
        for j in range(3):
            b = core * 3 + j
            l, cb = divmod(b, 12)
            aws.append(ada_w[l][:, cb * 512:(cb + 1) * 512].reshape(8, 128, 512))
            abs_.append(ada_b[l][cb * 512:(cb + 1) * 512])
        in_maps.append({"cT": cT, "aw": np.ascontiguousarray(np.stack(aws)), "ab": np.ascontiguousarray(np.stack(abs_))})
    res = run(build_k0(), in_maps)
    mods = np.zeros((L, 3, 6144), np.float32)
    for core in range(NCORE):
        for j in range(3):
            b = core * 3 + j
            l, cb = divmod(b, 12)
            mods[l, :, cb * 512:(cb + 1) * 512] = res[core]["out"][j]
    return mods


def load_bcast(P, queue, dram_buf, n, name):
    t = P.sb([128, n], name=name)
    P.dma(queue, t[:], dram_buf.t.ap().partition_broadcast(128), reads=[dram_buf], writes=[t])
    return t


def norm_mod_tile(P, xt, h, A, B, scr, ss, eng2="pool"):
    P.act(scr, scr[:], xt, xt[:], AF.Square, scale=1.0 / 32.0, accum=ss[:], wr=[ss])
    P.ts("dve", ss, ss[:], ss, ss[:], 1e-6, None, ALU.add)
    P.op("act", lambda e: e.sqrt(out=ss[:], in_=ss[:]), reads=[ss], writes=[ss])
    P.op("dve", lambda e: e.reciprocal(out=ss[:], in_=ss[:]), reads=[ss], writes=[ss])
    P.stt("dve", h, h[:], xt, xt[:], ss[:, 0:1], A, A[:], ALU.mult, ALU.mult, rd=[ss])
    P.tt(eng2, h, h[:], h, h[:], B, B[:], ALU.add)


def transpose_tile(P, h, hT, ident, pts, nk=8, evac=("act", "dve")):
    for half in range((nk + 3) // 4):
        pt = pts[half % len(pts)]
        n4 = min(4, nk - half * 4)
        for q in range(n4):
            k = half * 4 + q
            P.tr(pt, pt[:, q * 128:(q + 1) * 128], h, h[:, k * 128:(k + 1) * 128], ident, ident[:])
        P.cp(evac[half % len(evac)], hT, hT[:, half * 4:half * 4 + n4, :],
             pt, pt[:, 0:n4 * 128].rearrange("p (k t) -> p k t", k=n4))


def load_weight_bf16(P, wdram_ap_fn, W, nk, ncols, stage, wdram, colchunk=None):
    qs = ("sync", "act", "pool")
    cs = ("pool", "dve", "act")
    for k in range(nk):
        st = stage[k % len(stage)]
        P.dma(qs[k % 3], st[:, 0:ncols], wdram_ap_fn(k), reads=[wdram], writes=[st])
        P.cp(cs[k % 3], W, W[:, k, :], st, st[:, 0:ncols])


def build_k1(NT, ncols=IN_DIM, n_lat=None):
    if n_lat is None:
        n_lat = NT - 1
    nc = bass.Bass("TRN2", target_bir_lowering=False)
    P = Prog(nc)
    xt = P.dram("xt", [NT, 128, D], F32, kind="ExternalInput")
    w = P.dram("w", [D, ncols], F32, kind="ExternalInput")
    g = P.dram("g", [D], F32, kind="ExternalInput")
    vecs = {n: P.dram(n, [D], F32, kind="ExternalInput") for n in ("sh_l", "sc_l", "sh_c", "sc_c")}
    idn = P.dram("idn", [128, 128], F32, kind="ExternalInput")
    out = P.dram("out", [NT, 128, ncols], F32, kind="ExternalOutput")

    ident = P.sb([128, 128], name="ident")
    P.dma("sync", ident[:], idn.t.ap(), reads=[idn], writes=[ident])
    gb = load_bcast(P, "act", g, D, "gb")
    A_l = load_bcast(P, "pool", vecs["sc_l"], D, "A_l")
    B_l = load_bcast(P, "sync", vecs["sh_l"], D, "B_l")
    A_c = load_bcast(P, "act", vecs["sc_c"], D, "A_c")
    B_c = load_bcast(P, "pool", vecs["sh_c"], D, "B_c")
    for A in (A_l, A_c):
        P.stt("dve", A, A[:], A, A[:], 1.0, gb, gb[:], ALU.add, ALU.mult)

    W = P.sb([128, 8, ncols], BF16, name="W")
    stage = [P.sb([128, ncols], name="stg%d" % i) for i in range(2)]
    load_weight_bf16(P, lambda k: w.t.ap()[k * 128:(k + 1) * 128, :], W, 8, ncols, stage, w)

    xs = [P.sb([128, D], name="x%d" % i) for i in range(2)]
    hs = [P.sb([128, D], name="h%d" % i) for i in range(2)]
    scr = P.sb([128, D], name="scr")
    sss = [P.sb([128, 1], name="ss%d" % i) for i in range(2)]
    hTs = [P.sb([128, 8, 128], BF16, name="hT%d" % i) for i in range(2)]
    outs = [P.sb([128, ncols], name="ot%d" % i) for i in range(2)]
    pts = [P.ps([128, 512], name="pt%d" % i) for i in range(2)]
    pms = [P.ps([128, 512], name="pm%d" % i) for i in range(4)]
    ncb = (ncols + 511) // 512
    pmi = 0
    for t in range(NT):
        x_, h_, ss_, hT_, o_ = xs[t % 2], hs[t % 2], sss[t % 2], hTs[t % 2], outs[t % 2]
        P.dma(("sync", "pool")[t % 2], x_[:], xt.t.ap()[t], reads=[xt], writes=[x_])
        A, B = (A_l, B_l) if t < n_lat else (A_c, B_c)
        norm_mod_tile(P, x_, h_, A, B, scr, ss_)
        transpose_tile(P, h_, hT_, ident, pts)
        for cb in range(ncb):
            c0 = cb * 512
            cw = min(512, ncols - c0)
            pm = pms[pmi % 4]
            pmi += 1
            for k in range(8):
                P.mm(pm, pm[:, 0:cw], hT_, hT_[:, k, :], W, W[:, k, c0:c0 + cw], start=(k == 0), stop=(k == 7))
            P.cp(("act", "dve")[cb % 2], o_, o_[:, c0:c0 + cw], pm, pm[:, 0:cw])
        P.dma("sync", out.t.ap()[t], o_[:], reads=[o_], writes=[out])
    P.finish([out])
    return nc


NCH = 130
NBLK = 65


def consts():
    t = np.arange(128)
    U = (t[:, None] <= t[None, :]).astype(np.float32)
    NEG = np.where(t[None, :] < t[:, None], -30000.0, 0.0).astype(np.float32)
    return {"idn": np.eye(128, dtype=np.float32), "U": U, "NEG": NEG, "ones": np.ones((128, 128), np.float32)}


def load_consts(P, names=("idn", "U", "NEG", "ones")):
    out = {}
    for i, n in enumerate(names):
        d = P.dram(n, [128, 128], F32, kind="ExternalInput")
        s = P.sb([128, 128], name="c_" + n)
        P.dma(("sync", "act", "pool")[i % 3], s[:], d.t.ap(), reads=[d], writes=[s])
        out[n] = s
    return out


def conv_block(P, eng, ut, acc, cv, cw, cb, npart, n=256, bias=True):
    sl = slice(0, npart)
    P.ts(eng, acc, acc[sl, 0:n], ut, ut[sl, 0:n], cw[sl, 0:1], None, ALU.mult, rd=[cw])
    for k in range(1, 5):
        P.stt(eng, acc, acc[sl, 0:n], ut, ut[sl, k:k + n], cw[sl, k:k + 1], acc, acc[sl, 0:n], ALU.mult, ALU.add, rd=[cw])
    if bias:
        P.act(cv, cv[sl, 0:n], acc, acc[sl, 0:n], AF.Silu, bias=cb[sl, 0:1], rd=[cb])
    else:
        P.act(cv, cv[sl, 0:n], acc, acc[sl, 0:n], AF.Silu)


def softplus_inplace(P, t, ap):
    P.act(t, ap, t, ap, AF.Exp)
    P.ts("dve", t, ap, t, ap, 1.0, None, ALU.add)
    P.act(t, ap, t, ap, AF.Ln)


def cum_tables(P, C, la, pC1, pC2, nh=3):
    N = NCH * nh
    flat = lambda b: b[:].rearrange("p c h -> p (c h)")
    T = {}
    for n in ("negac", "eac", "wj", "eL"):
        T[n] = P.sb([128, NCH, nh], name="tb_" + n)
    P.mm(pC1, pC1[:, 0:N], C["U"], C["U"][:], la, flat(la))
    P.mm(pC2, pC2[:, 0:N], C["ones"], C["ones"][:], la, flat(la))
    P.ts("dve", T["negac"], flat(T["negac"]), pC1, pC1[:, 0:N], -1.0, None, ALU.mult)
    P.act(T["eac"], flat(T["eac"]), pC1, pC1[:, 0:N], AF.Exp)
    P.act(T["eL"], flat(T["eL"]), pC2, pC2[:, 0:N], AF.Exp)
    P.tt("dve", T["wj"], flat(T["wj"]), pC2, pC2[:, 0:N], T["negac"], flat(T["negac"]), ALU.add)
    P.act(T["wj"], flat(T["wj"]), T["wj"], flat(T["wj"]), AF.Exp)
    return T


def decay_mats(P, C, la, T, c, pA, rhs_t, decT, nh=3):
    for h in range(nh):
        r = rhs_t[h % len(rhs_t)]
        P.ts(("dve", "pool")[h % 2], r, r[:], C["U"], C["U"][:], la[:, c, h:h + 1], None, ALU.mult, rd=[la])
        P.mm(pA, pA[:, h * 128:(h + 1) * 128], C["ones"], C["ones"][:], r, r[:], start=True, stop=False)
        P.mm(pA, pA[:, h * 128:(h + 1) * 128], C["idn"], C["idn"][:], C["NEG"], C["NEG"][:], start=False, stop=True)
    for h in range(nh):
        P.act(decT, decT[:, h, :], pA, pA[:, h * 128:(h + 1) * 128], AF.Exp, bias=T["negac"][:, c, h:h + 1], rd=[T["negac"]])


def build_k2s():
    nc = bass.Bass("TRN2", target_bir_lowering=False)
    P = Prog(nc)
    u = P.dram("u", [448, NBLK, 260], F32, kind="ExternalInput")
    cwd = P.dram("cw", [448, 5], F32, kind="ExternalInput")
    cbd = P.dram("cb", [448, 1], F32, kind="ExternalInput")
    dtr = P.dram("dtr", [128, NCH, 3], F32, kind="ExternalInput")
    dtb = P.dram("dtb", [3], F32, kind="ExternalInput")
    alog = P.dram("alog", [3], F32, kind="ExternalInput")
    yout = P.dram("y", [NCH, 128, 192], F32, kind="ExternalOutput")
    xout = P.dram("xs", [NCH, 128, 192], F32, kind="ExternalOutput")
    C = load_consts(P)
    offs = (0, 128, 192, 320)
    nps = (128, 64, 128, 128)
    cw, cb = [], []
    for i in range(4):
        a = P.sb([128, 5], name="cw%d" % i)
        b = P.sb([128, 1], name="cb%d" % i)
        P.dma("sync", a[0:nps[i], :], cwd.t.ap()[offs[i]:offs[i] + nps[i], :], reads=[cwd], writes=[a])
        P.dma("act", b[0:nps[i], :], cbd.t.ap()[offs[i]:offs[i] + nps[i], :], reads=[cbd], writes=[b])
        cw.append(a)
        cb.append(b)
    dt = P.sb([128, NCH, 3], name="dt")
    la = P.sb([128, NCH, 3], name="la")
    dtw = P.sb([128, NCH, 3], name="dtw")
    dtbb = P.sb([128, 3], name="dtbb")
    Ab = P.sb([128, 3], name="Ab")
    P.dma("sync", dt[:], dtr.t.ap(), reads=[dtr], writes=[dt])
    P.dma("act", dtbb[:], dtb.t.ap().partition_broadcast(128), reads=[dtb], writes=[dtbb])
    P.dma("pool", Ab[:], alog.t.ap().partition_broadcast(128), reads=[alog], writes=[Ab])
    P.act(Ab, Ab[:], Ab, Ab[:], AF.Exp)
    P.ts("dve", Ab, Ab[:], Ab, Ab[:], -1.0, None, ALU.mult)
    for h in range(3):
        P.ts("dve", dt, dt[:, :, h], dt, dt[:, :, h], dtbb[:, h:h + 1], None, ALU.add, rd=[dtbb])
    fl = lambda b: b[:].rearrange("p c h -> p (c h)")
    softplus_inplace(P, dt, fl(dt))
    for h in range(3):
        P.ts("dve", la, la[:, :, h], dt, dt[:, :, h], Ab[:, h:h + 1], None, ALU.mult, rd=[Ab])
    pC1 = P.ps([128, 512], name="pC1")
    pC2 = P.ps([128, 512], name="pC2")
    T = cum_tables(P, C, la, pC1, pC2)
    P.tt("dve", dtw, fl(dtw), dt, fl(dt), T["wj"], fl(T["wj"]), ALU.mult)

    banks = [pC1, pC2] + [P.ps([1], name="bk%d" % i) for i in range(6)]
    bankset = [banks[0:3], banks[3:6]]
    bY2, bS = banks[6], banks[7]
    S = P.sb([128, 192], name="S")
    P.op("dve", lambda e: e.memset(S[:], 0.0), writes=[S])
    ut = [[P.sb([128, 260], name="ut%d_%d" % (i, j)) for j in range(2)] for i in range(4)]
    acc = [P.sb([128, 256], name="acc%d" % i) for i in range(4)]
    cv = [[P.sb([128, 256], name="cv%d_%d" % (i, j)) for j in range(2)] for i in range(4)]

    def mkset(n):
        return {"rhs": [P.sb([128, 128], name="rhs%d_%d" % (i, n)) for i in range(3)],
                "decT": P.sb([128, 3, 128], name="decT%d" % n), "Wt": P.sb([128, 3, 128], name="Wt%d" % n),
                "xdt": P.sb([128, 192], name="xdt%d" % n)}

    def mkhand(n, par):
        return {"tk": P.sb([128, 320], name="tok%d_%d" % (n, par)), "xw": P.sb([128, 192], name="xw%d_%d" % (n, par)),
                "ysb": P.sb([128, 192], name="ysb%d_%d" % (n, par))}

    sets = [mkset(0), mkset(1)]
    hands = [[mkhand(n, par) for par in range(2)] for n in range(2)]
    yo = [P.sb([128, 192], name="yo%d" % i) for i in range(2)]

    def pre(c, cc, j, st, bk, hd):
        bT, bA, bY1 = bk
        tk, xw, ysb = hd["tk"], hd["xw"], hd["ysb"]
        decT, Wt, xdt = st["decT"], st["Wt"], st["xdt"]
        xA, xB, BT, CT = cv[0][j], cv[1][j], cv[2][j], cv[3][j]
        ck = slice(cc * 128, (cc + 1) * 128)
        P.tr(bT, bT[:, 0:128], xA, xA[:, ck], C["idn"], C["idn"][:])
        P.tr(bT, bT[:, 128:192], xB, xB[0:64, ck], C["idn"], C["idn"][0:64, 0:64])
        P.tr(bT, bT[:, 192:320], BT, BT[:, ck], C["idn"], C["idn"][:])
        yield
        P.cp("act", tk, tk[:], bT, bT[:, 0:320])
        P.dma("sync", xout.t.ap()[c], tk[:, 0:192], reads=[tk], writes=[xout])
        for h in range(3):
            r = st["rhs"][h]
            P.ts(("dve", "pool")[h % 2], r, r[:], C["U"], C["U"][:], la[:, c, h:h + 1], None, ALU.mult, rd=[la])
        yield
        for h in range(3):
            r = st["rhs"][h]
            P.mm(bA, bA[:, h * 128:(h + 1) * 128], C["ones"], C["ones"][:], r, r[:], start=True, stop=False)
            P.mm(bA, bA[:, h * 128:(h + 1) * 128], C["idn"], C["idn"][:], C["NEG"], C["NEG"][:], start=False, stop=True)
        P.mm(bT, bT[:, 0:128], BT, BT[:, ck], CT, CT[:, ck])
        yield
        for h in range(3):
            P.act(decT, decT[:, h, :], bA, bA[:, h * 128:(h + 1) * 128], AF.Exp, bias=T["negac"][:, c, h:h + 1], rd=[T["negac"]])
        for h in range(3):
            hs = slice(h * 64, (h + 1) * 64)
            P.ts("pool", xdt, xdt[:, hs], tk, tk[:, hs], dt[:, c, h:h + 1], None, ALU.mult, rd=[dt])
            P.ts("pool", xw, xw[:, hs], tk, tk[:, hs], dtw[:, c, h:h + 1], None, ALU.mult, rd=[dtw])
        yield
        for h in range(3):
            P.tt("dve", Wt, Wt[:, h, :], bT, bT[:, 0:128], decT, decT[:, h, :], ALU.mult)
        yield
        for h in range(3):
            hs = slice(h * 64, (h + 1) * 64)
            P.mm(bY1, bY1[:, hs], Wt, Wt[:, h, :], xdt, xdt[:, hs])
        yield
        P.cp("act", ysb, ysb[:], bY1, bY1[:, 0:192])
        yield

    def rec(c, cc, j, hd):
        tk, xw, ysb = hd["tk"], hd["xw"], hd["ysb"]
        CT = cv[3][j]
        ck = slice(cc * 128, (cc + 1) * 128)
        for h in range(3):
            hs = slice(h * 64, (h + 1) * 64)
            P.mm(bY2, bY2[:, hs], CT, CT[:, ck], S, S[:, hs])
            P.mm(bS, bS[:, hs], tk, tk[:, 192:320], xw, xw[:, hs])
        yield
        y_ = yo[c % 2]
        for h in range(3):
            hs = slice(h * 64, (h + 1) * 64)
            P.stt("dve", y_, y_[:, hs], bY2, bY2[:, hs], T["eac"][:, c, h:h + 1], ysb, ysb[:, hs], ALU.mult, ALU.add, rd=[T["eac"]])
        for h in range(3):
            hs = slice(h * 64, (h + 1) * 64)
            P.stt("dve", S, S[:, hs], S, S[:, hs], T["eL"][:, c, h:h + 1], bS, bS[:, hs], ALU.mult, ALU.add, rd=[T["eL"]])
        P.dma("sync", yout.t.ap()[c], y_[:], reads=[y_], writes=[yout])
        yield

    def chain(gs):
        for g in gs:
            for _ in g:
                yield

    def roundrobin(gens):
        gens = list(gens)
        while gens:
            for g in list(gens):
                try:
                    next(g)
                except StopIteration:
                    gens.remove(g)

    pending = []
    for b in range(NBLK):
        j = b % 2
        for i in range(4):
            P.dma(("sync", "act")[i % 2], ut[i][j][0:nps[i], :], u.t.ap()[offs[i]:offs[i] + nps[i], b, :],
                  reads=[u], writes=[ut[i][j]])
            conv_block(P, "dve", ut[i][j], acc[i], cv[i][j], cw[i], cb[i], nps[i])
        gens = [pre(2 * b, 0, j, sets[0], bankset[0], hands[0][j]), pre(2 * b + 1, 1, j, sets[1], bankset[1], hands[1][j])]
        if pending:
            gens.append(chain(pending))
        roundrobin(gens)
        pending = [rec(2 * b, 0, j, hands[0][j]), rec(2 * b + 1, 1, j, hands[1][j])]
    roundrobin([chain(pending)])
    P.finish([yout, xout])
    return nc


def windows(seg, n=256):
    ch, T = seg.shape
    p = np.zeros((ch, T + 4), np.float32)
    p[:, 2:T + 2] = seg
    idx = (np.arange(T // n)[:, None] * n + np.arange(n + 4)[None, :])
    return p[:, idx]


def prep_k2s(pl, pc, conv_w, conv_b, a_log, dt_bias, core):
    s, d, g = core // 4, (core // 2) % 2, core % 2
    c0 = 256 + 384
    chans = np.concatenate([np.arange(g * 192, g * 192 + 192), 384 + g * 128 + np.arange(128), 384 + 256 + g * 128 + np.arange(128)])
    segs = []
    for arr in (pc[s], pl[s]):
        a = arr[:, c0 + chans]
        if d == 1:
            a = a[::-1]
        segs.append(windows(np.ascontiguousarray(a.T)))
    u = np.ascontiguousarray(np.concatenate(segs, axis=1))
    cw = conv_w[:, chans].T
    if d == 1:
        cw = cw[:, ::-1]
    dcol = 256 + 384 + 896 + d * 6 + g * 3
    dts = []
    for arr in (pc[s], pl[s]):
        a = arr[:, dcol:dcol + 3]
        if d == 1:
            a = a[::-1]
        dts.append(a)
    dtr = np.concatenate(dts, 0).reshape(NCH, 128, 3).transpose(1, 0, 2)
    m = {"u": u, "cw": np.ascontiguousarray(cw), "cb": np.ascontiguousarray(conv_b[chans][:, None]),
         "dtr": np.ascontiguousarray(dtr), "dtb": np.ascontiguousarray(dt_bias[d, g * 3:g * 3 + 3]),
         "alog": np.ascontiguousarray(a_log[d, g * 3:g * 3 + 3])}
    m.update(consts())
    return m


GRID_W = 64


def consts_g():
    c = consts()
    t = np.arange(128)
    c["POS"] = np.where(t[None, :] >= t[:, None], 30000.0, 0.0).astype(np.float32)
    return c


def build_k2g(nblk=NBLK):
    nc = bass.Bass("TRN2", target_bir_lowering=False)
    P = Prog(nc)
    u = P.dram("u", [576, NBLK, 260], F32, kind="ExternalInput")
    cwd = P.dram("cw", [576, 5], F32, kind="ExternalInput")
    ard = P.dram("araw", [128, NCH, 3], F32, kind="ExternalInput")
    brd = P.dram("braw", [128, NCH, 3], F32, kind="ExternalInput")
    dtb = P.dram("dtb", [3], F32, kind="ExternalInput")
    alog = P.dram("alog", [3], F32, kind="ExternalInput")
    oout = P.dram("o", [NCH, 128, 192], F32, kind="ExternalOutput")
    C = load_consts(P, ("idn", "U", "NEG", "ones", "POS"))
    idn = C["idn"]
    offs = (0, 128, 192, 320, 384, 512)
    nps = (128, 64, 128, 64, 128, 64)
    cw = []
    for i in range(6):
        a = P.sb([128, 5], name="cw%d" % i)
        P.dma(("sync", "act")[i % 2], a[0:nps[i], :], cwd.t.ap()[offs[i]:offs[i] + nps[i], :], reads=[cwd], writes=[a])
        cw.append(a)
    fl = lambda b: b[:].rearrange("p c h -> p (c h)")
    N3 = NCH * 3
    la = P.sb([128, NCH, 3], name="la")
    beta = P.sb([128, NCH, 3], name="beta")
    dtbb = P.sb([128, 3], name="dtbb")
    Ab = P.sb([128, 3], name="Ab")
    P.dma("sync", la[:], ard.t.ap(), reads=[ard], writes=[la])
    P.dma("pool", beta[:], brd.t.ap(), reads=[brd], writes=[beta])
    P.dma("act", dtbb[:], dtb.t.ap().partition_broadcast(128), reads=[dtb], writes=[dtbb])
    P.dma("pool", Ab[:], alog.t.ap().partition_broadcast(128), reads=[alog], writes=[Ab])
    P.act(Ab, Ab[:], Ab, Ab[:], AF.Exp)
    P.ts("dve", Ab, Ab[:], Ab, Ab[:], -1.0, None, ALU.mult)
    for h in range(3):
        P.ts("dve", la, la[:, :, h], la, la[:, :, h], dtbb[:, h:h + 1], None, ALU.add, rd=[dtbb])
    softplus_inplace(P, la, fl(la))
    for h in range(3):
        P.ts("dve", la, la[:, :, h], la, la[:, :, h], Ab[:, h:h + 1], None, ALU.mult, rd=[Ab])
    P.act(beta, fl(beta), beta, fl(beta), AF.Sigmoid)
    banks = [P.ps([128, 512], name="bank%d" % i) for i in range(8)]
    ac = P.sb([128, NCH, 3], name="ac")
    negac = P.sb([128, NCH, 3], name="negac")
    eac = P.sb([128, NCH, 3], name="eac")
    wj = P.sb([128, NCH, 3], name="wj")
    eL = P.sb([128, NCH, 3], name="eL")
    be = P.sb([128, NCH, 3], name="be")
    nbeta = P.sb([128, NCH, 3], name="nbeta")
    pC1, pC2 = banks[3], banks[4]
    P.mm(pC1, pC1[:, 0:N3], C["U"], C["U"][:], la, fl(la))
    P.mm(pC2, pC2[:, 0:N3], C["ones"], C["ones"][:], la, fl(la))
    P.cp("dve", ac, fl(ac), pC1, pC1[:, 0:N3])
    P.ts("dve", negac, fl(negac), ac, fl(ac), -1.0, None, ALU.mult)
    P.act(eac, fl(eac), ac, fl(ac), AF.Exp)
    P.act(eL, fl(eL), pC2, pC2[:, 0:N3], AF.Exp)
    P.tt("dve", wj, fl(wj), pC2, pC2[:, 0:N3], negac, fl(negac), ALU.add)
    P.act(wj, fl(wj), wj, fl(wj), AF.Exp)
    P.tt("dve", be, fl(be), beta, fl(beta), eac, fl(eac), ALU.mult)
    P.ts("dve", nbeta, fl(nbeta), beta, fl(beta), -1.0, None, ALU.mult)

    I3 = P.sb([128, 3, 128], name="I3")
    for h in range(3):
        P.cp("pool", I3, I3[:, h, :], idn, idn[:])
    S = P.sb([64, 192], name="S")
    P.op("dve", lambda e: e.memset(S[:], 0.0), writes=[S])

    ut = [[P.sb([128, 260], name="ut%d_%d" % (i, j)) for j in range(2)] for i in range(6)]
    acc = [P.sb([128, 256], name="acc%d" % i) for i in range(6)]
    cv = [[P.sb([128, 256], name="cv%d_%d" % (i, j)) for j in range(2)] for i in range(6)]

    def mkset(n):
        d = {}
        for nm, shp in (("qk", [128, 384]), ("vt", [128, 192]), ("sq", [128, 384]), ("rs", [128, 6]), ("qkn", [128, 384]),
                        ("kT", [64, 384]), ("kbe", [128, 192]), ("vb", [128, 192]), ("decT", [128, 3, 128]),
                        ("decS", [128, 3, 128]), ("Np", [128, 3, 128]), ("Mp", [128, 3, 128]), ("Tt", [128, 3, 128])):
            d[nm] = P.sb(shp, name="%s_%d" % (nm, n))
        d["rhs"] = [P.sb([128, 128], name="rhs%d_%d" % (i, n)) for i in range(3)]
        return d

    def mkhand(n, par):
        d = {}
        for nm, shp in (("usb", [128, 192]), ("wT", [64, 384]), ("qT", [64, 384]), ("attnT", [128, 3, 128]), ("kend", [128, 192])):
            d[nm] = P.sb(shp, name="%s_%d_%d" % (nm, n, par))
        return d

    sets = [mkset(0), mkset(1)]
    hands = [[mkhand(n, par) for par in range(2)] for n in range(2)]
    bankset = [banks[0:3], banks[3:6]]
    bR6, bR7 = banks[6], banks[7]
    vnew = P.sb([128, 192], name="vnew")
    o2 = P.sb([128, 192], name="o2")
    oo = [P.sb([128, 192], name="oo%d" % i) for i in range(2)]
    f3 = lambda b: b[:].rearrange("p h n -> p (h n)")
    H = lambda h: slice(h * 64, (h + 1) * 64)
    H2 = lambda h: slice(h * 128, (h + 1) * 128)

    def pre(c, cc, j, st, bk, hd):
        b0, b1, b2 = bk
        qk, vt, sq, rs, qkn, kT = st["qk"], st["vt"], st["sq"], st["rs"], st["qkn"], st["kT"]
        kbe, vb, decT, decS, Np, Mp, Tt, rhs_t = st["kbe"], st["vb"], st["decT"], st["decS"], st["Np"], st["Mp"], st["Tt"], st["rhs"]
        usb, wT, qT, attnT, kend = hd["usb"], hd["wT"], hd["qT"], hd["attnT"], hd["kend"]
        ck = slice(cc * 128, (cc + 1) * 128)
        pT1, pT2 = b0, b1
        for a in range(3):
            pt = pT1 if a < 2 else pT2
            base = (a % 2) * 192
            t01, t2 = cv[2 * a][j], cv[2 * a + 1][j]
            P.tr(pt, pt[:, base:base + 128], t01, t01[:, ck], idn, idn[:])
            P.tr(pt, pt[:, base + 128:base + 192], t2, t2[0:64, ck], idn, idn[0:64, 0:64])
        yield
        P.cp("act", qk, qk[:], pT1, pT1[:, 0:384])
        P.cp("dve", vt, vt[:], pT2, pT2[:, 0:192])
        yield
        P.tt("pool", sq, sq[:], qk, qk[:], qk, qk[:], ALU.mult)
        yield
        P.op("dve", lambda e: e.reduce_sum(out=rs[:], in_=sq[:].rearrange("p (a d) -> p a d", d=64), axis=AX.X),
             reads=[sq], writes=[rs])
        P.ts("dve", rs, rs[:], rs, rs[:], 1e-6, None, ALU.add)
        yield
        P.op("act", lambda e: e.sqrt(out=rs[:], in_=rs[:]), reads=[rs], writes=[rs])
        yield
        P.op("dve", lambda e: e.reciprocal(out=rs[:], in_=rs[:]), reads=[rs], writes=[rs])
        P.ts("dve", rs, rs[:, 0:3], rs, rs[:, 0:3], 0.125, None, ALU.mult)
        yield
        for a in range(6):
            P.ts(("dve", "pool")[a % 2], qkn, qkn[:, H(a)], qk, qk[:, H(a)], rs[:, a:a + 1], None, ALU.mult, rd=[rs])
        yield
        pKT, pQT = b2, b0
        for h in range(3):
            P.tr(pKT, pKT[0:64, H2(h)], qkn, qkn[:, 192 + h * 64:192 + (h + 1) * 64], idn, idn[:])
            P.tr(pQT, pQT[0:64, H2(h)], qkn, qkn[:, h * 64:(h + 1) * 64], idn, idn[:])
        yield
        P.cp("act", kT, kT[:], pKT, pKT[0:64, 0:384])
        P.cp("dve", qT, qT[:], pQT, pQT[0:64, 0:384])
        for h in range(3):
            kn_h = qkn[:, 192 + h * 64:192 + (h + 1) * 64]
            P.ts("pool", kbe, kbe[:, H(h)], qkn, kn_h, be[:, c, h:h + 1], None, ALU.mult, rd=[be])
            P.ts("pool", vb, vb[:, H(h)], vt, vt[:, H(h)], beta[:, c, h:h + 1], None, ALU.mult, rd=[beta])
            P.ts("pool", kend, kend[:, H(h)], qkn, kn_h, wj[:, c, h:h + 1], None, ALU.mult, rd=[wj])
        yield
        pA1, pA2 = b0, b1
        for h in range(3):
            r = rhs_t[h]
            P.ts(("dve", "pool")[h % 2], r, r[:], C["U"], C["U"][:], la[:, c, h:h + 1], None, ALU.mult, rd=[la])
        yield
        for h in range(3):
            r = rhs_t[h]
            P.mm(pA1, pA1[:, H2(h)], C["ones"], C["ones"][:], r, r[:], start=True, stop=False)
            P.mm(pA1, pA1[:, H2(h)], idn, idn[:], C["NEG"], C["NEG"][:], start=False, stop=True)
            P.mm(pA2, pA2[:, H2(h)], C["ones"], C["ones"][:], r, r[:], start=True, stop=False)
            P.mm(pA2, pA2[:, H2(h)], idn, idn[:], C["POS"], C["POS"][:], start=False, stop=True)
        yield
        for h in range(3):
            P.act(decT, decT[:, h, :], pA1, pA1[:, H2(h)], AF.Exp, bias=negac[:, c, h:h + 1], rd=[negac])
            P.act(decS, decS[:, h, :], pA2, pA2[:, H2(h)], AF.Exp, bias=ac[:, c, h:h + 1], scale=-1.0, rd=[ac])
        yield
        pKK, pQK = b2, b0
        for h in range(3):
            P.mm(pKK, pKK[:, H2(h)], kT, kT[:, H2(h)], kT, kT[:, H2(h)])
            P.mm(pQK, pQK[:, H2(h)], kT, kT[:, H2(h)], qT, qT[:, H2(h)])
        yield
        for h in range(3):
            P.stt("dve", Np, Np[:, h, :], pKK, pKK[:, H2(h)], nbeta[:, c, h:h + 1], decS, decS[:, h, :], ALU.mult, ALU.mult, rd=[nbeta])
            P.tt("dve", attnT, attnT[:, h, :], pQK, pQK[:, H2(h)], decT, decT[:, h, :], ALU.mult)
        yield
        pN, pM, pTt = b0, b1, b2
        for h in range(3):
            P.tr(pM, pM[:, H2(h)], Np, Np[:, h, :], idn, idn[:])
        yield
        P.cp("act", Mp, f3(Mp), pM, pM[:, 0:384])
        yield
        P.tt("pool", Tt, f3(Tt), Mp, f3(Mp), I3, f3(I3), ALU.add)
        for step in range(6):
            last = step == 5
            for h in range(3):
                P.mm(pN, pN[:, H2(h)], Mp, Mp[:, h, :], Np, Np[:, h, :])
                if not last:
                    P.mm(pM, pM[:, H2(h)], Np, Np[:, h, :], Mp, Mp[:, h, :])
            yield
            P.cp("act", Np, f3(Np), pN, pN[:, 0:384])
            if not last:
                P.cp("dve", Mp, f3(Mp), pM, pM[:, 0:384])
            yield
            for h in range(3):
                P.mm(pTt, pTt[:, H2(h)], Np, Np[:, h, :], Tt, Tt[:, h, :])
            yield
            P.tt("dve", Tt, f3(Tt), Tt, f3(Tt), pTt, pTt[:, 0:384], ALU.add)
        yield
        pU, pWT = b0, b1
        for h in range(3):
            P.mm(pU, pU[:, H(h)], Tt, Tt[:, h, :], vb, vb[:, H(h)])
            P.mm(pWT, pWT[0:64, H2(h)], kbe, kbe[:, H(h)], Tt, Tt[:, h, :])
        yield
        P.cp("act", usb, usb[:], pU, pU[:, 0:192])
        P.cp("dve", wT, wT[:], pWT, pWT[0:64, 0:384])
        yield

    def rec(c, hd):
        usb, wT, qT, attnT, kend = hd["usb"], hd["wT"], hd["qT"], hd["attnT"], hd["kend"]
        pWS, pO1, pO2, pSn = bR6, bR7, bR6, bR6
        for h in range(3):
            P.mm(pWS, pWS[:, H(h)], wT, wT[:, H2(h)], S, S[:, H(h)])
            P.mm(pO1, pO1[:, H(h)], qT, qT[:, H2(h)], S, S[:, H(h)])
        yield
        P.tt("dve", vnew, vnew[:], usb, usb[:], pWS, pWS[:, 0:192], ALU.subtract)
        yield
        for h in range(3):
            P.mm(pO2, pO2[:, H(h)], attnT, attnT[:, h, :], vnew, vnew[:, H(h)])
        yield
        P.cp("act", o2, o2[:], pO2, pO2[:, 0:192])
        yield
        for h in range(3):
            P.mm(pSn, pSn[0:64, H(h)], kend, kend[:, H(h)], vnew, vnew[:, H(h)])
        o_ = oo[c % 2]
        for h in range(3):
            P.stt("dve", o_, o_[:, H(h)], pO1, pO1[:, H(h)], eac[:, c, h:h + 1], o2, o2[:, H(h)], ALU.mult, ALU.add, rd=[eac])
        yield
        for h in range(3):
            P.stt("dve", S, S[:, H(h)], S, S[:, H(h)], eL[0:64, c, h:h + 1], pSn, pSn[0:64, H(h)], ALU.mult, ALU.add, rd=[eL])
        P.dma("sync", oout.t.ap()[c], o_[:], reads=[o_], writes=[oout])
        yield

    def chain(gs):
        for g in gs:
            for _ in g:
                yield

    def roundrobin(gens):
        gens = list(gens)
        while gens:
            for g in list(gens):
                try:
                    next(g)
                except StopIteration:
                    gens.remove(g)

    pending = []
    for b in range(nblk):
        j = b % 2
        for i in range(6):
            P.dma(("sync", "act")[i % 2], ut[i][j][0:nps[i], :], u.t.ap()[offs[i]:offs[i] + nps[i], b, :],
                  reads=[u], writes=[ut[i][j]])
            conv_block(P, "dve", ut[i][j], acc[i], cv[i][j], cw[i], None, nps[i], bias=False)
        gens = [pre(2 * b, 0, j, sets[0], bankset[0], hands[0][j]), pre(2 * b + 1, 1, j, sets[1], bankset[1], hands[1][j])]
        if pending:
            gens.append(chain(pending))
        roundrobin(gens)
        pending = [rec(2 * b, hands[0][j]), rec(2 * b + 1, hands[1][j])]
    roundrobin([chain(pending)])
    P.finish([oout])
    return nc


def to_cm(a):
    T, Cc = a.shape
    return a.reshape(T // GRID_W, GRID_W, Cc).transpose(1, 0, 2).reshape(T, Cc)


def from_cm(a):
    T, Cc = a.shape
    return a.reshape(GRID_W, T // GRID_W, Cc).transpose(1, 0, 2).reshape(T, Cc)


def prep_k2g(pl, pc, conv_w, a_log, dt_bias, core):
    s, d, g = core // 4, (core // 2) % 2, core % 2
    q0 = 1548
    chans = np.concatenate([a * 384 + g * 192 + np.arange(192) for a in range(3)])
    acol = 3084 + d * 6 + g * 3
    bcol = 3096 + d * 6 + g * 3
    segs, ars, brs = [], [], []
    for arr, cm in ((pc[s], False), (pl[s], True)):
        a = arr[:, q0 + chans]
        ar = arr[:, acol:acol + 3]
        br = arr[:, bcol:bcol + 3]
        if cm:
            a, ar, br = to_cm(a), to_cm(ar), to_cm(br)
        if d == 1:
            a, ar, br = a[::-1], ar[::-1], br[::-1]
        segs.append(windows(np.ascontiguousarray(a.T)))
        ars.append(ar)
        brs.append(br)
    cw = conv_w[:, chans].T
    if d == 1:
        cw = cw[:, ::-1]
    tm = lambda lst: np.ascontiguousarray(np.concatenate(lst, 0).reshape(NCH, 128, 3).transpose(1, 0, 2))
    m = {"u": np.ascontiguousarray(np.concatenate(segs, axis=1)), "cw": np.ascontiguousarray(cw),
         "araw": tm(ars), "braw": tm(brs), "dtb": np.ascontiguousarray(dt_bias[d, g * 3:g * 3 + 3]),
         "alog": np.ascontiguousarray(a_log[d, g * 3:g * 3 + 3])}
    m.update(consts_g())
    return m


PKW = 1024 + 256 + 7 * 384
POOL_WINDOWS = (2, 4, 8, 16)


def build_k3(NT, n_lat=None):
    if n_lat is None:
        n_lat = NT - 1
    nc = bass.Bass("TRN2", target_bir_lowering=False)
    P = Prog(nc)
    pk = P.dram("pk", [NT, 128, PKW], F32, kind="ExternalInput")
    halo = P.dram("halo", [NT, 16, 256], F32, kind="ExternalInput")
    band = P.dram("band", [NT, 144, 512], F32, kind="ExternalInput")
    wout = P.dram("wout", [D, D], F32, kind="ExternalInput")
    pwd = P.dram("pw", [64, 4, 128], F32, kind="ExternalInput")
    psd = P.dram("pscale", [128, 2], F32, kind="ExternalInput")
    rwd = P.dram("rw", [128, 8, 16], F32, kind="ExternalInput")
    idn = P.dram("idn", [128, 128], F32, kind="ExternalInput")
    v384 = {n: P.dram(n, [384], F32, kind="ExternalInput") for n in ("dvec", "sng", "gng")}
    v1k = {n: P.dram(n, [D], F32, kind="ExternalInput") for n in ("g1_l", "g1_c", "n2g", "sh_l", "sc_l", "sh_c", "sc_c")}
    x2o = P.dram("x2", [NT, 128, D], F32, kind="ExternalOutput")
    hTo = P.dram("h2T", [NT, 128, 8, 128], F32, kind="ExternalOutput")
    affo = P.dram("aff", [NT, 128, 16], F32, kind="ExternalOutput")

    ident = P.sb([128, 128], name="ident")
    P.dma("sync", ident[:], idn.t.ap(), reads=[idn], writes=[ident])
    pw = P.sb([64, 4, 128], name="pw")
    P.dma("act", pw[:], pwd.t.ap(), reads=[pwd], writes=[pw])
    psc = P.sb([128, 2], name="psc")
    P.dma("sync", psc[:], psd.t.ap(), reads=[psd], writes=[psc])
    rw = P.sb([128, 8, 16], name="rw")
    P.dma("act", rw[:], rwd.t.ap(), reads=[rwd], writes=[rw])
    b384 = {n: load_bcast(P, ("sync", "act")[i % 2], v384[n], 384, "b_" + n) for i, n in enumerate(v384)}
    b1k = {n: load_bcast(P, ("sync", "act")[i % 2], v1k[n], D, "b_" + n) for i, n in enumerate(v1k)}
    for n in ("sc_l", "sc_c"):
        A = b1k[n]
        P.stt("dve", A, A[:], A, A[:], 1.0, b1k["n2g"], b1k["n2g"][:], ALU.add, ALU.mult)
    W = P.sb([128, 8, D], BF16, name="W")
    stage = [P.sb([128, D], name="stg%d" % i) for i in range(2)]
    load_weight_bf16(P, lambda k: wout.t.ap()[k * 128:(k + 1) * 128, :], W, 8, D, stage, wout)

    pks = [P.sb([128, PKW], name="pk%d" % i) for i in range(2)]
    hls = [P.sb([16, 256], name="hl%d" % i) for i in range(2)]
    bds = [P.sb([128, 512], name="bd%d" % i) for i in range(2)]
    bd2s = [P.sb([16, 512], name="bdh%d" % i) for i in range(2)]
    dT = P.sb([64, 4, 128], name="dT")
    mixT = [P.sb([128, 8, 128], BF16, name="mixT%d" % i) for i in range(2)]
    t1 = P.sb([128, 384], name="t1")
    ys = P.sb([128, 384], name="ys")
    sz = P.sb([128, 384], name="sz")
    scr3 = P.sb([128, 384], name="scr3")
    ss1 = P.sb([128, 1], name="ss1")
    yo = P.sb([128, 768], name="yo")
    og = P.sb([128, 384], name="og")
    sq = P.sb([128, 384], name="sq")
    rs6 = P.sb([128, 6], name="rs6")
    sg = P.sb([128, 384], name="sg")
    mx = P.sb([128, 512], name="mx")
    x2s = [P.sb([128, D], name="x2_%d" % i) for i in range(2)]
    h2s = [P.sb([128, D], name="h2_%d" % i) for i in range(2)]
    scr = P.sb([128, D], name="scr")
    ss2 = P.sb([128, 1], name="ss2")
    h2Ts = [P.sb([128, 8, 128], name="h2T%d" % i) for i in range(2)]
    lg = P.sb([128, 16], name="lg")
    ex = P.sb([128, 16], name="ex")
    m1 = P.sb([128, 1], name="m1")
    s1 = P.sb([128, 1], name="s1")
    afs = [P.sb([128, 16], name="af%d" % i) for i in range(2)]
    pD, pP, pr = P.ps([1], name="pD"), P.ps([1], name="pP"), P.ps([1], name="pr")
    pts = [P.ps([1], name="pt%d" % i) for i in range(2)]
    pms = [P.ps([1], name="pm%d" % i) for i in range(2)]

    for t in range(NT):
        lat = t < n_lat
        pk_ = pks[t % 2]
        hl, bd, bd2, mT = hls[t % 2], bds[t % 2], bd2s[t % 2], mixT[t % 2]
        P.dma("sync", pk_[:], pk.t.ap()[t], reads=[pk], writes=[pk_])
        P.dma("act", hl[:], halo.t.ap()[t], reads=[halo], writes=[hl])
        P.dma("act", bd[:], band.t.ap()[t, 0:128, :], reads=[band], writes=[bd])
        P.dma("act", bd2[:], band.t.ap()[t, 128:144, :], reads=[band], writes=[bd2])
        xo, uo = 0, 1024
        yf, yb, xs, z, of, ob, gt = [slice(1280 + i * 384, 1280 + (i + 1) * 384) for i in range(7)]
        for g in range(4):
            P.mm(pD, pD[0:64, g * 128:(g + 1) * 128], pk_, pk_[:, uo + g * 64:uo + (g + 1) * 64], bd, bd[:, g * 128:(g + 1) * 128],
                 start=True, stop=False)
            P.mm(pD, pD[0:64, g * 128:(g + 1) * 128], hl, hl[:, g * 64:(g + 1) * 64], bd2, bd2[:, g * 128:(g + 1) * 128],
                 start=False, stop=True)
        P.cp("act", dT, dT[:].rearrange("p g t -> p (g t)"), pD, pD[0:64, :])
        for cch in range(2):
            for gg in range(2):
                g = cch * 2 + gg
                P.mm(pP, pP[:, cch * 128:(cch + 1) * 128], pw, pw[:, g, :], dT, dT[:, g, :], start=(gg == 0), stop=(gg == 1))
        for cch in range(2):
            P.ts("dve", mT, mT[:, cch, :], pP, pP[:, cch * 128:(cch + 1) * 128], psc[:, cch:cch + 1], None, ALU.mult, rd=[psc])
        P.tt("pool", ys, ys[:], pk_, pk_[:, yf], pk_, pk_[:, yb], ALU.add)
        P.tt("pool", t1, t1[:], pk_, pk_[:, xs], b384["dvec"], b384["dvec"][:], ALU.mult)
        P.tt("pool", ys, ys[:], ys, ys[:], t1, t1[:], ALU.add)
        P.act(sz, sz[:], pk_, pk_[:, z], AF.Silu)
        P.tt("dve", ys, ys[:], ys, ys[:], sz, sz[:], ALU.mult)
        P.act(scr3, scr3[:], ys, ys[:], AF.Square, scale=float(384 ** -0.5), accum=ss1[:], wr=[ss1])
        P.ts("dve", ss1, ss1[:], ss1, ss1[:], 1e-6, None, ALU.add)
        P.op("act", lambda e: e.sqrt(out=ss1[:], in_=ss1[:]), reads=[ss1], writes=[ss1])
        P.op("dve", lambda e: e.reciprocal(out=ss1[:], in_=ss1[:]), reads=[ss1], writes=[ss1])
        P.stt("dve", yo, yo[:, 0:384], ys, ys[:], ss1[:, 0:1], b384["sng"], b384["sng"][:], ALU.mult, ALU.mult, rd=[ss1])
        P.tt("pool", og, og[:], pk_, pk_[:, of], pk_, pk_[:, ob], ALU.add)
        P.tt("pool", sq, sq[:], og, og[:], og, og[:], ALU.mult)
        P.op("dve", lambda e: e.reduce_sum(out=rs6[:], in_=sq[:].rearrange("p (a d) -> p a d", d=64), axis=AX.X),
             reads=[sq], writes=[rs6])
        P.ts("dve", rs6, rs6[:], rs6, rs6[:], 1.0 / 64.0, 1e-6, ALU.mult, ALU.add)
        P.op("act", lambda e: e.sqrt(out=rs6[:], in_=rs6[:]), reads=[rs6], writes=[rs6])
        P.op("dve", lambda e: e.reciprocal(out=rs6[:], in_=rs6[:]), reads=[rs6], writes=[rs6])
        for a in range(6):
            P.ts(("dve", "pool")[a % 2], og, og[:, a * 64:(a + 1) * 64], og, og[:, a * 64:(a + 1) * 64], rs6[:, a:a + 1], None, ALU.mult, rd=[rs6])
        P.act(sg, sg[:], pk_, pk_[:, gt], AF.Silu)
        P.tt("pool", og, og[:], og, og[:], b384["gng"], b384["gng"][:], ALU.mult)
        P.tt("dve", yo, yo[:, 384:768], og, og[:], sg, sg[:], ALU.mult)
        for half in range(2):
            pt = pts[half]
            for q in range(3):
                k = half * 3 + q
                P.tr(pt, pt[:, q * 128:(q + 1) * 128], yo, yo[:, k * 128:(k + 1) * 128], ident, ident[:])
            P.cp(("act", "dve")[half], mT, mT[:, 2 + half * 3:5 + half * 3, :], pt, pt[:, 0:384].rearrange("p (k t) -> p k t", k=3))
        x2 = x2s[t % 2]
        g1 = b1k["g1_l"] if lat else b1k["g1_c"]
        for cb in range(2):
            pm = pms[cb]
            cs = slice(cb * 512, (cb + 1) * 512)
            for k in range(8):
                P.mm(pm, pm[:, :], mT, mT[:, k, :], W, W[:, k, cs], start=(k == 0), stop=(k == 7))
            P.tt("dve", mx, mx[:], pm, pm[:, :], g1, g1[:, cs], ALU.mult)
            P.tt("pool", x2, x2[:, cs], mx, mx[:], pk_, pk_[:, cb * 512:(cb + 1) * 512], ALU.add)
        P.dma("sync", x2o.t.ap()[t], x2[:], reads=[x2], writes=[x2o])
        h2, h2T = h2s[t % 2], h2Ts[t % 2]
        A2, B2 = (b1k["sc_l"], b1k["sh_l"]) if lat else (b1k["sc_c"], b1k["sh_c"])
        norm_mod_tile(P, x2, h2, A2, B2, scr, ss2)
        transpose_tile(P, h2, h2T, ident, pts)
        P.dma("sync", hTo.t.ap()[t], h2T[:], reads=[h2T], writes=[hTo])
        for k in range(8):
            P.mm(pr, pr[:, 0:16], h2T, h2T[:, k, :], rw, rw[:, k, :], start=(k == 0), stop=(k == 7))
        P.cp("act", lg, lg[:], pr, pr[:, 0:16])
        P.op("dve", lambda e: e.reduce_max(out=m1[:], in_=lg[:], axis=AX.X), reads=[lg], writes=[m1])
        P.ts("dve", m1, m1[:], m1, m1[:], -1.0, None, ALU.mult)
        P.act(ex, ex[:], lg, lg[:], AF.Exp, bias=m1[:, 0:1], accum=s1[:], rd=[m1], wr=[s1])
        P.op("dve", lambda e: e.reciprocal(out=s1[:], in_=s1[:]), reads=[s1], writes=[s1])
        af = afs[t % 2]
        P.ts("dve", af, af[:], ex, ex[:], s1[:, 0:1], None, ALU.mult, rd=[s1])
        P.dma("sync", affo.t.ap()[t], af[:], reads=[af], writes=[affo])
    P.finish([x2o, hTo, affo])
    return nc


def pool_band(pos, T):
    src = np.concatenate([pos, pos[0] - 8 + np.arange(8), pos[-1] + 1 + np.arange(8)])
    out = np.zeros((144, 4, 128), np.float32)
    for g, w in enumerate(POOL_WINDOWS):
        lo = np.clip(pos - w // 2, 0, T)
        hi = np.clip(pos + w // 2, 0, T)
        cnt = np.maximum(hi - lo, 1).astype(np.float32)
        m = (src[:, None] >= lo[None, :]) & (src[:, None] < hi[None, :])
        out[:, g, :] = m / cnt[None, :]
        out[np.arange(128), g, np.arange(128)] -= 1.0
    return out.reshape(144, 512)


NE = 16
FF = 512


def bisect_threshold(P, affs, J, kcap, ones, pcnt, name):
    lo = P.sb([128, NE], name=name + "_lo")
    hi = P.sb([128, NE], name=name + "_hi")
    mid = P.sb([128, NE], name=name + "_mid")
    cnt = P.sb([128, NE], name=name + "_cnt")
    pred = P.sb([128, NE], name=name + "_pred")
    tmp = P.sb([128, NE], name=name + "_tmp")
    cmp_ = P.sb([128, NE, J], name=name + "_cmp")
    P.op("dve", lambda e: e.memset(lo[:], 0.0), writes=[lo])
    P.op("dve", lambda e: e.memset(hi[:], 1.0), writes=[hi])
    for it in range(34):
        P.tt("dve", mid, mid[:], lo, lo[:], hi, hi[:], ALU.add)
        P.ts("dve", mid, mid[:], mid, mid[:], 0.5, None, ALU.mult)
        for e_ in range(NE):
            P.ts(("dve", "pool")[e_ % 2], cmp_, cmp_[:, e_, :], affs, affs[:, e_, :], mid[:, e_:e_ + 1], None, ALU.is_ge, rd=[mid])
        P.op("dve", lambda e: e.reduce_sum(out=cnt[:], in_=cmp_[:], axis=AX.X), reads=[cmp_], writes=[cnt])
        P.mm(pcnt, pcnt[:, 0:NE], ones, ones[:], cnt, cnt[:])
        P.ts("dve", pred, pred[:], pcnt, pcnt[:, 0:NE], float(kcap) - 0.5, None, ALU.is_ge)
        P.tt("dve", tmp, tmp[:], mid, mid[:], lo, lo[:], ALU.subtract)
        P.tt("dve", tmp, tmp[:], tmp, tmp[:], pred, pred[:], ALU.mult)
        P.tt("dve", lo, lo[:], lo, lo[:], tmp, tmp[:], ALU.add)
        P.tt("dve", tmp, tmp[:], hi, hi[:], mid, mid[:], ALU.subtract)
        P.tt("dve", tmp, tmp[:], tmp, tmp[:], pred, pred[:], ALU.mult)
        P.tt("dve", hi, hi[:], mid, mid[:], tmp, tmp[:], ALU.add)
    return lo


def build_k4(NT, n_lat, kcap_lat, kcap_ctx, J_lat, J_ctx, final_norm, passes):
    nc = bass.Bass("TRN2", target_bir_lowering=False)
    P = Prog(nc)
    x2d = P.dram("x2", [NT, 128, D], F32, kind="ExternalInput")
    hTd = P.dram("h2T", [NT, 128, 8, 128], F32, kind="ExternalInput")
    afd = P.dram("aff", [NT, 128, NE], F32, kind="ExternalInput")
    asl = P.dram("affs_l", [128, NE, J_lat], F32, kind="ExternalInput")
    asc = P.dram("affs_c", [128, NE, J_ctx], F32, kind="ExternalInput")
    wgd = P.dram("wg", [NE, D, FF], F32, kind="ExternalInput")
    wud = P.dram("wu", [NE, D, FF], F32, kind="ExternalInput")
    wdd = P.dram("wd", [NE, FF, D], F32, kind="ExternalInput")
    onesd = P.dram("ones", [128, 128], F32, kind="ExternalInput")
    v1k = {n: P.dram(n, [D], F32, kind="ExternalInput") for n in ("g2_l", "g2_c", "fng")}
    outd = P.dram("out", [NT, 128, D], F32, kind="ExternalOutput")

    ones = P.sb([128, 128], name="ones")
    P.dma("sync", ones[:], onesd.t.ap(), reads=[onesd], writes=[ones])
    b1k = {n: load_bcast(P, ("sync", "act")[i % 2], v1k[n], D, "b_" + n) for i, n in enumerate(v1k)}
    pcnt = P.ps([1], name="pcnt")
    affs_l = P.sb([128, NE, J_lat], name="affs_l")
    P.dma("sync", affs_l[:], asl.t.ap(), reads=[asl], writes=[affs_l])
    thr_l = bisect_threshold(P, affs_l, J_lat, kcap_lat, ones, pcnt, "bl")
    thr_c = None
    if n_lat < NT:
        affs_c = P.sb([128, NE, J_ctx], name="affs_c")
        P.dma("act", affs_c[:], asc.t.ap(), reads=[asc], writes=[affs_c])
        thr_c = bisect_threshold(P, affs_c, J_ctx, kcap_ctx, ones, pcnt, "bc")
    afo = P.sb([128, NT, NE], name="afo")
    gw = P.sb([128, NT, NE], name="gw")
    P.dma("sync", afo[:], afd.t.ap().rearrange("t p e -> p t e"), reads=[afd], writes=[afo])
    for t in range(NT):
        thr = thr_l if t < n_lat else thr_c
        P.tt("dve", gw, gw[:, t, :], afo, afo[:, t, :], thr, thr[:], ALU.is_ge)
        P.tt("dve", gw, gw[:, t, :], gw, gw[:, t, :], afo, afo[:, t, :], ALU.mult)

    maxt = max(sum(t1 - t0 for (t0, t1) in ps_) for ps_ in passes)
    hT = P.sb([128, 8, maxt * 128], BF16, name="hT")
    hst = [P.sb([128, 8, 128], name="hst%d" % i) for i in range(2)]
    acc = P.sb([128, maxt, D], name="acc")
    Wg = [P.sb([128, 8, FF], BF16, name="Wg%d" % i) for i in range(2)]
    Wu = [P.sb([128, 8, FF], BF16, name="Wu%d" % i) for i in range(2)]
    Wd = [P.sb([128, 4, D], BF16, name="Wd%d" % i) for i in range(2)]
    stg = [P.sb([128, 2048], name="stg%d" % i) for i in range(2)]
    sil = [P.sb([128, 512], name="sil%d" % i) for i in range(2)]
    hidT = [P.sb([128, 4, 512], BF16, name="hidT%d" % i) for i in range(2)]
    xt = [P.sb([128, D], name="xt%d" % i) for i in range(2)]
    scr = P.sb([128, D], name="scr")
    ssf = P.sb([128, 1], name="ssf")
    pg = [P.ps([1], name="pg%d" % i) for i in range(2)]
    pu = [P.ps([1], name="pu%d" % i) for i in range(2)]
    pd = [P.ps([1], name="pd%d" % i) for i in range(2)]
    sti = 0
    wi = 0
    gi = 0
    for ps_ in passes:
        tiles = [t for (t0, t1) in ps_ for t in range(t0, t1)]
        loc = {t: i for i, t in enumerate(tiles)}
        for i, t in enumerate(tiles):
            h_ = hst[i % 2]
            P.dma(("sync", "act")[i % 2], h_[:], hTd.t.ap()[t], reads=[hTd], writes=[h_])
            P.cp(("pool", "act")[i % 2], hT, hT[:, :, i * 128:(i + 1) * 128], h_, h_[:])
        P.op("pool", lambda e: e.memset(acc[:], 0.0), writes=[acc])
        def load_w(ex):
            nonlocal sti
            wg_, wu_, wd_ = Wg[ex % 2], Wu[ex % 2], Wd[ex % 2]
            for (wt, src, nk) in ((wg_, wgd, 8), (wu_, wud, 8), (wd_, wdd, 4)):
                for hf in range(2):
                    s_ = stg[sti % 2]
                    sti += 1
                    k0, k1 = hf * nk // 2, (hf + 1) * nk // 2
                    sv = s_[:].rearrange("p (k f) -> p k f", k=nk // 2)
                    P.dma(("sync", "act")[sti % 2], sv, src.t.ap()[ex].rearrange("(k p) f -> p k f", p=128)[:, k0:k1, :],
                          reads=[src], writes=[s_])
                    P.cp(("pool", "act", "dve")[sti % 3], wt, wt[:, k0:k1, :], s_, sv)

        def gateup(ex, t0, t1, hd):
            wg_, wu_ = Wg[ex % 2], Wu[ex % 2]
            n = (t1 - t0) * 128
            c0 = loc[t0] * 128
            for fc in range(4):
                pg_, pu_, sl_ = pg[fc % 2], pu[fc % 2], sil[fc % 2]
                for k in range(8):
                    P.mm(pg_, pg_[:, 0:n], wg_, wg_[:, k, fc * 128:(fc + 1) * 128], hT, hT[:, k, c0:c0 + n], start=(k == 0), stop=(k == 7))
                for k in range(8):
                    P.mm(pu_, pu_[:, 0:n], wu_, wu_[:, k, fc * 128:(fc + 1) * 128], hT, hT[:, k, c0:c0 + n], start=(k == 0), stop=(k == 7))
                P.act(sl_, sl_[:, 0:n], pg_, pg_[:, 0:n], AF.Silu)
                P.tt("dve", hd, hd[:, fc, 0:n], pu_, pu_[:, 0:n], sl_, sl_[:, 0:n], ALU.mult)
                yield

        def down(ex, t0, t1, hd):
            wd_ = Wd[ex % 2]
            for t in range(t0, t1):
                i = loc[t]
                tl = slice((t - t0) * 128, (t - t0 + 1) * 128)
                for half in range(2):
                    pd_ = pd[half]
                    cs = slice(half * 512, (half + 1) * 512)
                    for fc in range(4):
                        P.mm(pd_, pd_[:, :], hd, hd[:, fc, tl], wd_, wd_[:, fc, cs], start=(fc == 0), stop=(fc == 3))
                    P.stt("dve", acc, acc[:, i, cs], pd_, pd_[:, :], gw[:, t, ex:ex + 1], acc, acc[:, i, cs], ALU.mult, ALU.add, rd=[gw])
                yield

        def roundrobin(gens):
            gens = list(gens)
            while gens:
                for g in list(gens):
                    try:
                        next(g)
                    except StopIteration:
                        gens.remove(g)

        items = [(ex, t0, t1, gidx) for ex in range(NE) for gidx, (t0, t1) in enumerate(ps_)]
        pf = min(1, len(ps_) - 1)
        prev = None
        load_w(0)
        for (ex, t0, t1, gidx) in items:
            if gidx == pf and ex + 1 < NE:
                load_w(ex + 1)
            hd = hidT[gi % 2]
            gi += 1
            gens = [gateup(ex, t0, t1, hd)]
            if prev is not None:
                gens.append(down(*prev))
            roundrobin(gens)
            prev = (ex, t0, t1, hd)
        roundrobin([down(*prev)])
        for t in tiles:
            i = loc[t]
            x_ = xt[t % 2]
            g2 = b1k["g2_l"] if t < n_lat else b1k["g2_c"]
            P.dma("act", x_[:], x2d.t.ap()[t], reads=[x2d], writes=[x_])
            P.tt("pool", acc, acc[:, i, :], acc, acc[:, i, :], g2, g2[:], ALU.mult)
            P.tt("pool", x_, x_[:], x_, x_[:], acc, acc[:, i, :], ALU.add)
            if final_norm:
                P.act(scr, scr[:], x_, x_[:], AF.Square, scale=1.0 / 32.0, accum=ssf[:], wr=[ssf])
                P.ts("dve", ssf, ssf[:], ssf, ssf[:], 1e-6, None, ALU.add)
                P.op("act", lambda e: e.sqrt(out=ssf[:], in_=ssf[:]), reads=[ssf], writes=[ssf])
                P.op("dve", lambda e: e.reciprocal(out=ssf[:], in_=ssf[:]), reads=[ssf], writes=[ssf])
                P.stt("dve", x_, x_[:], x_, x_[:], ssf[:, 0:1], b1k["fng"], b1k["fng"][:], ALU.mult, ALU.mult, rd=[ssf])
            P.dma("sync", outd.t.ap()[t], x_[:], reads=[x_], writes=[outd])
    P.finish([outd])
    return nc


B, T, CTX = 2, 16384, 256
NLT = 32


def tiles_of(lat, ctx, core, with_ctx=True):
    s, q = core // 4, core % 4
    Cc = lat.shape[-1]
    lt = lat[s, q * 4096:(q + 1) * 4096].reshape(NLT, 128, Cc)
    if not with_ctx:
        return np.ascontiguousarray(lt)
    ct = np.zeros((1, 128, Cc), np.float32)
    n = min(128, CTX - q * 64)
    ct[0, :n] = ctx[s, q * 64:q * 64 + n]
    return np.concatenate([lt, ct], 0)


def untile(res, key, Cc, with_ctx=True):
    lat = np.zeros((B, T, Cc), np.float32)
    ctx = np.zeros((B, CTX, Cc), np.float32)
    for core in range(NCORE):
        s, q = core // 4, core % 4
        r = res[core][key]
        lat[s, q * 4096:(q + 1) * 4096] = r[:NLT].reshape(4096, Cc)
        if with_ctx:
            ctx[s, q * 64:(q + 1) * 64] = r[NLT, :64]
    return lat, ctx


def unseq(y, d, cm):
    y = y.reshape(NCH * 128, -1)
    c_, l_ = y[:CTX], y[CTX:]
    if d == 1:
        c_, l_ = c_[::-1], l_[::-1]
    if cm:
        l_ = from_cm(l_)
    return c_, l_


_band_cache = {}


def band_for(pos0, Tseq):
    key = (pos0 if (pos0 == 0 or pos0 + 128 + 8 > Tseq) else -1, Tseq)
    if key not in _band_cache:
        _band_cache[key] = pool_band(pos0 + np.arange(128), Tseq)
    return _band_cache[key]


def halo_for(u, pos0):
    Tseq = u.shape[0]
    h = np.zeros((16, 256), np.float32)
    for i in range(8):
        a = pos0 - 8 + i
        if 0 <= a < Tseq:
            h[i] = u[a]
        b_ = pos0 + 128 + i
        if 0 <= b_ < Tseq:
            h[8 + i] = u[b_]
    return h


def stage_proj(l, x, ctx, mods, p):
    f32 = _f32
    m = mods[l]
    seg = lambda r, i: f32(m[r, i * 1024:(i + 1) * 1024])
    idn = np.eye(128, dtype=np.float32)
    in_maps = []
    for core in range(NCORE):
        s = core // 4
        in_maps.append({"xt": tiles_of(x, ctx, core), "w": f32(p["w_in"][l]), "g": f32(p["norm1_g"][l]),
                        "sh_l": seg(s, 0), "sc_l": seg(s, 1), "sh_c": seg(2, 0), "sc_c": seg(2, 1), "idn": idn})
    res = run(build_k1(NLT + 1), in_maps)
    return untile(res, "out", 3108)


def stage_scans(l, pl, pc, p):
    f32 = _f32
    res = run(build_k2s(), [prep_k2s(pl, pc, f32(p["ssd_conv_w"][l]), f32(p["ssd_conv_b"][l]), f32(p["ssd_a_log"][l]),
                                     f32(p["ssd_dt_bias"][l]), core) for core in range(NCORE)])
    ys_l = np.zeros((B, 2, T, 384), np.float32)
    ys_c = np.zeros((B, 2, CTX, 384), np.float32)
    xs_l = np.zeros((B, T, 384), np.float32)
    xs_c = np.zeros((B, CTX, 384), np.float32)
    for core in range(NCORE):
        s, d, g = core // 4, (core // 2) % 2, core % 2
        c_, l_ = unseq(res[core]["y"], d, False)
        ys_l[s, d, :, g * 192:(g + 1) * 192] = l_
        ys_c[s, d, :, g * 192:(g + 1) * 192] = c_
        if d == 0:
            c_, l_ = unseq(res[core]["xs"], 0, False)
            xs_l[s, :, g * 192:(g + 1) * 192] = l_
            xs_c[s, :, g * 192:(g + 1) * 192] = c_
    del res
    res = run(build_k2g(), [prep_k2g(pl, pc, f32(p["gdn_conv_w"][l]), f32(p["gdn_a_log"][l]), f32(p["gdn_dt_bias"][l]), core)
                            for core in range(NCORE)])
    os_l = np.zeros((B, 2, T, 384), np.float32)
    os_c = np.zeros((B, 2, CTX, 384), np.float32)
    for core in range(NCORE):
        s, d, g = core // 4, (core // 2) % 2, core % 2
        c_, l_ = unseq(res[core]["o"], d, True)
        os_l[s, d, :, g * 192:(g + 1) * 192] = l_
        os_c[s, d, :, g * 192:(g + 1) * 192] = c_
    return ys_l, ys_c, xs_l, xs_c, os_l, os_c


def stage_post(l, x, ctx, pl, pc, scans, mods, p):
    f32 = _f32
    ys_l, ys_c, xs_l, xs_c, os_l, os_c = scans
    m = mods[l]
    seg = lambda r, i: f32(m[r, i * 1024:(i + 1) * 1024])
    idn = np.eye(128, dtype=np.float32)
    pwp = np.zeros((64, 4, 128), np.float32)
    for g in range(4):
        pwp[:, g, (g % 2) * 64:(g % 2) * 64 + 64] = p["pool_w"][l][g]
    common = {"wout": f32(p["w_out"][l]), "pw": pwp, "pscale": f32(np.asarray(p["pool_scale"][l]).reshape(2, 128).T),
              "rw": f32(np.asarray(p["router_w"][l]).reshape(8, 128, 16).transpose(1, 0, 2)), "idn": idn,
              "dvec": f32(np.repeat(np.asarray(p["ssd_d"][l]), 64)), "sng": f32(p["ssd_norm_g"][l]),
              "gng": f32(np.tile(np.asarray(p["gdn_norm_g"][l]), 6)), "n2g": f32(p["norm2_g"][l])}
    in_maps = []
    for core in range(NCORE):
        s, q = core // 4, core % 4
        ls = slice(q * 4096, (q + 1) * 4096)
        lat_p = np.concatenate([x[s, ls], pl[s, ls, 0:256], ys_l[s, 0, ls], ys_l[s, 1, ls], xs_l[s, ls], pl[s, ls, 256:640],
                                os_l[s, 0, ls], os_l[s, 1, ls], pl[s, ls, 2700:3084]], axis=-1).reshape(NLT, 128, PKW)
        n = min(128, CTX - q * 64)
        cs_ = slice(q * 64, q * 64 + n)
        ctx_p = np.zeros((1, 128, PKW), np.float32)
        ctx_p[0, :n] = np.concatenate([ctx[s, cs_], pc[s, cs_, 0:256], ys_c[s, 0, cs_], ys_c[s, 1, cs_], xs_c[s, cs_],
                                       pc[s, cs_, 256:640], os_c[s, 0, cs_], os_c[s, 1, cs_], pc[s, cs_, 2700:3084]], axis=-1)
        halo = np.stack([halo_for(pl[s, :, 0:256], q * 4096 + i * 128) for i in range(NLT)] + [halo_for(pc[s, :, 0:256], q * 64)])
        band = np.stack([band_for(q * 4096 + i * 128, T) for i in range(NLT)] + [band_for(q * 64, CTX)])
        d_ = {"pk": np.ascontiguousarray(np.concatenate([lat_p, ctx_p], 0)), "halo": f32(halo), "band": f32(band),
              "g1_l": seg(s, 2), "g1_c": seg(2, 2), "sh_l": seg(s, 3), "sc_l": seg(s, 4), "sh_c": seg(2, 3), "sc_c": seg(2, 4)}
        d_.update(common)
        in_maps.append(d_)
    return run(build_k3(NLT + 1), in_maps)


def stage_moe(l, res3, mods, p, last):
    f32 = _f32
    m = mods[l]
    seg = lambda r, i: f32(m[r, i * 1024:(i + 1) * 1024])
    ones = np.ones((128, 128), np.float32)
    aff_l, aff_c = untile(res3, "aff", 16)
    with_ctx = not last
    NT = NLT + 1 if with_ctx else NLT
    passes = [[(q * 8, q * 8 + 4), (q * 8 + 4, q * 8 + 8)] for q in range(4)]
    if with_ctx:
        passes[3].append((NLT, NLT + 1))
    in_maps = []
    for core in range(NCORE):
        s = core // 4
        in_maps.append({"x2": f32(res3[core]["x2"][:NT]), "h2T": f32(res3[core]["h2T"][:NT]), "aff": f32(res3[core]["aff"][:NT]),
                        "affs_l": f32(aff_l[s].reshape(128, 128, 16).transpose(0, 2, 1)),
                        "affs_c": f32(aff_c[s].reshape(128, 2, 16).transpose(0, 2, 1)),
                        "wg": f32(p["exp_w_gate"][l]), "wu": f32(p["exp_w_up"][l]), "wd": f32(p["exp_w_down"][l]), "ones": ones,
                        "g2_l": seg(s, 5), "g2_c": seg(2, 5), "fng": f32(p["final_norm_g"])})
    res = run(build_k4(NT, NLT, 2 * T // 16, 2 * CTX // 16, 128, 2, last, passes), in_maps)
    return untile(res, "out", 1024, with_ctx=with_ctx)


def _f32(a):
    return np.ascontiguousarray(np.asarray(a, dtype=np.float32))


def kernel(**p):
    x, ctx = _f32(p["x"]), _f32(p["ctx"])
    L = p["ada_w"].shape[0]
    mods = run_k0(_f32(p["c"]), _f32(p["c_ctx"]), _f32(p["ada_w"]), _f32(p["ada_b"]))
    for l in range(L):
        last = l == L - 1
        pl, pc = stage_proj(l, x, ctx, mods, p)
        scans = stage_scans(l, pl, pc, p)
        res3 = stage_post(l, x, ctx, pl, pc, scans, mods, p)
        del scans, pl, pc
        x, ctx_new = stage_moe(l, res3, mods, p, last)
        del res3
        if not last:
            ctx = ctx_new
    return x
```

```python
import contextlib
import numpy as np
import concourse.bass as bass
import concourse.mybir as mybir
from concourse.bass_utils import run_bass_kernel_spmd

F32 = mybir.dt.float32
BF16 = mybir.dt.bfloat16
ALU = mybir.AluOpType
AF = mybir.ActivationFunctionType
AX = mybir.AxisListType


class Buf:
    def __init__(self, t=None, name=""):
        self.t = t
        self.name = name
        self.w = None
        self.r = []
        self.excl = False

    def __getitem__(self, k):
        return self.t[k]


class Prog:
    ENGS = ("sync", "act", "dve", "pool", "pe")

    def __init__(self, nc, n_dma_sems=40):
        self.nc = nc
        self.es = contextlib.ExitStack()
        self.q = {e: [] for e in self.ENGS}
        self.EPOCH = 4000
        self.esem = {}
        self.seq = {e: 0 for e in ("act", "dve", "pool", "pe")}
        self.dsem = [nc.alloc_semaphore("ds_%d" % i) for i in range(n_dma_sems)]
        self.dcnt = [0] * n_dma_sems
        self.dlast = [None] * n_dma_sems
        self.dnext = 0
        self.waited = {e: {} for e in self.ENGS}
        self.nbuf = 0

    def sb(self, shape, dtype=F32, name=None):
        self.nbuf += 1
        name = "s_" + (name or "sb%d" % self.nbuf)
        t = self.es.enter_context(self.nc.sbuf_tensor(name, list(shape), dtype))
        return Buf(t, name)

    def ps(self, shape, dtype=F32, name=None):
        self.nbuf += 1
        name = "p_" + (name or "ps%d" % self.nbuf)
        t = self.es.enter_context(self.nc.psum_tensor(name, [128, 512], F32))
        b = Buf(t, name)
        b.excl = True
        return b

    def dram(self, name, shape, dtype=F32, kind="Internal"):
        t = self.nc.dram_tensor(name, list(shape), dtype, kind=kind)
        return Buf(t, name)

    def _need(self, eng, reads, writes):
        evs = []
        for b in reads:
            if b.w is not None:
                evs.append(b.w)
            if b.excl:
                evs.extend(b.r)
        for b in writes:
            if b.w is not None:
                evs.append(b.w)
            evs.extend(b.r)
        best = {}
        for (k, v) in evs:
            if eng == "pe" and k[0] == "pe":
                continue
            if best.get(k, 0) < v:
                best[k] = v
        out = []
        wd = self.waited[eng]
        for k, v in best.items():
            if wd.get(k, 0) >= v:
                continue
            wd[k] = v
            out.append((k, v))
        return out

    def _mark(self, ev, reads, writes):
        for b in reads:
            b.r.append(ev)
        for b in writes:
            b.w = ev
            b.r = []

    def op(self, eng, fn, reads=(), writes=()):
        waits = self._need(eng, reads, writes)
        ep, v = divmod(self.seq[eng], self.EPOCH)
        self.seq[eng] += 1
        key = (eng, ep)
        if key not in self.esem:
            self.esem[key] = self.nc.alloc_semaphore("es_%s_%d" % key)
        ev = (key, v + 1)
        self._mark(ev, reads, writes)
        self.q[eng].append((waits, fn, (key, 1)))

    def dma(self, queue, out, in_, reads=(), writes=(), **kw):
        i = self.dnext
        self.dnext = (self.dnext + 1) % len(self.dsem)
        waits = self._need(queue, reads, writes)
        key = ("d", i)
        if self.dcnt[i] > 0 and self.waited[queue].get(key, 0) < self.dcnt[i]:
            self.waited[queue][key] = self.dcnt[i]
            waits.append((key, self.dcnt[i]))
        self.dcnt[i] += 16
        ev = (key, self.dcnt[i])
        self._mark(ev, reads, writes)
        self.q[queue].append((waits, lambda e: e.dma_start(out=out, in_=in_, **kw), (key, 16)))
        return ev

    def mm(self, O, o, A, a, B, b, start=True, stop=True):
        self.op("pe", lambda e: e.matmul(o, lhsT=a, rhs=b, start=start, stop=stop), reads=[A, B], writes=[O])

    def tr(self, O, o, A, a, I, i):
        self.op("pe", lambda e: e.transpose(out=o, in_=a, identity=i), reads=[A, I], writes=[O])

    def act(self, O, o, A, a, func, bias=None, scale=1.0, accum=None, rd=(), wr=()):
        kw = {}
        if bias is not None:
            kw["bias"] = bias
        if accum is not None:
            kw["accum_out"] = accum
        self.op("act", lambda e: e.activation(out=o, in_=a, func=func, scale=scale, **kw),
                reads=[A] + list(rd), writes=[O] + list(wr))

    def tt(self, eng, O, o, A, a, B, b, op):
        self.op(eng, lambda e: e.tensor_tensor(out=o, in0=a, in1=b, op=op), reads=[A, B], writes=[O])

    def ts(self, eng, O, o, A, a, s1, s2, op0, op1=None, rd=()):
        if op1 is None:
            self.op(eng, lambda e: e.tensor_scalar(out=o, in0=a, scalar1=s1, scalar2=None, op0=op0),
                    reads=[A] + list(rd), writes=[O])
        else:
            self.op(eng, lambda e: e.tensor_scalar(out=o, in0=a, scalar1=s1, scalar2=s2, op0=op0, op1=op1),
                    reads=[A] + list(rd), writes=[O])

    def stt(self, eng, O, o, A, a, sc, B, b, op0, op1, rd=()):
        self.op(eng, lambda e: e.scalar_tensor_tensor(out=o, in0=a, scalar=sc, in1=b, op0=op0, op1=op1),
                reads=[A, B] + list(rd), writes=[O])

    def cp(self, eng, O, o, A, a):
        if eng == "act":
            self.op("act", lambda e: e.copy(out=o, in_=a), reads=[A], writes=[O])
        else:
            self.op(eng, lambda e: e.tensor_copy(out=o, in_=a), reads=[A], writes=[O])

    def _sem(self, k):
        if k[0] == "d":
            return self.dsem[k[1]]
        return self.esem[k]

    def finish(self, final_bufs):
        evs = []
        for b in final_bufs:
            if b.w is not None:
                evs.append(b.w)
        fw = []
        for (k, v) in evs:
            fw.append((k, v))
        nc = self.nc
        q = self.q
        semf = self._sem

        def replay(e, lst, tail=()):
            for waits, fn, inc in lst:
                for (k, v) in waits:
                    e.wait_ge(semf(k), v)
                ins = fn(e)
                ins.then_inc(semf(inc[0]), inc[1])
            for (k, v) in tail:
                e.wait_ge(semf(k), v)

        with nc.Block() as block:
            @block.sync
            def _(e):
                replay(e, q["sync"], fw)

            @block.scalar
            def _(e):
                replay(e, q["act"])

            @block.vector
            def _(e):
                replay(e, q["dve"])

            @block.gpsimd
            def _(e):
                replay(e, q["pool"])

            @block.tensor
            def _(e):
                replay(e, q["pe"])
        self.es.close()


D = 1024
IN_DIM = 3108
NCORE = 8


def run(nc, in_maps):
    res = run_bass_kernel_spmd(nc, in_maps, core_ids=list(range(NCORE)))
    return res.results


def build_k0():
    nc = bass.Bass("TRN2", target_bir_lowering=False)
    P = Prog(nc)
    cT = P.dram("cT", [128, 8, 3], F32, kind="ExternalInput")
    aw = P.dram("aw", [3, 8, 128, 512], F32, kind="ExternalInput")
    ab = P.dram("ab", [3, 512], F32, kind="ExternalInput")
    out = P.dram("out", [3, 3, 512], F32, kind="ExternalOutput")
    sc = P.sb([128, 8, 3])
    P.dma("sync", sc[:], cT.t.ap(), reads=[cT], writes=[sc])
    P.act(sc, sc[:], sc, sc[:], AF.Silu)
    for j in range(3):
        w = P.sb([128, 8, 512], name="w%d" % j)
        bt = P.sb([3, 512], name="b%d" % j)
        ot = P.sb([3, 512], name="o%d" % j)
        pm = P.ps([3, 512], name="pm%d" % j)
        P.dma(("sync", "act", "pool")[j], w[:], aw.t.ap()[j].rearrange("k p n -> p k n"), reads=[aw], writes=[w])
        P.dma("sync", bt[:], ab.t.ap()[j].partition_broadcast(3), reads=[ab], writes=[bt])
        for k in range(8):
            P.mm(pm, pm[0:3, :], sc, sc[:, k, :], w, w[:, k, :], start=(k == 0), stop=(k == 7))
        P.tt("dve", ot, ot[:], pm, pm[0:3, :], bt, bt[:], ALU.add)
        P.dma("sync", out.t.ap()[j], ot[:], reads=[ot], writes=[out])
    P.finish([out])
    return nc


def run_k0(c, c_ctx, ada_w, ada_b):
    L = ada_w.shape[0]
    cv = np.stack([c[0], c[1], c_ctx], axis=0)
    cT = np.ascontiguousarray(cv.reshape(3, 8, 128).transpose(2, 1, 0))
    nblk = L * 12
    assert nblk == 24
    in_maps = []
    for core in range(NCORE):
        aws, abs_ = [], []
        for j in range(3):
            b = core * 3 + j
            l, cb = divmod(b, 12)
            aws.append(ada_w[l][:, cb * 512:(cb + 1) * 512].reshape(8, 128, 512))
            abs_.append(ada_b[l][cb * 512:(cb + 1) * 512])
        in_maps.append({"cT": cT, "aw": np.ascontiguousarray(np.stack(aws)), "ab": np.ascontiguousarray(np.stack(abs_))})
    res = run(build_k0(), in_maps)
    mods = np.zeros((L, 3, 6144), np.float32)
    for core in range(NCORE):
        for j in range(3):
            b = core * 3 + j
            l, cb = divmod(b, 12)
            mods[l, :, cb * 512:(cb + 1) * 512] = res[core]["out"][j]
    return mods


def load_bcast(P, queue, dram_buf, n, name):
    t = P.sb([128, n], name=name)
    P.dma(queue, t[:], dram_buf.t.ap().partition_broadcast(128), reads=[dram_buf], writes=[t])
    return t


def norm_mod_tile(P, xt, h, A, B, scr, ss, eng2="pool"):
    P.act(scr, scr[:], xt, xt[:], AF.Square, scale=1.0 / 32.0, accum=ss[:], wr=[ss])
    P.ts("dve", ss, ss[:], ss, ss[:], 1e-6, None, ALU.add)
    P.op("act", lambda e: e.sqrt(out=ss[:], in_=ss[:]), reads=[ss], writes=[ss])
    P.op("dve", lambda e: e.reciprocal(out=ss[:], in_=ss[:]), reads=[ss], writes=[ss])
    P.stt("dve", h, h[:], xt, xt[:], ss[:, 0:1], A, A[:], ALU.mult, ALU.mult, rd=[ss])
    P.tt(eng2, h, h[:], h, h[:], B, B[:], ALU.add)


def transpose_tile(P, h, hT, ident, pts, nk=8, evac=("act", "dve")):
    for half in range((nk + 3) // 4):
        pt = pts[half % len(pts)]
        n4 = min(4, nk - half * 4)
        for q in range(n4):
            k = half * 4 + q
            P.tr(pt, pt[:, q * 128:(q + 1) * 128], h, h[:, k * 128:(k + 1) * 128], ident, ident[:])
        P.cp(evac[half % len(evac)], hT, hT[:, half * 4:half * 4 + n4, :],
             pt, pt[:, 0:n4 * 128].rearrange("p (k t) -> p k t", k=n4))


def load_weight_bf16(P, wdram_ap_fn, W, nk, ncols, stage, wdram, colchunk=None):
    qs = ("sync", "act", "pool")
    cs = ("pool", "dve", "act")
    for k in range(nk):
        st = stage[k % len(stage)]
        P.dma(qs[k % 3], st[:, 0:ncols], wdram_ap_fn(k), reads=[wdram], writes=[st])
        P.cp(cs[k % 3], W, W[:, k, :], st, st[:, 0:ncols])


def build_k1(NT, ncols=IN_DIM, n_lat=None):
    if n_lat is None:
        n_lat = NT - 1
    nc = bass.Bass("TRN2", target_bir_lowering=False)
    P = Prog(nc)
    xt = P.dram("xt", [NT, 128, D], F32, kind="ExternalInput")
    w = P.dram("w", [D, ncols], F32, kind="ExternalInput")
    g = P.dram("g", [D], F32, kind="ExternalInput")
    vecs = {n: P.dram(n, [D], F32, kind="ExternalInput") for n in ("sh_l", "sc_l", "sh_c", "sc_c")}
    idn = P.dram("idn", [128, 128], F32, kind="ExternalInput")
    out = P.dram("out", [NT, 128, ncols], F32, kind="ExternalOutput")

    ident = P.sb([128, 128], name="ident")
    P.dma("sync", ident[:], idn.t.ap(), reads=[idn], writes=[ident])
    gb = load_bcast(P, "act", g, D, "gb")
    A_l = load_bcast(P, "pool", vecs["sc_l"], D, "A_l")
    B_l = load_bcast(P, "sync", vecs["sh_l"], D, "B_l")
    A_c = load_bcast(P, "act", vecs["sc_c"], D, "A_c")
    B_c = load_bcast(P, "pool", vecs["sh_c"], D, "B_c")
    for A in (A_l, A_c):
        P.stt("dve", A, A[:], A, A[:], 1.0, gb, gb[:], ALU.add, ALU.mult)

    W = P.sb([128, 8, ncols], BF16, name="W")
    stage = [P.sb([128, ncols], name="stg%d" % i) for i in range(2)]
    load_weight_bf16(P, lambda k: w.t.ap()[k * 128:(k + 1) * 128, :], W, 8, ncols, stage, w)

    xs = [P.sb([128, D], name="x%d" % i) for i in range(2)]
    hs = [P.sb([128, D], name="h%d" % i) for i in range(2)]
    scr = P.sb([128, D], name="scr")
    sss = [P.sb([128, 1], name="ss%d" % i) for i in range(2)]
    hTs = [P.sb([128, 8, 128], BF16, name="hT%d" % i) for i in range(2)]
    outs = [P.sb([128, ncols], name="ot%d" % i) for i in range(2)]
    pts = [P.ps([128, 512], name="pt%d" % i) for i in range(2)]
    pms = [P.ps([128, 512], name="pm%d" % i) for i in range(4)]
    ncb = (ncols + 511) // 512
    scrs = [scr, P.sb([128, D], name="scr_b")]

    def tile_gen(t, n):
        x_, h_, ss_, hT_, o_, sc_ = xs[n], hs[n], sss[n], hTs[n], outs[n], scrs[n]
        pt = pts[n]
        pm2 = pms[2 * n:2 * n + 2]
        P.dma(("sync", "act")[n], x_[:], xt.t.ap()[t], reads=[xt], writes=[x_])
        yield
        A, B = (A_l, B_l) if t < n_lat else (A_c, B_c)
        P.act(sc_, sc_[:], x_, x_[:], AF.Square, scale=1.0 / 32.0, accum=ss_[:], wr=[ss_])
        yield
        P.ts("dve", ss_, ss_[:], ss_, ss_[:], 1e-6, None, ALU.add)
        yield
        P.op("act", lambda e: e.sqrt(out=ss_[:], in_=ss_[:]), reads=[ss_], writes=[ss_])
        yield
        P.op("dve", lambda e: e.reciprocal(out=ss_[:], in_=ss_[:]), reads=[ss_], writes=[ss_])
        P.stt("dve", h_, h_[:], x_, x_[:], ss_[:, 0:1], A, A[:], ALU.mult, ALU.mult, rd=[ss_])
        yield
        P.tt("pool", h_, h_[:], h_, h_[:], B, B[:], ALU.add)
        yield
        for half in range(2):
            for q in range(4):
                k = half * 4 + q
                P.tr(pt, pt[:, q * 128:(q + 1) * 128], h_, h_[:, k * 128:(k + 1) * 128], ident, ident[:])
            yield
            P.cp(("act", "dve")[half], hT_, hT_[:, half * 4:half * 4 + 4, :], pt, pt[:, 0:512].rearrange("p (k t) -> p k t", k=4))
            yield
        for cb in range(ncb):
            c0 = cb * 512
            cw = min(512, ncols - c0)
            pm = pm2[cb % 2]
            for k in range(8):
                P.mm(pm, pm[:, 0:cw], hT_, hT_[:, k, :], W, W[:, k, c0:c0 + cw], start=(k == 0), stop=(k == 7))
            yield
            P.cp(("act", "dve")[cb % 2], o_, o_[:, c0:c0 + cw], pm, pm[:, 0:cw])
            yield
        P.dma("sync", out.t.ap()[t], o_[:], reads=[o_], writes=[out])
        yield

    def roundrobin(gens):
        gens = list(gens)
        while gens:
            for g in list(gens):
                try:
                    next(g)
                except StopIteration:
                    gens.remove(g)

    for t in range(0, NT, 2):
        gens = [tile_gen(t, 0)]
        if t + 1 < NT:
            gens.append(tile_gen(t + 1, 1))
        roundrobin(gens)
    P.finish([out])
    return nc


NCH = 130
NBLK = 65


def consts():
    t = np.arange(128)
    U = (t[:, None] <= t[None, :]).astype(np.float32)
    NEG = np.where(t[None, :] < t[:, None], -30000.0, 0.0).astype(np.float32)
    return {"idn": np.eye(128, dtype=np.float32), "U": U, "NEG": NEG, "ones": np.ones((128, 128), np.float32)}


def load_consts(P, names=("idn", "U", "NEG", "ones")):
    out = {}
    for i, n in enumerate(names):
        d = P.dram(n, [128, 128], F32, kind="ExternalInput")
        s = P.sb([128, 128], name="c_" + n)
        P.dma(("sync", "act", "pool")[i % 3], s[:], d.t.ap(), reads=[d], writes=[s])
        out[n] = s
    return out


def conv_block(P, eng, ut, acc, cv, cw, cb, npart, n=256, bias=True):
    sl = slice(0, npart)
    P.ts(eng, acc, acc[sl, 0:n], ut, ut[sl, 0:n], cw[sl, 0:1], None, ALU.mult, rd=[cw])
    for k in range(1, 5):
        P.stt(eng, acc, acc[sl, 0:n], ut, ut[sl, k:k + n], cw[sl, k:k + 1], acc, acc[sl, 0:n], ALU.mult, ALU.add, rd=[cw])
    if bias:
        P.act(cv, cv[sl, 0:n], acc, acc[sl, 0:n], AF.Silu, bias=cb[sl, 0:1], rd=[cb])
    else:
        P.act(cv, cv[sl, 0:n], acc, acc[sl, 0:n], AF.Silu)


def softplus_inplace(P, t, ap):
    P.act(t, ap, t, ap, AF.Exp)
    P.ts("dve", t, ap, t, ap, 1.0, None, ALU.add)
    P.act(t, ap, t, ap, AF.Ln)


def cum_tables(P, C, la, pC1, pC2, nh=3):
    N = NCH * nh
    flat = lambda b: b[:].rearrange("p c h -> p (c h)")
    T = {}
    for n in ("negac", "eac", "wj", "eL"):
        T[n] = P.sb([128, NCH, nh], name="tb_" + n)
    P.mm(pC1, pC1[:, 0:N], C["U"], C["U"][:], la, flat(la))
    P.mm(pC2, pC2[:, 0:N], C["ones"], C["ones"][:], la, flat(la))
    P.ts("dve", T["negac"], flat(T["negac"]), pC1, pC1[:, 0:N], -1.0, None, ALU.mult)
    P.act(T["eac"], flat(T["eac"]), pC1, pC1[:, 0:N], AF.Exp)
    P.act(T["eL"], flat(T["eL"]), pC2, pC2[:, 0:N], AF.Exp)
    P.tt("dve", T["wj"], flat(T["wj"]), pC2, pC2[:, 0:N], T["negac"], flat(T["negac"]), ALU.add)
    P.act(T["wj"], flat(T["wj"]), T["wj"], flat(T["wj"]), AF.Exp)
    return T


def decay_mats(P, C, la, T, c, pA, rhs_t, decT, nh=3):
    for h in range(nh):
        r = rhs_t[h % len(rhs_t)]
        P.ts(("dve", "pool")[h % 2], r, r[:], C["U"], C["U"][:], la[:, c, h:h + 1], None, ALU.mult, rd=[la])
        P.mm(pA, pA[:, h * 128:(h + 1) * 128], C["ones"], C["ones"][:], r, r[:], start=True, stop=False)
        P.mm(pA, pA[:, h * 128:(h + 1) * 128], C["idn"], C["idn"][:], C["NEG"], C["NEG"][:], start=False, stop=True)
    for h in range(nh):
        P.act(decT, decT[:, h, :], pA, pA[:, h * 128:(h + 1) * 128], AF.Exp, bias=T["negac"][:, c, h:h + 1], rd=[T["negac"]])


def build_k2s():
    nc = bass.Bass("TRN2", target_bir_lowering=False)
    P = Prog(nc)
    u = P.dram("u", [448, NBLK, 260], F32, kind="ExternalInput")
    cwd = P.dram("cw", [448, 5], F32, kind="ExternalInput")
    cbd = P.dram("cb", [448, 1], F32, kind="ExternalInput")
    dtr = P.dram("dtr", [128, NCH, 3], F32, kind="ExternalInput")
    dtb = P.dram("dtb", [3], F32, kind="ExternalInput")
    alog = P.dram("alog", [3], F32, kind="ExternalInput")
    yout = P.dram("y", [NCH, 128, 192], F32, kind="ExternalOutput")
    xout = P.dram("xs", [NCH, 128, 192], F32, kind="ExternalOutput")
    C = load_consts(P)
    offs = (0, 128, 192, 320)
    nps = (128, 64, 128, 128)
    cw, cb = [], []
    for i in range(4):
        a = P.sb([128, 5], name="cw%d" % i)
        b = P.sb([128, 1], name="cb%d" % i)
        P.dma("sync", a[0:nps[i], :], cwd.t.ap()[offs[i]:offs[i] + nps[i], :], reads=[cwd], writes=[a])
        P.dma("act", b[0:nps[i], :], cbd.t.ap()[offs[i]:offs[i] + nps[i], :], reads=[cbd], writes=[b])
        cw.append(a)
        cb.append(b)
    dt = P.sb([128, NCH, 3], name="dt")
    la = P.sb([128, NCH, 3], name="la")
    dtw = P.sb([128, NCH, 3], name="dtw")
    dtbb = P.sb([128, 3], name="dtbb")
    Ab = P.sb([128, 3], name="Ab")
    P.dma("sync", dt[:], dtr.t.ap(), reads=[dtr], writes=[dt])
    P.dma("act", dtbb[:], dtb.t.ap().partition_broadcast(128), reads=[dtb], writes=[dtbb])
    P.dma("pool", Ab[:], alog.t.ap().partition_broadcast(128), reads=[alog], writes=[Ab])
    P.act(Ab, Ab[:], Ab, Ab[:], AF.Exp)
    P.ts("dve", Ab, Ab[:], Ab, Ab[:], -1.0, None, ALU.mult)
    for h in range(3):
        P.ts("dve", dt, dt[:, :, h], dt, dt[:, :, h], dtbb[:, h:h + 1], None, ALU.add, rd=[dtbb])
    fl = lambda b: b[:].rearrange("p c h -> p (c h)")
    softplus_inplace(P, dt, fl(dt))
    for h in range(3):
        P.ts("dve", la, la[:, :, h], dt, dt[:, :, h], Ab[:, h:h + 1], None, ALU.mult, rd=[Ab])
    pC1 = P.ps([128, 512], name="pC1")
    pC2 = P.ps([128, 512], name="pC2")
    T = cum_tables(P, C, la, pC1, pC2)
    P.tt("dve", dtw, fl(dtw), dt, fl(dt), T["wj"], fl(T["wj"]), ALU.mult)

    banks = [pC1, pC2] + [P.ps([1], name="bk%d" % i) for i in range(6)]
    bankset = [banks[0:3], banks[3:6]]
    bY2, bS = banks[6], banks[7]
    S = P.sb([128, 192], name="S")
    P.op("dve", lambda e: e.memset(S[:], 0.0), writes=[S])
    ut = [[P.sb([128, 260], name="ut%d_%d" % (i, j)) for j in range(2)] for i in range(4)]
    acc = [P.sb([128, 256], name="acc%d" % i) for i in range(4)]
    cv = [[P.sb([128, 256], name="cv%d_%d" % (i, j)) for j in range(2)] for i in range(4)]

    def mkset(n):
        return {"rhs": [P.sb([128, 128], name="rhs%d_%d" % (i, n)) for i in range(3)],
                "decT": P.sb([128, 3, 128], name="decT%d" % n), "Wt": P.sb([128, 3, 128], name="Wt%d" % n),
                "xdt": P.sb([128, 192], name="xdt%d" % n)}

    def mkhand(n, par):
        return {"tk": P.sb([128, 320], name="tok%d_%d" % (n, par)), "xw": P.sb([128, 192], name="xw%d_%d" % (n, par)),
                "ysb": P.sb([128, 192], name="ysb%d_%d" % (n, par))}

    sets = [mkset(0), mkset(1)]
    hands = [[mkhand(n, par) for par in range(2)] for n in range(2)]
    yo = [P.sb([128, 192], name="yo%d" % i) for i in range(2)]

    def pre(c, cc, j, st, bk, hd):
        bT, bA, bY1 = bk
        tk, xw, ysb = hd["tk"], hd["xw"], hd["ysb"]
        decT, Wt, xdt = st["decT"], st["Wt"], st["xdt"]
        xA, xB, BT, CT = cv[0][j], cv[1][j], cv[2][j], cv[3][j]
        ck = slice(cc * 128, (cc + 1) * 128)
        P.tr(bT, bT[:, 0:128], xA, xA[:, ck], C["idn"], C["idn"][:])
        P.tr(bT, bT[:, 128:192], xB, xB[0:64, ck], C["idn"], C["idn"][0:64, 0:64])
        P.tr(bT, bT[:, 192:320], BT, BT[:, ck], C["idn"], C["idn"][:])
        yield
        P.cp("act", tk, tk[:], bT, bT[:, 0:320])
        P.dma("sync", xout.t.ap()[c], tk[:, 0:192], reads=[tk], writes=[xout])
        for h in range(3):
            r = st["rhs"][h]
            P.ts(("dve", "pool")[h % 2], r, r[:], C["U"], C["U"][:], la[:, c, h:h + 1], None, ALU.mult, rd=[la])
        yield
        for h in range(3):
            r = st["rhs"][h]
            P.mm(bA, bA[:, h * 128:(h + 1) * 128], C["ones"], C["ones"][:], r, r[:], start=True, stop=False)
            P.mm(bA, bA[:, h * 128:(h + 1) * 128], C["idn"], C["idn"][:], C["NEG"], C["NEG"][:], start=False, stop=True)
        P.mm(bT, bT[:, 0:128], BT, BT[:, ck], CT, CT[:, ck])
        yield
        for h in range(3):
            P.act(decT, decT[:, h, :], bA, bA[:, h * 128:(h + 1) * 128], AF.Exp, bias=T["negac"][:, c, h:h + 1], rd=[T["negac"]])
        for h in range(3):
            hs = slice(h * 64, (h + 1) * 64)
            P.ts("pool", xdt, xdt[:, hs], tk, tk[:, hs], dt[:, c, h:h + 1], None, ALU.mult, rd=[dt])
            P.ts("pool", xw, xw[:, hs], tk, tk[:, hs], dtw[:, c, h:h + 1], None, ALU.mult, rd=[dtw])
        yield
        for h in range(3):
            P.tt("dve", Wt, Wt[:, h, :], bT, bT[:, 0:128], decT, decT[:, h, :], ALU.mult)
        yield
        for h in range(3):
            hs = slice(h * 64, (h + 1) * 64)
            P.mm(bY1, bY1[:, hs], Wt, Wt[:, h, :], xdt, xdt[:, hs])
        yield
        P.cp("act", ysb, ysb[:], bY1, bY1[:, 0:192])
        yield

    def rec(c, cc, j, hd):
        tk, xw, ysb = hd["tk"], hd["xw"], hd["ysb"]
        CT = cv[3][j]
        ck = slice(cc * 128, (cc + 1) * 128)
        for h in range(3):
            hs = slice(h * 64, (h + 1) * 64)
            P.mm(bY2, bY2[:, hs], CT, CT[:, ck], S, S[:, hs])
            P.mm(bS, bS[:, hs], tk, tk[:, 192:320], xw, xw[:, hs])
        yield
        y_ = yo[c % 2]
        for h in range(3):
            hs = slice(h * 64, (h + 1) * 64)
            P.stt("dve", y_, y_[:, hs], bY2, bY2[:, hs], T["eac"][:, c, h:h + 1], ysb, ysb[:, hs], ALU.mult, ALU.add, rd=[T["eac"]])
        for h in range(3):
            hs = slice(h * 64, (h + 1) * 64)
            P.stt("dve", S, S[:, hs], S, S[:, hs], T["eL"][:, c, h:h + 1], bS, bS[:, hs], ALU.mult, ALU.add, rd=[T["eL"]])
        P.dma("sync", yout.t.ap()[c], y_[:], reads=[y_], writes=[yout])
        yield

    def chain(gs):
        for g in gs:
            for _ in g:
                yield

    def roundrobin(gens):
        gens = list(gens)
        while gens:
            for g in list(gens):
                try:
                    next(g)
                except StopIteration:
                    gens.remove(g)

    pending = []
    for b in range(NBLK):
        j = b % 2
        for i in range(4):
            P.dma(("sync", "act")[i % 2], ut[i][j][0:nps[i], :], u.t.ap()[offs[i]:offs[i] + nps[i], b, :],
                  reads=[u], writes=[ut[i][j]])
            conv_block(P, "dve", ut[i][j], acc[i], cv[i][j], cw[i], cb[i], nps[i])
        gens = [pre(2 * b, 0, j, sets[0], bankset[0], hands[0][j]), pre(2 * b + 1, 1, j, sets[1], bankset[1], hands[1][j])]
        if pending:
            gens.append(chain(pending))
        roundrobin(gens)
        pending = [rec(2 * b, 0, j, hands[0][j]), rec(2 * b + 1, 1, j, hands[1][j])]
    roundrobin([chain(pending)])
    P.finish([yout, xout])
    return nc


def windows(seg, n=256):
    ch, T = seg.shape
    p = np.zeros((ch, T + 4), np.float32)
    p[:, 2:T + 2] = seg
    idx = (np.arange(T // n)[:, None] * n + np.arange(n + 4)[None, :])
    return p[:, idx]


def prep_k2s(pl, pc, conv_w, conv_b, a_log, dt_bias, core):
    s, d, g = core // 4, (core // 2) % 2, core % 2
    c0 = 256 + 384
    chans = np.concatenate([np.arange(g * 192, g * 192 + 192), 384 + g * 128 + np.arange(128), 384 + 256 + g * 128 + np.arange(128)])
    segs = []
    for arr in (pc[s], pl[s]):
        a = arr[:, c0 + chans]
        if d == 1:
            a = a[::-1]
        segs.append(windows(np.ascontiguousarray(a.T)))
    u = np.ascontiguousarray(np.concatenate(segs, axis=1))
    cw = conv_w[:, chans].T
    if d == 1:
        cw = cw[:, ::-1]
    dcol = 256 + 384 + 896 + d * 6 + g * 3
    dts = []
    for arr in (pc[s], pl[s]):
        a = arr[:, dcol:dcol + 3]
        if d == 1:
            a = a[::-1]
        dts.append(a)
    dtr = np.concatenate(dts, 0).reshape(NCH, 128, 3).transpose(1, 0, 2)
    m = {"u": u, "cw": np.ascontiguousarray(cw), "cb": np.ascontiguousarray(conv_b[chans][:, None]),
         "dtr": np.ascontiguousarray(dtr), "dtb": np.ascontiguousarray(dt_bias[d, g * 3:g * 3 + 3]),
         "alog": np.ascontiguousarray(a_log[d, g * 3:g * 3 + 3])}
    m.update(consts())
    return m


GRID_W = 64


def consts_g():
    c = consts()
    t = np.arange(128)
    c["POS"] = np.where(t[None, :] >= t[:, None], 30000.0, 0.0).astype(np.float32)
    return c


def build_k2g(nblk=NBLK):
    nc = bass.Bass("TRN2", target_bir_lowering=False)
    P = Prog(nc)
    u = P.dram("u", [576, NBLK, 260], F32, kind="ExternalInput")
    cwd = P.dram("cw", [576, 5], F32, kind="ExternalInput")
    ard = P.dram("araw", [128, NCH, 3], F32, kind="ExternalInput")
    brd = P.dram("braw", [128, NCH, 3], F32, kind="ExternalInput")
    dtb = P.dram("dtb", [3], F32, kind="ExternalInput")
    alog = P.dram("alog", [3], F32, kind="ExternalInput")
    oout = P.dram("o", [NCH, 128, 192], F32, kind="ExternalOutput")
    C = load_consts(P, ("idn", "U", "NEG", "ones", "POS"))
    idn = C["idn"]
    offs = (0, 128, 192, 320, 384, 512)
    nps = (128, 64, 128, 64, 128, 64)
    cw = []
    for i in range(6):
        a = P.sb([128, 5], name="cw%d" % i)
        P.dma(("sync", "act")[i % 2], a[0:nps[i], :], cwd.t.ap()[offs[i]:offs[i] + nps[i], :], reads=[cwd], writes=[a])
        cw.append(a)
    fl = lambda b: b[:].rearrange("p c h -> p (c h)")
    N3 = NCH * 3
    la = P.sb([128, NCH, 3], name="la")
    beta = P.sb([128, NCH, 3], name="beta")
    dtbb = P.sb([128, 3], name="dtbb")
    Ab = P.sb([128, 3], name="Ab")
    P.dma("sync", la[:], ard.t.ap(), reads=[ard], writes=[la])
    P.dma("pool", beta[:], brd.t.ap(), reads=[brd], writes=[beta])
    P.dma("act", dtbb[:], dtb.t.ap().partition_broadcast(128), reads=[dtb], writes=[dtbb])
    P.dma("pool", Ab[:], alog.t.ap().partition_broadcast(128), reads=[alog], writes=[Ab])
    P.act(Ab, Ab[:], Ab, Ab[:], AF.Exp)
    P.ts("dve", Ab, Ab[:], Ab, Ab[:], -1.0, None, ALU.mult)
    for h in range(3):
        P.ts("dve", la, la[:, :, h], la, la[:, :, h], dtbb[:, h:h + 1], None, ALU.add, rd=[dtbb])
    softplus_inplace(P, la, fl(la))
    for h in range(3):
        P.ts("dve", la, la[:, :, h], la, la[:, :, h], Ab[:, h:h + 1], None, ALU.mult, rd=[Ab])
    P.act(beta, fl(beta), beta, fl(beta), AF.Sigmoid)
    banks = [P.ps([128, 512], name="bank%d" % i) for i in range(8)]
    ac = P.sb([128, NCH, 3], name="ac")
    negac = P.sb([128, NCH, 3], name="negac")
    eac = P.sb([128, NCH, 3], name="eac")
    wj = P.sb([128, NCH, 3], name="wj")
    eL = P.sb([128, NCH, 3], name="eL")
    be = P.sb([128, NCH, 3], name="be")
    nbeta = P.sb([128, NCH, 3], name="nbeta")
    pC1, pC2 = banks[3], banks[4]
    P.mm(pC1, pC1[:, 0:N3], C["U"], C["U"][:], la, fl(la))
    P.mm(pC2, pC2[:, 0:N3], C["ones"], C["ones"][:], la, fl(la))
    P.cp("dve", ac, fl(ac), pC1, pC1[:, 0:N3])
    P.ts("dve", negac, fl(negac), ac, fl(ac), -1.0, None, ALU.mult)
    P.act(eac, fl(eac), ac, fl(ac), AF.Exp)
    P.act(eL, fl(eL), pC2, pC2[:, 0:N3], AF.Exp)
    P.tt("dve", wj, fl(wj), pC2, pC2[:, 0:N3], negac, fl(negac), ALU.add)
    P.act(wj, fl(wj), wj, fl(wj), AF.Exp)
    P.tt("dve", be, fl(be), beta, fl(beta), eac, fl(eac), ALU.mult)
    P.ts("dve", nbeta, fl(nbeta), beta, fl(beta), -1.0, None, ALU.mult)

    I3 = P.sb([128, 3, 128], name="I3")
    for h in range(3):
        P.cp("pool", I3, I3[:, h, :], idn, idn[:])
    S = P.sb([64, 192], name="S")
    P.op("dve", lambda e: e.memset(S[:], 0.0), writes=[S])

    ut = [[P.sb([128, 260], name="ut%d_%d" % (i, j)) for j in range(2)] for i in range(6)]
    acc = [P.sb([128, 256], name="acc%d" % i) for i in range(6)]
    cv = [[P.sb([128, 256], name="cv%d_%d" % (i, j)) for j in range(2)] for i in range(6)]

    def mkset(n):
        d = {}
        for nm, shp in (("qk", [128, 384]), ("vt", [128, 192]), ("sq", [128, 384]), ("rs", [128, 6]), ("qkn", [128, 384]),
                        ("kT", [64, 384]), ("kbe", [128, 192]), ("vb", [128, 192]), ("decT", [128, 3, 128]),
                        ("decS", [128, 3, 128]), ("Np", [128, 3, 128]), ("Mp", [128, 3, 128]), ("Tt", [128, 3, 128])):
            d[nm] = P.sb(shp, name="%s_%d" % (nm, n))
        d["rhs"] = [P.sb([128, 128], name="rhs%d_%d" % (i, n)) for i in range(3)]
        return d

    def mkhand(n, par):
        d = {}
        for nm, shp in (("usb", [128, 192]), ("wT", [64, 384]), ("qT", [64, 384]), ("attnT", [128, 3, 128]), ("kend", [128, 192])):
            d[nm] = P.sb(shp, name="%s_%d_%d" % (nm, n, par))
        return d

    sets = [mkset(0), mkset(1)]
    hands = [[mkhand(n, par) for par in range(2)] for n in range(2)]
    bankset = [banks[0:3], banks[3:6]]
    bR6, bR7 = banks[6], banks[7]
    vnew = P.sb([128, 192], name="vnew")
    o2 = P.sb([128, 192], name="o2")
    oo = [P.sb([128, 192], name="oo%d" % i) for i in range(2)]
    f3 = lambda b: b[:].rearrange("p h n -> p (h n)")
    H = lambda h: slice(h * 64, (h + 1) * 64)
    H2 = lambda h: slice(h * 128, (h + 1) * 128)

    def pre(c, cc, j, st, bk, hd):
        b0, b1, b2 = bk
        qk, vt, sq, rs, qkn, kT = st["qk"], st["vt"], st["sq"], st["rs"], st["qkn"], st["kT"]
        kbe, vb, decT, decS, Np, Mp, Tt, rhs_t = st["kbe"], st["vb"], st["decT"], st["decS"], st["Np"], st["Mp"], st["Tt"], st["rhs"]
        usb, wT, qT, attnT, kend = hd["usb"], hd["wT"], hd["qT"], hd["attnT"], hd["kend"]
        ck = slice(cc * 128, (cc + 1) * 128)
        pT1, pT2 = b0, b1
        for a in range(3):
            pt = pT1 if a < 2 else pT2
            base = (a % 2) * 192
            t01, t2 = cv[2 * a][j], cv[2 * a + 1][j]
            P.tr(pt, pt[:, base:base + 128], t01, t01[:, ck], idn, idn[:])
            P.tr(pt, pt[:, base + 128:base + 192], t2, t2[0:64, ck], idn, idn[0:64, 0:64])
        yield
        P.cp("act", qk, qk[:], pT1, pT1[:, 0:384])
        P.cp("dve", vt, vt[:], pT2, pT2[:, 0:192])
        yield
        P.tt("pool", sq, sq[:], qk, qk[:], qk, qk[:], ALU.mult)
        yield
        P.op("dve", lambda e: e.reduce_sum(out=rs[:], in_=sq[:].rearrange("p (a d) -> p a d", d=64), axis=AX.X),
             reads=[sq], writes=[rs])
        P.ts("dve", rs, rs[:], rs, rs[:], 1e-6, None, ALU.add)
        yield
        P.op("act", lambda e: e.sqrt(out=rs[:], in_=rs[:]), reads=[rs], writes=[rs])
        yield
        P.op("dve", lambda e: e.reciprocal(out=rs[:], in_=rs[:]), reads=[rs], writes=[rs])
        P.ts("dve", rs, rs[:, 0:3], rs, rs[:, 0:3], 0.125, None, ALU.mult)
        yield
        for a in range(6):
            P.ts(("dve", "pool")[a % 2], qkn, qkn[:, H(a)], qk, qk[:, H(a)], rs[:, a:a + 1], None, ALU.mult, rd=[rs])
        yield
        pKT, pQT = b2, b0
        for h in range(3):
            P.tr(pKT, pKT[0:64, H2(h)], qkn, qkn[:, 192 + h * 64:192 + (h + 1) * 64], idn, idn[:])
            P.tr(pQT, pQT[0:64, H2(h)], qkn, qkn[:, h * 64:(h + 1) * 64], idn, idn[:])
        yield
        P.cp("act", kT, kT[:], pKT, pKT[0:64, 0:384])
        P.cp("dve", qT, qT[:], pQT, pQT[0:64, 0:384])
        for h in range(3):
            kn_h = qkn[:, 192 + h * 64:192 + (h + 1) * 64]
            P.ts("pool", kbe, kbe[:, H(h)], qkn, kn_h, be[:, c, h:h + 1], None, ALU.mult, rd=[be])
            P.ts("pool", vb, vb[:, H(h)], vt, vt[:, H(h)], beta[:, c, h:h + 1], None, ALU.mult, rd=[beta])
            P.ts("pool", kend, kend[:, H(h)], qkn, kn_h, wj[:, c, h:h + 1], None, ALU.mult, rd=[wj])
        yield
        pA1, pA2 = b0, b1
        for h in range(3):
            r = rhs_t[h]
            P.ts(("dve", "pool")[h % 2], r, r[:], C["U"], C["U"][:], la[:, c, h:h + 1], None, ALU.mult, rd=[la])
        yield
        for h in range(3):
            r = rhs_t[h]
            P.mm(pA1, pA1[:, H2(h)], C["ones"], C["ones"][:], r, r[:], start=True, stop=False)
            P.mm(pA1, pA1[:, H2(h)], idn, idn[:], C["NEG"], C["NEG"][:], start=False, stop=True)
            P.mm(pA2, pA2[:, H2(h)], C["ones"], C["ones"][:], r, r[:], start=True, stop=False)
            P.mm(pA2, pA2[:, H2(h)], idn, idn[:], C["POS"], C["POS"][:], start=False, stop=True)
        yield
        for h in range(3):
            P.act(decT, decT[:, h, :], pA1, pA1[:, H2(h)], AF.Exp, bias=negac[:, c, h:h + 1], rd=[negac])
            P.act(decS, decS[:, h, :], pA2, pA2[:, H2(h)], AF.Exp, bias=ac[:, c, h:h + 1], scale=-1.0, rd=[ac])
        yield
        pKK, pQK = b2, b0
        for h in range(3):
            P.mm(pKK, pKK[:, H2(h)], kT, kT[:, H2(h)], kT, kT[:, H2(h)])
            P.mm(pQK, pQK[:, H2(h)], kT, kT[:, H2(h)], qT, qT[:, H2(h)])
        yield
        for h in range(3):
            P.stt("dve", Np, Np[:, h, :], pKK, pKK[:, H2(h)], nbeta[:, c, h:h + 1], decS, decS[:, h, :], ALU.mult, ALU.mult, rd=[nbeta])
            P.tt("dve", attnT, attnT[:, h, :], pQK, pQK[:, H2(h)], decT, decT[:, h, :], ALU.mult)
        yield
        pN, pM, pTt = b0, b1, b2
        for h in range(3):
            P.tr(pM, pM[:, H2(h)], Np, Np[:, h, :], idn, idn[:])
        yield
        P.cp("act", Mp, f3(Mp), pM, pM[:, 0:384])
        yield
        P.tt("pool", Tt, f3(Tt), Mp, f3(Mp), I3, f3(I3), ALU.add)
        for step in range(6):
            last = step == 5
            for h in range(3):
                P.mm(pN, pN[:, H2(h)], Mp, Mp[:, h, :], Np, Np[:, h, :])
                if not last:
                    P.mm(pM, pM[:, H2(h)], Np, Np[:, h, :], Mp, Mp[:, h, :])
            yield
            P.cp("act", Np, f3(Np), pN, pN[:, 0:384])
            if not last:
                P.cp("dve", Mp, f3(Mp), pM, pM[:, 0:384])
            yield
            for h in range(3):
                P.mm(pTt, pTt[:, H2(h)], Np, Np[:, h, :], Tt, Tt[:, h, :])
            yield
            P.tt("dve", Tt, f3(Tt), Tt, f3(Tt), pTt, pTt[:, 0:384], ALU.add)
        yield
        pU, pWT = b0, b1
        for h in range(3):
            P.mm(pU, pU[:, H(h)], Tt, Tt[:, h, :], vb, vb[:, H(h)])
            P.mm(pWT, pWT[0:64, H2(h)], kbe, kbe[:, H(h)], Tt, Tt[:, h, :])
        yield
        P.cp("act", usb, usb[:], pU, pU[:, 0:192])
        P.cp("dve", wT, wT[:], pWT, pWT[0:64, 0:384])
        yield

    def rec(c, hd):
        usb, wT, qT, attnT, kend = hd["usb"], hd["wT"], hd["qT"], hd["attnT"], hd["kend"]
        pWS, pO1, pO2, pSn = bR6, bR7, bR6, bR6
        for h in range(3):
            P.mm(pWS, pWS[:, H(h)], wT, wT[:, H2(h)], S, S[:, H(h)])
            P.mm(pO1, pO1[:, H(h)], qT, qT[:, H2(h)], S, S[:, H(h)])
        yield
        P.tt("dve", vnew, vnew[:], usb, usb[:], pWS, pWS[:, 0:192], ALU.subtract)
        yield
        for h in range(3):
            P.mm(pO2, pO2[:, H(h)], attnT, attnT[:, h, :], vnew, vnew[:, H(h)])
        yield
        P.cp("act", o2, o2[:], pO2, pO2[:, 0:192])
        yield
        for h in range(3):
            P.mm(pSn, pSn[0:64, H(h)], kend, kend[:, H(h)], vnew, vnew[:, H(h)])
        o_ = oo[c % 2]
        for h in range(3):
            P.stt("dve", o_, o_[:, H(h)], pO1, pO1[:, H(h)], eac[:, c, h:h + 1], o2, o2[:, H(h)], ALU.mult, ALU.add, rd=[eac])
        yield
        for h in range(3):
            P.stt("dve", S, S[:, H(h)], S, S[:, H(h)], eL[0:64, c, h:h + 1], pSn, pSn[0:64, H(h)], ALU.mult, ALU.add, rd=[eL])
        P.dma("sync", oout.t.ap()[c], o_[:], reads=[o_], writes=[oout])
        yield

    def chain(gs):
        for g in gs:
            for _ in g:
                yield

    def roundrobin(gens):
        gens = list(gens)
        while gens:
            for g in list(gens):
                try:
                    next(g)
                except StopIteration:
                    gens.remove(g)

    pending = []
    for b in range(nblk):
        j = b % 2
        for i in range(6):
            P.dma(("sync", "act")[i % 2], ut[i][j][0:nps[i], :], u.t.ap()[offs[i]:offs[i] + nps[i], b, :],
                  reads=[u], writes=[ut[i][j]])
            conv_block(P, "dve", ut[i][j], acc[i], cv[i][j], cw[i], None, nps[i], bias=False)
        gens = [pre(2 * b, 0, j, sets[0], bankset[0], hands[0][j]), pre(2 * b + 1, 1, j, sets[1], bankset[1], hands[1][j])]
        if pending:
            gens.append(chain(pending))
        roundrobin(gens)
        pending = [rec(2 * b, hands[0][j]), rec(2 * b + 1, hands[1][j])]
    roundrobin([chain(pending)])
    P.finish([oout])
    return nc


def to_cm(a):
    T, Cc = a.shape
    return a.reshape(T // GRID_W, GRID_W, Cc).transpose(1, 0, 2).reshape(T, Cc)


def from_cm(a):
    T, Cc = a.shape
    return a.reshape(GRID_W, T // GRID_W, Cc).transpose(1, 0, 2).reshape(T, Cc)


def prep_k2g(pl, pc, conv_w, a_log, dt_bias, core):
    s, d, g = core // 4, (core // 2) % 2, core % 2
    q0 = 1548
    chans = np.concatenate([a * 384 + g * 192 + np.arange(192) for a in range(3)])
    acol = 3084 + d * 6 + g * 3
    bcol = 3096 + d * 6 + g * 3
    segs, ars, brs = [], [], []
    for arr, cm in ((pc[s], False), (pl[s], True)):
        a = arr[:, q0 + chans]
        ar = arr[:, acol:acol + 3]
        br = arr[:, bcol:bcol + 3]
        if cm:
            a, ar, br = to_cm(a), to_cm(ar), to_cm(br)
        if d == 1:
            a, ar, br = a[::-1], ar[::-1], br[::-1]
        segs.append(windows(np.ascontiguousarray(a.T)))
        ars.append(ar)
        brs.append(br)
    cw = conv_w[:, chans].T
    if d == 1:
        cw = cw[:, ::-1]
    tm = lambda lst: np.ascontiguousarray(np.concatenate(lst, 0).reshape(NCH, 128, 3).transpose(1, 0, 2))
    m = {"u": np.ascontiguousarray(np.concatenate(segs, axis=1)), "cw": np.ascontiguousarray(cw),
         "araw": tm(ars), "braw": tm(brs), "dtb": np.ascontiguousarray(dt_bias[d, g * 3:g * 3 + 3]),
         "alog": np.ascontiguousarray(a_log[d, g * 3:g * 3 + 3])}
    m.update(consts_g())
    return m


PKW = 1024 + 256 + 7 * 384
POOL_WINDOWS = (2, 4, 8, 16)


def build_k3(NT, n_lat=None):
    if n_lat is None:
        n_lat = NT - 1
    nc = bass.Bass("TRN2", target_bir_lowering=False)
    P = Prog(nc)
    pk = P.dram("pk", [NT, 128, PKW], F32, kind="ExternalInput")
    halo = P.dram("halo", [NT, 16, 256], F32, kind="ExternalInput")
    band = P.dram("band", [NT, 144, 512], F32, kind="ExternalInput")
    wout = P.dram("wout", [D, D], F32, kind="ExternalInput")
    pwd = P.dram("pw", [64, 4, 128], F32, kind="ExternalInput")
    psd = P.dram("pscale", [128, 2], F32, kind="ExternalInput")
    rwd = P.dram("rw", [128, 8, 16], F32, kind="ExternalInput")
    idn = P.dram("idn", [128, 128], F32, kind="ExternalInput")
    v384 = {n: P.dram(n, [384], F32, kind="ExternalInput") for n in ("dvec", "sng", "gng")}
    v1k = {n: P.dram(n, [D], F32, kind="ExternalInput") for n in ("g1_l", "g1_c", "n2g", "sh_l", "sc_l", "sh_c", "sc_c")}
    x2o = P.dram("x2", [NT, 128, D], F32, kind="ExternalOutput")
    hTo = P.dram("h2T", [NT, 128, 8, 128], F32, kind="ExternalOutput")
    affo = P.dram("aff", [NT, 128, 16], F32, kind="ExternalOutput")

    ident = P.sb([128, 128], name="ident")
    P.dma("sync", ident[:], idn.t.ap(), reads=[idn], writes=[ident])
    pw = P.sb([64, 4, 128], name="pw")
    P.dma("act", pw[:], pwd.t.ap(), reads=[pwd], writes=[pw])
    psc = P.sb([128, 2], name="psc")
    P.dma("sync", psc[:], psd.t.ap(), reads=[psd], writes=[psc])
    rw = P.sb([128, 8, 16], name="rw")
    P.dma("act", rw[:], rwd.t.ap(), reads=[rwd], writes=[rw])
    b384 = {n: load_bcast(P, ("sync", "act")[i % 2], v384[n], 384, "b_" + n) for i, n in enumerate(v384)}
    b1k = {n: load_bcast(P, ("sync", "act")[i % 2], v1k[n], D, "b_" + n) for i, n in enumerate(v1k)}
    for n in ("sc_l", "sc_c"):
        A = b1k[n]
        P.stt("dve", A, A[:], A, A[:], 1.0, b1k["n2g"], b1k["n2g"][:], ALU.add, ALU.mult)
    W = P.sb([128, 8, D], BF16, name="W")
    stage = [P.sb([128, D], name="stg%d" % i) for i in range(2)]
    load_weight_bf16(P, lambda k: wout.t.ap()[k * 128:(k + 1) * 128, :], W, 8, D, stage, wout)

    def mkset(n):
        d = {}
        for nm, shp, dt_ in (("pk", [128, PKW], F32), ("hl", [16, 256], F32), ("bd", [128, 512], F32), ("bd2", [16, 512], F32),
                             ("dT", [64, 4, 128], F32), ("mT", [128, 8, 128], BF16), ("t1", [128, 384], F32), ("ys", [128, 384], F32),
                             ("sz", [128, 384], F32), ("scr3", [128, 384], F32), ("ss1", [128, 1], F32), ("yo", [128, 768], F32),
                             ("og", [128, 384], F32), ("sq", [128, 384], F32), ("rs6", [128, 6], F32), ("sg", [128, 384], F32),
                             ("mx", [128, 512], F32), ("x2", [128, D], F32), ("h2", [128, D], F32), ("scr", [128, D], F32),
                             ("ss2", [128, 1], F32), ("h2T", [128, 8, 128], F32), ("lg", [128, 16], F32), ("ex", [128, 16], F32),
                             ("m1", [128, 1], F32), ("s1", [128, 1], F32), ("af", [128, 16], F32)):
            d[nm] = P.sb(shp, dt_, name="%s_%d" % (nm, n))
        d["bA"], d["bT"] = P.ps([1], name="bA%d" % n), P.ps([1], name="bT%d" % n)
        d["pms"] = [P.ps([1], name="bM%d_%d" % (i, n)) for i in range(2)]
        return d

    sets = [mkset(0), mkset(1)]

    def tile_gen(t, st):
        lat = t < n_lat
        pk_, hl, bd, bd2, mT, dT = st["pk"], st["hl"], st["bd"], st["bd2"], st["mT"], st["dT"]
        t1, ys, sz, scr3, ss1, yo, og, sq, rs6, sg, mx = [st[k] for k in ("t1", "ys", "sz", "scr3", "ss1", "yo", "og", "sq", "rs6", "sg", "mx")]
        pD = pP = pr = st["bA"]
        pts = [st["bT"]]
        pms = st["pms"]
        P.dma("sync", pk_[:], pk.t.ap()[t], reads=[pk], writes=[pk_])
        P.dma("act", hl[:], halo.t.ap()[t], reads=[halo], writes=[hl])
        P.dma("act", bd[:], band.t.ap()[t, 0:128, :], reads=[band], writes=[bd])
        P.dma("act", bd2[:], band.t.ap()[t, 128:144, :], reads=[band], writes=[bd2])
        yield
        xo, uo = 0, 1024
        yf, yb, xs, z, of, ob, gt = [slice(1280 + i * 384, 1280 + (i + 1) * 384) for i in range(7)]
        for g in range(4):
            P.mm(pD, pD[0:64, g * 128:(g + 1) * 128], pk_, pk_[:, uo + g * 64:uo + (g + 1) * 64], bd, bd[:, g * 128:(g + 1) * 128],
                 start=True, stop=False)
            P.mm(pD, pD[0:64, g * 128:(g + 1) * 128], hl, hl[:, g * 64:(g + 1) * 64], bd2, bd2[:, g * 128:(g + 1) * 128],
                 start=False, stop=True)
        P.tt("pool", ys, ys[:], pk_, pk_[:, yf], pk_, pk_[:, yb], ALU.add)
        P.tt("pool", t1, t1[:], pk_, pk_[:, xs], b384["dvec"], b384["dvec"][:], ALU.mult)
        P.act(sz, sz[:], pk_, pk_[:, z], AF.Silu)
        yield
        P.cp("act", dT, dT[:].rearrange("p g t -> p (g t)"), pD, pD[0:64, :])
        P.tt("pool", ys, ys[:], ys, ys[:], t1, t1[:], ALU.add)
        yield
        for cch in range(2):
            for gg in range(2):
                g = cch * 2 + gg
                P.mm(pP, pP[:, cch * 128:(cch + 1) * 128], pw, pw[:, g, :], dT, dT[:, g, :], start=(gg == 0), stop=(gg == 1))
        P.tt("dve", ys, ys[:], ys, ys[:], sz, sz[:], ALU.mult)
        yield
        for cch in range(2):
            P.ts("dve", mT, mT[:, cch, :], pP, pP[:, cch * 128:(cch + 1) * 128], psc[:, cch:cch + 1], None, ALU.mult, rd=[psc])
        P.act(scr3, scr3[:], ys, ys[:], AF.Square, scale=float(384 ** -0.5), accum=ss1[:], wr=[ss1])
        P.tt("pool", og, og[:], pk_, pk_[:, of], pk_, pk_[:, ob], ALU.add)
        P.tt("pool", sq, sq[:], og, og[:], og, og[:], ALU.mult)
        yield
        P.ts("dve", ss1, ss1[:], ss1, ss1[:], 1e-6, None, ALU.add)
        P.op("dve", lambda e: e.reduce_sum(out=rs6[:], in_=sq[:].rearrange("p (a d) -> p a d", d=64), axis=AX.X),
             reads=[sq], writes=[rs6])
        P.ts("dve", rs6, rs6[:], rs6, rs6[:], 1.0 / 64.0, 1e-6, ALU.mult, ALU.add)
        yield
        P.op("act", lambda e: e.sqrt(out=ss1[:], in_=ss1[:]), reads=[ss1], writes=[ss1])
        P.op("act", lambda e: e.sqrt(out=rs6[:], in_=rs6[:]), reads=[rs6], writes=[rs6])
        P.act(sg, sg[:], pk_, pk_[:, gt], AF.Silu)
        yield
        P.op("dve", lambda e: e.reciprocal(out=ss1[:], in_=ss1[:]), reads=[ss1], writes=[ss1])
        P.op("dve", lambda e: e.reciprocal(out=rs6[:], in_=rs6[:]), reads=[rs6], writes=[rs6])
        yield
        P.stt("dve", yo, yo[:, 0:384], ys, ys[:], ss1[:, 0:1], b384["sng"], b384["sng"][:], ALU.mult, ALU.mult, rd=[ss1])
        for a in range(6):
            P.ts(("dve", "pool")[a % 2], og, og[:, a * 64:(a + 1) * 64], og, og[:, a * 64:(a + 1) * 64], rs6[:, a:a + 1], None, ALU.mult, rd=[rs6])
        yield
        P.tt("pool", og, og[:], og, og[:], b384["gng"], b384["gng"][:], ALU.mult)
        yield
        P.tt("dve", yo, yo[:, 384:768], og, og[:], sg, sg[:], ALU.mult)
        yield
        for half in range(2):
            pt = pts[0]
            for q in range(3):
                k = half * 3 + q
                P.tr(pt, pt[:, q * 128:(q + 1) * 128], yo, yo[:, k * 128:(k + 1) * 128], ident, ident[:])
            yield
            P.cp(("act", "dve")[half], mT, mT[:, 2 + half * 3:5 + half * 3, :], pt, pt[:, 0:384].rearrange("p (k t) -> p k t", k=3))
            yield
        x2 = st["x2"]
        g1 = b1k["g1_l"] if lat else b1k["g1_c"]
        for cb in range(2):
            pm = pms[cb]
            cs = slice(cb * 512, (cb + 1) * 512)
            for k in range(8):
                P.mm(pm, pm[:, :], mT, mT[:, k, :], W, W[:, k, cs], start=(k == 0), stop=(k == 7))
            yield
            P.tt("dve", mx, mx[:], pm, pm[:, :], g1, g1[:, cs], ALU.mult)
            yield
            P.tt("pool", x2, x2[:, cs], mx, mx[:], pk_, pk_[:, cb * 512:(cb + 1) * 512], ALU.add)
            yield
        P.dma("sync", x2o.t.ap()[t], x2[:], reads=[x2], writes=[x2o])
        h2, h2T, scr, ss2 = st["h2"], st["h2T"], st["scr"], st["ss2"]
        lg, ex, m1, s1, af = st["lg"], st["ex"], st["m1"], st["s1"], st["af"]
        A2, B2 = (b1k["sc_l"], b1k["sh_l"]) if lat else (b1k["sc_c"], b1k["sh_c"])
        P.act(scr, scr[:], x2, x2[:], AF.Square, scale=1.0 / 32.0, accum=ss2[:], wr=[ss2])
        yield
        P.ts("dve", ss2, ss2[:], ss2, ss2[:], 1e-6, None, ALU.add)
        yield
        P.op("act", lambda e: e.sqrt(out=ss2[:], in_=ss2[:]), reads=[ss2], writes=[ss2])
        yield
        P.op("dve", lambda e: e.reciprocal(out=ss2[:], in_=ss2[:]), reads=[ss2], writes=[ss2])
        P.stt("dve", h2, h2[:], x2, x2[:], ss2[:, 0:1], A2, A2[:], ALU.mult, ALU.mult, rd=[ss2])
        yield
        P.tt("pool", h2, h2[:], h2, h2[:], B2, B2[:], ALU.add)
        yield
        for half in range(2):
            pt = pts[0]
            for q in range(4):
                k = half * 4 + q
                P.tr(pt, pt[:, q * 128:(q + 1) * 128], h2, h2[:, k * 128:(k + 1) * 128], ident, ident[:])
            yield
            P.cp(("act", "dve")[half], h2T, h2T[:, half * 4:half * 4 + 4, :], pt, pt[:, 0:512].rearrange("p (k t) -> p k t", k=4))
            yield
        P.dma("sync", hTo.t.ap()[t], h2T[:], reads=[h2T], writes=[hTo])
        for k in range(8):
            P.mm(pr, pr[:, 0:16], h2T, h2T[:, k, :], rw, rw[:, k, :], start=(k == 0), stop=(k == 7))
        yield
        P.cp("act", lg, lg[:], pr, pr[:, 0:16])
        yield
        P.op("dve", lambda e: e.reduce_max(out=m1[:], in_=lg[:], axis=AX.X), reads=[lg], writes=[m1])
        P.ts("dve", m1, m1[:], m1, m1[:], -1.0, None, ALU.mult)
        yield
        P.act(ex, ex[:], lg, lg[:], AF.Exp, bias=m1[:, 0:1], accum=s1[:], rd=[m1], wr=[s1])
        yield
        P.op("dve", lambda e: e.reciprocal(out=s1[:], in_=s1[:]), reads=[s1], writes=[s1])
        P.ts("dve", af, af[:], ex, ex[:], s1[:, 0:1], None, ALU.mult, rd=[s1])
        P.dma("sync", affo.t.ap()[t], af[:], reads=[af], writes=[affo])
        yield

    def roundrobin(gens):
        gens = list(gens)
        while gens:
            for g in list(gens):
                try:
                    next(g)
                except StopIteration:
                    gens.remove(g)

    for t in range(0, NT, 2):
        gens = [tile_gen(t, sets[0])]
        if t + 1 < NT:
            gens.append(tile_gen(t + 1, sets[1]))
        roundrobin(gens)
    P.finish([x2o, hTo, affo])
    return nc


def pool_band(pos, T):
    src = np.concatenate([pos, pos[0] - 8 + np.arange(8), pos[-1] + 1 + np.arange(8)])
    out = np.zeros((144, 4, 128), np.float32)
    for g, w in enumerate(POOL_WINDOWS):
        lo = np.clip(pos - w // 2, 0, T)
        hi = np.clip(pos + w // 2, 0, T)
        cnt = np.maximum(hi - lo, 1).astype(np.float32)
        m = (src[:, None] >= lo[None, :]) & (src[:, None] < hi[None, :])
        out[:, g, :] = m / cnt[None, :]
        out[np.arange(128), g, np.arange(128)] -= 1.0
    return out.reshape(144, 512)


NE = 16
FF = 512


def bisect_threshold(P, affs, J, kcap, ones, pcnt, name):
    lo = P.sb([128, NE], name=name + "_lo")
    hi = P.sb([128, NE], name=name + "_hi")
    mid = P.sb([128, NE], name=name + "_mid")
    cnt = P.sb([128, NE], name=name + "_cnt")
    pred = P.sb([128, NE], name=name + "_pred")
    tmp = P.sb([128, NE], name=name + "_tmp")
    cmp_ = P.sb([128, NE, J], name=name + "_cmp")
    P.op("dve", lambda e: e.memset(lo[:], 0.0), writes=[lo])
    P.op("dve", lambda e: e.memset(hi[:], 1.0), writes=[hi])
    for it in range(34):
        P.tt("dve", mid, mid[:], lo, lo[:], hi, hi[:], ALU.add)
        P.ts("dve", mid, mid[:], mid, mid[:], 0.5, None, ALU.mult)
        for e_ in range(NE):
            P.ts(("dve", "pool")[e_ % 2], cmp_, cmp_[:, e_, :], affs, affs[:, e_, :], mid[:, e_:e_ + 1], None, ALU.is_ge, rd=[mid])
        P.op("dve", lambda e: e.reduce_sum(out=cnt[:], in_=cmp_[:], axis=AX.X), reads=[cmp_], writes=[cnt])
        P.mm(pcnt, pcnt[:, 0:NE], ones, ones[:], cnt, cnt[:])
        P.ts("dve", pred, pred[:], pcnt, pcnt[:, 0:NE], float(kcap) - 0.5, None, ALU.is_ge)
        P.tt("dve", tmp, tmp[:], mid, mid[:], lo, lo[:], ALU.subtract)
        P.tt("dve", tmp, tmp[:], tmp, tmp[:], pred, pred[:], ALU.mult)
        P.tt("dve", lo, lo[:], lo, lo[:], tmp, tmp[:], ALU.add)
        P.tt("dve", tmp, tmp[:], hi, hi[:], mid, mid[:], ALU.subtract)
        P.tt("dve", tmp, tmp[:], tmp, tmp[:], pred, pred[:], ALU.mult)
        P.tt("dve", hi, hi[:], mid, mid[:], tmp, tmp[:], ALU.add)
    return lo


def build_k4(NT, n_lat, kcap_lat, kcap_ctx, J_lat, J_ctx, final_norm, passes):
    nc = bass.Bass("TRN2", target_bir_lowering=False)
    P = Prog(nc)
    x2d = P.dram("x2", [NT, 128, D], F32, kind="ExternalInput")
    hTd = P.dram("h2T", [NT, 128, 8, 128], F32, kind="ExternalInput")
    afd = P.dram("aff", [NT, 128, NE], F32, kind="ExternalInput")
    asl = P.dram("affs_l", [128, NE, J_lat], F32, kind="ExternalInput")
    asc = P.dram("affs_c", [128, NE, J_ctx], F32, kind="ExternalInput")
    wgd = P.dram("wg", [NE, D, FF], F32, kind="ExternalInput")
    wud = P.dram("wu", [NE, D, FF], F32, kind="ExternalInput")
    wdd = P.dram("wd", [NE, FF, D], F32, kind="ExternalInput")
    onesd = P.dram("ones", [128, 128], F32, kind="ExternalInput")
    v1k = {n: P.dram(n, [D], F32, kind="ExternalInput") for n in ("g2_l", "g2_c", "fng")}
    outd = P.dram("out", [NT, 128, D], F32, kind="ExternalOutput")

    ones = P.sb([128, 128], name="ones")
    P.dma("sync", ones[:], onesd.t.ap(), reads=[onesd], writes=[ones])
    b1k = {n: load_bcast(P, ("sync", "act")[i % 2], v1k[n], D, "b_" + n) for i, n in enumerate(v1k)}
    pcnt = P.ps([1], name="pcnt")
    affs_l = P.sb([128, NE, J_lat], name="affs_l")
    P.dma("sync", affs_l[:], asl.t.ap(), reads=[asl], writes=[affs_l])
    thr_l = bisect_threshold(P, affs_l, J_lat, kcap_lat, ones, pcnt, "bl")
    thr_c = None
    if n_lat < NT:
        affs_c = P.sb([128, NE, J_ctx], name="affs_c")
        P.dma("act", affs_c[:], asc.t.ap(), reads=[asc], writes=[affs_c])
        thr_c = bisect_threshold(P, affs_c, J_ctx, kcap_ctx, ones, pcnt, "bc")
    afo = P.sb([128, NT, NE], name="afo")
    gw = P.sb([128, NT, NE], name="gw")
    P.dma("sync", afo[:], afd.t.ap().rearrange("t p e -> p t e"), reads=[afd], writes=[afo])
    for t in range(NT):
        thr = thr_l if t < n_lat else thr_c
        P.tt("dve", gw, gw[:, t, :], afo, afo[:, t, :], thr, thr[:], ALU.is_ge)
        P.tt("dve", gw, gw[:, t, :], gw, gw[:, t, :], afo, afo[:, t, :], ALU.mult)

    maxt = max(sum(t1 - t0 for (t0, t1) in ps_) for ps_ in passes)
    hT = P.sb([128, 8, maxt * 128], BF16, name="hT")
    hst = [P.sb([128, 8, 128], name="hst%d" % i) for i in range(2)]
    acc = P.sb([128, maxt, D], name="acc")
    Wg = [P.sb([128, 8, FF], BF16, name="Wg%d" % i) for i in range(2)]
    Wu = [P.sb([128, 8, FF], BF16, name="Wu%d" % i) for i in range(2)]
    Wd = [P.sb([128, 4, D], BF16, name="Wd%d" % i) for i in range(2)]
    stg = [P.sb([128, 2048], name="stg%d" % i) for i in range(2)]
    sil = [P.sb([128, 512], name="sil%d" % i) for i in range(2)]
    hidT = [P.sb([128, 4, 512], BF16, name="hidT%d" % i) for i in range(2)]
    xt = [P.sb([128, D], name="xt%d" % i) for i in range(2)]
    scr = P.sb([128, D], name="scr")
    ssf = P.sb([128, 1], name="ssf")
    pg = [P.ps([1], name="pg%d" % i) for i in range(2)]
    pu = [P.ps([1], name="pu%d" % i) for i in range(2)]
    pd = [P.ps([1], name="pd%d" % i) for i in range(2)]
    sti = 0
    wi = 0
    gi = 0
    for ps_ in passes:
        tiles = [t for (t0, t1) in ps_ for t in range(t0, t1)]
        loc = {t: i for i, t in enumerate(tiles)}
        for i, t in enumerate(tiles):
            h_ = hst[i % 2]
            P.dma(("sync", "act")[i % 2], h_[:], hTd.t.ap()[t], reads=[hTd], writes=[h_])
            P.cp(("pool", "act")[i % 2], hT, hT[:, :, i * 128:(i + 1) * 128], h_, h_[:])
        P.op("pool", lambda e: e.memset(acc[:], 0.0), writes=[acc])
        def load_w(ex):
            nonlocal sti
            wg_, wu_, wd_ = Wg[ex % 2], Wu[ex % 2], Wd[ex % 2]
            for (wt, src, nk) in ((wg_, wgd, 8), (wu_, wud, 8), (wd_, wdd, 4)):
                for hf in range(2):
                    s_ = stg[sti % 2]
                    sti += 1
                    k0, k1 = hf * nk // 2, (hf + 1) * nk // 2
                    sv = s_[:].rearrange("p (k f) -> p k f", k=nk // 2)
                    P.dma(("sync", "act")[sti % 2], sv, src.t.ap()[ex].rearrange("(k p) f -> p k f", p=128)[:, k0:k1, :],
                          reads=[src], writes=[s_])
                    P.cp(("pool", "act", "dve")[sti % 3], wt, wt[:, k0:k1, :], s_, sv)

        def gateup(ex, t0, t1, hd):
            wg_, wu_ = Wg[ex % 2], Wu[ex % 2]
            n = (t1 - t0) * 128
            c0 = loc[t0] * 128
            for fc in range(4):
                pg_, pu_, sl_ = pg[fc % 2], pu[fc % 2], sil[fc % 2]
                for k in range(8):
                    P.mm(pg_, pg_[:, 0:n], wg_, wg_[:, k, fc * 128:(fc + 1) * 128], hT, hT[:, k, c0:c0 + n], start=(k == 0), stop=(k == 7))
                for k in range(8):
                    P.mm(pu_, pu_[:, 0:n], wu_, wu_[:, k, fc * 128:(fc + 1) * 128], hT, hT[:, k, c0:c0 + n], start=(k == 0), stop=(k == 7))
                P.act(sl_, sl_[:, 0:n], pg_, pg_[:, 0:n], AF.Silu)
                P.tt("dve", hd, hd[:, fc, 0:n], pu_, pu_[:, 0:n], sl_, sl_[:, 0:n], ALU.mult)
                yield

        def down(ex, t0, t1, hd):
            wd_ = Wd[ex % 2]
            for t in range(t0, t1):
                i = loc[t]
                tl = slice((t - t0) * 128, (t - t0 + 1) * 128)
                for half in range(2):
                    pd_ = pd[half]
                    cs = slice(half * 512, (half + 1) * 512)
                    for fc in range(4):
                        P.mm(pd_, pd_[:, :], hd, hd[:, fc, tl], wd_, wd_[:, fc, cs], start=(fc == 0), stop=(fc == 3))
                    P.stt("dve", acc, acc[:, i, cs], pd_, pd_[:, :], gw[:, t, ex:ex + 1], acc, acc[:, i, cs], ALU.mult, ALU.add, rd=[gw])
                yield

        def roundrobin(gens):
            gens = list(gens)
            while gens:
                for g in list(gens):
                    try:
                        next(g)
                    except StopIteration:
                        gens.remove(g)

        items = [(ex, t0, t1, gidx) for ex in range(NE) for gidx, (t0, t1) in enumerate(ps_)]
        pf = min(1, len(ps_) - 1)
        prev = None
        load_w(0)
        for (ex, t0, t1, gidx) in items:
            if gidx == pf and ex + 1 < NE:
                load_w(ex + 1)
            hd = hidT[gi % 2]
            gi += 1
            gens = [gateup(ex, t0, t1, hd)]
            if prev is not None:
                gens.append(down(*prev))
            roundrobin(gens)
            prev = (ex, t0, t1, hd)
        roundrobin([down(*prev)])
        for t in tiles:
            i = loc[t]
            x_ = xt[t % 2]
            g2 = b1k["g2_l"] if t < n_lat else b1k["g2_c"]
            P.dma("act", x_[:], x2d.t.ap()[t], reads=[x2d], writes=[x_])
            P.tt("pool", acc, acc[:, i, :], acc, acc[:, i, :], g2, g2[:], ALU.mult)
            P.tt("pool", x_, x_[:], x_, x_[:], acc, acc[:, i, :], ALU.add)
            if final_norm:
                P.act(scr, scr[:], x_, x_[:], AF.Square, scale=1.0 / 32.0, accum=ssf[:], wr=[ssf])
                P.ts("dve", ssf, ssf[:], ssf, ssf[:], 1e-6, None, ALU.add)
                P.op("act", lambda e: e.sqrt(out=ssf[:], in_=ssf[:]), reads=[ssf], writes=[ssf])
                P.op("dve", lambda e: e.reciprocal(out=ssf[:], in_=ssf[:]), reads=[ssf], writes=[ssf])
                P.stt("dve", x_, x_[:], x_, x_[:], ssf[:, 0:1], b1k["fng"], b1k["fng"][:], ALU.mult, ALU.mult, rd=[ssf])
            P.dma("sync", outd.t.ap()[t], x_[:], reads=[x_], writes=[outd])
    P.finish([outd])
    return nc


B, T, CTX = 2, 16384, 256
NLT = 32


def tiles_of(lat, ctx, core, with_ctx=True):
    s, q = core // 4, core % 4
    Cc = lat.shape[-1]
    lt = lat[s, q * 4096:(q + 1) * 4096].reshape(NLT, 128, Cc)
    if not with_ctx:
        return np.ascontiguousarray(lt)
    ct = np.zeros((1, 128, Cc), np.float32)
    n = min(128, CTX - q * 64)
    ct[0, :n] = ctx[s, q * 64:q * 64 + n]
    return np.concatenate([lt, ct], 0)


def untile(res, key, Cc, with_ctx=True):
    lat = np.zeros((B, T, Cc), np.float32)
    ctx = np.zeros((B, CTX, Cc), np.float32)
    for core in range(NCORE):
        s, q = core // 4, core % 4
        r = res[core][key]
        lat[s, q * 4096:(q + 1) * 4096] = r[:NLT].reshape(4096, Cc)
        if with_ctx:
            ctx[s, q * 64:(q + 1) * 64] = r[NLT, :64]
    return lat, ctx


def unseq(y, d, cm):
    y = y.reshape(NCH * 128, -1)
    c_, l_ = y[:CTX], y[CTX:]
    if d == 1:
        c_, l_ = c_[::-1], l_[::-1]
    if cm:
        l_ = from_cm(l_)
    return c_, l_


_band_cache = {}


def band_for(pos0, Tseq):
    key = (pos0 if (pos0 == 0 or pos0 + 128 + 8 > Tseq) else -1, Tseq)
    if key not in _band_cache:
        _band_cache[key] = pool_band(pos0 + np.arange(128), Tseq)
    return _band_cache[key]


def halo_for(u, pos0):
    Tseq = u.shape[0]
    h = np.zeros((16, 256), np.float32)
    for i in range(8):
        a = pos0 - 8 + i
        if 0 <= a < Tseq:
            h[i] = u[a]
        b_ = pos0 + 128 + i
        if 0 <= b_ < Tseq:
            h[8 + i] = u[b_]
    return h


def stage_proj(l, x, ctx, mods, p):
    f32 = _f32
    m = mods[l]
    seg = lambda r, i: f32(m[r, i * 1024:(i + 1) * 1024])
    idn = np.eye(128, dtype=np.float32)
    in_maps = []
    for core in range(NCORE):
        s = core // 4
        in_maps.append({"xt": tiles_of(x, ctx, core), "w": f32(p["w_in"][l]), "g": f32(p["norm1_g"][l]),
                        "sh_l": seg(s, 0), "sc_l": seg(s, 1), "sh_c": seg(2, 0), "sc_c": seg(2, 1), "idn": idn})
    res = run(build_k1(NLT + 1), in_maps)
    return untile(res, "out", 3108)


def stage_scans(l, pl, pc, p):
    f32 = _f32
    res = run(build_k2s(), [prep_k2s(pl, pc, f32(p["ssd_conv_w"][l]), f32(p["ssd_conv_b"][l]), f32(p["ssd_a_log"][l]),
                                     f32(p["ssd_dt_bias"][l]), core) for core in range(NCORE)])
    ys_l = np.zeros((B, 2, T, 384), np.float32)
    ys_c = np.zeros((B, 2, CTX, 384), np.float32)
    xs_l = np.zeros((B, T, 384), np.float32)
    xs_c = np.zeros((B, CTX, 384), np.float32)
    for core in range(NCORE):
        s, d, g = core // 4, (core // 2) % 2, core % 2
        c_, l_ = unseq(res[core]["y"], d, False)
        ys_l[s, d, :, g * 192:(g + 1) * 192] = l_
        ys_c[s, d, :, g * 192:(g + 1) * 192] = c_
        if d == 0:
            c_, l_ = unseq(res[core]["xs"], 0, False)
            xs_l[s, :, g * 192:(g + 1) * 192] = l_
            xs_c[s, :, g * 192:(g + 1) * 192] = c_
    del res
    res = run(build_k2g(), [prep_k2g(pl, pc, f32(p["gdn_conv_w"][l]), f32(p["gdn_a_log"][l]), f32(p["gdn_dt_bias"][l]), core)
                            for core in range(NCORE)])
    os_l = np.zeros((B, 2, T, 384), np.float32)
    os_c = np.zeros((B, 2, CTX, 384), np.float32)
    for core in range(NCORE):
        s, d, g = core // 4, (core // 2) % 2, core % 2
        c_, l_ = unseq(res[core]["o"], d, True)
        os_l[s, d, :, g * 192:(g + 1) * 192] = l_
        os_c[s, d, :, g * 192:(g + 1) * 192] = c_
    return ys_l, ys_c, xs_l, xs_c, os_l, os_c


def stage_post(l, x, ctx, pl, pc, scans, mods, p):
    f32 = _f32
    ys_l, ys_c, xs_l, xs_c, os_l, os_c = scans
    m = mods[l]
    seg = lambda r, i: f32(m[r, i * 1024:(i + 1) * 1024])
    idn = np.eye(128, dtype=np.float32)
    pwp = np.zeros((64, 4, 128), np.float32)
    for g in range(4):
        pwp[:, g, (g % 2) * 64:(g % 2) * 64 + 64] = p["pool_w"][l][g]
    common = {"wout": f32(p["w_out"][l]), "pw": pwp, "pscale": f32(np.asarray(p["pool_scale"][l]).reshape(2, 128).T),
              "rw": f32(np.asarray(p["router_w"][l]).reshape(8, 128, 16).transpose(1, 0, 2)), "idn": idn,
              "dvec": f32(np.repeat(np.asarray(p["ssd_d"][l]), 64)), "sng": f32(p["ssd_norm_g"][l]),
              "gng": f32(np.tile(np.asarray(p["gdn_norm_g"][l]), 6)), "n2g": f32(p["norm2_g"][l])}
    in_maps = []
    for core in range(NCORE):
        s, q = core // 4, core % 4
        ls = slice(q * 4096, (q + 1) * 4096)
        lat_p = np.concatenate([x[s, ls], pl[s, ls, 0:256], ys_l[s, 0, ls], ys_l[s, 1, ls], xs_l[s, ls], pl[s, ls, 256:640],
                                os_l[s, 0, ls], os_l[s, 1, ls], pl[s, ls, 2700:3084]], axis=-1).reshape(NLT, 128, PKW)
        n = min(128, CTX - q * 64)
        cs_ = slice(q * 64, q * 64 + n)
        ctx_p = np.zeros((1, 128, PKW), np.float32)
        ctx_p[0, :n] = np.concatenate([ctx[s, cs_], pc[s, cs_, 0:256], ys_c[s, 0, cs_], ys_c[s, 1, cs_], xs_c[s, cs_],
                                       pc[s, cs_, 256:640], os_c[s, 0, cs_], os_c[s, 1, cs_], pc[s, cs_, 2700:3084]], axis=-1)
        halo = np.stack([halo_for(pl[s, :, 0:256], q * 4096 + i * 128) for i in range(NLT)] + [halo_for(pc[s, :, 0:256], q * 64)])
        band = np.stack([band_for(q * 4096 + i * 128, T) for i in range(NLT)] + [band_for(q * 64, CTX)])
        d_ = {"pk": np.ascontiguousarray(np.concatenate([lat_p, ctx_p], 0)), "halo": f32(halo), "band": f32(band),
              "g1_l": seg(s, 2), "g1_c": seg(2, 2), "sh_l": seg(s, 3), "sc_l": seg(s, 4), "sh_c": seg(2, 3), "sc_c": seg(2, 4)}
        d_.update(common)
        in_maps.append(d_)
    return run(build_k3(NLT + 1), in_maps)


def stage_moe(l, res3, mods, p, last):
    f32 = _f32
    m = mods[l]
    seg = lambda r, i: f32(m[r, i * 1024:(i + 1) * 1024])
    ones = np.ones((128, 128), np.float32)
    aff_l, aff_c = untile(res3, "aff", 16)
    with_ctx = not last
    NT = NLT + 1 if with_ctx else NLT
    passes = [[(q * 8, q * 8 + 4), (q * 8 + 4, q * 8 + 8)] for q in range(4)]
    if with_ctx:
        passes[3].append((NLT, NLT + 1))
    in_maps = []
    for core in range(NCORE):
        s = core // 4
        in_maps.append({"x2": f32(res3[core]["x2"][:NT]), "h2T": f32(res3[core]["h2T"][:NT]), "aff": f32(res3[core]["aff"][:NT]),
                        "affs_l": f32(aff_l[s].reshape(128, 128, 16).transpose(0, 2, 1)),
                        "affs_c": f32(aff_c[s].reshape(128, 2, 16).transpose(0, 2, 1)),
                        "wg": f32(p["exp_w_gate"][l]), "wu": f32(p["exp_w_up"][l]), "wd": f32(p["exp_w_down"][l]), "ones": ones,
                        "g2_l": seg(s, 5), "g2_c": seg(2, 5), "fng": f32(p["final_norm_g"])})
    res = run(build_k4(NT, NLT, 2 * T // 16, 2 * CTX // 16, 128, 2, last, passes), in_maps)
    return untile(res, "out", 1024, with_ctx=with_ctx)


def _f32(a):
    return np.ascontiguousarray(np.asarray(a, dtype=np.float32))


def kernel(**p):
    x, ctx = _f32(p["x"]), _f32(p["ctx"])
    L = p["ada_w"].shape[0]
    mods = run_k0(_f32(p["c"]), _f32(p["c_ctx"]), _f32(p["ada_w"]), _f32(p["ada_b"]))
    for l in range(L):
        last = l == L - 1
        pl, pc = stage_proj(l, x, ctx, mods, p)
        scans = stage_scans(l, pl, pc, p)
        res3 = stage_post(l, x, ctx, pl, pc, scans, mods, p)
        del scans, pl, pc
        x, ctx_new = stage_moe(l, res3, mods, p, last)
        del res3
        if not last:
            ctx = ctx_new
    return x
```

```python
import contextlib
import numpy as np
import concourse.bass as bass
import concourse.mybir as mybir
from concourse.bass_utils import run_bass_kernel_spmd

F32 = mybir.dt.float32
BF16 = mybir.dt.bfloat16
ALU = mybir.AluOpType
AF = mybir.ActivationFunctionType
AX = mybir.AxisListType


class Buf:
    def __init__(self, t=None, name=""):
        self.t = t
        self.name = name
        self.w = None
        self.r = []
        self.excl = False

    def __getitem__(self, k):
        return self.t[k]


class Prog:
    ENGS = ("sync", "act", "dve", "pool", "pe")

    def __init__(self, nc, n_dma_sems=40):
        self.nc = nc
        self.es = contextlib.ExitStack()
        self.q = {e: [] for e in self.ENGS}
        self.EPOCH = 4000
        self.esem = {}
        self.seq = {e: 0 for e in ("act", "dve", "pool", "pe")}
        self.dsem = [nc.alloc_semaphore("ds_%d" % i) for i in range(n_dma_sems)]
        self.dcnt = [0] * n_dma_sems
        self.dlast = [None] * n_dma_sems
        self.dnext = 0
        self.waited = {e: {} for e in self.ENGS}
        self.nbuf = 0

    def sb(self, shape, dtype=F32, name=None):
        self.nbuf += 1
        name = "s_" + (name or "sb%d" % self.nbuf)
        t = self.es.enter_context(self.nc.sbuf_tensor(name, list(shape), dtype))
        return Buf(t, name)

    def ps(self, shape, dtype=F32, name=None):
        self.nbuf += 1
        name = "p_" + (name or "ps%d" % self.nbuf)
        t = self.es.enter_context(self.nc.psum_tensor(name, [128, 512], F32))
        b = Buf(t, name)
        b.excl = True
        return b

    def dram(self, name, shape, dtype=F32, kind="Internal"):
        t = self.nc.dram_tensor(name, list(shape), dtype, kind=kind)
        return Buf(t, name)

    def _need(self, eng, reads, writes):
        evs = []
        for b in reads:
            if b.w is not None:
                evs.append(b.w)
            if b.excl:
                evs.extend(b.r)
        for b in writes:
            if b.w is not None:
                evs.append(b.w)
            evs.extend(b.r)
        best = {}
        for (k, v) in evs:
            if eng == "pe" and k[0] == "pe":
                continue
            if best.get(k, 0) < v:
                best[k] = v
        out = []
        wd = self.waited[eng]
        for k, v in best.items():
            if wd.get(k, 0) >= v:
                continue
            wd[k] = v
            out.append((k, v))
        return out

    def _mark(self, ev, reads, writes):
        for b in reads:
            b.r.append(ev)
        for b in writes:
            b.w = ev
            b.r = []

    def op(self, eng, fn, reads=(), writes=()):
        waits = self._need(eng, reads, writes)
        ep, v = divmod(self.seq[eng], self.EPOCH)
        self.seq[eng] += 1
        key = (eng, ep)
        if key not in self.esem:
            self.esem[key] = self.nc.alloc_semaphore("es_%s_%d" % key)
        ev = (key, v + 1)
        self._mark(ev, reads, writes)
        self.q[eng].append((waits, fn, (key, 1)))

    def dma(self, queue, out, in_, reads=(), writes=(), **kw):
        if queue == "pool":
            queue = ("sync", "act")[self.dnext % 2]
        i = self.dnext
        self.dnext = (self.dnext + 1) % len(self.dsem)
        waits = self._need(queue, reads, writes)
        key = ("d", i)
        if self.dcnt[i] > 0 and self.waited[queue].get(key, 0) < self.dcnt[i]:
            self.waited[queue][key] = self.dcnt[i]
            waits.append((key, self.dcnt[i]))
        self.dcnt[i] += 16
        ev = (key, self.dcnt[i])
        self._mark(ev, reads, writes)
        self.q[queue].append((waits, lambda e: e.dma_start(out=out, in_=in_, **kw), (key, 16)))
        return ev

    def mm(self, O, o, A, a, B, b, start=True, stop=True):
        self.op("pe", lambda e: e.matmul(o, lhsT=a, rhs=b, start=start, stop=stop), reads=[A, B], writes=[O])

    def tr(self, O, o, A, a, I, i):
        self.op("pe", lambda e: e.transpose(out=o, in_=a, identity=i), reads=[A, I], writes=[O])

    def act(self, O, o, A, a, func, bias=None, scale=1.0, accum=None, rd=(), wr=()):
        kw = {}
        if bias is not None:
            kw["bias"] = bias
        if accum is not None:
            kw["accum_out"] = accum
        self.op("act", lambda e: e.activation(out=o, in_=a, func=func, scale=scale, **kw),
                reads=[A] + list(rd), writes=[O] + list(wr))

    def tt(self, eng, O, o, A, a, B, b, op):
        self.op(eng, lambda e: e.tensor_tensor(out=o, in0=a, in1=b, op=op), reads=[A, B], writes=[O])

    def ts(self, eng, O, o, A, a, s1, s2, op0, op1=None, rd=()):
        if op1 is None:
            self.op(eng, lambda e: e.tensor_scalar(out=o, in0=a, scalar1=s1, scalar2=None, op0=op0),
                    reads=[A] + list(rd), writes=[O])
        else:
            self.op(eng, lambda e: e.tensor_scalar(out=o, in0=a, scalar1=s1, scalar2=s2, op0=op0, op1=op1),
                    reads=[A] + list(rd), writes=[O])

    def stt(self, eng, O, o, A, a, sc, B, b, op0, op1, rd=()):
        self.op(eng, lambda e: e.scalar_tensor_tensor(out=o, in0=a, scalar=sc, in1=b, op0=op0, op1=op1),
                reads=[A, B] + list(rd), writes=[O])

    def cp(self, eng, O, o, A, a):
        if eng == "act":
            self.op("act", lambda e: e.copy(out=o, in_=a), reads=[A], writes=[O])
        else:
            self.op(eng, lambda e: e.tensor_copy(out=o, in_=a), reads=[A], writes=[O])

    def _sem(self, k):
        if k[0] == "d":
            return self.dsem[k[1]]
        return self.esem[k]

    def finish(self, final_bufs):
        evs = []
        for b in final_bufs:
            if b.w is not None:
                evs.append(b.w)
        fw = []
        for (k, v) in evs:
            fw.append((k, v))
        nc = self.nc
        q = self.q
        semf = self._sem

        def replay(e, lst, tail=()):
            for waits, fn, inc in lst:
                for (k, v) in waits:
                    e.wait_ge(semf(k), v)
                ins = fn(e)
                ins.then_inc(semf(inc[0]), inc[1])
            for (k, v) in tail:
                e.wait_ge(semf(k), v)

        with nc.Block() as block:
            @block.sync
            def _(e):
                replay(e, q["sync"], fw)

            @block.scalar
            def _(e):
                replay(e, q["act"])

            @block.vector
            def _(e):
                replay(e, q["dve"])

            @block.gpsimd
            def _(e):
                replay(e, q["pool"])

            @block.tensor
            def _(e):
                replay(e, q["pe"])
        self.es.close()


D = 1024
IN_DIM = 3108
NCORE = 8


def run(nc, in_maps):
    res = run_bass_kernel_spmd(nc, in_maps, core_ids=list(range(NCORE)))
    return res.results


def build_k0():
    nc = bass.Bass("TRN2", target_bir_lowering=False)
    P = Prog(nc)
    cT = P.dram("cT", [128, 8, 3], F32, kind="ExternalInput")
    aw = P.dram("aw", [3, 8, 128, 512], F32, kind="ExternalInput")
    ab = P.dram("ab", [3, 512], F32, kind="ExternalInput")
    out = P.dram("out", [3, 3, 512], F32, kind="ExternalOutput")
    sc = P.sb([128, 8, 3])
    P.dma("sync", sc[:], cT.t.ap(), reads=[cT], writes=[sc])
    P.act(sc, sc[:], sc, sc[:], AF.Silu)
    for j in range(3):
        w = P.sb([128, 8, 512], name="w%d" % j)
        bt = P.sb([3, 512], name="b%d" % j)
        ot = P.sb([3, 512], name="o%d" % j)
        pm = P.ps([3, 512], name="pm%d" % j)
        P.dma(("sync", "act", "pool")[j], w[:], aw.t.ap()[j].rearrange("k p n -> p k n"), reads=[aw], writes=[w])
        P.dma("sync", bt[:], ab.t.ap()[j].partition_broadcast(3), reads=[ab], writes=[bt])
        for k in range(8):
            P.mm(pm, pm[0:3, :], sc, sc[:, k, :], w, w[:, k, :], start=(k == 0), stop=(k == 7))
        P.tt("dve", ot, ot[:], pm, pm[0:3, :], bt, bt[:], ALU.add)
        P.dma("sync", out.t.ap()[j], ot[:], reads=[ot], writes=[out])
    P.finish([out])
    return nc


def run_k0(c, c_ctx, ada_w, ada_b):
    L = ada_w.shape[0]
    cv = np.stack([c[0], c[1], c_ctx], axis=0)
    cT = np.ascontiguousarray(cv.reshape(3, 8, 128).transpose(2, 1, 0))
    nblk = L * 12
    assert nblk == 24
    in_maps = []
    for core in range(NCORE):
        aws, abs_ = [], []
        for j in range(3):
            b = core * 3 + j
            l, cb = divmod(b, 12)
            aws.append(ada_w[l][:, cb * 512:(cb + 1) * 512].reshape(8, 128, 512))
            abs_.append(ada_b[l][cb * 512:(cb + 1) * 512])
        in_maps.append({"cT": cT, "aw": np.ascontiguousarray(np.stack(aws)), "ab": np.ascontiguousarray(np.stack(abs_))})
    res = run(build_k0(), in_maps)
    mods = np.zeros((L, 3, 6144), np.float32)
    for core in range(NCORE):
        for j in range(3):
            b = core * 3 + j
            l, cb = divmod(b, 12)
            mods[l, :, cb * 512:(cb + 1) * 512] = res[core]["out"][j]
    return mods


def load_bcast(P, queue, dram_buf, n, name):
    t = P.sb([128, n], name=name)
    P.dma(queue, t[:], dram_buf.t.ap().partition_broadcast(128), reads=[dram_buf], writes=[t])
    return t


def norm_mod_tile(P, xt, h, A, B, scr, ss, eng2="pool"):
    P.act(scr, scr[:], xt, xt[:], AF.Square, scale=1.0 / 32.0, accum=ss[:], wr=[ss])
    P.ts("dve", ss, ss[:], ss, ss[:], 1e-6, None, ALU.add)
    P.op("act", lambda e: e.sqrt(out=ss[:], in_=ss[:]), reads=[ss], writes=[ss])
    P.op("dve", lambda e: e.reciprocal(out=ss[:], in_=ss[:]), reads=[ss], writes=[ss])
    P.stt("dve", h, h[:], xt, xt[:], ss[:, 0:1], A, A[:], ALU.mult, ALU.mult, rd=[ss])
    P.tt(eng2, h, h[:], h, h[:], B, B[:], ALU.add)


def transpose_tile(P, h, hT, ident, pts, nk=8, evac=("act", "dve")):
    for half in range((nk + 3) // 4):
        pt = pts[half % len(pts)]
        n4 = min(4, nk - half * 4)
        for q in range(n4):
            k = half * 4 + q
            P.tr(pt, pt[:, q * 128:(q + 1) * 128], h, h[:, k * 128:(k + 1) * 128], ident, ident[:])
        P.cp(evac[half % len(evac)], hT, hT[:, half * 4:half * 4 + n4, :],
             pt, pt[:, 0:n4 * 128].rearrange("p (k t) -> p k t", k=n4))


def load_weight_bf16(P, wdram_ap_fn, W, nk, ncols, stage, wdram, colchunk=None):
    qs = ("sync", "act", "pool")
    cs = ("pool", "dve", "act")
    for k in range(nk):
        st = stage[k % len(stage)]
        P.dma(qs[k % 3], st[:, 0:ncols], wdram_ap_fn(k), reads=[wdram], writes=[st])
        P.cp(cs[k % 3], W, W[:, k, :], st, st[:, 0:ncols])


def build_k1(NT, ncols=IN_DIM, n_lat=None):
    if n_lat is None:
        n_lat = NT - 1
    nc = bass.Bass("TRN2", target_bir_lowering=False)
    P = Prog(nc)
    xt = P.dram("xt", [NT, 128, D], F32, kind="ExternalInput")
    w = P.dram("w", [D, ncols], F32, kind="ExternalInput")
    g = P.dram("g", [D], F32, kind="ExternalInput")
    vecs = {n: P.dram(n, [D], F32, kind="ExternalInput") for n in ("sh_l", "sc_l", "sh_c", "sc_c")}
    idn = P.dram("idn", [128, 128], F32, kind="ExternalInput")
    out = P.dram("out", [NT, 128, ncols], F32, kind="ExternalOutput")

    ident = P.sb([128, 128], name="ident")
    P.dma("sync", ident[:], idn.t.ap(), reads=[idn], writes=[ident])
    gb = load_bcast(P, "act", g, D, "gb")
    A_l = load_bcast(P, "pool", vecs["sc_l"], D, "A_l")
    B_l = load_bcast(P, "sync", vecs["sh_l"], D, "B_l")
    A_c = load_bcast(P, "act", vecs["sc_c"], D, "A_c")
    B_c = load_bcast(P, "pool", vecs["sh_c"], D, "B_c")
    for A in (A_l, A_c):
        P.stt("dve", A, A[:], A, A[:], 1.0, gb, gb[:], ALU.add, ALU.mult)

    W = P.sb([128, 8, ncols], BF16, name="W")
    stage = [P.sb([128, ncols], name="stg%d" % i) for i in range(2)]
    load_weight_bf16(P, lambda k: w.t.ap()[k * 128:(k + 1) * 128, :], W, 8, ncols, stage, w)

    xs = [P.sb([128, D], name="x%d" % i) for i in range(2)]
    hs = [P.sb([128, D], name="h%d" % i) for i in range(2)]
    scr = P.sb([128, D], name="scr")
    sss = [P.sb([128, 1], name="ss%d" % i) for i in range(2)]
    hTs = [P.sb([128, 8, 128], BF16, name="hT%d" % i) for i in range(2)]
    outs = [P.sb([128, ncols], name="ot%d" % i) for i in range(2)]
    pts = [P.ps([128, 512], name="pt%d" % i) for i in range(2)]
    pms = [P.ps([128, 512], name="pm%d" % i) for i in range(4)]
    ncb = (ncols + 511) // 512
    scrs = [scr, P.sb([128, D], name="scr_b")]

    def tile_gen(t, n):
        x_, h_, ss_, hT_, o_, sc_ = xs[n], hs[n], sss[n], hTs[n], outs[n], scrs[n]
        pt = pts[n]
        pm2 = pms[2 * n:2 * n + 2]
        P.dma(("sync", "act")[n], x_[:], xt.t.ap()[t], reads=[xt], writes=[x_])
        yield
        A, B = (A_l, B_l) if t < n_lat else (A_c, B_c)
        P.act(sc_, sc_[:], x_, x_[:], AF.Square, scale=1.0 / 32.0, accum=ss_[:], wr=[ss_])
        yield
        P.ts("dve", ss_, ss_[:], ss_, ss_[:], 1e-6, None, ALU.add)
        yield
        P.op("act", lambda e: e.sqrt(out=ss_[:], in_=ss_[:]), reads=[ss_], writes=[ss_])
        yield
        P.op("dve", lambda e: e.reciprocal(out=ss_[:], in_=ss_[:]), reads=[ss_], writes=[ss_])
        P.stt("dve", h_, h_[:], x_, x_[:], ss_[:, 0:1], A, A[:], ALU.mult, ALU.mult, rd=[ss_])
        yield
        P.tt("pool", h_, h_[:], h_, h_[:], B, B[:], ALU.add)
        yield
        for half in range(2):
            for q in range(4):
                k = half * 4 + q
                P.tr(pt, pt[:, q * 128:(q + 1) * 128], h_, h_[:, k * 128:(k + 1) * 128], ident, ident[:])
            yield
            P.cp(("act", "dve")[half], hT_, hT_[:, half * 4:half * 4 + 4, :], pt, pt[:, 0:512].rearrange("p (k t) -> p k t", k=4))
            yield
        for cb in range(ncb):
            c0 = cb * 512
            cw = min(512, ncols - c0)
            pm = pm2[cb % 2]
            for k in range(8):
                P.mm(pm, pm[:, 0:cw], hT_, hT_[:, k, :], W, W[:, k, c0:c0 + cw], start=(k == 0), stop=(k == 7))
            yield
            P.cp(("act", "dve")[cb % 2], o_, o_[:, c0:c0 + cw], pm, pm[:, 0:cw])
            yield
        P.dma("sync", out.t.ap()[t], o_[:], reads=[o_], writes=[out])
        yield

    def roundrobin(gens):
        gens = list(gens)
        while gens:
            for g in list(gens):
                try:
                    next(g)
                except StopIteration:
                    gens.remove(g)

    for t in range(0, NT, 2):
        gens = [tile_gen(t, 0)]
        if t + 1 < NT:
            gens.append(tile_gen(t + 1, 1))
        roundrobin(gens)
    P.finish([out])
    return nc


NCH = 130
NBLK = 65


def consts():
    t = np.arange(128)
    U = (t[:, None] <= t[None, :]).astype(np.float32)
    NEG = np.where(t[None, :] < t[:, None], -30000.0, 0.0).astype(np.float32)
    return {"idn": np.eye(128, dtype=np.float32), "U": U, "NEG": NEG, "ones": np.ones((128, 128), np.float32)}


def load_consts(P, names=("idn", "U", "NEG", "ones")):
    out = {}
    for i, n in enumerate(names):
        d = P.dram(n, [128, 128], F32, kind="ExternalInput")
        s = P.sb([128, 128], name="c_" + n)
        P.dma(("sync", "act", "pool")[i % 3], s[:], d.t.ap(), reads=[d], writes=[s])
        out[n] = s
    return out


def conv_block(P, eng, ut, acc, cv, cw, cb, npart, n=256, bias=True):
    sl = slice(0, npart)
    P.ts(eng, acc, acc[sl, 0:n], ut, ut[sl, 0:n], cw[sl, 0:1], None, ALU.mult, rd=[cw])
    for k in range(1, 5):
        P.stt(eng, acc, acc[sl, 0:n], ut, ut[sl, k:k + n], cw[sl, k:k + 1], acc, acc[sl, 0:n], ALU.mult, ALU.add, rd=[cw])
    if bias:
        P.act(cv, cv[sl, 0:n], acc, acc[sl, 0:n], AF.Silu, bias=cb[sl, 0:1], rd=[cb])
    else:
        P.act(cv, cv[sl, 0:n], acc, acc[sl, 0:n], AF.Silu)


def softplus_inplace(P, t, ap):
    P.act(t, ap, t, ap, AF.Exp)
    P.ts("dve", t, ap, t, ap, 1.0, None, ALU.add)
    P.act(t, ap, t, ap, AF.Ln)


def cum_tables(P, C, la, pC1, pC2, nh=3):
    N = NCH * nh
    flat = lambda b: b[:].rearrange("p c h -> p (c h)")
    T = {}
    for n in ("negac", "eac", "wj", "eL"):
        T[n] = P.sb([128, NCH, nh], name="tb_" + n)
    P.mm(pC1, pC1[:, 0:N], C["U"], C["U"][:], la, flat(la))
    P.mm(pC2, pC2[:, 0:N], C["ones"], C["ones"][:], la, flat(la))
    P.ts("dve", T["negac"], flat(T["negac"]), pC1, pC1[:, 0:N], -1.0, None, ALU.mult)
    P.act(T["eac"], flat(T["eac"]), pC1, pC1[:, 0:N], AF.Exp)
    P.act(T["eL"], flat(T["eL"]), pC2, pC2[:, 0:N], AF.Exp)
    P.tt("dve", T["wj"], flat(T["wj"]), pC2, pC2[:, 0:N], T["negac"], flat(T["negac"]), ALU.add)
    P.act(T["wj"], flat(T["wj"]), T["wj"], flat(T["wj"]), AF.Exp)
    return T


def decay_mats(P, C, la, T, c, pA, rhs_t, decT, nh=3):
    for h in range(nh):
        r = rhs_t[h % len(rhs_t)]
        P.ts(("dve", "pool")[h % 2], r, r[:], C["U"], C["U"][:], la[:, c, h:h + 1], None, ALU.mult, rd=[la])
        P.mm(pA, pA[:, h * 128:(h + 1) * 128], C["ones"], C["ones"][:], r, r[:], start=True, stop=False)
        P.mm(pA, pA[:, h * 128:(h + 1) * 128], C["idn"], C["idn"][:], C["NEG"], C["NEG"][:], start=False, stop=True)
    for h in range(nh):
        P.act(decT, decT[:, h, :], pA, pA[:, h * 128:(h + 1) * 128], AF.Exp, bias=T["negac"][:, c, h:h + 1], rd=[T["negac"]])


def build_k2s():
    nc = bass.Bass("TRN2", target_bir_lowering=False)
    P = Prog(nc)
    u = P.dram("u", [448, NBLK, 260], F32, kind="ExternalInput")
    cwd = P.dram("cw", [448, 5], F32, kind="ExternalInput")
    cbd = P.dram("cb", [448, 1], F32, kind="ExternalInput")
    dtr = P.dram("dtr", [128, NCH, 3], F32, kind="ExternalInput")
    dtb = P.dram("dtb", [3], F32, kind="ExternalInput")
    alog = P.dram("alog", [3], F32, kind="ExternalInput")
    yout = P.dram("y", [NCH, 128, 192], F32, kind="ExternalOutput")
    xout = P.dram("xs", [NCH, 128, 192], F32, kind="ExternalOutput")
    C = load_consts(P)
    offs = (0, 128, 192, 320)
    nps = (128, 64, 128, 128)
    cw, cb = [], []
    for i in range(4):
        a = P.sb([128, 5], name="cw%d" % i)
        b = P.sb([128, 1], name="cb%d" % i)
        P.dma("sync", a[0:nps[i], :], cwd.t.ap()[offs[i]:offs[i] + nps[i], :], reads=[cwd], writes=[a])
        P.dma("act", b[0:nps[i], :], cbd.t.ap()[offs[i]:offs[i] + nps[i], :], reads=[cbd], writes=[b])
        cw.append(a)
        cb.append(b)
    dt = P.sb([128, NCH, 3], name="dt")
    la = P.sb([128, NCH, 3], name="la")
    dtw = P.sb([128, NCH, 3], name="dtw")
    dtbb = P.sb([128, 3], name="dtbb")
    Ab = P.sb([128, 3], name="Ab")
    P.dma("sync", dt[:], dtr.t.ap(), reads=[dtr], writes=[dt])
    P.dma("act", dtbb[:], dtb.t.ap().partition_broadcast(128), reads=[dtb], writes=[dtbb])
    P.dma("pool", Ab[:], alog.t.ap().partition_broadcast(128), reads=[alog], writes=[Ab])
    P.act(Ab, Ab[:], Ab, Ab[:], AF.Exp)
    P.ts("dve", Ab, Ab[:], Ab, Ab[:], -1.0, None, ALU.mult)
    for h in range(3):
        P.ts("dve", dt, dt[:, :, h], dt, dt[:, :, h], dtbb[:, h:h + 1], None, ALU.add, rd=[dtbb])
    fl = lambda b: b[:].rearrange("p c h -> p (c h)")
    softplus_inplace(P, dt, fl(dt))
    for h in range(3):
        P.ts("dve", la, la[:, :, h], dt, dt[:, :, h], Ab[:, h:h + 1], None, ALU.mult, rd=[Ab])
    pC1 = P.ps([128, 512], name="pC1")
    pC2 = P.ps([128, 512], name="pC2")
    T = cum_tables(P, C, la, pC1, pC2)
    P.tt("dve", dtw, fl(dtw), dt, fl(dt), T["wj"], fl(T["wj"]), ALU.mult)

    banks = [pC1, pC2] + [P.ps([1], name="bk%d" % i) for i in range(6)]
    bankset = [banks[0:3], banks[3:6]]
    bY2, bS = banks[6], banks[7]
    S = P.sb([128, 192], name="S")
    P.op("dve", lambda e: e.memset(S[:], 0.0), writes=[S])
    ut = [[P.sb([128, 260], name="ut%d_%d" % (i, j)) for j in range(2)] for i in range(4)]
    acc = [P.sb([128, 256], name="acc%d" % i) for i in range(4)]
    cv = [[P.sb([128, 256], name="cv%d_%d" % (i, j)) for j in range(2)] for i in range(4)]

    def mkset(n):
        return {"rhs": [P.sb([128, 128], name="rhs%d_%d" % (i, n)) for i in range(3)],
                "decT": P.sb([128, 3, 128], name="decT%d" % n), "Wt": P.sb([128, 3, 128], name="Wt%d" % n),
                "xdt": P.sb([128, 192], name="xdt%d" % n)}

    def mkhand(n, par):
        return {"tk": P.sb([128, 320], name="tok%d_%d" % (n, par)), "xw": P.sb([128, 192], name="xw%d_%d" % (n, par)),
                "ysb": P.sb([128, 192], name="ysb%d_%d" % (n, par)), "ct": P.sb([128, 128], name="ct%d_%d" % (n, par))}

    sets = [mkset(0), mkset(1)]
    hands = [[mkhand(n, par) for par in range(2)] for n in range(2)]
    yo = [P.sb([128, 192], name="yo%d" % i) for i in range(2)]

    def pre(c, cc, j, st, bk, hd):
        bT, bA, bY1 = bk
        tk, xw, ysb = hd["tk"], hd["xw"], hd["ysb"]
        decT, Wt, xdt = st["decT"], st["Wt"], st["xdt"]
        xA, xB, BT, CT = cv[0][j], cv[1][j], cv[2][j], cv[3][j]
        ck = slice(cc * 128, (cc + 1) * 128)
        P.tr(bT, bT[:, 0:128], xA, xA[:, ck], C["idn"], C["idn"][:])
        P.tr(bT, bT[:, 128:192], xB, xB[0:64, ck], C["idn"], C["idn"][0:64, 0:64])
        P.tr(bT, bT[:, 192:320], BT, BT[:, ck], C["idn"], C["idn"][:])
        yield
        P.cp("act", tk, tk[:], bT, bT[:, 0:320])
        P.cp("pool", hd["ct"], hd["ct"][:], CT, CT[:, ck])
        P.dma("sync", xout.t.ap()[c], tk[:, 0:192], reads=[tk], writes=[xout])
        for h in range(3):
            r = st["rhs"][h]
            P.ts(("dve", "pool")[h % 2], r, r[:], C["U"], C["U"][:], la[:, c, h:h + 1], None, ALU.mult, rd=[la])
        yield
        for h in range(3):
            r = st["rhs"][h]
            P.mm(bA, bA[:, h * 128:(h + 1) * 128], C["ones"], C["ones"][:], r, r[:], start=True, stop=False)
            P.mm(bA, bA[:, h * 128:(h + 1) * 128], C["idn"], C["idn"][:], C["NEG"], C["NEG"][:], start=False, stop=True)
        P.mm(bT, bT[:, 0:128], BT, BT[:, ck], CT, CT[:, ck])
        yield
        for h in range(3):
            P.act(decT, decT[:, h, :], bA, bA[:, h * 128:(h + 1) * 128], AF.Exp, bias=T["negac"][:, c, h:h + 1], rd=[T["negac"]])
        for h in range(3):
            hs = slice(h * 64, (h + 1) * 64)
            P.ts("pool", xdt, xdt[:, hs], tk, tk[:, hs], dt[:, c, h:h + 1], None, ALU.mult, rd=[dt])
            P.ts("pool", xw, xw[:, hs], tk, tk[:, hs], dtw[:, c, h:h + 1], None, ALU.mult, rd=[dtw])
        yield
        for h in range(3):
            P.tt("dve", Wt, Wt[:, h, :], bT, bT[:, 0:128], decT, decT[:, h, :], ALU.mult)
        yield
        for h in range(3):
            hs = slice(h * 64, (h + 1) * 64)
            P.mm(bY1, bY1[:, hs], Wt, Wt[:, h, :], xdt, xdt[:, hs])
        yield
        P.cp("act", ysb, ysb[:], bY1, bY1[:, 0:192])
        yield

    def rec(c, cc, j, hd):
        tk, xw, ysb, ct = hd["tk"], hd["xw"], hd["ysb"], hd["ct"]
        for h in range(3):
            hs = slice(h * 64, (h + 1) * 64)
            P.mm(bY2, bY2[:, hs], ct, ct[:], S, S[:, hs])
            P.mm(bS, bS[:, hs], tk, tk[:, 192:320], xw, xw[:, hs])
        yield
        y_ = yo[c % 2]
        for h in range(3):
            hs = slice(h * 64, (h + 1) * 64)
            P.stt("dve", y_, y_[:, hs], bY2, bY2[:, hs], T["eac"][:, c, h:h + 1], ysb, ysb[:, hs], ALU.mult, ALU.add, rd=[T["eac"]])
        for h in range(3):
            hs = slice(h * 64, (h + 1) * 64)
            P.stt("dve", S, S[:, hs], S, S[:, hs], T["eL"][:, c, h:h + 1], bS, bS[:, hs], ALU.mult, ALU.add, rd=[T["eL"]])
        P.dma("sync", yout.t.ap()[c], y_[:], reads=[y_], writes=[yout])
        yield

    def chain(gs):
        for g in gs:
            for _ in g:
                yield

    def roundrobin(gens):
        gens = list(gens)
        while gens:
            for g in list(gens):
                try:
                    next(g)
                except StopIteration:
                    gens.remove(g)

    def conv_gen(b):
        j = b % 2
        for i in range(4):
            P.dma(("sync", "act")[i % 2], ut[i][j][0:nps[i], :], u.t.ap()[offs[i]:offs[i] + nps[i], b, :],
                  reads=[u], writes=[ut[i][j]])
            conv_block(P, "dve", ut[i][j], acc[i], cv[i][j], cw[i], cb[i], nps[i])
            yield

    pending = []
    roundrobin([conv_gen(0)])
    for b in range(NBLK):
        j = b % 2
        gens = [pre(2 * b, 0, j, sets[0], bankset[0], hands[0][j]), pre(2 * b + 1, 1, j, sets[1], bankset[1], hands[1][j])]
        if pending:
            gens.append(chain(pending))
        if b + 1 < NBLK:
            gens.append(conv_gen(b + 1))
        roundrobin(gens)
        pending = [rec(2 * b, 0, j, hands[0][j]), rec(2 * b + 1, 1, j, hands[1][j])]
    roundrobin([chain(pending)])
    P.finish([yout, xout])
    return nc


def windows(seg, n=256):
    ch, T = seg.shape
    p = np.zeros((ch, T + 4), np.float32)
    p[:, 2:T + 2] = seg
    idx = (np.arange(T // n)[:, None] * n + np.arange(n + 4)[None, :])
    return p[:, idx]


def prep_k2s(pl, pc, conv_w, conv_b, a_log, dt_bias, core):
    s, d, g = core // 4, (core // 2) % 2, core % 2
    c0 = 256 + 384
    chans = np.concatenate([np.arange(g * 192, g * 192 + 192), 384 + g * 128 + np.arange(128), 384 + 256 + g * 128 + np.arange(128)])
    segs = []
    for arr in (pc[s], pl[s]):
        a = arr[:, c0 + chans]
        if d == 1:
            a = a[::-1]
        segs.append(windows(np.ascontiguousarray(a.T)))
    u = np.ascontiguousarray(np.concatenate(segs, axis=1))
    cw = conv_w[:, chans].T
    if d == 1:
        cw = cw[:, ::-1]
    dcol = 256 + 384 + 896 + d * 6 + g * 3
    dts = []
    for arr in (pc[s], pl[s]):
        a = arr[:, dcol:dcol + 3]
        if d == 1:
            a = a[::-1]
        dts.append(a)
    dtr = np.concatenate(dts, 0).reshape(NCH, 128, 3).transpose(1, 0, 2)
    m = {"u": u, "cw": np.ascontiguousarray(cw), "cb": np.ascontiguousarray(conv_b[chans][:, None]),
         "dtr": np.ascontiguousarray(dtr), "dtb": np.ascontiguousarray(dt_bias[d, g * 3:g * 3 + 3]),
         "alog": np.ascontiguousarray(a_log[d, g * 3:g * 3 + 3])}
    m.update(consts())
    return m


GRID_W = 64


def consts_g():
    c = consts()
    t = np.arange(128)
    c["POS"] = np.where(t[None, :] >= t[:, None], 30000.0, 0.0).astype(np.float32)
    return c


def build_k2g(nblk=NBLK):
    nc = bass.Bass("TRN2", target_bir_lowering=False)
    P = Prog(nc)
    u = P.dram("u", [576, NBLK, 260], F32, kind="ExternalInput")
    cwd = P.dram("cw", [576, 5], F32, kind="ExternalInput")
    ard = P.dram("araw", [128, NCH, 3], F32, kind="ExternalInput")
    brd = P.dram("braw", [128, NCH, 3], F32, kind="ExternalInput")
    dtb = P.dram("dtb", [3], F32, kind="ExternalInput")
    alog = P.dram("alog", [3], F32, kind="ExternalInput")
    oout = P.dram("o", [NCH, 128, 192], F32, kind="ExternalOutput")
    C = load_consts(P, ("idn", "U", "NEG", "ones", "POS"))
    idn = C["idn"]
    offs = (0, 128, 192, 320, 384, 512)
    nps = (128, 64, 128, 64, 128, 64)
    cw = []
    for i in range(6):
        a = P.sb([128, 5], name="cw%d" % i)
        P.dma(("sync", "act")[i % 2], a[0:nps[i], :], cwd.t.ap()[offs[i]:offs[i] + nps[i], :], reads=[cwd], writes=[a])
        cw.append(a)
    fl = lambda b: b[:].rearrange("p c h -> p (c h)")
    N3 = NCH * 3
    la = P.sb([128, NCH, 3], name="la")
    beta = P.sb([128, NCH, 3], name="beta")
    dtbb = P.sb([128, 3], name="dtbb")
    Ab = P.sb([128, 3], name="Ab")
    P.dma("sync", la[:], ard.t.ap(), reads=[ard], writes=[la])
    P.dma("pool", beta[:], brd.t.ap(), reads=[brd], writes=[beta])
    P.dma("act", dtbb[:], dtb.t.ap().partition_broadcast(128), reads=[dtb], writes=[dtbb])
    P.dma("pool", Ab[:], alog.t.ap().partition_broadcast(128), reads=[alog], writes=[Ab])
    P.act(Ab, Ab[:], Ab, Ab[:], AF.Exp)
    P.ts("dve", Ab, Ab[:], Ab, Ab[:], -1.0, None, ALU.mult)
    for h in range(3):
        P.ts("dve", la, la[:, :, h], la, la[:, :, h], dtbb[:, h:h + 1], None, ALU.add, rd=[dtbb])
    softplus_inplace(P, la, fl(la))
    for h in range(3):
        P.ts("dve", la, la[:, :, h], la, la[:, :, h], Ab[:, h:h + 1], None, ALU.mult, rd=[Ab])
    P.act(beta, fl(beta), beta, fl(beta), AF.Sigmoid)
    banks = [P.ps([128, 512], name="bank%d" % i) for i in range(8)]
    ac = P.sb([128, NCH, 3], name="ac")
    negac = P.sb([128, NCH, 3], name="negac")
    eac = P.sb([128, NCH, 3], name="eac")
    wj = P.sb([128, NCH, 3], name="wj")
    eL = P.sb([128, NCH, 3], name="eL")
    be = P.sb([128, NCH, 3], name="be")
    nbeta = P.sb([128, NCH, 3], name="nbeta")
    pC1, pC2 = banks[3], banks[4]
    P.mm(pC1, pC1[:, 0:N3], C["U"], C["U"][:], la, fl(la))
    P.mm(pC2, pC2[:, 0:N3], C["ones"], C["ones"][:], la, fl(la))
    P.cp("dve", ac, fl(ac), pC1, pC1[:, 0:N3])
    P.ts("dve", negac, fl(negac), ac, fl(ac), -1.0, None, ALU.mult)
    P.act(eac, fl(eac), ac, fl(ac), AF.Exp)
    P.act(eL, fl(eL), pC2, pC2[:, 0:N3], AF.Exp)
    P.tt("dve", wj, fl(wj), pC2, pC2[:, 0:N3], negac, fl(negac), ALU.add)
    P.act(wj, fl(wj), wj, fl(wj), AF.Exp)
    P.tt("dve", be, fl(be), beta, fl(beta), eac, fl(eac), ALU.mult)
    P.ts("dve", nbeta, fl(nbeta), beta, fl(beta), -1.0, None, ALU.mult)

    I3 = P.sb([128, 3, 128], name="I3")
    for h in range(3):
        P.cp("pool", I3, I3[:, h, :], idn, idn[:])
    S = P.sb([64, 192], name="S")
    P.op("dve", lambda e: e.memset(S[:], 0.0), writes=[S])

    ut = [[P.sb([128, 260], name="ut%d_%d" % (i, j)) for j in range(2)] for i in range(6)]
    acc = [P.sb([128, 256], name="acc%d" % i) for i in range(6)]
    cv = [[P.sb([128, 256], name="cv%d_%d" % (i, j)) for j in range(2)] for i in range(6)]

    def mkset(n):
        d = {}
        for nm, shp in (("qk", [128, 384]), ("vt", [128, 192]), ("sq", [128, 384]), ("rs", [128, 6]), ("qkn", [128, 384]),
                        ("kT", [64, 384]), ("kbe", [128, 192]), ("vb", [128, 192]), ("decT", [128, 3, 128]),
                        ("decS", [128, 3, 128]), ("Np", [128, 3, 128]), ("Mp", [128, 3, 128]), ("Tt", [128, 3, 128])):
            d[nm] = P.sb(shp, name="%s_%d" % (nm, n))
        d["rhs"] = [P.sb([128, 128], name="rhs%d_%d" % (i, n)) for i in range(3)]
        return d

    def mkhand(n, par):
        d = {}
        for nm, shp in (("usb", [128, 192]), ("wT", [64, 384]), ("qT", [64, 384]), ("attnT", [128, 3, 128]), ("kend", [128, 192])):
            d[nm] = P.sb(shp, name="%s_%d_%d" % (nm, n, par))
        return d

    sets = [mkset(0), mkset(1)]
    hands = [[mkhand(n, par) for par in range(2)] for n in range(2)]
    bankset = [banks[0:3], banks[3:6]]
    bR6, bR7 = banks[6], banks[7]
    vnew = P.sb([128, 192], name="vnew")
    o2 = P.sb([128, 192], name="o2")
    oo = [P.sb([128, 192], name="oo%d" % i) for i in range(2)]
    f3 = lambda b: b[:].rearrange("p h n -> p (h n)")
    H = lambda h: slice(h * 64, (h + 1) * 64)
    H2 = lambda h: slice(h * 128, (h + 1) * 128)

    def pre(c, cc, j, st, bk, hd):
        b0, b1, b2 = bk
        qk, vt, sq, rs, qkn, kT = st["qk"], st["vt"], st["sq"], st["rs"], st["qkn"], st["kT"]
        kbe, vb, decT, decS, Np, Mp, Tt, rhs_t = st["kbe"], st["vb"], st["decT"], st["decS"], st["Np"], st["Mp"], st["Tt"], st["rhs"]
        usb, wT, qT, attnT, kend = hd["usb"], hd["wT"], hd["qT"], hd["attnT"], hd["kend"]
        ck = slice(cc * 128, (cc + 1) * 128)
        pT1, pT2 = b0, b1
        for a in range(3):
            pt = pT1 if a < 2 else pT2
            base = (a % 2) * 192
            t01, t2 = cv[2 * a][j], cv[2 * a + 1][j]
            P.tr(pt, pt[:, base:base + 128], t01, t01[:, ck], idn, idn[:])
            P.tr(pt, pt[:, base + 128:base + 192], t2, t2[0:64, ck], idn, idn[0:64, 0:64])
        yield
        P.cp("act", qk, qk[:], pT1, pT1[:, 0:384])
        P.cp("dve", vt, vt[:], pT2, pT2[:, 0:192])
        yield
        P.tt("pool", sq, sq[:], qk, qk[:], qk, qk[:], ALU.mult)
        yield
        P.op("dve", lambda e: e.reduce_sum(out=rs[:], in_=sq[:].rearrange("p (a d) -> p a d", d=64), axis=AX.X),
             reads=[sq], writes=[rs])
        P.ts("dve", rs, rs[:], rs, rs[:], 1e-6, None, ALU.add)
        yield
        P.op("act", lambda e: e.sqrt(out=rs[:], in_=rs[:]), reads=[rs], writes=[rs])
        yield
        P.op("dve", lambda e: e.reciprocal(out=rs[:], in_=rs[:]), reads=[rs], writes=[rs])
        P.ts("dve", rs, rs[:, 0:3], rs, rs[:, 0:3], 0.125, None, ALU.mult)
        yield
        for a in range(6):
            P.ts(("dve", "pool")[a % 2], qkn, qkn[:, H(a)], qk, qk[:, H(a)], rs[:, a:a + 1], None, ALU.mult, rd=[rs])
        yield
        pKT, pQT = b2, b0
        for h in range(3):
            P.tr(pKT, pKT[0:64, H2(h)], qkn, qkn[:, 192 + h * 64:192 + (h + 1) * 64], idn, idn[:])
            P.tr(pQT, pQT[0:64, H2(h)], qkn, qkn[:, h * 64:(h + 1) * 64], idn, idn[:])
        yield
        P.cp("act", kT, kT[:], pKT, pKT[0:64, 0:384])
        P.cp("dve", qT, qT[:], pQT, pQT[0:64, 0:384])
        for h in range(3):
            kn_h = qkn[:, 192 + h * 64:192 + (h + 1) * 64]
            P.ts("pool", kbe, kbe[:, H(h)], qkn, kn_h, be[:, c, h:h + 1], None, ALU.mult, rd=[be])
            P.ts("pool", vb, vb[:, H(h)], vt, vt[:, H(h)], beta[:, c, h:h + 1], None, ALU.mult, rd=[beta])
            P.ts("pool", kend, kend[:, H(h)], qkn, kn_h, wj[:, c, h:h + 1], None, ALU.mult, rd=[wj])
        yield
        pA1, pA2 = b0, b1
        for h in range(3):
            r = rhs_t[h]
            P.ts(("dve", "pool")[h % 2], r, r[:], C["U"], C["U"][:], la[:, c, h:h + 1], None, ALU.mult, rd=[la])
        yield
        for h in range(3):
            r = rhs_t[h]
            P.mm(pA1, pA1[:, H2(h)], C["ones"], C["ones"][:], r, r[:], start=True, stop=False)
            P.mm(pA1, pA1[:, H2(h)], idn, idn[:], C["NEG"], C["NEG"][:], start=False, stop=True)
            P.mm(pA2, pA2[:, H2(h)], C["ones"], C["ones"][:], r, r[:], start=True, stop=False)
            P.mm(pA2, pA2[:, H2(h)], idn, idn[:], C["POS"], C["POS"][:], start=False, stop=True)
        yield
        for h in range(3):
            P.act(decT, decT[:, h, :], pA1, pA1[:, H2(h)], AF.Exp, bias=negac[:, c, h:h + 1], rd=[negac])
            P.act(decS, decS[:, h, :], pA2, pA2[:, H2(h)], AF.Exp, bias=ac[:, c, h:h + 1], scale=-1.0, rd=[ac])
        yield
        pKK, pQK = b2, b0
        for h in range(3):
            P.mm(pKK, pKK[:, H2(h)], kT, kT[:, H2(h)], kT, kT[:, H2(h)])
            P.mm(pQK, pQK[:, H2(h)], kT, kT[:, H2(h)], qT, qT[:, H2(h)])
        yield
        for h in range(3):
            P.stt("dve", Np, Np[:, h, :], pKK, pKK[:, H2(h)], nbeta[:, c, h:h + 1], decS, decS[:, h, :], ALU.mult, ALU.mult, rd=[nbeta])
            P.tt("dve", attnT, attnT[:, h, :], pQK, pQK[:, H2(h)], decT, decT[:, h, :], ALU.mult)
        yield
        pN, pM, pTt = b0, b1, b2
        for h in range(3):
            P.tr(pM, pM[:, H2(h)], Np, Np[:, h, :], idn, idn[:])
        yield
        P.cp("act", Mp, f3(Mp), pM, pM[:, 0:384])
        yield
        P.tt("pool", Tt, f3(Tt), Mp, f3(Mp), I3, f3(I3), ALU.add)
        for step in range(6):
            last = step == 5
            for h in range(3):
                P.mm(pN, pN[:, H2(h)], Mp, Mp[:, h, :], Np, Np[:, h, :])
                if not last:
                    P.mm(pM, pM[:, H2(h)], Np, Np[:, h, :], Mp, Mp[:, h, :])
            yield
            P.cp("act", Np, f3(Np), pN, pN[:, 0:384])
            if not last:
                P.cp("dve", Mp, f3(Mp), pM, pM[:, 0:384])
            yield
            for h in range(3):
                P.mm(pTt, pTt[:, H2(h)], Np, Np[:, h, :], Tt, Tt[:, h, :])
            yield
            P.tt("dve", Tt, f3(Tt), Tt, f3(Tt), pTt, pTt[:, 0:384], ALU.add)
        yield
        pU, pWT = b0, b1
        for h in range(3):
            P.mm(pU, pU[:, H(h)], Tt, Tt[:, h, :], vb, vb[:, H(h)])
            P.mm(pWT, pWT[0:64, H2(h)], kbe, kbe[:, H(h)], Tt, Tt[:, h, :])
        yield
        P.cp("act", usb, usb[:], pU, pU[:, 0:192])
        P.cp("dve", wT, wT[:], pWT, pWT[0:64, 0:384])
        yield

    def rec(c, hd):
        usb, wT, qT, attnT, kend = hd["usb"], hd["wT"], hd["qT"], hd["attnT"], hd["kend"]
        pWS, pO1, pO2, pSn = bR6, bR7, bR6, bR6
        for h in range(3):
            P.mm(pWS, pWS[:, H(h)], wT, wT[:, H2(h)], S, S[:, H(h)])
            P.mm(pO1, pO1[:, H(h)], qT, qT[:, H2(h)], S, S[:, H(h)])
        yield
        P.tt("dve", vnew, vnew[:], usb, usb[:], pWS, pWS[:, 0:192], ALU.subtract)
        yield
        for h in range(3):
            P.mm(pO2, pO2[:, H(h)], attnT, attnT[:, h, :], vnew, vnew[:, H(h)])
        yield
        P.cp("act", o2, o2[:], pO2, pO2[:, 0:192])
        yield
        for h in range(3):
            P.mm(pSn, pSn[0:64, H(h)], kend, kend[:, H(h)], vnew, vnew[:, H(h)])
        o_ = oo[c % 2]
        for h in range(3):
            P.stt("dve", o_, o_[:, H(h)], pO1, pO1[:, H(h)], eac[:, c, h:h + 1], o2, o2[:, H(h)], ALU.mult, ALU.add, rd=[eac])
        yield
        for h in range(3):
            P.stt("dve", S, S[:, H(h)], S, S[:, H(h)], eL[0:64, c, h:h + 1], pSn, pSn[0:64, H(h)], ALU.mult, ALU.add, rd=[eL])
        P.dma("sync", oout.t.ap()[c], o_[:], reads=[o_], writes=[oout])
        yield

    def chain(gs):
        for g in gs:
            for _ in g:
                yield

    def roundrobin(gens):
        gens = list(gens)
        while gens:
            for g in list(gens):
                try:
                    next(g)
                except StopIteration:
                    gens.remove(g)

    def conv_gen(b):
        j = b % 2
        for i in range(6):
            P.dma(("sync", "act")[i % 2], ut[i][j][0:nps[i], :], u.t.ap()[offs[i]:offs[i] + nps[i], b, :],
                  reads=[u], writes=[ut[i][j]])
            conv_block(P, "dve", ut[i][j], acc[i], cv[i][j], cw[i], None, nps[i], bias=False)
            yield

    pending = []
    roundrobin([conv_gen(0)])
    for b in range(nblk):
        j = b % 2
        gens = [pre(2 * b, 0, j, sets[0], bankset[0], hands[0][j]), pre(2 * b + 1, 1, j, sets[1], bankset[1], hands[1][j])]
        if pending:
            gens.append(chain(pending))
        if b + 1 < nblk:
            gens.append(conv_gen(b + 1))
        roundrobin(gens)
        pending = [rec(2 * b, hands[0][j]), rec(2 * b + 1, hands[1][j])]
    roundrobin([chain(pending)])
    P.finish([oout])
    return nc


def to_cm(a):
    T, Cc = a.shape
    return a.reshape(T // GRID_W, GRID_W, Cc).transpose(1, 0, 2).reshape(T, Cc)


def from_cm(a):
    T, Cc = a.shape
    return a.reshape(GRID_W, T // GRID_W, Cc).transpose(1, 0, 2).reshape(T, Cc)


def prep_k2g(pl, pc, conv_w, a_log, dt_bias, core):
    s, d, g = core // 4, (core // 2) % 2, core % 2
    q0 = 1548
    chans = np.concatenate([a * 384 + g * 192 + np.arange(192) for a in range(3)])
    acol = 3084 + d * 6 + g * 3
    bcol = 3096 + d * 6 + g * 3
    segs, ars, brs = [], [], []
    for arr, cm in ((pc[s], False), (pl[s], True)):
        a = arr[:, q0 + chans]
        ar = arr[:, acol:acol + 3]
        br = arr[:, bcol:bcol + 3]
        if cm:
            a, ar, br = to_cm(a), to_cm(ar), to_cm(br)
        if d == 1:
            a, ar, br = a[::-1], ar[::-1], br[::-1]
        segs.append(windows(np.ascontiguousarray(a.T)))
        ars.append(ar)
        brs.append(br)
    cw = conv_w[:, chans].T
    if d == 1:
        cw = cw[:, ::-1]
    tm = lambda lst: np.ascontiguousarray(np.concatenate(lst, 0).reshape(NCH, 128, 3).transpose(1, 0, 2))
    m = {"u": np.ascontiguousarray(np.concatenate(segs, axis=1)), "cw": np.ascontiguousarray(cw),
         "araw": tm(ars), "braw": tm(brs), "dtb": np.ascontiguousarray(dt_bias[d, g * 3:g * 3 + 3]),
         "alog": np.ascontiguousarray(a_log[d, g * 3:g * 3 + 3])}
    m.update(consts_g())
    return m


PKW = 1024 + 256 + 7 * 384
POOL_WINDOWS = (2, 4, 8, 16)


def build_k3(NT, n_lat=None):
    if n_lat is None:
        n_lat = NT - 1
    nc = bass.Bass("TRN2", target_bir_lowering=False)
    P = Prog(nc)
    pk = P.dram("pk", [NT, 128, PKW], F32, kind="ExternalInput")
    halo = P.dram("halo", [NT, 16, 256], F32, kind="ExternalInput")
    band = P.dram("band", [NT, 144, 512], F32, kind="ExternalInput")
    wout = P.dram("wout", [D, D], F32, kind="ExternalInput")
    pwd = P.dram("pw", [64, 4, 128], F32, kind="ExternalInput")
    psd = P.dram("pscale", [128, 2], F32, kind="ExternalInput")
    rwd = P.dram("rw", [128, 8, 16], F32, kind="ExternalInput")
    idn = P.dram("idn", [128, 128], F32, kind="ExternalInput")
    v384 = {n: P.dram(n, [384], F32, kind="ExternalInput") for n in ("dvec", "sng", "gng")}
    v1k = {n: P.dram(n, [D], F32, kind="ExternalInput") for n in ("g1_l", "g1_c", "n2g", "sh_l", "sc_l", "sh_c", "sc_c")}
    x2o = P.dram("x2", [NT, 128, D], F32, kind="ExternalOutput")
    hTo = P.dram("h2T", [NT, 128, 8, 128], F32, kind="ExternalOutput")
    affo = P.dram("aff", [NT, 128, 16], F32, kind="ExternalOutput")

    ident = P.sb([128, 128], name="ident")
    P.dma("sync", ident[:], idn.t.ap(), reads=[idn], writes=[ident])
    pw = P.sb([64, 4, 128], name="pw")
    P.dma("act", pw[:], pwd.t.ap(), reads=[pwd], writes=[pw])
    psc = P.sb([128, 2], name="psc")
    P.dma("sync", psc[:], psd.t.ap(), reads=[psd], writes=[psc])
    rw = P.sb([128, 8, 16], name="rw")
    P.dma("act", rw[:], rwd.t.ap(), reads=[rwd], writes=[rw])
    b384 = {n: load_bcast(P, ("sync", "act")[i % 2], v384[n], 384, "b_" + n) for i, n in enumerate(v384)}
    b1k = {n: load_bcast(P, ("sync", "act")[i % 2], v1k[n], D, "b_" + n) for i, n in enumerate(v1k)}
    for n in ("sc_l", "sc_c"):
        A = b1k[n]
        P.stt("dve", A, A[:], A, A[:], 1.0, b1k["n2g"], b1k["n2g"][:], ALU.add, ALU.mult)
    W = P.sb([128, 8, D], BF16, name="W")
    stage = [P.sb([128, D], name="stg%d" % i) for i in range(2)]
    load_weight_bf16(P, lambda k: wout.t.ap()[k * 128:(k + 1) * 128, :], W, 8, D, stage, wout)

    def mkset(n):
        d = {}
        for nm, shp, dt_ in (("pk", [128, PKW], F32), ("hl", [16, 256], F32), ("bd", [128, 512], F32), ("bd2", [16, 512], F32),
                             ("dT", [64, 4, 128], F32), ("mT", [128, 8, 128], BF16), ("t1", [128, 384], F32), ("ys", [128, 384], F32),
                             ("sz", [128, 384], F32), ("scr3", [128, 384], F32), ("ss1", [128, 1], F32), ("yo", [128, 768], F32),
                             ("og", [128, 384], F32), ("sq", [128, 384], F32), ("rs6", [128, 6], F32), ("sg", [128, 384], F32),
                             ("mx", [128, 512], F32), ("x2", [128, D], F32), ("h2", [128, D], F32), ("scr", [128, D], F32),
                             ("ss2", [128, 1], F32), ("h2T", [128, 8, 128], F32), ("lg", [128, 16], F32), ("ex", [128, 16], F32),
                             ("m1", [128, 1], F32), ("s1", [128, 1], F32), ("af", [128, 16], F32)):
            d[nm] = P.sb(shp, dt_, name="%s_%d" % (nm, n))
        d["bA"], d["bT"] = P.ps([1], name="bA%d" % n), P.ps([1], name="bT%d" % n)
        d["pms"] = [P.ps([1], name="bM%d_%d" % (i, n)) for i in range(2)]
        return d

    sets = [mkset(0), mkset(1)]

    def tile_gen(t, st):
        lat = t < n_lat
        pk_, hl, bd, bd2, mT, dT = st["pk"], st["hl"], st["bd"], st["bd2"], st["mT"], st["dT"]
        t1, ys, sz, scr3, ss1, yo, og, sq, rs6, sg, mx = [st[k] for k in ("t1", "ys", "sz", "scr3", "ss1", "yo", "og", "sq", "rs6", "sg", "mx")]
        pD = pP = pr = st["bA"]
        pts = [st["bT"]]
        pms = st["pms"]
        P.dma("sync", pk_[:], pk.t.ap()[t], reads=[pk], writes=[pk_])
        P.dma("act", hl[:], halo.t.ap()[t], reads=[halo], writes=[hl])
        P.dma("act", bd[:], band.t.ap()[t, 0:128, :], reads=[band], writes=[bd])
        P.dma("act", bd2[:], band.t.ap()[t, 128:144, :], reads=[band], writes=[bd2])
        yield
        xo, uo = 0, 1024
        yf, yb, xs, z, of, ob, gt = [slice(1280 + i * 384, 1280 + (i + 1) * 384) for i in range(7)]
        for g in range(4):
            P.mm(pD, pD[0:64, g * 128:(g + 1) * 128], pk_, pk_[:, uo + g * 64:uo + (g + 1) * 64], bd, bd[:, g * 128:(g + 1) * 128],
                 start=True, stop=False)
            P.mm(pD, pD[0:64, g * 128:(g + 1) * 128], hl, hl[:, g * 64:(g + 1) * 64], bd2, bd2[:, g * 128:(g + 1) * 128],
                 start=False, stop=True)
        P.tt("pool", ys, ys[:], pk_, pk_[:, yf], pk_, pk_[:, yb], ALU.add)
        P.tt("pool", t1, t1[:], pk_, pk_[:, xs], b384["dvec"], b384["dvec"][:], ALU.mult)
        P.act(sz, sz[:], pk_, pk_[:, z], AF.Silu)
        yield
        P.cp("act", dT, dT[:].rearrange("p g t -> p (g t)"), pD, pD[0:64, :])
        P.tt("pool", ys, ys[:], ys, ys[:], t1, t1[:], ALU.add)
        yield
        for cch in range(2):
            for gg in range(2):
                g = cch * 2 + gg
                P.mm(pP, pP[:, cch * 128:(cch + 1) * 128], pw, pw[:, g, :], dT, dT[:, g, :], start=(gg == 0), stop=(gg == 1))
        P.tt("dve", ys, ys[:], ys, ys[:], sz, sz[:], ALU.mult)
        yield
        for cch in range(2):
            P.ts("dve", mT, mT[:, cch, :], pP, pP[:, cch * 128:(cch + 1) * 128], psc[:, cch:cch + 1], None, ALU.mult, rd=[psc])
        P.act(scr3, scr3[:], ys, ys[:], AF.Square, scale=float(384 ** -0.5), accum=ss1[:], wr=[ss1])
        P.tt("pool", og, og[:], pk_, pk_[:, of], pk_, pk_[:, ob], ALU.add)
        P.tt("pool", sq, sq[:], og, og[:], og, og[:], ALU.mult)
        yield
        P.ts("dve", ss1, ss1[:], ss1, ss1[:], 1e-6, None, ALU.add)
        P.op("dve", lambda e: e.reduce_sum(out=rs6[:], in_=sq[:].rearrange("p (a d) -> p a d", d=64), axis=AX.X),
             reads=[sq], writes=[rs6])
        P.ts("dve", rs6, rs6[:], rs6, rs6[:], 1.0 / 64.0, 1e-6, ALU.mult, ALU.add)
        yield
        P.op("act", lambda e: e.sqrt(out=ss1[:], in_=ss1[:]), reads=[ss1], writes=[ss1])
        P.op("act", lambda e: e.sqrt(out=rs6[:], in_=rs6[:]), reads=[rs6], writes=[rs6])
        P.act(sg, sg[:], pk_, pk_[:, gt], AF.Silu)
        yield
        P.op("dve", lambda e: e.reciprocal(out=ss1[:], in_=ss1[:]), reads=[ss1], writes=[ss1])
        P.op("dve", lambda e: e.reciprocal(out=rs6[:], in_=rs6[:]), reads=[rs6], writes=[rs6])
        yield
        P.stt("dve", yo, yo[:, 0:384], ys, ys[:], ss1[:, 0:1], b384["sng"], b384["sng"][:], ALU.mult, ALU.mult, rd=[ss1])
        for a in range(6):
            P.ts(("dve", "pool")[a % 2], og, og[:, a * 64:(a + 1) * 64], og, og[:, a * 64:(a + 1) * 64], rs6[:, a:a + 1], None, ALU.mult, rd=[rs6])
        yield
        P.tt("pool", og, og[:], og, og[:], b384["gng"], b384["gng"][:], ALU.mult)
        yield
        P.tt("dve", yo, yo[:, 384:768], og, og[:], sg, sg[:], ALU.mult)
        yield
        for half in range(2):
            pt = pts[0]
            for q in range(3):
                k = half * 3 + q
                P.tr(pt, pt[:, q * 128:(q + 1) * 128], yo, yo[:, k * 128:(k + 1) * 128], ident, ident[:])
            yield
            P.cp(("act", "dve")[half], mT, mT[:, 2 + half * 3:5 + half * 3, :], pt, pt[:, 0:384].rearrange("p (k t) -> p k t", k=3))
            yield
        x2 = st["x2"]
        g1 = b1k["g1_l"] if lat else b1k["g1_c"]
        for cb in range(2):
            pm = pms[cb]
            cs = slice(cb * 512, (cb + 1) * 512)
            for k in range(8):
                P.mm(pm, pm[:, :], mT, mT[:, k, :], W, W[:, k, cs], start=(k == 0), stop=(k == 7))
            yield
            P.tt("dve", mx, mx[:], pm, pm[:, :], g1, g1[:, cs], ALU.mult)
            yield
            P.tt("pool", x2, x2[:, cs], mx, mx[:], pk_, pk_[:, cb * 512:(cb + 1) * 512], ALU.add)
            yield
        P.dma("sync", x2o.t.ap()[t], x2[:], reads=[x2], writes=[x2o])
        h2, h2T, scr, ss2 = st["h2"], st["h2T"], st["scr"], st["ss2"]
        lg, ex, m1, s1, af = st["lg"], st["ex"], st["m1"], st["s1"], st["af"]
        A2, B2 = (b1k["sc_l"], b1k["sh_l"]) if lat else (b1k["sc_c"], b1k["sh_c"])
        P.act(scr, scr[:], x2, x2[:], AF.Square, scale=1.0 / 32.0, accum=ss2[:], wr=[ss2])
        yield
        P.ts("dve", ss2, ss2[:], ss2, ss2[:], 1e-6, None, ALU.add)
        yield
        P.op("act", lambda e: e.sqrt(out=ss2[:], in_=ss2[:]), reads=[ss2], writes=[ss2])
        yield
        P.op("dve", lambda e: e.reciprocal(out=ss2[:], in_=ss2[:]), reads=[ss2], writes=[ss2])
        P.stt("dve", h2, h2[:], x2, x2[:], ss2[:, 0:1], A2, A2[:], ALU.mult, ALU.mult, rd=[ss2])
        yield
        P.tt("pool", h2, h2[:], h2, h2[:], B2, B2[:], ALU.add)
        yield
        for half in range(2):
            pt = pts[0]
            for q in range(4):
                k = half * 4 + q
                P.tr(pt, pt[:, q * 128:(q + 1) * 128], h2, h2[:, k * 128:(k + 1) * 128], ident, ident[:])
            yield
            P.cp(("act", "dve")[half], h2T, h2T[:, half * 4:half * 4 + 4, :], pt, pt[:, 0:512].rearrange("p (k t) -> p k t", k=4))
            yield
        P.dma("sync", hTo.t.ap()[t], h2T[:], reads=[h2T], writes=[hTo])
        for k in range(8):
            P.mm(pr, pr[:, 0:16], h2T, h2T[:, k, :], rw, rw[:, k, :], start=(k == 0), stop=(k == 7))
        yield
        P.cp("act", lg, lg[:], pr, pr[:, 0:16])
        yield
        P.op("dve", lambda e: e.reduce_max(out=m1[:], in_=lg[:], axis=AX.X), reads=[lg], writes=[m1])
        P.ts("dve", m1, m1[:], m1, m1[:], -1.0, None, ALU.mult)
        yield
        P.act(ex, ex[:], lg, lg[:], AF.Exp, bias=m1[:, 0:1], accum=s1[:], rd=[m1], wr=[s1])
        yield
        P.op("dve", lambda e: e.reciprocal(out=s1[:], in_=s1[:]), reads=[s1], writes=[s1])
        P.ts("dve", af, af[:], ex, ex[:], s1[:, 0:1], None, ALU.mult, rd=[s1])
        P.dma("sync", affo.t.ap()[t], af[:], reads=[af], writes=[affo])
        yield

    def roundrobin(gens):
        gens = list(gens)
        while gens:
            for g in list(gens):
                try:
                    next(g)
                except StopIteration:
                    gens.remove(g)

    for t in range(0, NT, 2):
        gens = [tile_gen(t, sets[0])]
        if t + 1 < NT:
            gens.append(tile_gen(t + 1, sets[1]))
        roundrobin(gens)
    P.finish([x2o, hTo, affo])
    return nc


def pool_band(pos, T):
    src = np.concatenate([pos, pos[0] - 8 + np.arange(8), pos[-1] + 1 + np.arange(8)])
    out = np.zeros((144, 4, 128), np.float32)
    for g, w in enumerate(POOL_WINDOWS):
        lo = np.clip(pos - w // 2, 0, T)
        hi = np.clip(pos + w // 2, 0, T)
        cnt = np.maximum(hi - lo, 1).astype(np.float32)
        m = (src[:, None] >= lo[None, :]) & (src[:, None] < hi[None, :])
        out[:, g, :] = m / cnt[None, :]
        out[np.arange(128), g, np.arange(128)] -= 1.0
    return out.reshape(144, 512)


NE = 16
FF = 512


def bisect_threshold(P, affs, J, kcap, ones, pcnt, name):
    lo = P.sb([128, NE], name=name + "_lo")
    hi = P.sb([128, NE], name=name + "_hi")
    mid = P.sb([128, NE], name=name + "_mid")
    cnt = P.sb([128, NE], name=name + "_cnt")
    pred = P.sb([128, NE], name=name + "_pred")
    tmp = P.sb([128, NE], name=name + "_tmp")
    cmp_ = P.sb([128, NE, J], name=name + "_cmp")
    P.op("dve", lambda e: e.memset(lo[:], 0.0), writes=[lo])
    P.op("dve", lambda e: e.memset(hi[:], 1.0), writes=[hi])
    for it in range(34):
        P.tt("dve", mid, mid[:], lo, lo[:], hi, hi[:], ALU.add)
        P.ts("dve", mid, mid[:], mid, mid[:], 0.5, None, ALU.mult)
        for e_ in range(NE):
            P.ts(("dve", "pool")[e_ % 2], cmp_, cmp_[:, e_, :], affs, affs[:, e_, :], mid[:, e_:e_ + 1], None, ALU.is_ge, rd=[mid])
        P.op("dve", lambda e: e.reduce_sum(out=cnt[:], in_=cmp_[:], axis=AX.X), reads=[cmp_], writes=[cnt])
        P.mm(pcnt, pcnt[:, 0:NE], ones, ones[:], cnt, cnt[:])
        P.ts("dve", pred, pred[:], pcnt, pcnt[:, 0:NE], float(kcap) - 0.5, None, ALU.is_ge)
        P.tt("dve", tmp, tmp[:], mid, mid[:], lo, lo[:], ALU.subtract)
        P.tt("dve", tmp, tmp[:], tmp, tmp[:], pred, pred[:], ALU.mult)
        P.tt("dve", lo, lo[:], lo, lo[:], tmp, tmp[:], ALU.add)
        P.tt("dve", tmp, tmp[:], hi, hi[:], mid, mid[:], ALU.subtract)
        P.tt("dve", tmp, tmp[:], tmp, tmp[:], pred, pred[:], ALU.mult)
        P.tt("dve", hi, hi[:], mid, mid[:], tmp, tmp[:], ALU.add)
    return lo


def build_k4(NT, n_lat, kcap_lat, kcap_ctx, J_lat, J_ctx, final_norm, passes):
    nc = bass.Bass("TRN2", target_bir_lowering=False)
    P = Prog(nc)
    x2d = P.dram("x2", [NT, 128, D], F32, kind="ExternalInput")
    hTd = P.dram("h2T", [NT, 128, 8, 128], F32, kind="ExternalInput")
    afd = P.dram("aff", [NT, 128, NE], F32, kind="ExternalInput")
    asl = P.dram("affs_l", [128, NE, J_lat], F32, kind="ExternalInput")
    asc = P.dram("affs_c", [128, NE, J_ctx], F32, kind="ExternalInput")
    wgd = P.dram("wg", [NE, D, FF], F32, kind="ExternalInput")
    wud = P.dram("wu", [NE, D, FF], F32, kind="ExternalInput")
    wdd = P.dram("wd", [NE, FF, D], F32, kind="ExternalInput")
    onesd = P.dram("ones", [128, 128], F32, kind="ExternalInput")
    v1k = {n: P.dram(n, [D], F32, kind="ExternalInput") for n in ("g2_l", "g2_c", "fng")}
    outd = P.dram("out", [NT, 128, D], F32, kind="ExternalOutput")

    ones = P.sb([128, 128], name="ones")
    P.dma("sync", ones[:], onesd.t.ap(), reads=[onesd], writes=[ones])
    b1k = {n: load_bcast(P, ("sync", "act")[i % 2], v1k[n], D, "b_" + n) for i, n in enumerate(v1k)}
    pcnt = P.ps([1], name="pcnt")
    affs_l = P.sb([128, NE, J_lat], name="affs_l")
    P.dma("sync", affs_l[:], asl.t.ap(), reads=[asl], writes=[affs_l])
    thr_l = bisect_threshold(P, affs_l, J_lat, kcap_lat, ones, pcnt, "bl")
    thr_c = None
    if n_lat < NT:
        affs_c = P.sb([128, NE, J_ctx], name="affs_c")
        P.dma("act", affs_c[:], asc.t.ap(), reads=[asc], writes=[affs_c])
        thr_c = bisect_threshold(P, affs_c, J_ctx, kcap_ctx, ones, pcnt, "bc")
    afo = P.sb([128, NT, NE], name="afo")
    gw = P.sb([128, NT, NE], name="gw")
    P.dma("sync", afo[:], afd.t.ap().rearrange("t p e -> p t e"), reads=[afd], writes=[afo])
    for t in range(NT):
        thr = thr_l if t < n_lat else thr_c
        P.tt("dve", gw, gw[:, t, :], afo, afo[:, t, :], thr, thr[:], ALU.is_ge)
        P.tt("dve", gw, gw[:, t, :], gw, gw[:, t, :], afo, afo[:, t, :], ALU.mult)

    maxt = max(sum(t1 - t0 for (t0, t1) in ps_) for ps_ in passes)
    hT = P.sb([128, 8, maxt * 128], BF16, name="hT")
    hst = [P.sb([128, 8, 128], name="hst%d" % i) for i in range(2)]
    acc = P.sb([128, maxt, D], name="acc")
    Wg = [P.sb([128, 8, FF], BF16, name="Wg%d" % i) for i in range(2)]
    Wu = [P.sb([128, 8, FF], BF16, name="Wu%d" % i) for i in range(2)]
    Wd = [P.sb([128, 4, D], BF16, name="Wd%d" % i) for i in range(2)]
    stg = [P.sb([128, 2048], name="stg%d" % i) for i in range(2)]
    sil = [P.sb([128, 512], name="sil%d" % i) for i in range(2)]
    hidT = [P.sb([128, 4, 512], BF16, name="hidT%d" % i) for i in range(2)]
    xt = [P.sb([128, D], name="xt%d" % i) for i in range(2)]
    scr = P.sb([128, D], name="scr")
    ssf = P.sb([128, 1], name="ssf")
    pg = [P.ps([1], name="pg%d" % i) for i in range(2)]
    pu = [P.ps([1], name="pu%d" % i) for i in range(2)]
    pd = [P.ps([1], name="pd%d" % i) for i in range(2)]
    sti = 0
    wi = 0
    gi = 0
    for ps_ in passes:
        tiles = [t for (t0, t1) in ps_ for t in range(t0, t1)]
        loc = {t: i for i, t in enumerate(tiles)}
        for i, t in enumerate(tiles):
            h_ = hst[i % 2]
            P.dma(("sync", "act")[i % 2], h_[:], hTd.t.ap()[t], reads=[hTd], writes=[h_])
            P.cp(("pool", "act")[i % 2], hT, hT[:, :, i * 128:(i + 1) * 128], h_, h_[:])
        P.op("pool", lambda e: e.memset(acc[:], 0.0), writes=[acc])
        def load_w(ex):
            nonlocal sti
            wg_, wu_, wd_ = Wg[ex % 2], Wu[ex % 2], Wd[ex % 2]
            for (wt, src, nk) in ((wg_, wgd, 8), (wu_, wud, 8), (wd_, wdd, 4)):
                for hf in range(2):
                    s_ = stg[sti % 2]
                    sti += 1
                    k0, k1 = hf * nk // 2, (hf + 1) * nk // 2
                    sv = s_[:].rearrange("p (k f) -> p k f", k=nk // 2)
                    P.dma(("sync", "act")[sti % 2], sv, src.t.ap()[ex].rearrange("(k p) f -> p k f", p=128)[:, k0:k1, :],
                          reads=[src], writes=[s_])
                    P.cp(("pool", "act", "dve")[sti % 3], wt, wt[:, k0:k1, :], s_, sv)

        def gateup(ex, t0, t1, hd):
            wg_, wu_ = Wg[ex % 2], Wu[ex % 2]
            n = (t1 - t0) * 128
            c0 = loc[t0] * 128
            for fc in range(4):
                pg_, pu_, sl_ = pg[fc % 2], pu[fc % 2], sil[fc % 2]
                for k in range(8):
                    P.mm(pg_, pg_[:, 0:n], wg_, wg_[:, k, fc * 128:(fc + 1) * 128], hT, hT[:, k, c0:c0 + n], start=(k == 0), stop=(k == 7))
                for k in range(8):
                    P.mm(pu_, pu_[:, 0:n], wu_, wu_[:, k, fc * 128:(fc + 1) * 128], hT, hT[:, k, c0:c0 + n], start=(k == 0), stop=(k == 7))
                P.act(sl_, sl_[:, 0:n], pg_, pg_[:, 0:n], AF.Silu)
                P.tt("dve", hd, hd[:, fc, 0:n], pu_, pu_[:, 0:n], sl_, sl_[:, 0:n], ALU.mult)
                yield

        def down(ex, t0, t1, hd):
            wd_ = Wd[ex % 2]
            for t in range(t0, t1):
                i = loc[t]
                tl = slice((t - t0) * 128, (t - t0 + 1) * 128)
                for half in range(2):
                    pd_ = pd[half]
                    cs = slice(half * 512, (half + 1) * 512)
                    for fc in range(4):
                        P.mm(pd_, pd_[:, :], hd, hd[:, fc, tl], wd_, wd_[:, fc, cs], start=(fc == 0), stop=(fc == 3))
                    P.stt("dve", acc, acc[:, i, cs], pd_, pd_[:, :], gw[:, t, ex:ex + 1], acc, acc[:, i, cs], ALU.mult, ALU.add, rd=[gw])
                yield

        def roundrobin(gens):
            gens = list(gens)
            while gens:
                for g in list(gens):
                    try:
                        next(g)
                    except StopIteration:
                        gens.remove(g)

        items = [(ex, t0, t1, gidx) for ex in range(NE) for gidx, (t0, t1) in enumerate(ps_)]
        pf = min(1, len(ps_) - 1)
        prev = None
        load_w(0)
        for (ex, t0, t1, gidx) in items:
            if gidx == pf and ex + 1 < NE:
                load_w(ex + 1)
            hd = hidT[gi % 2]
            gi += 1
            gens = [gateup(ex, t0, t1, hd)]
            if prev is not None:
                gens.append(down(*prev))
            roundrobin(gens)
            prev = (ex, t0, t1, hd)
        roundrobin([down(*prev)])
        for t in tiles:
            i = loc[t]
            x_ = xt[t % 2]
            g2 = b1k["g2_l"] if t < n_lat else b1k["g2_c"]
            P.dma("act", x_[:], x2d.t.ap()[t], reads=[x2d], writes=[x_])
            P.tt("pool", acc, acc[:, i, :], acc, acc[:, i, :], g2, g2[:], ALU.mult)
            P.tt("pool", x_, x_[:], x_, x_[:], acc, acc[:, i, :], ALU.add)
            if final_norm:
                P.act(scr, scr[:], x_, x_[:], AF.Square, scale=1.0 / 32.0, accum=ssf[:], wr=[ssf])
                P.ts("dve", ssf, ssf[:], ssf, ssf[:], 1e-6, None, ALU.add)
                P.op("act", lambda e: e.sqrt(out=ssf[:], in_=ssf[:]), reads=[ssf], writes=[ssf])
                P.op("dve", lambda e: e.reciprocal(out=ssf[:], in_=ssf[:]), reads=[ssf], writes=[ssf])
                P.stt("dve", x_, x_[:], x_, x_[:], ssf[:, 0:1], b1k["fng"], b1k["fng"][:], ALU.mult, ALU.mult, rd=[ssf])
            P.dma("sync", outd.t.ap()[t], x_[:], reads=[x_], writes=[outd])
    P.finish([outd])
    return nc


B, T, CTX = 2, 16384, 256
NLT = 32


def tiles_of(lat, ctx, core, with_ctx=True):
    s, q = core // 4, core % 4
    Cc = lat.shape[-1]
    lt = lat[s, q * 4096:(q + 1) * 4096].reshape(NLT, 128, Cc)
    if not with_ctx:
        return np.ascontiguousarray(lt)
    ct = np.zeros((1, 128, Cc), np.float32)
    n = min(128, CTX - q * 64)
    ct[0, :n] = ctx[s, q * 64:q * 64 + n]
    return np.concatenate([lt, ct], 0)


def untile(res, key, Cc, with_ctx=True):
    lat = np.zeros((B, T, Cc), np.float32)
    ctx = np.zeros((B, CTX, Cc), np.float32)
    for core in range(NCORE):
        s, q = core // 4, core % 4
        r = res[core][key]
        lat[s, q * 4096:(q + 1) * 4096] = r[:NLT].reshape(4096, Cc)
        if with_ctx:
            ctx[s, q * 64:(q + 1) * 64] = r[NLT, :64]
    return lat, ctx


def unseq(y, d, cm):
    y = y.reshape(NCH * 128, -1)
    c_, l_ = y[:CTX], y[CTX:]
    if d == 1:
        c_, l_ = c_[::-1], l_[::-1]
    if cm:
        l_ = from_cm(l_)
    return c_, l_


_band_cache = {}


def band_for(pos0, Tseq):
    key = (pos0 if (pos0 == 0 or pos0 + 128 + 8 > Tseq) else -1, Tseq)
    if key not in _band_cache:
        _band_cache[key] = pool_band(pos0 + np.arange(128), Tseq)
    return _band_cache[key]


def halo_for(u, pos0):
    Tseq = u.shape[0]
    h = np.zeros((16, 256), np.float32)
    for i in range(8):
        a = pos0 - 8 + i
        if 0 <= a < Tseq:
            h[i] = u[a]
        b_ = pos0 + 128 + i
        if 0 <= b_ < Tseq:
            h[8 + i] = u[b_]
    return h


def stage_proj(l, x, ctx, mods, p):
    f32 = _f32
    m = mods[l]
    seg = lambda r, i: f32(m[r, i * 1024:(i + 1) * 1024])
    idn = np.eye(128, dtype=np.float32)
    in_maps = []
    for core in range(NCORE):
        s = core // 4
        in_maps.append({"xt": tiles_of(x, ctx, core), "w": f32(p["w_in"][l]), "g": f32(p["norm1_g"][l]),
                        "sh_l": seg(s, 0), "sc_l": seg(s, 1), "sh_c": seg(2, 0), "sc_c": seg(2, 1), "idn": idn})
    res = run(build_k1(NLT + 1), in_maps)
    return untile(res, "out", 3108)


def stage_scans(l, pl, pc, p):
    f32 = _f32
    res = run(build_k2s(), [prep_k2s(pl, pc, f32(p["ssd_conv_w"][l]), f32(p["ssd_conv_b"][l]), f32(p["ssd_a_log"][l]),
                                     f32(p["ssd_dt_bias"][l]), core) for core in range(NCORE)])
    ys_l = np.zeros((B, 2, T, 384), np.float32)
    ys_c = np.zeros((B, 2, CTX, 384), np.float32)
    xs_l = np.zeros((B, T, 384), np.float32)
    xs_c = np.zeros((B, CTX, 384), np.float32)
    for core in range(NCORE):
        s, d, g = core // 4, (core // 2) % 2, core % 2
        c_, l_ = unseq(res[core]["y"], d, False)
        ys_l[s, d, :, g * 192:(g + 1) * 192] = l_
        ys_c[s, d, :, g * 192:(g + 1) * 192] = c_
        if d == 0:
            c_, l_ = unseq(res[core]["xs"], 0, False)
            xs_l[s, :, g * 192:(g + 1) * 192] = l_
            xs_c[s, :, g * 192:(g + 1) * 192] = c_
    del res
    res = run(build_k2g(), [prep_k2g(pl, pc, f32(p["gdn_conv_w"][l]), f32(p["gdn_a_log"][l]), f32(p["gdn_dt_bias"][l]), core)
                            for core in range(NCORE)])
    os_l = np.zeros((B, 2, T, 384), np.float32)
    os_c = np.zeros((B, 2, CTX, 384), np.float32)
    for core in range(NCORE):
        s, d, g = core // 4, (core // 2) % 2, core % 2
        c_, l_ = unseq(res[core]["o"], d, True)
        os_l[s, d, :, g * 192:(g + 1) * 192] = l_
        os_c[s, d, :, g * 192:(g + 1) * 192] = c_
    return ys_l, ys_c, xs_l, xs_c, os_l, os_c


def stage_post(l, x, ctx, pl, pc, scans, mods, p):
    f32 = _f32
    ys_l, ys_c, xs_l, xs_c, os_l, os_c = scans
    m = mods[l]
    seg = lambda r, i: f32(m[r, i * 1024:(i + 1) * 1024])
    idn = np.eye(128, dtype=np.float32)
    pwp = np.zeros((64, 4, 128), np.float32)
    for g in range(4):
        pwp[:, g, (g % 2) * 64:(g % 2) * 64 + 64] = p["pool_w"][l][g]
    common = {"wout": f32(p["w_out"][l]), "pw": pwp, "pscale": f32(np.asarray(p["pool_scale"][l]).reshape(2, 128).T),
              "rw": f32(np.asarray(p["router_w"][l]).reshape(8, 128, 16).transpose(1, 0, 2)), "idn": idn,
              "dvec": f32(np.repeat(np.asarray(p["ssd_d"][l]), 64)), "sng": f32(p["ssd_norm_g"][l]),
              "gng": f32(np.tile(np.asarray(p["gdn_norm_g"][l]), 6)), "n2g": f32(p["norm2_g"][l])}
    in_maps = []
    for core in range(NCORE):
        s, q = core // 4, core % 4
        ls = slice(q * 4096, (q + 1) * 4096)
        lat_p = np.concatenate([x[s, ls], pl[s, ls, 0:256], ys_l[s, 0, ls], ys_l[s, 1, ls], xs_l[s, ls], pl[s, ls, 256:640],
                                os_l[s, 0, ls], os_l[s, 1, ls], pl[s, ls, 2700:3084]], axis=-1).reshape(NLT, 128, PKW)
        n = min(128, CTX - q * 64)
        cs_ = slice(q * 64, q * 64 + n)
        ctx_p = np.zeros((1, 128, PKW), np.float32)
        ctx_p[0, :n] = np.concatenate([ctx[s, cs_], pc[s, cs_, 0:256], ys_c[s, 0, cs_], ys_c[s, 1, cs_], xs_c[s, cs_],
                                       pc[s, cs_, 256:640], os_c[s, 0, cs_], os_c[s, 1, cs_], pc[s, cs_, 2700:3084]], axis=-1)
        halo = np.stack([halo_for(pl[s, :, 0:256], q * 4096 + i * 128) for i in range(NLT)] + [halo_for(pc[s, :, 0:256], q * 64)])
        band = np.stack([band_for(q * 4096 + i * 128, T) for i in range(NLT)] + [band_for(q * 64, CTX)])
        d_ = {"pk": np.ascontiguousarray(np.concatenate([lat_p, ctx_p], 0)), "halo": f32(halo), "band": f32(band),
              "g1_l": seg(s, 2), "g1_c": seg(2, 2), "sh_l": seg(s, 3), "sc_l": seg(s, 4), "sh_c": seg(2, 3), "sc_c": seg(2, 4)}
        d_.update(common)
        in_maps.append(d_)
    return run(build_k3(NLT + 1), in_maps)


def stage_moe(l, res3, mods, p, last):
    f32 = _f32
    m = mods[l]
    seg = lambda r, i: f32(m[r, i * 1024:(i + 1) * 1024])
    ones = np.ones((128, 128), np.float32)
    aff_l, aff_c = untile(res3, "aff", 16)
    with_ctx = not last
    NT = NLT + 1 if with_ctx else NLT
    passes = [[(q * 8, q * 8 + 4), (q * 8 + 4, q * 8 + 8)] for q in range(4)]
    if with_ctx:
        passes[3].append((NLT, NLT + 1))
    in_maps = []
    for core in range(NCORE):
        s = core // 4
        in_maps.append({"x2": f32(res3[core]["x2"][:NT]), "h2T": f32(res3[core]["h2T"][:NT]), "aff": f32(res3[core]["aff"][:NT]),
                        "affs_l": f32(aff_l[s].reshape(128, 128, 16).transpose(0, 2, 1)),
                        "affs_c": f32(aff_c[s].reshape(128, 2, 16).transpose(0, 2, 1)),
                        "wg": f32(p["exp_w_gate"][l]), "wu": f32(p["exp_w_up"][l]), "wd": f32(p["exp_w_down"][l]), "ones": ones,
                        "g2_l": seg(s, 5), "g2_c": seg(2, 5), "fng": f32(p["final_norm_g"])})
    res = run(build_k4(NT, NLT, 2 * T // 16, 2 * CTX // 16, 128, 2, last, passes), in_maps)
    return untile(res, "out", 1024, with_ctx=with_ctx)


def _f32(a):
    return np.ascontiguousarray(np.asarray(a, dtype=np.float32))


def kernel(**p):
    x, ctx = _f32(p["x"]), _f32(p["ctx"])
    L = p["ada_w"].shape[0]
    mods = run_k0(_f32(p["c"]), _f32(p["c_ctx"]), _f32(p["ada_w"]), _f32(p["ada_b"]))
    for l in range(L):
        last = l == L - 1
        pl, pc = stage_proj(l, x, ctx, mods, p)
        scans = stage_scans(l, pl, pc, p)
        res3 = stage_post(l, x, ctx, pl, pc, scans, mods, p)
        del scans, pl, pc
        x, ctx_new = stage_moe(l, res3, mods, p, last)
        del res3
        if not last:
            ctx = ctx_new
    return x
```
